# Optimizing a Trainium2 kernel written in Bass

```python
import jax, jax.numpy as jnp
from jax import lax
import numpy as np

D_MODEL = 1024
BATCH = 4
SEQ = 8192
DEPTH = 2

MEM_LEN = 256
N_EVEN = (DEPTH + 1) // 2
N_ODD = DEPTH // 2
EPS = 1e-6

A_HEADS = 4
A_WIDTH = D_MODEL // 2
HEAD_DIM = A_WIDTH // A_HEADS
IDX_HEADS = 16
IDX_DIM = 64
TOPK_MAX = 256
Q_BLOCK = 128
ROPE_THETA = 500000.0

POOL_WINDOWS = (2, 4, 8, 16)
B_WIDTH = D_MODEL - A_WIDTH
POOL_GROUP = B_WIDTH // len(POOL_WINDOWS)

AB_SIZES = (A_WIDTH, A_WIDTH, A_WIDTH, B_WIDTH, IDX_HEADS * IDX_DIM, IDX_HEADS, IDX_DIM)
IN_AB = sum(AB_SIZES)
AB_SPLITS = [int(s) for s in np.cumsum(AB_SIZES)[:-1]]

D_CONV = D_MODEL
CONV_WIDTH = 31

C_HEADS = 4
C_HEAD_DIM = D_MODEL // C_HEADS

D_FF = -(-8 * D_MODEL // (3 * 256)) * 256

kernel_name = 'hybrid_dsa_pool_conformer_trunk'


def rmsnorm(x, g):
    xf = x.astype(jnp.float32)
    y = xf * lax.rsqrt(jnp.mean(xf * xf, axis=-1, keepdims=True) + EPS)
    return (y * g.astype(jnp.float32)).astype(x.dtype)


def layernorm(x, g, b):
    xf = x.astype(jnp.float32)
    mu = jnp.mean(xf, axis=-1, keepdims=True)
    var = jnp.mean(jnp.square(xf - mu), axis=-1, keepdims=True)
    y = (xf - mu) * lax.rsqrt(var + EPS)
    return (y * g.astype(jnp.float32) + b.astype(jnp.float32)).astype(x.dtype)


def rope_partial(x, pos):
    d = x.shape[-1]
    rot = d // 4
    half = rot // 2
    inv = ROPE_THETA ** (-jnp.arange(half, dtype=jnp.float32) * 2.0 / rot)
    ang = pos.astype(jnp.float32)[:, None] * inv[None, :]
    cos = jnp.cos(ang)[:, None, :]
    sin = jnp.sin(ang)[:, None, :]
    xf = x.astype(jnp.float32)
    x1, x2, rest = xf[..., :half], xf[..., half:rot], xf[..., rot:]
    out = jnp.concatenate([x1 * cos - x2 * sin, x2 * cos + x1 * sin, rest], axis=-1)
    return out.astype(x.dtype)


def dsa_attention(q, k, v, iq, iw, ik):
    bsz, t_len = q.shape[0], q.shape[1]
    topk = min(TOPK_MAX, t_len // 4)
    n_blocks = t_len // Q_BLOCK
    key_pos = jnp.arange(t_len)
    bidx = jnp.arange(bsz)[:, None, None]
    scale = HEAD_DIM ** -0.5

    def block(i):
        start = i * Q_BLOCK
        qb = lax.dynamic_slice_in_dim(q, start, Q_BLOCK, axis=1)
        iqb = lax.dynamic_slice_in_dim(iq, start, Q_BLOCK, axis=1)
        iwb = lax.dynamic_slice_in_dim(iw, start, Q_BLOCK, axis=1)
        qpos = start + jnp.arange(Q_BLOCK)
        s = jnp.einsum('bqhd,bsd->bqhs', iqb, ik).astype(jnp.float32)
        scores = jnp.einsum('bqhs,bqh->bqs', jax.nn.relu(s), iwb.astype(jnp.float32))
        causal = key_pos[None, :] <= qpos[:, None]
        scores = jnp.where(causal[None], scores, -jnp.inf)
        _, idx = lax.top_k(scores, topk)
        valid = idx <= qpos[None, :, None]
        ks = k[bidx, idx]
        vs = v[bidx, idx]
        logits = jnp.einsum('bqhd,bqkhd->bhqk', qb, ks).astype(jnp.float32) * scale
        logits = jnp.where(valid[:, None], logits, -jnp.inf)
        p = jax.nn.softmax(logits, axis=-1).astype(v.dtype)
        return jnp.einsum('bhqk,bqkhd->bqhd', p, vs)

    out = lax.map(block, jnp.arange(n_blocks))
    return jnp.transpose(out, (1, 0, 2, 3, 4)).reshape(q.shape)


def multiscale_pool(u, pool_w):
    t_len = u.shape[1]
    uf = u.astype(jnp.float32)
    cs = jnp.cumsum(uf, axis=1)
    t = jnp.arange(t_len)
    outs = []
    for g, w in enumerate(POOL_WINDOWS):
        sl = slice(g * POOL_GROUP, (g + 1) * POOL_GROUP)
        c = cs[..., sl]
        shifted = jnp.pad(c, ((0, 0), (w, 0), (0, 0)))[:, :t_len]
        cnt = jnp.minimum(t + 1, w).astype(jnp.float32)[None, :, None]
        outs.append((c - shifted) / cnt - uf[..., sl])
    pooled = jnp.stack(outs, axis=2).astype(u.dtype)
    y = jnp.einsum('btgc,gcd->btgd', pooled, pool_w)
    return y.reshape(u.shape)


def mixer_ab(xn, w_in, q_gain, k_gain, pool_w, pool_scale, w_out):
    bsz, t_len, _ = xn.shape
    proj = xn @ w_in
    q, k, v, u, iq, iw, ik = jnp.split(proj, AB_SPLITS, axis=-1)
    pos = jnp.arange(t_len)
    q = rope_partial(rmsnorm(q.reshape(bsz, t_len, A_HEADS, HEAD_DIM), q_gain), pos)
    k = rope_partial(rmsnorm(k.reshape(bsz, t_len, A_HEADS, HEAD_DIM), k_gain), pos)
    v = v.reshape(bsz, t_len, A_HEADS, HEAD_DIM)
    iq = rope_partial(iq.reshape(bsz, t_len, IDX_HEADS, IDX_DIM), pos)
    ik = rope_partial(ik.reshape(bsz, t_len, 1, IDX_DIM), pos)[:, :, 0]
    iw = iw * (IDX_HEADS ** -0.5 * IDX_DIM ** -0.5)
    a = dsa_attention(q, k, v, iq, iw, ik).reshape(bsz, t_len, A_WIDTH)
    b = multiscale_pool(u, pool_w) * pool_scale
    return jnp.concatenate([a, b], axis=-1) @ w_out


def conv_module(xn, w_in, b_in, dw_w, dw_b, ln_g, ln_b, w_out):
    h = xn @ w_in + b_in
    a, gate = jnp.split(h, 2, axis=-1)
    h = a * jax.nn.sigmoid(gate)
    h = lax.conv_general_dilated(
        h, dw_w[:, None, :], window_strides=(1,), padding=[(CONV_WIDTH - 1, 0)],
        dimension_numbers=('NWC', 'WIO', 'NWC'), feature_group_count=D_CONV) + dw_b
    h = jax.nn.silu(layernorm(h, ln_g, ln_b))
    return h @ w_out


def cross_attn(hn, memn, wq, wk, wv, q_gain, k_gain, wo):
    bsz, t_len, _ = hn.shape
    m_len = memn.shape[1]
    q = rmsnorm((hn @ wq).reshape(bsz, t_len, C_HEADS, C_HEAD_DIM), q_gain)
    k = rmsnorm((memn @ wk).reshape(bsz, m_len, C_HEADS, C_HEAD_DIM), k_gain)
    v = (memn @ wv).reshape(bsz, m_len, C_HEADS, C_HEAD_DIM)
    logits = jnp.einsum('bthd,bmhd->bhtm', q, k).astype(jnp.float32) * C_HEAD_DIM ** -0.5
    p = jax.nn.softmax(logits, axis=-1).astype(v.dtype)
    o = jnp.einsum('bhtm,bmhd->bthd', p, v).reshape(bsz, t_len, D_MODEL)
    return o @ wo


def swiglu(xn, w_gate, w_up, w_down):
    return (jax.nn.silu(xn @ w_gate) * (xn @ w_up)) @ w_down


def setup_inputs(seed: int = 0) -> dict:
    key = jax.random.key(seed)
    ks = iter(jax.random.split(key, 40))

    def dense(shape, fan_in):
        return jax.random.normal(next(ks), shape, jnp.float32) * fan_in ** -0.5

    def gain(shape):
        return 1.0 + 0.05 * jax.random.normal(next(ks), shape, jnp.float32)

    def bias(shape):
        return 0.02 * jax.random.normal(next(ks), shape, jnp.float32)

    return {
        'x': jax.random.normal(next(ks), (BATCH, SEQ, D_MODEL), jnp.float32),
        'mem': jax.random.normal(next(ks), (BATCH, MEM_LEN, D_MODEL), jnp.float32),
        'norm_mix': gain((DEPTH, D_MODEL)),
        'norm_cross': gain((DEPTH, D_MODEL)),
        'norm_mem': gain((DEPTH, D_MODEL)),
        'norm_ffn': gain((DEPTH, D_MODEL)),
        'w_in_ab': dense((N_EVEN, D_MODEL, IN_AB), D_MODEL),
        'a_q_norm': gain((N_EVEN, HEAD_DIM)),
        'a_k_norm': gain((N_EVEN, HEAD_DIM)),
        'pool_w': dense((N_EVEN, len(POOL_WINDOWS), POOL_GROUP, POOL_GROUP), POOL_GROUP),
        'pool_scale': gain((N_EVEN, B_WIDTH)),
        'w_out_ab': dense((N_EVEN, D_MODEL, D_MODEL), D_MODEL),
        'conv_w_in': dense((N_ODD, D_MODEL, 2 * D_CONV), D_MODEL),
        'conv_b_in': bias((N_ODD, 2 * D_CONV)),
        'conv_dw_w': dense((N_ODD, CONV_WIDTH, D_CONV), CONV_WIDTH),
        'conv_dw_b': bias((N_ODD, D_CONV)),
        'conv_ln_g': gain((N_ODD, D_CONV)),
        'conv_ln_b': bias((N_ODD, D_CONV)),
        'conv_w_out': dense((N_ODD, D_CONV, D_MODEL), D_CONV),
        'cross_wq': dense((DEPTH, D_MODEL, D_MODEL), D_MODEL),
        'cross_wk': dense((DEPTH, D_MODEL, D_MODEL), D_MODEL),
        'cross_wv': dense((DEPTH, D_MODEL, D_MODEL), D_MODEL),
        'cross_q_norm': gain((DEPTH, C_HEAD_DIM)),
        'cross_k_norm': gain((DEPTH, C_HEAD_DIM)),
        'cross_wo': dense((DEPTH, D_MODEL, D_MODEL), D_MODEL),
        'ffn_w_gate': dense((DEPTH, D_MODEL, D_FF), D_MODEL),
        'ffn_w_up': dense((DEPTH, D_MODEL, D_FF), D_MODEL),
        'ffn_w_down': dense((DEPTH, D_FF, D_MODEL), D_FF),
    }


def reference(x, mem, norm_mix, norm_cross, norm_mem, norm_ffn, w_in_ab, a_q_norm, a_k_norm,
              pool_w, pool_scale, w_out_ab, conv_w_in, conv_b_in, conv_dw_w, conv_dw_b,
              conv_ln_g, conv_ln_b, conv_w_out, cross_wq, cross_wk, cross_wv, cross_q_norm,
              cross_k_norm, cross_wo, ffn_w_gate, ffn_w_up, ffn_w_down):
    h = x
    for l in range(DEPTH):
        xn = rmsnorm(h, norm_mix[l])
        if l % 2 == 0:
            e = l // 2
            h = h + mixer_ab(xn, w_in_ab[e], a_q_norm[e], a_k_norm[e], pool_w[e],
                             pool_scale[e], w_out_ab[e])
        else:
            o = l // 2
            h = h + conv_module(xn, conv_w_in[o], conv_b_in[o], conv_dw_w[o], conv_dw_b[o],
                                conv_ln_g[o], conv_ln_b[o], conv_w_out[o])
        h = h + cross_attn(rmsnorm(h, norm_cross[l]), rmsnorm(mem, norm_mem[l]),
                           cross_wq[l], cross_wk[l], cross_wv[l], cross_q_norm[l],
                           cross_k_norm[l], cross_wo[l])
        h = h + swiglu(rmsnorm(h, norm_ffn[l]), ffn_w_gate[l], ffn_w_up[l], ffn_w_down[l])
    return h
```

```python
import numpy as np
import ml_dtypes
import concourse.bass as bass
import concourse.mybir as mybir
from concourse.bass_utils import run_bass_kernel_spmd

F32 = mybir.dt.float32
BF16 = mybir.dt.bfloat16
AF = mybir.ActivationFunctionType
ALU = mybir.AluOpType
AX = mybir.AxisListType

D = 1024
SEQ = 8192
NT_SEQ = SEQ // 128
CH_TILES = 16
SLOTS_PER_CH = CH_TILES + 1
NSLOT = 2 * SLOTS_PER_CH
NOWN = NSLOT * 128
MEM = 256
DFF = 2816
EPS = 1e-6
NEG = -1.0e30
NITER = 24
MBW = 3072

SAME_ENGINE_SYNC = True
SEQ_PE = True
SKEW = False
SKEW_A = True


class Buf:
    def __init__(self, name, ap_fn):
        self.name = name
        self.ap_fn = ap_fn
        self.w = {}
        self.r = {}
        self.dsem = None
        self.dval = 0

    def __getitem__(self, idx):
        return self.ap_fn()[idx]


class Eng:
    def __init__(self, name, inst, is_pe=False):
        self.name = name
        self.inst = inst
        self.sem = None
        self.cnt = 0
        self.known = {}
        self.is_pe = is_pe


class Sched:
    EPOCH = 30000

    def __init__(self, nc):
        self.nc = nc
        self.pe = Eng("pe", nc.tensor, True)
        self.act = Eng("act", nc.scalar)
        self.dve = Eng("dve", nc.vector)
        self.pool = Eng("pool", nc.gpsimd)
        self.sp = Eng("sp", nc.sync)
        self.engs = [self.pe, self.act, self.dve, self.pool, self.sp]
        self.nsem = 0
        self.all_dma_bufs = []
        self.nbuf = 0
        self.scopes = []
        self.live = []
        self.all_sems = []

    def new_sem(self, name):
        self.nsem += 1
        sm_ = self.nc.alloc_semaphore(f"{name}_{self.nsem}")
        self.all_sems.append(sm_)
        return sm_

    def sbuf(self, name, shape, dtype):
        self.nbuf += 1
        if self.scopes:
            t = self.scopes[-1].enter_context(self.nc.sbuf_tensor(f"{name}_{self.nbuf}", list(shape), dtype))
        else:
            t = self.nc.alloc_sbuf_tensor(f"{name}_{self.nbuf}", list(shape), dtype)
        b = Buf(name, lambda: t)
        self.live.append(b)
        return b

    def push(self):
        import contextlib
        self.scopes.append(contextlib.ExitStack())

    def pop(self):
        self.barrier()
        self.scopes.pop().close()

    def barrier(self):
        evs = {}
        for e in self.engs:
            if e.sem is not None and e.cnt > 0:
                evs[id(e.sem)] = (e.sem, e.cnt)
        for b in self.live:
            if b.dsem is not None and b.dval > 0:
                evs[id(b.dsem)] = (b.dsem, b.dval)
        for e in self.engs:
            for k, (sm, v) in evs.items():
                if e.sem is not None and sm is e.sem:
                    continue
                if e.known.get(k, 0) >= v:
                    continue
                e.inst.wait_ge(sm, v)
                e.known[k] = v

    def psum(self, name, shape, dtype):
        self.nbuf += 1
        t = self.nc.alloc_psum_tensor(f"{name}_{self.nbuf}", list(shape), dtype)
        return Buf(name, lambda: t)

    def dram(self, name, shape, dtype, kind="Internal"):
        t = self.nc.dram_tensor(name, list(shape), dtype, kind=kind)
        a = t.ap()
        return Buf(name, lambda: a)

    def view(self, name, buf, idx):
        return Buf(name, lambda: buf.ap_fn()[idx])

    def _collect(self, eng, reads, writes):
        need = {}

        def add(d):
            for s, v in d.items():
                k = id(s)
                if k not in need or need[k][1] < v:
                    need[k] = (s, v)
        for b in reads:
            add(b.w)
        for b in writes:
            add(b.w)
            add(b.r)
        for k, (s, v) in need.items():
            if eng.sem is not None and s is eng.sem:
                if eng.is_pe or not SAME_ENGINE_SYNC:
                    continue
            if eng.known.get(k, 0) >= v:
                continue
            eng.inst.wait_ge(s, v)
            eng.known[k] = v

    def op(self, eng, fn, reads=(), writes=(), partial=()):
        allw = list(writes) + list(partial)
        self._collect(eng, reads, allw)
        if eng.sem is None or eng.cnt >= self.EPOCH:
            eng.sem = self.new_sem(eng.name)
            eng.cnt = 0
        ins = fn()
        eng.cnt += 1
        ins.then_inc(eng.sem, 1)
        s, v = eng.sem, eng.cnt
        for b in reads:
            if b.r.get(s, 0) < v:
                b.r[s] = v
        for b in writes:
            b.w = {s: v}
            b.r = {}
        for b in partial:
            b.w[s] = v
        return ins

    def dma(self, out_buf, out_ap, in_buf, in_ap, eng=None, sbuf_side=None, **kw):
        eng = eng or self.sp
        owner = sbuf_side
        self._collect(eng, [in_buf], [out_buf])
        if owner.dsem is None or owner.dval >= self.EPOCH:
            owner.dsem = self.new_sem("d" + owner.name)
            owner.dval = 0
        ins = eng.inst.dma_start(out=out_ap, in_=in_ap, **kw)
        owner.dval += 16
        ins.then_inc(owner.dsem, 16)
        s, v = owner.dsem, owner.dval
        if in_buf.r.get(s, 0) < v:
            in_buf.r[s] = v
        out_buf.w[s] = v
        return ins

    def load(self, dst, dst_ap, src, src_ap, eng=None, **kw):
        return self.dma(dst, dst_ap, src, src_ap, eng=eng, sbuf_side=dst, **kw)

    def store(self, dst, dst_ap, src, src_ap, eng=None, **kw):
        return self.dma(dst, dst_ap, src, src_ap, eng=eng, sbuf_side=src, **kw)

    def finish(self, bufs):
        self._collect(self.sp, bufs, [])


class Ring:
    def __init__(self, bufs):
        self.bufs = bufs
        self.i = 0

    def next(self):
        b = self.bufs[self.i % len(self.bufs)]
        self.i += 1
        return b


def own_tiles(half):
    tiles = []
    for ci in range(2):
        start = (2 * ci + half) * CH_TILES
        tiles.append(start - 1)
        tiles.extend(range(start, start + CH_TILES))
    return tiles


def slot_nkt(slot):
    ci, i = divmod(slot, SLOTS_PER_CH)
    t1 = (2 * ci + 1) * CH_TILES + i - 1
    return t1 + 1


def slot_first_uncertain(slot):
    ci, i = divmod(slot, SLOTS_PER_CH)
    t0 = 2 * ci * CH_TILES + i - 1
    return max(t0, 0)


def rope_tables(pos, head_dim):
    rot = head_dim // 4
    half = rot // 2
    inv = 500000.0 ** (-np.arange(half, dtype=np.float32) * 2.0 / rot)
    ang = pos.astype(np.float32)[None, :] * inv[:, None].astype(np.float32)
    cos = np.cos(ang).astype(np.float32)
    sin = np.sin(ang).astype(np.float32)
    C = np.ones((128, len(pos)), np.float32)
    S = np.zeros((128, len(pos)), np.float32)
    for h0 in range(0, 128, head_dim):
        C[h0:h0 + half] = cos
        C[h0 + half:h0 + rot] = cos
        S[h0:h0 + half] = -sin
        S[h0 + half:h0 + rot] = sin
    return C, S


def perm_matrix(head_dim):
    rot = head_dim // 4
    half = rot // 2
    P = np.zeros((128, 128), np.float32)
    for h0 in range(0, 128, head_dim):
        for i in range(half):
            P[h0 + half + i, h0 + i] = 1.0
            P[h0 + i, h0 + half + i] = 1.0
    return P


def build_program(stop_after=None, slots=None):
    nc = bass.Bass("TRN2", target_bir_lowering=False)
    S = Sched(nc)
    pe, act, dve, pool, sp = S.pe, S.act, S.dve, S.pool, S.sp
    dbg = {}

    def din(name, shape, dtype=F32):
        return S.dram(name, shape, dtype, kind="ExternalInput")

    xT_seq = din("xT_seq", [128, 8, SEQ])
    xT_own = din("xT_own", [128, 8, NOWN])
    memT = din("memT", [128, 8, MEM])
    qadj = din("qadj", [128, NSLOT])
    iota_in = din("iota", [128, MBW])
    c128s = din("c128s", [128, SEQ]); s128s = din("s128s", [128, SEQ])
    c64s = din("c64s", [128, SEQ]); s64s = din("s64s", [128, SEQ])
    c128o = din("c128o", [128, NOWN]); s128o = din("s128o", [128, NOWN])
    c64o = din("c64o", [128, NOWN]); s64o = din("s64o", [128, NOWN])
    p128_in = din("p128", [128, 128]); p64_in = din("p64", [128, 128])
    ident_in = din("ident", [128, 128])
    poolcorr_in = din("poolcorr", [128, 2, 4, 16])
    haloflag_in = din("haloflag", [128, 2])
    w_keys = din("w_keys", [D, 1152])
    w_own = din("w_own", [D, 2064])
    vecs = din("vecs", [128, 64])
    pool_w = din("pool_w", [4, 128, 128])
    w_out_ab = din("w_out_ab", [D, D])
    conv_w_in = din("conv_w_in", [D, 2 * D])
    conv_dw = din("conv_dw", [128, 8, 31])
    conv_w_out = din("conv_w_out", [D, D])
    cross_wq = din("cross_wq", [2, D, D]); cross_wk = din("cross_wk", [2, D, D])
    cross_wv = din("cross_wv", [2, D, D]); cross_wo = din("cross_wo", [2, D, D])
    ffn_wg = din("ffn_wg", [2, D, DFF]); ffn_wu = din("ffn_wu", [2, D, DFF])
    ffn_wd = din("ffn_wd", [2, DFF, D])
    out_hT = S.dram("out_hT", [128, 8, 32 * 128], F32, kind="ExternalOutput")

    KT = S.dram("KT", [128, 4, SEQ], BF16)
    Vd = S.dram("Vd", [SEQ, 512], BF16)
    QT = S.dram("QT", [128, 4, NOWN], BF16)
    IQT = S.dram("IQT", [128, 8, NOWN], BF16)
    BT = S.dram("BT", [128, 4, NOWN], BF16)
    AT = S.dram("AT", [128, 4, NOWN], BF16)
    HT = S.dram("HT", [128, 8, NOWN], F32)
    HID = S.dram("HID", [128, 22, NOWN], BF16)

    ones_m = S.sbuf("ones_m", [128, 128], BF16)
    ones_h = S.sbuf("ones_h", [128, 128], BF16)
    ones_c = S.sbuf("ones_c", [128, 128], BF16)
    ones_1 = S.sbuf("ones_1", [128, 128], BF16)
    ident = S.sbuf("ident", [128, 128], BF16)
    p128 = S.sbuf("p128", [128, 128], BF16)
    p64 = S.sbuf("p64", [128, 128], BF16)
    cst_f = S.sbuf("cst_f", [128, 3, 128], F32)
    vec = S.sbuf("vec", [128, 64], F32)
    qadj_sb = S.sbuf("qadj_sb", [128, NSLOT], F32)
    eps_t = S.sbuf("eps_t", [128, 1], F32)

    S.op(pool, lambda: nc.gpsimd.memset(ones_m[:], 1.0 / 1024), writes=[ones_m])
    S.op(pool, lambda: nc.gpsimd.memset(ones_h[:], 1.0 / 128), writes=[ones_h])
    S.op(pool, lambda: nc.gpsimd.memset(ones_c[:], 1.0 / 256), writes=[ones_c])
    S.op(pool, lambda: nc.gpsimd.memset(ones_1[:], 1.0), writes=[ones_1])
    S.op(pool, lambda: nc.gpsimd.memset(eps_t[:], EPS), writes=[eps_t])
    S.load(cst_f, cst_f[:, 0, :], ident_in, ident_in[:, :])
    S.load(cst_f, cst_f[:, 1, :], p128_in, p128_in[:, :])
    S.load(cst_f, cst_f[:, 2, :], p64_in, p64_in[:, :])
    S.load(vec, vec[:], vecs, vecs[:, :])
    S.load(qadj_sb, qadj_sb[:], qadj, qadj[:, :])
    S.op(dve, lambda: nc.vector.tensor_copy(out=ident[:], in_=cst_f[:, 0, :]), reads=[cst_f], writes=[ident])
    S.op(dve, lambda: nc.vector.tensor_copy(out=p128[:], in_=cst_f[:, 1, :]), reads=[cst_f], writes=[p128])
    S.op(dve, lambda: nc.vector.tensor_copy(out=p64[:], in_=cst_f[:, 2, :]), reads=[cst_f], writes=[p64])

    V_NMIX0, V_NMIX1, V_NCROSS0, V_NCROSS1, V_NMEM0, V_NMEM1, V_NFFN0, V_NFFN1 = [8 * i for i in range(8)]
    vec2_in = din("vecs2", [128, 64])
    vec2 = S.sbuf("vec2", [128, 64], F32)
    S.load(vec2, vec2[:], vec2_in, vec2_in[:, :])
    V2_AQ, V2_AK = 0, 1
    V2_PSCALE = 2
    V2_CQ0, V2_CQ1, V2_CK0, V2_CK1 = 6, 8, 10, 12
    V2_BIN = 14
    V2_DWB = 30
    V2_LNG = 38
    V2_LNB = 46

    PS = [S.psum(f"ps{i}", [128, 512], F32) for i in range(7)]
    PSB = S.psum("psb", [128, 1024], BF16)

    def load_w_cast(dst, col_dst, w_buf, w_ap, col0, M):
        K = w_ap.shape[0]
        for kc in range(K // 128):
            m0 = 0
            while m0 < M:
                mm_ = min(2048, M - m0)
                S.load(dst, dst[:, kc, col_dst + m0:col_dst + m0 + mm_], w_buf,
                       w_ap[kc * 128:(kc + 1) * 128, col0 + m0:col0 + m0 + mm_], eng=pool)
                m0 += mm_

    def mm(ps_ap, lhsT, rhs, start, stop, reads, ps_buf):
        S.op(pe, lambda: nc.tensor.matmul(ps_ap, lhsT=lhsT, rhs=rhs, start=start, stop=stop),
             reads=reads, partial=[ps_buf] if not start else (), writes=[ps_buf] if start else ())

    def rstd_from_ms(ps_buf, n, out_buf, tmp_buf):
        S.op(act, lambda: nc.scalar.activation(out=tmp_buf[:, :n], in_=ps_buf[:, :n], func=AF.Sqrt,
                                               bias=eps_t[:, 0:1], scale=1.0),
             reads=[ps_buf, eps_t], writes=[tmp_buf])
        S.op(dve, lambda: nc.vector.reciprocal(out=out_buf[:, :n], in_=tmp_buf[:, :n]),
             reads=[tmp_buf], writes=[out_buf])

    def own_blocks(include_halo=True):
        res = []
        for ci in range(2):
            base = ci * SLOTS_PER_CH * 128
            if include_halo:
                res.append((base, 128, ci, True, False))
            for i in range(4):
                res.append((base + 128 + 512 * i, 512, ci, False, i == 0))
        return res

    class Common:
        pass

    def alloc_common():
        cm = Common()
        cm.xt_ring = Ring([S.sbuf(f"xt{i}", [128, 8, 512], F32) for i in range(2)])
        cm.sq_b = S.sbuf("sq", [128, 8, 512], BF16)
        cm.xn_b = S.sbuf("xn", [128, 8, 512], BF16)
        cm.rstd_b = S.sbuf("rstd", [128, 512], F32)
        cm.tmp_b = S.sbuf("tmpf", [128, 512], F32)
        return cm

    def norm_block(cm, src_buf, off, n, gcol):
        xt = cm.xt_ring.next()
        S.load(xt, xt[:, :, :n], src_buf, src_buf[:, :, off:off + n])
        S.op(act, lambda: nc.scalar.activation(out=cm.sq_b[:, :, :n], in_=xt[:, :, :n], func=AF.Square),
             reads=[xt], writes=[cm.sq_b])
        ps = PS[0]
        for c in range(8):
            mm(ps[:, :n], ones_m[:], cm.sq_b[:, c, :n], c == 0, c == 7, [ones_m, cm.sq_b], ps)
        rstd_from_ms(ps, n, cm.rstd_b, cm.tmp_b)
        for c in range(8):
            S.op(dve, lambda c=c: nc.vector.scalar_tensor_tensor(
                out=cm.xn_b[:, c, :n], in0=xt[:, c, :n], scalar=vec[:, gcol + c:gcol + c + 1],
                in1=cm.rstd_b[:, :n], op0=ALU.mult, op1=ALU.mult),
                reads=[xt, vec, cm.rstd_b], partial=[cm.xn_b])
        return xt

    def alloc_tmps():
        return (S.sbuf("sqh", [128, 512], BF16), S.sbuf("kn", [128, 512], BF16), S.sbuf("rk", [128, 512], F32),
                S.sbuf("tk", [128, 512], F32), S.sbuf("t1", [128, 512], F32), S.sbuf("t2", [128, 512], F32))

    def norm_head_rope(ps, n, gcol2, ones_t, pm, c_ap, s_ap, cs_bufs, out_ap, out_buf, do_norm, tmps):
        sqh, kn, rk, tk, t1, t2 = tmps
        ps2, ps3 = PS[5], PS[6]
        if do_norm:
            S.op(act, lambda: nc.scalar.activation(out=sqh[:, :n], in_=ps[:, :n], func=AF.Square),
                 reads=[ps], writes=[sqh])
            mm(ps2[:, :n], ones_t[:], sqh[:, :n], True, True, [ones_t, sqh], ps2)
            rstd_from_ms(ps2, n, rk, tk)
            S.op(dve, lambda: nc.vector.scalar_tensor_tensor(
                out=kn[:, :n], in0=ps[:, :n], scalar=vec2[:, gcol2:gcol2 + 1], in1=rk[:, :n],
                op0=ALU.mult, op1=ALU.mult), reads=[ps, vec2, rk], writes=[kn])
        else:
            S.op(act, lambda: nc.scalar.activation(out=kn[:, :n], in_=ps[:, :n], func=AF.Copy),
                 reads=[ps], writes=[kn])
        mm(ps3[:, :n], pm[:], kn[:, :n], True, True, [pm, kn], ps3)
        S.op(pool, lambda: nc.gpsimd.tensor_tensor(out=t1[:, :n], in0=kn[:, :n], in1=c_ap, op=ALU.mult),
             reads=[kn] + cs_bufs, writes=[t1])
        S.op(dve, lambda: nc.vector.tensor_tensor(out=t2[:, :n], in0=ps3[:, :n], in1=s_ap, op=ALU.mult),
             reads=[ps3] + cs_bufs, writes=[t2])
        if isinstance(out_ap, list):
            for (oap, obuf, p0, p1) in out_ap:
                S.op(dve, lambda oap=oap, p0=p0, p1=p1: nc.vector.tensor_tensor(out=oap, in0=t1[p0:p1, :n], in1=t2[p0:p1, :n], op=ALU.add),
                     reads=[t1, t2], partial=[obuf])
        else:
            S.op(dve, lambda: nc.vector.tensor_tensor(out=out_ap, in0=t1[:, :n], in1=t2[:, :n], op=ALU.add),
                 reads=[t1, t2], partial=[out_buf])

    proj_ps = Ring([PS[1], PS[2], PS[3], PS[4]])

    def proj(ps, w_sb, col0, ncols, rhs_list, n, rbufs):
        kc = len(rhs_list)
        for c in range(kc):
            mm(ps[:, :n], w_sb[:, c, col0:col0 + ncols], rhs_list[c], c == 0, c == kc - 1, [w_sb] + rbufs, ps)

    S.push()
    ikT0 = S.sbuf("ikT0", [128, SEQ], BF16)
    ikT1 = S.sbuf("ikT1", [128, SEQ], BF16)
    iw_sb = S.sbuf("iw_sb", [128, NSLOT, 16], F32)
    S.op(pool, lambda: nc.gpsimd.memset(ikT0[:], 0.0), writes=[ikT0])
    S.op(pool, lambda: nc.gpsimd.memset(ikT1[:], 0.0), writes=[ikT1])

    S.push()
    cm = alloc_common()
    tmps = alloc_tmps()
    cs_ring = Ring([S.sbuf(f"cs{i}", [128, 4, 512], F32) for i in range(2)])
    wk_sb = S.sbuf("wk_sb", [128, 8, 1152], BF16)
    load_w_cast(wk_sb, 0, w_keys, w_keys.ap_fn(), 0, 1152)
    kout_ring = Ring([S.sbuf(f"kout{i}", [128, 4, 512], BF16) for i in range(2)])
    vout_ring = Ring([S.sbuf(f"vout{i}", [128, 4, 512], BF16) for i in range(2)])
    for blk in range(SEQ // 512):
        off = blk * 512
        n = 512
        norm_block(cm, xT_seq, off, n, V_NMIX0)
        xn_b = cm.xn_b
        cs = cs_ring.next()
        S.load(cs, cs[:, 0, :], c128s, c128s[:, off:off + n])
        S.load(cs, cs[:, 1, :], s128s, s128s[:, off:off + n])
        S.load(cs, cs[:, 2, :], c64s, c64s[:, off:off + n])
        S.load(cs, cs[:, 3, :], s64s, s64s[:, off:off + n])
        kout = kout_ring.next()
        for kc in range(4):
            ps = proj_ps.next()
            proj(ps, wk_sb, kc * 128, 128, [xn_b[:, c, :n] for c in range(8)], n, [xn_b])
            norm_head_rope(ps, n, V2_AK, ones_h, p128, cs[:, 0, :n], cs[:, 1, :n], [cs],
                           kout[:, kc, :n], kout, True, tmps)
        S.store(KT, KT[:, :, off:off + n], kout, kout[:, :, :n])
        ps = proj_ps.next()
        proj(ps, wk_sb, 512, 128, [xn_b[:, c, :n] for c in range(8)], n, [xn_b])
        norm_head_rope(ps, n, 0, None, p64, cs[:, 2, :n], cs[:, 3, :n], [cs],
                       [(ikT0[0:64, off:off + n], ikT0, 0, 64), (ikT1[64:128, off:off + n], ikT1, 64, 128)],
                       None, False, tmps)
        vout = vout_ring.next()
        for tt in range(4):
            ps = proj_ps.next()
            for c in range(8):
                mm(ps[:, :], xn_b[:, c, tt * 128:(tt + 1) * 128], wk_sb[:, c, 640:1152], c == 0, c == 7,
                   [wk_sb, xn_b], ps)
            S.op(act, lambda tt=tt, ps=ps: nc.scalar.activation(out=vout[:, tt, :], in_=ps[:, :], func=AF.Copy),
                 reads=[ps], partial=[vout])
        S.store(Vd, Vd.ap_fn()[off:off + n, :].rearrange("(t p) d -> p t d", p=128), vout, vout[:, :, :])
    S.pop()

    S.push()
    cm = alloc_common()
    tmps = alloc_tmps()
    cs_ring = Ring([S.sbuf(f"cs{i}", [128, 4, 512], F32) for i in range(1)])
    wo_sb = S.sbuf("wo_sb", [128, 8, 2064], BF16)
    load_w_cast(wo_sb, 0, w_own, w_own.ap_fn(), 0, 2064)
    pw_sb = S.sbuf("pw_sb", [128, 4, 128], BF16)
    for g in range(4):
        S.load(pw_sb, pw_sb[:, g, :], pool_w, pool_w[g, :, :], eng=pool)
    pcorr = S.sbuf("pcorr", [128, 2, 4, 16], F32)
    S.load(pcorr, pcorr[:], poolcorr_in, poolcorr_in[:, :, :, :])
    qout_ring = Ring([S.sbuf(f"qout{i}", [128, 4, 512], BF16) for i in range(2)])
    iqout_ring = Ring([S.sbuf(f"iqout{i}", [128, 8, 512], BF16) for i in range(1)])
    bout_ring = Ring([S.sbuf(f"bout{i}", [128, 4, 512], BF16) for i in range(2)])
    Ug = [S.sbuf(f"U{g}", [128, 528], F32) for g in range(4)]
    sA = [S.sbuf(f"sA{g}", [128, 528], F32) for g in range(4)]
    sB = [S.sbuf(f"sB{g}", [128, 528], F32) for g in range(4)]
    pooled = [S.sbuf(f"pooled{g}", [128, 512], BF16) for g in range(4)]
    for (off, n, ci, is_halo, is_first) in own_blocks():
        norm_block(cm, xT_own, off, n, V_NMIX0)
        xn_b = cm.xn_b
        xl = [xn_b[:, c, :n] for c in range(8)]
        cs = cs_ring.next()
        S.load(cs, cs[:, 0, :n], c128o, c128o[:, off:off + n])
        S.load(cs, cs[:, 1, :n], s128o, s128o[:, off:off + n])
        S.load(cs, cs[:, 2, :n], c64o, c64o[:, off:off + n])
        S.load(cs, cs[:, 3, :n], s64o, s64o[:, off:off + n])
        qout = qout_ring.next()
        for kc in range(4):
            ps = proj_ps.next()
            proj(ps, wo_sb, kc * 128, 128, xl, n, [xn_b])
            norm_head_rope(ps, n, V2_AQ, ones_h, p128, cs[:, 0, :n], cs[:, 1, :n], [cs],
                           qout[:, kc, :n], qout, True, tmps)
        S.store(QT, QT[:, :, off:off + n], qout, qout[:, :, :n])
        iqout = iqout_ring.next()
        for kc in range(8):
            ps = proj_ps.next()
            proj(ps, wo_sb, 512 + kc * 128, 128, xl, n, [xn_b])
            norm_head_rope(ps, n, 0, None, p64, cs[:, 2, :n], cs[:, 3, :n], [cs],
                           iqout[:, kc, :n], iqout, False, tmps)
        S.store(IQT, IQT[:, :, off:off + n], iqout, iqout[:, :, :n])
        for tt in range(n // 128):
            slot = off // 128 + tt
            ps = proj_ps.next()
            for c in range(8):
                mm(ps[:, :16], xn_b[:, c, tt * 128:(tt + 1) * 128], wo_sb[:, c, 2048:2064], c == 0, c == 7,
                   [wo_sb, xn_b], ps)
            S.op(act, lambda ps=ps, slot=slot: nc.scalar.activation(out=iw_sb[:, slot, :], in_=ps[:, :16],
                                                                    func=AF.Copy, scale=1.0 / 32.0),
                 reads=[ps], partial=[iw_sb])
        L = 16 + n
        bout = bout_ring.next()
        for g in range(4):
            U = Ug[g]
            if is_halo:
                S.op(pool, lambda U=U: nc.gpsimd.memset(U[:, 0:16], 0.0), partial=[U])
            ps = proj_ps.next()
            proj(ps, wo_sb, 1536 + g * 128, 128, xl, n, [xn_b])
            S.op(act, lambda ps=ps, U=U: nc.scalar.activation(out=U[:, 16:L], in_=ps[:, :n], func=AF.Copy),
                 reads=[ps], partial=[U])
            a_, b_ = sA[g], sB[g]
            S.op(pool, lambda U=U, a_=a_: nc.gpsimd.tensor_tensor(out=a_[:, 1:L], in0=U[:, 1:L], in1=U[:, 0:L - 1], op=ALU.add),
                 reads=[U], writes=[a_])
            fin = a_
            if g >= 1:
                S.op(pool, lambda a_=a_, b_=b_: nc.gpsimd.tensor_tensor(out=b_[:, 3:L], in0=a_[:, 3:L], in1=a_[:, 1:L - 2], op=ALU.add),
                     reads=[a_], writes=[b_])
                fin = b_
            if g >= 2:
                S.op(pool, lambda a_=a_, b_=b_: nc.gpsimd.tensor_tensor(out=a_[:, 7:L], in0=b_[:, 7:L], in1=b_[:, 3:L - 4], op=ALU.add),
                     reads=[b_], writes=[a_])
                fin = a_
            if g >= 3:
                S.op(pool, lambda a_=a_, b_=b_: nc.gpsimd.tensor_tensor(out=b_[:, 15:L], in0=a_[:, 15:L], in1=a_[:, 7:L - 8], op=ALU.add),
                     reads=[a_], writes=[b_])
                fin = b_
            if is_first:
                S.op(dve, lambda fin=fin, g=g: nc.vector.tensor_tensor(out=fin[:, 16:32], in0=fin[:, 16:32],
                                                                       in1=pcorr[:, ci, g, :], op=ALU.mult),
                     reads=[pcorr, fin], partial=[fin])
            w_ = float(2 ** (g + 1))
            S.op(dve, lambda fin=fin, U=U, g=g: nc.vector.scalar_tensor_tensor(
                out=pooled[g][:, :n], in0=fin[:, 16:L], scalar=1.0 / w_, in1=U[:, 16:L],
                op0=ALU.mult, op1=ALU.subtract), reads=[fin, U], writes=[pooled[g]])
            S.op(pool, lambda U=U: nc.gpsimd.tensor_copy(out=U[:, 0:16], in_=U[:, n:n + 16]), reads=[U], partial=[U])
            ps = proj_ps.next()
            mm(ps[:, :n], pw_sb[:, g, :], pooled[g][:, :n], True, True, [pw_sb, pooled[g]], ps)
            S.op(act, lambda ps=ps, g=g: nc.scalar.activation(out=bout[:, g, :n], in_=ps[:, :n], func=AF.Copy,
                                                               scale=vec2[:, V2_PSCALE + g:V2_PSCALE + g + 1]),
                 reads=[ps, vec2], partial=[bout])
        S.store(BT, BT[:, :, off:off + n], bout, bout[:, :, :n])
    S.pop()

    S.push()
    iota_sb = S.sbuf("iota_sb", [128, MBW], F32)
    S.load(iota_sb, iota_sb[:], iota_in, iota_in[:, :])
    scores2 = [S.sbuf(f"scores{i}", [128, SEQ], F32) for i in range(2)]
    mbias = S.sbuf("mbias", [128, SEQ], BF16)
    junk = S.sbuf("junk", [128, SEQ // 2], BF16)
    mb2 = [S.sbuf(f"mb{i}", [128, MBW], BF16) for i in range(2)]
    tmpu = S.sbuf("tmpu", [128, MBW], F32)
    qt_ring = Ring([S.sbuf(f"qt{i}", [128, 4, 128], BF16) for i in range(2)])
    iqt_ring = Ring([S.sbuf(f"iqt{i}", [128, 8, 128], BF16) for i in range(2)])
    kb_ring = Ring([S.sbuf(f"kblk{i}", [128, 4, 512], BF16) for i in range(2)])
    vb_ring = Ring([S.sbuf(f"vblk{i}", [128, 4, 512], BF16) for i in range(2)])
    r_ring = Ring([S.sbuf(f"R{i}", [128, 512], BF16) for i in range(4)])
    p_ring = Ring([S.sbuf(f"P{i}", [128, 512], BF16) for i in range(3)])
    pt_ring = Ring([S.sbuf(f"PT{i}", [128, 512], BF16) for i in range(3)])
    diag2 = [S.sbuf(f"diag{i}", [128, 16, 128], BF16) for i in range(2)]
    sm = S.sbuf("sm", [128, 16], F32)
    hs = S.sbuf("hs", [128, 32], F32)
    pow2 = S.sbuf("pow2", [128, 32], F32)
    cntb = S.sbuf("cntb", [128, 2], F32)
    midr = Ring([S.sbuf(f"mid{i}", [128, 1], F32) for i in range(2)])
    eb = S.sbuf("eb", [128, 1], F32)
    rs = S.sbuf("rs", [128, 4, 16], F32)
    rsum = S.sbuf("rsum", [128, 4], F32)
    rrec = S.sbuf("rrec", [128, 4], F32)
    negone = S.sbuf("negone", [128, 4], F32)
    rjunk = S.sbuf("rjunk", [128, 16], F32)
    a_tok = S.sbuf("a_tok", [128, 512], BF16)
    aT_ring = Ring([S.sbuf(f"aT{i}", [128, 4, 128], BF16) for i in range(2)])
    for i in range(32):
        S.op(pool, lambda i=i: nc.gpsimd.memset(pow2[:, i:i + 1], 2.0 ** (-i)), partial=[pow2])
    S.op(pool, lambda: nc.gpsimd.memset(negone[:], -1.0), writes=[negone])
    s_ring = Ring([PS[0], PS[1]])
    sc_ring = Ring([PS[2]])
    l_ring = Ring([PS[4], PS[5]])
    Obank = PS[6]
    ps3b = Buf("ps3b", lambda: PS[3].ap_fn()[:, :].bitcast(BF16))
    PSBv = [S.view("psb0", PSB, (slice(None), slice(0, 512))), S.view("ps3b0", ps3b, (slice(None), slice(0, 512)))]
    ptp_ring = Ring(PSBv)
    SM_MX, SM_MN1, SM_MN2, SM_MN, SM_H, SM_TAU = range(6)
    MASKV = -30000.0

    def slot_geom(j):
        nkt = slot_nkt(j)
        nkb = (nkt + 3) // 4
        ub = slot_first_uncertain(j) // 4
        return nkb, nkb * 512, ub, (nkb - ub) * 512

    def stage_A(j):
        nkb, N, ub, W = slot_geom(j)
        assert W <= MBW
        scores = scores2[j % 2]; mb = mb2[j % 2]; diag = diag2[j % 2]
        iqt = iqt_ring.next()
        S.load(iqt, iqt[:], IQT, IQT[:, :, j * 128:(j + 1) * 128])
        for h in range(16):
            S.op(pool, lambda h=h: nc.gpsimd.tensor_scalar(out=diag[:, h, :], in0=ident[:], scalar1=iw_sb[:, j, h:h + 1],
                                                          scalar2=None, op0=ALU.mult),
                 reads=[ident, iw_sb], partial=[diag])
        S.op(pool, lambda: nc.gpsimd.tensor_scalar(out=mb[:, :W], in0=iota_sb[:, :W], scalar1=qadj_sb[:, j:j + 1],
                                                  scalar2=MASKV, op0=ALU.is_gt, op1=ALU.mult),
             reads=[iota_sb, qadj_sb], writes=[mb])
        yield
        items = [(kb, h) for kb in range(nkb) for h in range(16)]
        pend = None
        sc = None
        for (kb, h) in items + [(None, None)]:
            cur = None
            if kb is not None:
                sp_ = s_ring.next()
                ikp = ikT0 if h % 2 == 0 else ikT1
                mm(sp_[:, :], iqt[:, h // 2, :], ikp[:, kb * 512:(kb + 1) * 512], True, True,
                   [iqt, ikp], sp_)
                R = r_ring.next()
                S.op(act, lambda sp_=sp_, R=R: nc.scalar.activation(out=R[:], in_=sp_[:, :], func=AF.Relu),
                     reads=[sp_], writes=[R])
                cur = (kb, h, R)
            if pend is not None:
                pkb, ph, pR = pend
                if ph == 0:
                    sc = sc_ring.next()
                mm(sc[:, :], diag[:, ph, :], pR[:], ph == 0, (ph == 15 and pkb < ub), [diag, pR], sc)
                if ph == 15:
                    if pkb >= ub:
                        mm(sc[:, :], ident[:], mb[:, (pkb - ub) * 512:(pkb - ub + 1) * 512], False, True, [ident, mb], sc)
                    S.op(act, lambda sc=sc, pkb=pkb: nc.scalar.activation(out=scores[:, pkb * 512:(pkb + 1) * 512], in_=sc[:, :],
                                                                          func=AF.Copy), reads=[sc], partial=[scores])
            pend = cur
            if not (SKEW or SKEW_A) and pend is not None:
                pkb, ph, pR = pend
                if ph == 0:
                    sc = sc_ring.next()
                mm(sc[:, :], diag[:, ph, :], pR[:], ph == 0, (ph == 15 and pkb < ub), [diag, pR], sc)
                if ph == 15:
                    if pkb >= ub:
                        mm(sc[:, :], ident[:], mb[:, (pkb - ub) * 512:(pkb - ub + 1) * 512], False, True, [ident, mb], sc)
                    S.op(act, lambda sc=sc, pkb=pkb: nc.scalar.activation(out=scores[:, pkb * 512:(pkb + 1) * 512], in_=sc[:, :],
                                                                          func=AF.Copy), reads=[sc], partial=[scores])
                pend = None
            yield

    def stage_B(j):
        nkb, N, ub, W = slot_geom(j)
        scores = scores2[j % 2]; mb = mb2[j % 2]
        S.op(dve, lambda: nc.vector.tensor_reduce(out=sm[:, SM_MX:SM_MX + 1], in_=scores[:, :N], axis=AX.X, op=ALU.max),
             reads=[scores], partial=[sm])
        S.op(dve, lambda: nc.vector.scalar_tensor_tensor(out=tmpu[:, :W], in0=mb[:, :W], scalar=-2.0,
                                                         in1=scores[:, ub * 512:N], op0=ALU.mult, op1=ALU.add),
             reads=[mb, scores], writes=[tmpu])
        S.op(dve, lambda: nc.vector.tensor_reduce(out=sm[:, SM_MN2:SM_MN2 + 1], in_=tmpu[:, :W], axis=AX.X, op=ALU.min),
             reads=[tmpu], partial=[sm])
        if ub > 0:
            S.op(dve, lambda: nc.vector.tensor_reduce(out=sm[:, SM_MN1:SM_MN1 + 1], in_=scores[:, :ub * 512], axis=AX.X, op=ALU.min),
                 reads=[scores], partial=[sm])
            S.op(dve, lambda: nc.vector.tensor_tensor(out=sm[:, SM_MN:SM_MN + 1], in0=sm[:, SM_MN1:SM_MN1 + 1],
                                                      in1=sm[:, SM_MN2:SM_MN2 + 1], op=ALU.min), reads=[sm], partial=[sm])
        else:
            S.op(dve, lambda: nc.vector.tensor_copy(out=sm[:, SM_MN:SM_MN + 1], in_=sm[:, SM_MN2:SM_MN2 + 1]),
                 reads=[sm], partial=[sm])
        S.op(dve, lambda: nc.vector.tensor_scalar(out=sm[:, SM_H:SM_H + 1], in0=sm[:, SM_MX:SM_MX + 1],
                                                  scalar1=sm[:, SM_MN:SM_MN + 1], scalar2=0.50005, op0=ALU.subtract, op1=ALU.mult),
             reads=[sm], partial=[sm])
        mid = midr.next()
        S.op(dve, lambda mid=mid: nc.vector.tensor_scalar(out=mid[:], in0=sm[:, SM_MX:SM_MX + 1],
                                                          scalar1=sm[:, SM_MN:SM_MN + 1], scalar2=0.5, op0=ALU.add, op1=ALU.mult),
             reads=[sm], writes=[mid])
        S.op(dve, lambda: nc.vector.tensor_scalar(out=hs[:], in0=pow2[:], scalar1=sm[:, SM_H:SM_H + 1], scalar2=None, op0=ALU.mult),
             reads=[pow2, sm], writes=[hs])
        yield
        N1 = min(N, SEQ // 2)
        for it in range(NITER):
            S.op(dve, lambda mid=mid: nc.vector.tensor_scalar(out=junk[:, :N1], in0=scores[:, :N1], scalar1=mid[:, 0:1], scalar2=0.0,
                                                              op0=ALU.is_ge, op1=ALU.add, accum_out=cntb[:, 0:1]),
                 reads=[scores, mid], writes=[junk, cntb])
            if N > N1:
                S.op(dve, lambda mid=mid: nc.vector.tensor_scalar(out=junk[:, :N - N1], in0=scores[:, N1:N], scalar1=mid[:, 0:1],
                                                                  scalar2=cntb[:, 0:1], op0=ALU.is_ge, op1=ALU.add, accum_out=cntb[:, 1:2]),
                     reads=[scores, mid, cntb], writes=[junk], partial=[cntb])
                ccol = 1
            else:
                ccol = 0
            S.op(dve, lambda ccol=ccol: nc.vector.tensor_scalar(out=eb[:], in0=cntb[:, ccol:ccol + 1], scalar1=255.5, scalar2=0.5,
                                                                op0=ALU.is_ge, op1=ALU.subtract), reads=[cntb], writes=[eb])
            nmid = midr.next()
            S.op(dve, lambda mid=mid, nmid=nmid, it=it: nc.vector.scalar_tensor_tensor(
                out=nmid[:], in0=eb[:], scalar=hs[:, it:it + 1], in1=mid[:], op0=ALU.mult, op1=ALU.add),
                reads=[eb, hs, mid], writes=[nmid])
            mid = nmid
            yield
        S.op(dve, lambda mid=mid: nc.vector.scalar_tensor_tensor(
            out=sm[:, SM_TAU:SM_TAU + 1], in0=sm[:, SM_H:SM_H + 1], scalar=-(2.0 ** (-NITER)), in1=mid[:],
            op0=ALU.mult, op1=ALU.add), reads=[sm, mid], partial=[sm])
        S.op(dve, lambda: nc.vector.tensor_scalar(out=mbias[:, :N], in0=scores[:, :N], scalar1=sm[:, SM_TAU:SM_TAU + 1],
                                                  scalar2=MASKV, op0=ALU.is_lt, op1=ALU.mult),
             reads=[scores, sm], writes=[mbias])
        yield

    def stage_C(j):
        nkb, N, ub, W = slot_geom(j)
        qt = qt_ring.next()
        S.load(qt, qt[:], QT, QT[:, :, j * 128:(j + 1) * 128])
        S.op(pool, lambda: nc.gpsimd.memset(rs[:], 0.0), writes=[rs])
        yield
        items = [(kb, h) for kb in range(nkb) for h in range(4)]
        nI = len(items)
        st = {}
        blk = {}
        first_o = [True]

        def qk(i):
            kb, h = items[i]
            if h == 0:
                kblk = kb_ring.next(); vblk = vb_ring.next()
                S.load(kblk, kblk[:], KT, KT[:, :, kb * 512:(kb + 1) * 512])
                S.load(vblk, vblk[:], Vd, Vd.ap_fn()[kb * 512:(kb + 1) * 512, :].rearrange("(t p) d -> p t d", p=128))
                blk[kb] = (kblk, vblk)
            kblk, vblk = blk[kb]
            Lp = l_ring.next()
            mm(Lp[:, :], qt[:, h, :], kblk[:, h, :], True, False, [qt, kblk], Lp)
            mm(Lp[:, :], ident[:], mbias[:, kb * 512:(kb + 1) * 512], False, True, [ident, mbias], Lp)
            Pb = p_ring.next()
            S.op(act, lambda: nc.scalar.activation(out=Pb[:], in_=Lp[:, :], func=AF.Exp, scale=128.0 ** -0.5,
                                                   accum_out=rs[:, h, kb:kb + 1]),
                 reads=[Lp], writes=[Pb], partial=[rs])
            st[i] = [Pb, None]

        def tr(i):
            Pb = st[i][0]
            ptp = ptp_ring.next()
            for tt in range(4):
                S.op(pe, lambda tt=tt: nc.tensor.transpose(out=ptp[:, tt * 128:(tt + 1) * 128],
                                                           in_=Pb[:, tt * 128:(tt + 1) * 128], identity=ident[:]),
                     reads=[Pb, ident], writes=[ptp] if tt == 0 else (), partial=[ptp] if tt else ())
            PTb = pt_ring.next()
            S.op(act, lambda: nc.scalar.activation(out=PTb[:], in_=ptp[:, :], func=AF.Copy), reads=[ptp], writes=[PTb])
            st[i][1] = PTb

        def pv(i):
            kb, h = items[i]
            PTb = st[i][1]
            kblk, vblk = blk[kb]
            for tt in range(4):
                fo = first_o[0]
                first_o[0] = False
                S.op(pe, lambda tt=tt, fo=fo: nc.tensor.matmul(
                    Obank[:, h * 128:(h + 1) * 128], lhsT=PTb[:, tt * 128:(tt + 1) * 128],
                    rhs=vblk[:, tt, h * 128:(h + 1) * 128], start=fo, stop=(i == nI - 1 and tt == 3),
                    skip_group_check=True),
                    reads=[PTb, vblk], writes=[Obank] if fo else (), partial=() if fo else [Obank])
            del st[i]

        if SKEW:
            for g in range(nI + 2):
                if g < nI:
                    qk(g)
                if 1 <= g <= nI:
                    tr(g - 1)
                if g >= 2:
                    pv(g - 2)
                yield
        else:
            for g in range(nI):
                qk(g)
                tr(g)
                pv(g)
                yield
        for h in range(4):
            S.op(act, lambda h=h: nc.scalar.activation(out=rjunk[:, :], in_=rs[:, h, :], func=AF.Copy, accum_out=rsum[:, h:h + 1]),
                 reads=[rs], writes=[rjunk], partial=[rsum])
        S.op(pool, lambda: nc.gpsimd.tensor_tensor(out=rrec[:], in0=rsum[:], in1=negone[:], op=ALU.pow),
             reads=[rsum, negone], writes=[rrec])
        for h in range(4):
            S.op(act, lambda h=h: nc.scalar.activation(out=a_tok[:, h * 128:(h + 1) * 128], in_=Obank[:, h * 128:(h + 1) * 128],
                                                       func=AF.Copy, scale=rrec[:, h:h + 1]),
                 reads=[Obank, rrec], partial=[a_tok])
        ptp = ptp_ring.next()
        for h in range(4):
            S.op(pe, lambda h=h, ptp=ptp: nc.tensor.transpose(out=ptp[:, h * 128:(h + 1) * 128],
                                                              in_=a_tok[:, h * 128:(h + 1) * 128], identity=ident[:]),
                 reads=[a_tok, ident], writes=[ptp] if h == 0 else (), partial=[ptp] if h else ())
        aT = aT_ring.next()
        S.op(act, lambda ptp=ptp, aT=aT: nc.scalar.activation(out=aT[:].rearrange("p a b -> p (a b)"), in_=ptp[:, :], func=AF.Copy),
             reads=[ptp], writes=[aT])
        S.store(AT, AT[:, :, j * 128:(j + 1) * 128], aT, aT[:])
        yield

    def run_all(g):
        for _ in g:
            pass

    slot_list = list(slots) if slots is not None else list(range(NSLOT))
    ns = len(slot_list)
    run_all(stage_A(slot_list[0]))
    for si in range(ns + 1):
        gC = stage_C(slot_list[si - 1]) if si - 1 >= 0 else None
        gA = stage_A(slot_list[si + 1]) if si + 1 < ns else None
        gB = stage_B(slot_list[si]) if si < ns else None
        nC = slot_geom(slot_list[si - 1])[0] * 4 + 4 if gC is not None else 0
        nA = slot_geom(slot_list[si + 1])[0] * 16 + 2 if gA is not None else 0
        nB = NITER + 2 if gB is not None else 0
        live = {"A": gA, "B": gB, "C": gC}
        tot = {"A": nA, "B": nB, "C": nC}
        done = {"A": 0, "B": 0, "C": 0}
        wgt = {"A": 1.0, "B": 1.0, "C": 0.6}
        while any(g is not None for g in live.values()):
            best = None
            for k, g in live.items():
                if g is None:
                    continue
                frac = wgt[k] * done[k] / max(1, tot[k])
                if best is None or frac < best[0]:
                    best = (frac, k)
            k = best[1]
            if SEQ_PE and k == "A" and live["C"] is not None:
                k = "C"
            if k == "B" and done["B"] >= NITER + 1 and live["C"] is not None:
                k = "C"
            try:
                next(live[k])
                done[k] += 1
            except StopIteration:
                live[k] = None
    S.pop()
    S.pop()

    if stop_after == "3":
        dbg_a = S.dram("dbg_a", [128, 4, NOWN], BF16, kind="ExternalOutput")
        dbg_b = S.dram("dbg_b", [128, 4, NOWN], BF16, kind="ExternalOutput")
        S.push()
        big = S.sbuf("dbgbig", [128, 4, NOWN], BF16)
        S.load(big, big[:], AT, AT[:, :, :])
        S.store(dbg_a, dbg_a[:, :, :], big, big[:])
        S.load(big, big[:], BT, BT[:, :, :])
        S.store(dbg_b, dbg_b[:, :, :], big, big[:])
        S.finish([dbg_a, dbg_b])
        return nc

    GLU = S.dram("GLU", [128, 8, NOWN], BF16)
    hout_ring_holder = {}

    def residual_out(ps, n, c, res_buf, res_ap, hout):
        S.op(dve, lambda: nc.vector.tensor_tensor(out=hout[:, c, :n], in0=ps[:, :n], in1=res_ap, op=ALU.add),
             reads=[ps, res_buf], partial=[hout])

    S.push()
    wout_sb = S.sbuf("wout_sb", [128, 8, D], BF16)
    load_w_cast(wout_sb, 0, w_out_ab, w_out_ab.ap_fn(), 0, D)
    xt_ring = Ring([S.sbuf(f"xt4_{i}", [128, 8, 512], F32) for i in range(2)])
    ab_ring = Ring([S.sbuf(f"ab{i}", [128, 8, 512], BF16) for i in range(2)])
    hout_ring = Ring([S.sbuf(f"hout4_{i}", [128, 8, 512], F32) for i in range(2)])
    for (off, n, ci, is_halo, is_first) in own_blocks():
        xt = xt_ring.next(); ab = ab_ring.next(); hout = hout_ring.next()
        S.load(xt, xt[:, :, :n], xT_own, xT_own[:, :, off:off + n])
        S.load(ab, ab[:, 0:4, :n], AT, AT[:, :, off:off + n])
        S.load(ab, ab[:, 4:8, :n], BT, BT[:, :, off:off + n])
        for o in range(8):
            ps = proj_ps.next()
            proj(ps, wout_sb, o * 128, 128, [ab[:, c, :n] for c in range(8)], n, [ab])
            residual_out(ps, n, o, xt, xt[:, o, :n], hout)
        S.store(HT, HT[:, :, off:off + n], hout, hout[:, :, :n])
    S.pop()

    def cross_phase(l, blocks):
        S.push()
        gq, gk = (V2_CQ0, V2_CK0) if l == 0 else (V2_CQ1, V2_CK1)
        ncross = V_NCROSS0 if l == 0 else V_NCROSS1
        nmem = V_NMEM0 if l == 0 else V_NMEM1
        cm = alloc_common()
        kcT = S.sbuf("kcT", [128, 8, MEM], BF16)
        vc = S.sbuf("vc", [128, 2, D], BF16)
        sqc = S.sbuf("sqc", [128, 2, 512], BF16)
        rq = S.sbuf("rq", [128, 512], F32)
        tq = S.sbuf("tq", [128, 512], F32)
        S.push()
        wkv = S.sbuf("wkv", [128, 8, 2 * D], BF16)
        load_w_cast(wkv, 0, cross_wk, cross_wk.ap_fn()[l], 0, D)
        load_w_cast(wkv, D, cross_wv, cross_wv.ap_fn()[l], 0, D)
        norm_block(cm, memT, 0, MEM, nmem)
        xn_b = cm.xn_b
        n = MEM
        for hh in range(4):
            pss = [proj_ps.next(), proj_ps.next()]
            for dc in range(2):
                proj(pss[dc], wkv, (hh * 2 + dc) * 128, 128, [xn_b[:, c, :n] for c in range(8)], n, [xn_b])
                S.op(act, lambda dc=dc, pss=pss: nc.scalar.activation(out=sqc[:, dc, :n], in_=pss[dc][:, :n], func=AF.Square),
                     reads=[pss[dc]], partial=[sqc])
            ps2 = PS[5]
            for dc in range(2):
                mm(ps2[:, :n], ones_c[:], sqc[:, dc, :n], dc == 0, dc == 1, [ones_c, sqc], ps2)
            rstd_from_ms(ps2, n, rq, tq)
            for dc in range(2):
                S.op(dve, lambda dc=dc, pss=pss, hh=hh: nc.vector.scalar_tensor_tensor(
                    out=kcT[:, hh * 2 + dc, :n], in0=pss[dc][:, :n], scalar=vec2[:, gk + dc:gk + dc + 1], in1=rq[:, :n],
                    op0=ALU.mult, op1=ALU.mult), reads=[pss[dc], vec2, rq], partial=[kcT])
        for mt in range(2):
            for hf in range(2):
                ps = proj_ps.next()
                for c in range(8):
                    mm(ps[:, :], xn_b[:, c, mt * 128:(mt + 1) * 128], wkv[:, c, D + hf * 512:D + (hf + 1) * 512], c == 0, c == 7,
                       [wkv, xn_b], ps)
                S.op(act, lambda ps=ps, mt=mt, hf=hf: nc.scalar.activation(out=vc[:, mt, hf * 512:(hf + 1) * 512], in_=ps[:, :], func=AF.Copy),
                     reads=[ps], partial=[vc])
        S.pop()
        wqo = S.sbuf("wqo", [128, 8, 2 * D], BF16)
        load_w_cast(wqo, 0, cross_wq, cross_wq.ap_fn()[l], 0, D)
        load_w_cast(wqo, D, cross_wo, cross_wo.ap_fn()[l], 0, D)
        qn = S.sbuf("qn", [128, 8, 512], BF16)
        on = S.sbuf("on", [128, 8, 512], BF16)
        pc_ring = Ring([S.sbuf(f"pc{i}", [128, 2, 512], BF16) for i in range(2)])
        rden = S.sbuf("rden", [128, 512], F32)
        hout_ring = Ring([S.sbuf(f"houtc_{i}", [128, 8, 512], F32) for i in range(2)])
        for (off, n, ci, is_halo, is_first) in blocks:
            xt = norm_block(cm, HT, off, n, ncross)
            xn_b = cm.xn_b
            hout = hout_ring.next()
            for hh in range(4):
                pss = [proj_ps.next(), proj_ps.next()]
                for dc in range(2):
                    proj(pss[dc], wqo, (hh * 2 + dc) * 128, 128, [xn_b[:, c, :n] for c in range(8)], n, [xn_b])
                    S.op(act, lambda dc=dc, pss=pss: nc.scalar.activation(out=sqc[:, dc, :n], in_=pss[dc][:, :n], func=AF.Square),
                         reads=[pss[dc]], partial=[sqc])
                ps2 = PS[5]
                for dc in range(2):
                    mm(ps2[:, :n], ones_c[:], sqc[:, dc, :n], dc == 0, dc == 1, [ones_c, sqc], ps2)
                rstd_from_ms(ps2, n, rq, tq)
                for dc in range(2):
                    S.op(dve, lambda dc=dc, pss=pss, hh=hh: nc.vector.scalar_tensor_tensor(
                        out=qn[:, hh * 2 + dc, :n], in0=pss[dc][:, :n], scalar=vec2[:, gq + dc:gq + dc + 1], in1=rq[:, :n],
                        op0=ALU.mult, op1=ALU.mult), reads=[pss[dc], vec2, rq], partial=[qn])
                pc = pc_ring.next()
                for mt in range(2):
                    ps = proj_ps.next()
                    for dc in range(2):
                        mm(ps[:, :n], kcT[:, hh * 2 + dc, mt * 128:(mt + 1) * 128], qn[:, hh * 2 + dc, :n], dc == 0, dc == 1,
                           [kcT, qn], ps)
                    S.op(act, lambda ps=ps, mt=mt, pc=pc: nc.scalar.activation(out=pc[:, mt, :n], in_=ps[:, :n], func=AF.Exp,
                                                                                scale=1.0 / 16.0), reads=[ps], partial=[pc])
                psd = PS[6]
                for mt in range(2):
                    mm(psd[:, :n], ones_1[:], pc[:, mt, :n], mt == 0, mt == 1, [ones_1, pc], psd)
                S.op(dve, lambda psd=psd: nc.vector.reciprocal(out=rden[:, :n], in_=psd[:, :n]), reads=[psd], writes=[rden])
                for dc in range(2):
                    ps = proj_ps.next()
                    for mt in range(2):
                        mm(ps[:, :n], vc[:, mt, hh * 256 + dc * 128:hh * 256 + (dc + 1) * 128], pc[:, mt, :n], mt == 0, mt == 1,
                           [vc, pc], ps)
                    S.op(dve, lambda ps=ps, dc=dc, hh=hh: nc.vector.tensor_tensor(out=on[:, hh * 2 + dc, :n], in0=ps[:, :n],
                                                                                 in1=rden[:, :n], op=ALU.mult),
                         reads=[ps, rden], partial=[on])
            for o in range(8):
                ps = proj_ps.next()
                proj(ps, wqo, D + o * 128, 128, [on[:, c, :n] for c in range(8)], n, [on])
                residual_out(ps, n, o, xt, xt[:, o, :n], hout)
            S.store(HT, HT[:, :, off:off + n], hout, hout[:, :, :n])
        S.pop()

    def ffn_phase(l, blocks, final):
        nffn = V_NFFN0 if l == 0 else V_NFFN1
        S.push()
        cm = alloc_common()
        wgu = S.sbuf("wgu", [128, 8, 2 * DFF], BF16)
        load_w_cast(wgu, 0, ffn_wg, ffn_wg.ap_fn()[l], 0, DFF)
        load_w_cast(wgu, DFF, ffn_wu, ffn_wu.ap_fn()[l], 0, DFF)
        hid_ring = Ring([S.sbuf(f"hid{i}", [128, 22, 512], BF16) for i in range(2)])
        sg_ring = Ring([S.sbuf(f"sg{i}", [128, 512], F32) for i in range(2)])
        for (off, n, ci, is_halo, is_first) in blocks:
            norm_block(cm, HT, off, n, nffn)
            xn_b = cm.xn_b
            hid = hid_ring.next()
            xl = [xn_b[:, c, :n] for c in range(8)]
            for f in range(22):
                psg = proj_ps.next(); psu = proj_ps.next()
                proj(psg, wgu, f * 128, 128, xl, n, [xn_b])
                proj(psu, wgu, DFF + f * 128, 128, xl, n, [xn_b])
                sg = sg_ring.next()
                S.op(act, lambda psg=psg, sg=sg: nc.scalar.activation(out=sg[:, :n], in_=psg[:, :n], func=AF.Silu),
                     reads=[psg], writes=[sg])
                S.op(dve, lambda psu=psu, sg=sg, f=f: nc.vector.tensor_tensor(out=hid[:, f, :n], in0=psu[:, :n], in1=sg[:, :n], op=ALU.mult),
                     reads=[psu, sg], partial=[hid])
            S.store(HID, HID[:, :, off:off + n], hid, hid[:, :, :n])
        S.pop()
        S.push()
        wd = S.sbuf("wd", [128, 22, D], BF16)
        load_w_cast(wd, 0, ffn_wd, ffn_wd.ap_fn()[l], 0, D)
        hid_ring = Ring([S.sbuf(f"hidb{i}", [128, 22, 512], BF16) for i in range(2)])
        xt_ring = Ring([S.sbuf(f"xtd_{i}", [128, 8, 512], F32) for i in range(2)])
        hout_ring = Ring([S.sbuf(f"houtd_{i}", [128, 8, 512], F32) for i in range(2)])
        for (off, n, ci, is_halo, is_first) in blocks:
            hid = hid_ring.next(); xt = xt_ring.next(); hout = hout_ring.next()
            S.load(hid, hid[:, :, :n], HID, HID[:, :, off:off + n])
            S.load(xt, xt[:, :, :n], HT, HT[:, :, off:off + n])
            for o in range(8):
                ps = proj_ps.next()
                proj(ps, wd, o * 128, 128, [hid[:, f, :n] for f in range(22)], n, [hid])
                residual_out(ps, n, o, xt, xt[:, o, :n], hout)
            if final:
                slot0 = off // 128
                ci_, i_ = divmod(slot0, SLOTS_PER_CH)
                oo = (ci_ * CH_TILES + i_ - 1) * 128
                S.store(out_hT, out_hT[:, :, oo:oo + n], hout, hout[:, :, :n])
            else:
                S.store(HT, HT[:, :, off:off + n], hout, hout[:, :, :n])
        S.pop()

    cross_phase(0, own_blocks())
    ffn_phase(0, own_blocks(), False)

    S.push()
    cm = alloc_common()
    wci = S.sbuf("wci", [128, 8, 2 * D], BF16)
    load_w_cast(wci, 0, conv_w_in, conv_w_in.ap_fn(), 0, 2 * D)
    hflag = S.sbuf("hflag", [128, 2], F32)
    S.load(hflag, hflag[:], haloflag_in, haloflag_in[:, :])
    sig_ring = Ring([S.sbuf(f"sig{i}", [128, 512], F32) for i in range(2)])
    glu_ring = Ring([S.sbuf(f"glu{i}", [128, 8, 512], BF16) for i in range(2)])
    for (off, n, ci, is_halo, is_first) in own_blocks():
        norm_block(cm, HT, off, n, V_NMIX1)
        xn_b = cm.xn_b
        xl = [xn_b[:, c, :n] for c in range(8)]
        glu = glu_ring.next()
        for c8 in range(8):
            psa = proj_ps.next(); psg = proj_ps.next()
            proj(psa, wci, c8 * 128, 128, xl, n, [xn_b])
            proj(psg, wci, D + c8 * 128, 128, xl, n, [xn_b])
            sig = sig_ring.next()
            S.op(act, lambda psg=psg, sig=sig, c8=c8: nc.scalar.activation(out=sig[:, :n], in_=psg[:, :n], func=AF.Sigmoid,
                                                                          bias=vec2[:, V2_BIN + 8 + c8:V2_BIN + 9 + c8], scale=1.0),
                 reads=[psg, vec2], writes=[sig])
            S.op(dve, lambda psa=psa, sig=sig, c8=c8: nc.vector.scalar_tensor_tensor(
                out=glu[:, c8, :n], in0=psa[:, :n], scalar=vec2[:, V2_BIN + c8:V2_BIN + c8 + 1], in1=sig[:, :n],
                op0=ALU.add, op1=ALU.mult), reads=[psa, vec2, sig], partial=[glu])
        if is_halo:
            S.op(dve, lambda glu=glu: nc.vector.tensor_scalar(out=glu[:, :, :n], in0=glu[:, :, :n], scalar1=hflag[:, ci:ci + 1],
                                                             scalar2=None, op0=ALU.mult), reads=[hflag, glu], partial=[glu])
        S.store(GLU, GLU[:, :, off:off + n], glu, glu[:, :, :n])
    S.pop()

    S.push()
    wco = S.sbuf("wco", [128, 8, D], BF16)
    load_w_cast(wco, 0, conv_w_out, conv_w_out.ap_fn(), 0, D)
    dw_sb = S.sbuf("dw_sb", [128, 8, 31], F32)
    S.load(dw_sb, dw_sb[:], conv_dw, conv_dw[:, :, :])
    dg = S.sbuf("dg", [128, 8, 31, 128], BF16)
    for c8 in range(8):
        for k in range(31):
            S.op(act, lambda c8=c8, k=k: nc.scalar.activation(out=dg[:, c8, k, :], in_=ident[:], func=AF.Copy,
                                                              scale=dw_sb[:, c8, k:k + 1]), reads=[ident, dw_sb], partial=[dg])
    gin_ring = Ring([S.sbuf(f"gin{i}", [128, 8, 544], BF16) for i in range(2)])
    hc = S.sbuf("hc", [128, 8, 512], F32)
    hcb = S.sbuf("hcb", [128, 8, 512], BF16)
    hsq = S.sbuf("hsq", [128, 8, 512], BF16)
    mean_sb = S.sbuf("mean_sb", [128, 512], F32)
    var_sb = S.sbuf("var_sb", [128, 512], F32)
    rstd2 = S.sbuf("rstd2", [128, 512], F32)
    tv = S.sbuf("tv", [128, 512], F32)
    dtmp = Ring([S.sbuf(f"dtmp{i}", [128, 512], F32) for i in range(2)])
    sl = S.sbuf("sl", [128, 8, 512], BF16)
    xt_ring = Ring([S.sbuf(f"xtv_{i}", [128, 8, 512], F32) for i in range(1)])
    hout_ring = Ring([S.sbuf(f"houtv_{i}", [128, 8, 512], F32) for i in range(1)])
    for (off, n, ci, is_halo, is_first) in own_blocks(False):
        gin = gin_ring.next(); xt = xt_ring.next(); hout = hout_ring.next()
        S.load(gin, gin[:, :, :n + 30], GLU, GLU[:, :, off - 30:off + n])
        S.load(xt, xt[:, :, :n], HT, HT[:, :, off:off + n])
        for c8 in range(8):
            ps = proj_ps.next()
            for k in range(31):
                mm(ps[:, :n], dg[:, c8, k, :], gin[:, c8, k:k + n], k == 0, k == 30, [dg, gin], ps)
            S.op(act, lambda ps=ps, c8=c8: nc.scalar.activation(out=hc[:, c8, :n], in_=ps[:, :n], func=AF.Identity,
                                                                bias=vec2[:, V2_DWB + c8:V2_DWB + c8 + 1], scale=1.0),
                 reads=[ps, vec2], partial=[hc])
        S.op(dve, lambda: nc.vector.tensor_copy(out=hcb[:, :, :n], in_=hc[:, :, :n]), reads=[hc], writes=[hcb])
        S.op(act, lambda: nc.scalar.activation(out=hsq[:, :, :n], in_=hc[:, :, :n], func=AF.Square), reads=[hc], writes=[hsq])
        psm, psq = PS[5], PS[6]
        for c8 in range(8):
            mm(psm[:, :n], ones_m[:], hcb[:, c8, :n], c8 == 0, c8 == 7, [ones_m, hcb], psm)
        for c8 in range(8):
            mm(psq[:, :n], ones_m[:], hsq[:, c8, :n], c8 == 0, c8 == 7, [ones_m, hsq], psq)
        S.op(act, lambda: nc.scalar.activation(out=mean_sb[:, :n], in_=psm[:, :n], func=AF.Copy), reads=[psm], writes=[mean_sb])
        S.op(dve, lambda: nc.vector.tensor_tensor(out=var_sb[:, :n], in0=mean_sb[:, :n], in1=mean_sb[:, :n], op=ALU.mult),
             reads=[mean_sb], writes=[var_sb])
        S.op(dve, lambda: nc.vector.tensor_tensor(out=var_sb[:, :n], in0=psq[:, :n], in1=var_sb[:, :n], op=ALU.subtract),
             reads=[psq, var_sb], writes=[var_sb])
        S.op(act, lambda: nc.scalar.activation(out=tv[:, :n], in_=var_sb[:, :n], func=AF.Sqrt, bias=eps_t[:, 0:1], scale=1.0),
             reads=[var_sb, eps_t], writes=[tv])
        S.op(dve, lambda: nc.vector.reciprocal(out=rstd2[:, :n], in_=tv[:, :n]), reads=[tv], writes=[rstd2])
        for c8 in range(8):
            dt_ = dtmp.next()
            S.op(pool, lambda c8=c8, dt_=dt_: nc.gpsimd.tensor_tensor(out=dt_[:, :n], in0=hc[:, c8, :n], in1=mean_sb[:, :n], op=ALU.subtract),
                 reads=[hc, mean_sb], writes=[dt_])
            S.op(dve, lambda c8=c8, dt_=dt_: nc.vector.tensor_tensor(out=dt_[:, :n], in0=dt_[:, :n], in1=rstd2[:, :n], op=ALU.mult),
                 reads=[dt_, rstd2], writes=[dt_])
            S.op(act, lambda c8=c8, dt_=dt_: nc.scalar.activation(out=sl[:, c8, :n], in_=dt_[:, :n], func=AF.Silu,
                                                                  bias=vec2[:, V2_LNB + c8:V2_LNB + c8 + 1],
                                                                  scale=vec2[:, V2_LNG + c8:V2_LNG + c8 + 1]),
                 reads=[dt_, vec2], partial=[sl])
        for o in range(8):
            ps = proj_ps.next()
            proj(ps, wco, o * 128, 128, [sl[:, c, :n] for c in range(8)], n, [sl])
            residual_out(ps, n, o, xt, xt[:, o, :n], hout)
        S.store(HT, HT[:, :, off:off + n], hout, hout[:, :, :n])
    S.pop()

    cross_phase(1, own_blocks(False))
    ffn_phase(1, own_blocks(False), True)
    S.finish([out_hT])
    return nc


def _fm(a):
    t = a.shape[0]
    return np.ascontiguousarray(a.T.reshape(8, 128, t).transpose(1, 0, 2))


def _colvec(v):
    return np.ascontiguousarray(v.reshape(-1, 128).T)


def prepare_inputs(inputs, stop_after=None):
    f = lambda k: np.asarray(inputs[k], dtype=np.float32)
    x = f("x"); mem = f("mem")
    w_in = f("w_in_ab")[0]
    sp = np.cumsum((512, 512, 512, 512, 1024, 16, 64))[:-1]
    wq, wk, wv, wu, wiq, wiw, wik = np.split(w_in, sp, axis=1)
    w_keys = np.ascontiguousarray(np.concatenate([wk, wik, wik, wv], axis=1))
    w_own = np.ascontiguousarray(np.concatenate([wq, wiq, wu, wiw], axis=1))
    vecs = np.concatenate([_colvec(f(k)[l]) for k in ("norm_mix", "norm_cross", "norm_mem", "norm_ffn")
                           for l in range(2)], axis=1)
    vecs = np.ascontiguousarray(vecs.astype(np.float32))
    v2 = np.zeros((128, 64), np.float32)
    v2[:, 0] = f("a_q_norm")[0]; v2[:, 1] = f("a_k_norm")[0]
    v2[:, 2:6] = _colvec(f("pool_scale")[0])
    v2[:, 6:8] = _colvec(f("cross_q_norm")[0]); v2[:, 8:10] = _colvec(f("cross_q_norm")[1])
    v2[:, 10:12] = _colvec(f("cross_k_norm")[0]); v2[:, 12:14] = _colvec(f("cross_k_norm")[1])
    v2[:, 14:30] = _colvec(f("conv_b_in")[0])
    v2[:, 30:38] = _colvec(f("conv_dw_b")[0])
    v2[:, 38:46] = _colvec(f("conv_ln_g")[0])
    v2[:, 46:54] = _colvec(f("conv_ln_b")[0])
    conv_dw = np.ascontiguousarray(f("conv_dw_w")[0].T.reshape(8, 128, 31).transpose(1, 0, 2))
    seqpos = np.arange(SEQ)
    c128s, s128s = rope_tables(seqpos, 128)
    c64s, s64s = rope_tables(seqpos, 64)
    shared = dict(
        iota=np.ascontiguousarray(np.broadcast_to(np.arange(MBW, dtype=np.float32), (128, MBW))),
        c128s=c128s, s128s=s128s, c64s=c64s, s64s=s64s,
        p128=perm_matrix(128), p64=perm_matrix(64), ident=np.eye(128, dtype=np.float32),
        w_keys=w_keys, w_own=w_own, vecs=vecs, vecs2=v2,
        pool_w=f("pool_w")[0], w_out_ab=f("w_out_ab")[0], conv_w_in=f("conv_w_in")[0], conv_dw=conv_dw,
        conv_w_out=f("conv_w_out")[0], cross_wq=f("cross_wq"), cross_wk=f("cross_wk"),
        cross_wv=f("cross_wv"), cross_wo=f("cross_wo"), ffn_wg=f("ffn_w_gate"), ffn_wu=f("ffn_w_up"),
        ffn_wd=f("ffn_w_down"),
    )
    in_maps = []
    for core in range(8):
        b, half = divmod(core, 2)
        tiles = own_tiles(half)
        xo = np.zeros((NOWN, D), np.float32)
        pos = np.zeros(NOWN, np.int64)
        for s, t in enumerate(tiles):
            if t >= 0:
                xo[s * 128:(s + 1) * 128] = x[b, t * 128:(t + 1) * 128]
                pos[s * 128:(s + 1) * 128] = np.arange(t * 128, (t + 1) * 128)
        qa = np.zeros((128, NSLOT), np.float32)
        for s in range(NSLOT):
            base = (slot_first_uncertain(s) // 4) * 512
            qa[:, s] = pos[s * 128:(s + 1) * 128] - base
        c128o, s128o = rope_tables(pos, 128)
        c64o, s64o = rope_tables(pos, 64)
        pc = np.ones((128, 2, 4, 16), np.float32)
        hf = np.ones((128, 2), np.float32)
        if half == 0:
            hf[:, 0] = 0.0
            for g, w in enumerate((2, 4, 8, 16)):
                for t in range(16):
                    pc[:, 0, g, t] = w / min(t + 1, w)
        m = dict(shared)
        m.update(xT_seq=_fm(x[b]), xT_own=_fm(xo), memT=_fm(mem[b]), qadj=qa,
                 c128o=c128o, s128o=s128o, c64o=c64o, s64o=s64o, poolcorr=pc, haloflag=hf)
        in_maps.append(m)
    return in_maps


_NC_CACHE = {}


def kernel(**inputs):
    in_maps = prepare_inputs(inputs)
    if "nc" not in _NC_CACHE:
        _NC_CACHE["nc"] = build_program()
    nc = _NC_CACHE["nc"]
    res = run_bass_kernel_spmd(nc, in_maps, core_ids=list(range(8)))
    out = np.zeros((4, SEQ, D), np.float32)
    for core in range(8):
        b, half = divmod(core, 2)
        o = res.results[core]["out_hT"]
        o = o.transpose(2, 1, 0).reshape(32 * 128, D)
        for ci in range(2):
            start = (2 * ci + half) * CH_TILES * 128
            out[b, start:start + 2048] = o[ci * 2048:(ci + 1) * 2048]
    return out
```

```python
import numpy as np
import ml_dtypes
import concourse.bass as bass
import concourse.mybir as mybir
from concourse.bass_utils import run_bass_kernel_spmd

F32 = mybir.dt.float32
BF16 = mybir.dt.bfloat16
AF = mybir.ActivationFunctionType
ALU = mybir.AluOpType
AX = mybir.AxisListType

D = 1024
SEQ = 8192
NT_SEQ = SEQ // 128
CH_TILES = 16
SLOTS_PER_CH = CH_TILES + 1
NSLOT = 2 * SLOTS_PER_CH
NOWN = NSLOT * 128
MEM = 256
DFF = 2816
EPS = 1e-6
NEG = -1.0e30
NITER = 24
MBW = 3072

SAME_ENGINE_SYNC = True
SEQ_PE = True
SKEW = True
SKEW_A = True


class Buf:
    def __init__(self, name, ap_fn):
        self.name = name
        self.ap_fn = ap_fn
        self.w = {}
        self.r = {}
        self.dsem = None
        self.dval = 0

    def __getitem__(self, idx):
        return self.ap_fn()[idx]


class Eng:
    def __init__(self, name, inst, is_pe=False):
        self.name = name
        self.inst = inst
        self.sem = None
        self.cnt = 0
        self.known = {}
        self.is_pe = is_pe


class Sched:
    EPOCH = 30000

    def __init__(self, nc):
        self.nc = nc
        self.pe = Eng("pe", nc.tensor, True)
        self.act = Eng("act", nc.scalar)
        self.dve = Eng("dve", nc.vector)
        self.pool = Eng("pool", nc.gpsimd)
        self.sp = Eng("sp", nc.sync)
        self.engs = [self.pe, self.act, self.dve, self.pool, self.sp]
        self.nsem = 0
        self.all_dma_bufs = []
        self.nbuf = 0
        self.scopes = []
        self.live = []
        self.all_sems = []

    def new_sem(self, name):
        self.nsem += 1
        sm_ = self.nc.alloc_semaphore(f"{name}_{self.nsem}")
        self.all_sems.append(sm_)
        return sm_

    def sbuf(self, name, shape, dtype):
        self.nbuf += 1
        if self.scopes:
            t = self.scopes[-1].enter_context(self.nc.sbuf_tensor(f"{name}_{self.nbuf}", list(shape), dtype))
        else:
            t = self.nc.alloc_sbuf_tensor(f"{name}_{self.nbuf}", list(shape), dtype)
        b = Buf(name, lambda: t)
        self.live.append(b)
        return b

    def push(self):
        import contextlib
        self.scopes.append(contextlib.ExitStack())

    def pop(self):
        self.barrier()
        self.scopes.pop().close()

    def barrier(self):
        evs = {}
        for e in self.engs:
            if e.sem is not None and e.cnt > 0:
                evs[id(e.sem)] = (e.sem, e.cnt)
        for b in self.live:
            if b.dsem is not None and b.dval > 0:
                evs[id(b.dsem)] = (b.dsem, b.dval)
        for e in self.engs:
            for k, (sm, v) in evs.items():
                if e.sem is not None and sm is e.sem:
                    continue
                if e.known.get(k, 0) >= v:
                    continue
                e.inst.wait_ge(sm, v)
                e.known[k] = v

    def psum(self, name, shape, dtype):
        self.nbuf += 1
        t = self.nc.alloc_psum_tensor(f"{name}_{self.nbuf}", list(shape), dtype)
        return Buf(name, lambda: t)

    def dram(self, name, shape, dtype, kind="Internal"):
        t = self.nc.dram_tensor(name, list(shape), dtype, kind=kind)
        a = t.ap()
        return Buf(name, lambda: a)

    def view(self, name, buf, idx):
        return Buf(name, lambda: buf.ap_fn()[idx])

    def _collect(self, eng, reads, writes):
        need = {}

        def add(d):
            for s, v in d.items():
                k = id(s)
                if k not in need or need[k][1] < v:
                    need[k] = (s, v)
        for b in reads:
            add(b.w)
        for b in writes:
            add(b.w)
            add(b.r)
        for k, (s, v) in need.items():
            if eng.sem is not None and s is eng.sem:
                if eng.is_pe or not SAME_ENGINE_SYNC:
                    continue
            if eng.known.get(k, 0) >= v:
                continue
            eng.inst.wait_ge(s, v)
            eng.known[k] = v

    def op(self, eng, fn, reads=(), writes=(), partial=()):
        allw = list(writes) + list(partial)
        self._collect(eng, reads, allw)
        if eng.sem is None or eng.cnt >= self.EPOCH:
            eng.sem = self.new_sem(eng.name)
            eng.cnt = 0
        ins = fn()
        eng.cnt += 1
        ins.then_inc(eng.sem, 1)
        s, v = eng.sem, eng.cnt
        for b in reads:
            if b.r.get(s, 0) < v:
                b.r[s] = v
        for b in writes:
            b.w = {s: v}
            b.r = {}
        for b in partial:
            b.w[s] = v
        return ins

    def dma(self, out_buf, out_ap, in_buf, in_ap, eng=None, sbuf_side=None, **kw):
        eng = eng or self.sp
        owner = sbuf_side
        self._collect(eng, [in_buf], [out_buf])
        if owner.dsem is None or owner.dval >= self.EPOCH:
            owner.dsem = self.new_sem("d" + owner.name)
            owner.dval = 0
        ins = eng.inst.dma_start(out=out_ap, in_=in_ap, **kw)
        owner.dval += 16
        ins.then_inc(owner.dsem, 16)
        s, v = owner.dsem, owner.dval
        if in_buf.r.get(s, 0) < v:
            in_buf.r[s] = v
        out_buf.w[s] = v
        return ins

    def load(self, dst, dst_ap, src, src_ap, eng=None, **kw):
        return self.dma(dst, dst_ap, src, src_ap, eng=eng, sbuf_side=dst, **kw)

    def store(self, dst, dst_ap, src, src_ap, eng=None, **kw):
        return self.dma(dst, dst_ap, src, src_ap, eng=eng, sbuf_side=src, **kw)

    def finish(self, bufs):
        self._collect(self.sp, bufs, [])


class Ring:
    def __init__(self, bufs):
        self.bufs = bufs
        self.i = 0

    def next(self):
        b = self.bufs[self.i % len(self.bufs)]
        self.i += 1
        return b


def own_tiles(half):
    tiles = []
    for ci in range(2):
        start = (2 * ci + half) * CH_TILES
        tiles.append(start - 1)
        tiles.extend(range(start, start + CH_TILES))
    return tiles


def slot_nkt(slot):
    ci, i = divmod(slot, SLOTS_PER_CH)
    t1 = (2 * ci + 1) * CH_TILES + i - 1
    return t1 + 1


def slot_first_uncertain(slot):
    ci, i = divmod(slot, SLOTS_PER_CH)
    t0 = 2 * ci * CH_TILES + i - 1
    return max(t0, 0)


def rope_tables(pos, head_dim):
    rot = head_dim // 4
    half = rot // 2
    inv = 500000.0 ** (-np.arange(half, dtype=np.float32) * 2.0 / rot)
    ang = pos.astype(np.float32)[None, :] * inv[:, None].astype(np.float32)
    cos = np.cos(ang).astype(np.float32)
    sin = np.sin(ang).astype(np.float32)
    C = np.ones((128, len(pos)), np.float32)
    S = np.zeros((128, len(pos)), np.float32)
    for h0 in range(0, 128, head_dim):
        C[h0:h0 + half] = cos
        C[h0 + half:h0 + rot] = cos
        S[h0:h0 + half] = -sin
        S[h0 + half:h0 + rot] = sin
    return C, S


def perm_matrix(head_dim):
    rot = head_dim // 4
    half = rot // 2
    P = np.zeros((128, 128), np.float32)
    for h0 in range(0, 128, head_dim):
        for i in range(half):
            P[h0 + half + i, h0 + i] = 1.0
            P[h0 + i, h0 + half + i] = 1.0
    return P


def build_program(stop_after=None, slots=None):
    nc = bass.Bass("TRN2", target_bir_lowering=False)
    S = Sched(nc)
    pe, act, dve, pool, sp = S.pe, S.act, S.dve, S.pool, S.sp
    dbg = {}

    def din(name, shape, dtype=F32):
        return S.dram(name, shape, dtype, kind="ExternalInput")

    xT_seq = din("xT_seq", [128, 8, SEQ])
    xT_own = din("xT_own", [128, 8, NOWN])
    memT = din("memT", [128, 8, MEM])
    qadj = din("qadj", [128, NSLOT])
    iota_in = din("iota", [128, MBW])
    c128s = din("c128s", [128, SEQ]); s128s = din("s128s", [128, SEQ])
    c64s = din("c64s", [128, SEQ]); s64s = din("s64s", [128, SEQ])
    c128o = din("c128o", [128, NOWN]); s128o = din("s128o", [128, NOWN])
    c64o = din("c64o", [128, NOWN]); s64o = din("s64o", [128, NOWN])
    p128_in = din("p128", [128, 128]); p64_in = din("p64", [128, 128])
    ident_in = din("ident", [128, 128])
    poolcorr_in = din("poolcorr", [128, 2, 4, 16])
    haloflag_in = din("haloflag", [128, 2])
    w_keys = din("w_keys", [D, 1152])
    w_own = din("w_own", [D, 2064])
    vecs = din("vecs", [128, 64])
    pool_w = din("pool_w", [4, 128, 128])
    w_out_ab = din("w_out_ab", [D, D])
    conv_w_in = din("conv_w_in", [D, 2 * D])
    conv_dw = din("conv_dw", [128, 8, 31])
    conv_w_out = din("conv_w_out", [D, D])
    cross_wq = din("cross_wq", [2, D, D]); cross_wk = din("cross_wk", [2, D, D])
    cross_wv = din("cross_wv", [2, D, D]); cross_wo = din("cross_wo", [2, D, D])
    ffn_wg = din("ffn_wg", [2, D, DFF]); ffn_wu = din("ffn_wu", [2, D, DFF])
    ffn_wd = din("ffn_wd", [2, DFF, D])
    out_hT = S.dram("out_hT", [128, 8, 32 * 128], F32, kind="ExternalOutput")

    KT = S.dram("KT", [128, 4, SEQ], BF16)
    Vd = S.dram("Vd", [SEQ, 512], BF16)
    QT = S.dram("QT", [128, 4, NOWN], BF16)
    IQT = S.dram("IQT", [128, 8, NOWN], BF16)
    BT = S.dram("BT", [128, 4, NOWN], BF16)
    AT = S.dram("AT", [128, 4, NOWN], BF16)
    HT = S.dram("HT", [128, 8, NOWN], F32)
    HID = S.dram("HID", [128, 22, NOWN], BF16)

    ones_m = S.sbuf("ones_m", [128, 128], BF16)
    ones_h = S.sbuf("ones_h", [128, 128], BF16)
    ones_c = S.sbuf("ones_c", [128, 128], BF16)
    ones_1 = S.sbuf("ones_1", [128, 128], BF16)
    ident = S.sbuf("ident", [128, 128], BF16)
    p128 = S.sbuf("p128", [128, 128], BF16)
    p64 = S.sbuf("p64", [128, 128], BF16)
    cst_f = S.sbuf("cst_f", [128, 3, 128], F32)
    vec = S.sbuf("vec", [128, 64], F32)
    qadj_sb = S.sbuf("qadj_sb", [128, NSLOT], F32)
    eps_t = S.sbuf("eps_t", [128, 1], F32)

    S.op(pool, lambda: nc.gpsimd.memset(ones_m[:], 1.0 / 1024), writes=[ones_m])
    S.op(pool, lambda: nc.gpsimd.memset(ones_h[:], 1.0 / 128), writes=[ones_h])
    S.op(pool, lambda: nc.gpsimd.memset(ones_c[:], 1.0 / 256), writes=[ones_c])
    S.op(pool, lambda: nc.gpsimd.memset(ones_1[:], 1.0), writes=[ones_1])
    S.op(pool, lambda: nc.gpsimd.memset(eps_t[:], EPS), writes=[eps_t])
    S.load(cst_f, cst_f[:, 0, :], ident_in, ident_in[:, :])
    S.load(cst_f, cst_f[:, 1, :], p128_in, p128_in[:, :])
    S.load(cst_f, cst_f[:, 2, :], p64_in, p64_in[:, :])
    S.load(vec, vec[:], vecs, vecs[:, :])
    S.load(qadj_sb, qadj_sb[:], qadj, qadj[:, :])
    S.op(dve, lambda: nc.vector.tensor_copy(out=ident[:], in_=cst_f[:, 0, :]), reads=[cst_f], writes=[ident])
    S.op(dve, lambda: nc.vector.tensor_copy(out=p128[:], in_=cst_f[:, 1, :]), reads=[cst_f], writes=[p128])
    S.op(dve, lambda: nc.vector.tensor_copy(out=p64[:], in_=cst_f[:, 2, :]), reads=[cst_f], writes=[p64])

    V_NMIX0, V_NMIX1, V_NCROSS0, V_NCROSS1, V_NMEM0, V_NMEM1, V_NFFN0, V_NFFN1 = [8 * i for i in range(8)]
    vec2_in = din("vecs2", [128, 64])
    vec2 = S.sbuf("vec2", [128, 64], F32)
    S.load(vec2, vec2[:], vec2_in, vec2_in[:, :])
    V2_AQ, V2_AK = 0, 1
    V2_PSCALE = 2
    V2_CQ0, V2_CQ1, V2_CK0, V2_CK1 = 6, 8, 10, 12
    V2_BIN = 14
    V2_DWB = 30
    V2_LNG = 38
    V2_LNB = 46

    PS = [S.psum(f"ps{i}", [128, 512], F32) for i in range(7)]
    PSB = S.psum("psb", [128, 1024], BF16)

    def load_w_cast(dst, col_dst, w_buf, w_ap, col0, M):
        K = w_ap.shape[0]
        for kc in range(K // 128):
            m0 = 0
            while m0 < M:
                mm_ = min(2048, M - m0)
                S.load(dst, dst[:, kc, col_dst + m0:col_dst + m0 + mm_], w_buf,
                       w_ap[kc * 128:(kc + 1) * 128, col0 + m0:col0 + m0 + mm_], eng=pool)
                m0 += mm_

    def mm(ps_ap, lhsT, rhs, start, stop, reads, ps_buf):
        S.op(pe, lambda: nc.tensor.matmul(ps_ap, lhsT=lhsT, rhs=rhs, start=start, stop=stop),
             reads=reads, partial=[ps_buf] if not start else (), writes=[ps_buf] if start else ())

    def rstd_from_ms(ps_buf, n, out_buf, tmp_buf):
        S.op(act, lambda: nc.scalar.activation(out=tmp_buf[:, :n], in_=ps_buf[:, :n], func=AF.Sqrt,
                                               bias=eps_t[:, 0:1], scale=1.0),
             reads=[ps_buf, eps_t], writes=[tmp_buf])
        S.op(dve, lambda: nc.vector.reciprocal(out=out_buf[:, :n], in_=tmp_buf[:, :n]),
             reads=[tmp_buf], writes=[out_buf])

    def own_blocks(include_halo=True):
        res = []
        for ci in range(2):
            base = ci * SLOTS_PER_CH * 128
            if include_halo:
                res.append((base, 128, ci, True, False))
            for i in range(4):
                res.append((base + 128 + 512 * i, 512, ci, False, i == 0))
        return res

    class Common:
        pass

    def alloc_common():
        cm = Common()
        cm.xt_ring = Ring([S.sbuf(f"xt{i}", [128, 8, 512], F32) for i in range(2)])
        cm.sq_b = S.sbuf("sq", [128, 8, 512], BF16)
        cm.xn_b = S.sbuf("xn", [128, 8, 512], BF16)
        cm.rstd_b = S.sbuf("rstd", [128, 512], F32)
        cm.tmp_b = S.sbuf("tmpf", [128, 512], F32)
        return cm

    def norm_block(cm, src_buf, off, n, gcol):
        xt = cm.xt_ring.next()
        S.load(xt, xt[:, :, :n], src_buf, src_buf[:, :, off:off + n])
        S.op(act, lambda: nc.scalar.activation(out=cm.sq_b[:, :, :n], in_=xt[:, :, :n], func=AF.Square),
             reads=[xt], writes=[cm.sq_b])
        ps = PS[0]
        for c in range(8):
            mm(ps[:, :n], ones_m[:], cm.sq_b[:, c, :n], c == 0, c == 7, [ones_m, cm.sq_b], ps)
        rstd_from_ms(ps, n, cm.rstd_b, cm.tmp_b)
        for c in range(8):
            S.op(dve, lambda c=c: nc.vector.scalar_tensor_tensor(
                out=cm.xn_b[:, c, :n], in0=xt[:, c, :n], scalar=vec[:, gcol + c:gcol + c + 1],
                in1=cm.rstd_b[:, :n], op0=ALU.mult, op1=ALU.mult),
                reads=[xt, vec, cm.rstd_b], partial=[cm.xn_b])
        return xt

    def alloc_tmps():
        return (S.sbuf("sqh", [128, 512], BF16), S.sbuf("kn", [128, 512], BF16), S.sbuf("rk", [128, 512], F32),
                S.sbuf("tk", [128, 512], F32), S.sbuf("t1", [128, 512], F32), S.sbuf("t2", [128, 512], F32))

    def norm_head_rope(ps, n, gcol2, ones_t, pm, c_ap, s_ap, cs_bufs, out_ap, out_buf, do_norm, tmps):
        sqh, kn, rk, tk, t1, t2 = tmps
        ps2, ps3 = PS[5], PS[6]
        if do_norm:
            S.op(act, lambda: nc.scalar.activation(out=sqh[:, :n], in_=ps[:, :n], func=AF.Square),
                 reads=[ps], writes=[sqh])
            mm(ps2[:, :n], ones_t[:], sqh[:, :n], True, True, [ones_t, sqh], ps2)
            rstd_from_ms(ps2, n, rk, tk)
            S.op(dve, lambda: nc.vector.scalar_tensor_tensor(
                out=kn[:, :n], in0=ps[:, :n], scalar=vec2[:, gcol2:gcol2 + 1], in1=rk[:, :n],
                op0=ALU.mult, op1=ALU.mult), reads=[ps, vec2, rk], writes=[kn])
        else:
            S.op(act, lambda: nc.scalar.activation(out=kn[:, :n], in_=ps[:, :n], func=AF.Copy),
                 reads=[ps], writes=[kn])
        mm(ps3[:, :n], pm[:], kn[:, :n], True, True, [pm, kn], ps3)
        S.op(pool, lambda: nc.gpsimd.tensor_tensor(out=t1[:, :n], in0=kn[:, :n], in1=c_ap, op=ALU.mult),
             reads=[kn] + cs_bufs, writes=[t1])
        S.op(dve, lambda: nc.vector.tensor_tensor(out=t2[:, :n], in0=ps3[:, :n], in1=s_ap, op=ALU.mult),
             reads=[ps3] + cs_bufs, writes=[t2])
        if isinstance(out_ap, list):
            for (oap, obuf, p0, p1) in out_ap:
                S.op(dve, lambda oap=oap, p0=p0, p1=p1: nc.vector.tensor_tensor(out=oap, in0=t1[p0:p1, :n], in1=t2[p0:p1, :n], op=ALU.add),
                     reads=[t1, t2], partial=[obuf])
        else:
            S.op(dve, lambda: nc.vector.tensor_tensor(out=out_ap, in0=t1[:, :n], in1=t2[:, :n], op=ALU.add),
                 reads=[t1, t2], partial=[out_buf])

    proj_ps = Ring([PS[1], PS[2], PS[3], PS[4]])

    def proj(ps, w_sb, col0, ncols, rhs_list, n, rbufs):
        kc = len(rhs_list)
        for c in range(kc):
            mm(ps[:, :n], w_sb[:, c, col0:col0 + ncols], rhs_list[c], c == 0, c == kc - 1, [w_sb] + rbufs, ps)

    S.push()
    ikT0 = S.sbuf("ikT0", [128, SEQ], BF16)
    ikT1 = S.sbuf("ikT1", [128, SEQ], BF16)
    iw_sb = S.sbuf("iw_sb", [128, NSLOT, 16], F32)
    S.op(pool, lambda: nc.gpsimd.memset(ikT0[:], 0.0), writes=[ikT0])
    S.op(pool, lambda: nc.gpsimd.memset(ikT1[:], 0.0), writes=[ikT1])

    S.push()
    cm = alloc_common()
    tmps = alloc_tmps()
    cs_ring = Ring([S.sbuf(f"cs{i}", [128, 4, 512], F32) for i in range(2)])
    wk_sb = S.sbuf("wk_sb", [128, 8, 1152], BF16)
    load_w_cast(wk_sb, 0, w_keys, w_keys.ap_fn(), 0, 1152)
    kout_ring = Ring([S.sbuf(f"kout{i}", [128, 4, 512], BF16) for i in range(2)])
    vout_ring = Ring([S.sbuf(f"vout{i}", [128, 4, 512], BF16) for i in range(2)])
    for blk in range(SEQ // 512):
        off = blk * 512
        n = 512
        norm_block(cm, xT_seq, off, n, V_NMIX0)
        xn_b = cm.xn_b
        cs = cs_ring.next()
        S.load(cs, cs[:, 0, :], c128s, c128s[:, off:off + n])
        S.load(cs, cs[:, 1, :], s128s, s128s[:, off:off + n])
        S.load(cs, cs[:, 2, :], c64s, c64s[:, off:off + n])
        S.load(cs, cs[:, 3, :], s64s, s64s[:, off:off + n])
        kout = kout_ring.next()
        for kc in range(4):
            ps = proj_ps.next()
            proj(ps, wk_sb, kc * 128, 128, [xn_b[:, c, :n] for c in range(8)], n, [xn_b])
            norm_head_rope(ps, n, V2_AK, ones_h, p128, cs[:, 0, :n], cs[:, 1, :n], [cs],
                           kout[:, kc, :n], kout, True, tmps)
        S.store(KT, KT[:, :, off:off + n], kout, kout[:, :, :n])
        ps = proj_ps.next()
        proj(ps, wk_sb, 512, 128, [xn_b[:, c, :n] for c in range(8)], n, [xn_b])
        norm_head_rope(ps, n, 0, None, p64, cs[:, 2, :n], cs[:, 3, :n], [cs],
                       [(ikT0[0:64, off:off + n], ikT0, 0, 64), (ikT1[64:128, off:off + n], ikT1, 64, 128)],
                       None, False, tmps)
        vout = vout_ring.next()
        for tt in range(4):
            ps = proj_ps.next()
            for c in range(8):
                mm(ps[:, :], xn_b[:, c, tt * 128:(tt + 1) * 128], wk_sb[:, c, 640:1152], c == 0, c == 7,
                   [wk_sb, xn_b], ps)
            S.op(act, lambda tt=tt, ps=ps: nc.scalar.activation(out=vout[:, tt, :], in_=ps[:, :], func=AF.Copy),
                 reads=[ps], partial=[vout])
        S.store(Vd, Vd.ap_fn()[off:off + n, :].rearrange("(t p) d -> p t d", p=128), vout, vout[:, :, :])
    S.pop()

    S.push()
    cm = alloc_common()
    tmps = alloc_tmps()
    cs_ring = Ring([S.sbuf(f"cs{i}", [128, 4, 512], F32) for i in range(1)])
    wo_sb = S.sbuf("wo_sb", [128, 8, 2064], BF16)
    load_w_cast(wo_sb, 0, w_own, w_own.ap_fn(), 0, 2064)
    pw_sb = S.sbuf("pw_sb", [128, 4, 128], BF16)
    for g in range(4):
        S.load(pw_sb, pw_sb[:, g, :], pool_w, pool_w[g, :, :], eng=pool)
    pcorr = S.sbuf("pcorr", [128, 2, 4, 16], F32)
    S.load(pcorr, pcorr[:], poolcorr_in, poolcorr_in[:, :, :, :])
    qout_ring = Ring([S.sbuf(f"qout{i}", [128, 4, 512], BF16) for i in range(2)])
    iqout_ring = Ring([S.sbuf(f"iqout{i}", [128, 8, 512], BF16) for i in range(1)])
    bout_ring = Ring([S.sbuf(f"bout{i}", [128, 4, 512], BF16) for i in range(2)])
    Ug = [S.sbuf(f"U{g}", [128, 528], F32) for g in range(4)]
    sA = [S.sbuf(f"sA{g}", [128, 528], F32) for g in range(4)]
    sB = [S.sbuf(f"sB{g}", [128, 528], F32) for g in range(4)]
    pooled = [S.sbuf(f"pooled{g}", [128, 512], BF16) for g in range(4)]
    for (off, n, ci, is_halo, is_first) in own_blocks():
        norm_block(cm, xT_own, off, n, V_NMIX0)
        xn_b = cm.xn_b
        xl = [xn_b[:, c, :n] for c in range(8)]
        cs = cs_ring.next()
        S.load(cs, cs[:, 0, :n], c128o, c128o[:, off:off + n])
        S.load(cs, cs[:, 1, :n], s128o, s128o[:, off:off + n])
        S.load(cs, cs[:, 2, :n], c64o, c64o[:, off:off + n])
        S.load(cs, cs[:, 3, :n], s64o, s64o[:, off:off + n])
        qout = qout_ring.next()
        for kc in range(4):
            ps = proj_ps.next()
            proj(ps, wo_sb, kc * 128, 128, xl, n, [xn_b])
            norm_head_rope(ps, n, V2_AQ, ones_h, p128, cs[:, 0, :n], cs[:, 1, :n], [cs],
                           qout[:, kc, :n], qout, True, tmps)
        S.store(QT, QT[:, :, off:off + n], qout, qout[:, :, :n])
        iqout = iqout_ring.next()
        for kc in range(8):
            ps = proj_ps.next()
            proj(ps, wo_sb, 512 + kc * 128, 128, xl, n, [xn_b])
            norm_head_rope(ps, n, 0, None, p64, cs[:, 2, :n], cs[:, 3, :n], [cs],
                           iqout[:, kc, :n], iqout, False, tmps)
        S.store(IQT, IQT[:, :, off:off + n], iqout, iqout[:, :, :n])
        for tt in range(n // 128):
            slot = off // 128 + tt
            ps = proj_ps.next()
            for c in range(8):
                mm(ps[:, :16], xn_b[:, c, tt * 128:(tt + 1) * 128], wo_sb[:, c, 2048:2064], c == 0, c == 7,
                   [wo_sb, xn_b], ps)
            S.op(act, lambda ps=ps, slot=slot: nc.scalar.activation(out=iw_sb[:, slot, :], in_=ps[:, :16],
                                                                    func=AF.Copy, scale=1.0 / 32.0),
                 reads=[ps], partial=[iw_sb])
        L = 16 + n
        bout = bout_ring.next()
        for g in range(4):
            U = Ug[g]
            if is_halo:
                S.op(pool, lambda U=U: nc.gpsimd.memset(U[:, 0:16], 0.0), partial=[U])
            ps = proj_ps.next()
            proj(ps, wo_sb, 1536 + g * 128, 128, xl, n, [xn_b])
            S.op(act, lambda ps=ps, U=U: nc.scalar.activation(out=U[:, 16:L], in_=ps[:, :n], func=AF.Copy),
                 reads=[ps], partial=[U])
            a_, b_ = sA[g], sB[g]
            S.op(pool, lambda U=U, a_=a_: nc.gpsimd.tensor_tensor(out=a_[:, 1:L], in0=U[:, 1:L], in1=U[:, 0:L - 1], op=ALU.add),
                 reads=[U], writes=[a_])
            fin = a_
            if g >= 1:
                S.op(pool, lambda a_=a_, b_=b_: nc.gpsimd.tensor_tensor(out=b_[:, 3:L], in0=a_[:, 3:L], in1=a_[:, 1:L - 2], op=ALU.add),
                     reads=[a_], writes=[b_])
                fin = b_
            if g >= 2:
                S.op(pool, lambda a_=a_, b_=b_: nc.gpsimd.tensor_tensor(out=a_[:, 7:L], in0=b_[:, 7:L], in1=b_[:, 3:L - 4], op=ALU.add),
                     reads=[b_], writes=[a_])
                fin = a_
            if g >= 3:
                S.op(pool, lambda a_=a_, b_=b_: nc.gpsimd.tensor_tensor(out=b_[:, 15:L], in0=a_[:, 15:L], in1=a_[:, 7:L - 8], op=ALU.add),
                     reads=[a_], writes=[b_])
                fin = b_
            if is_first:
                S.op(dve, lambda fin=fin, g=g: nc.vector.tensor_tensor(out=fin[:, 16:32], in0=fin[:, 16:32],
                                                                       in1=pcorr[:, ci, g, :], op=ALU.mult),
                     reads=[pcorr, fin], partial=[fin])
            w_ = float(2 ** (g + 1))
            S.op(dve, lambda fin=fin, U=U, g=g: nc.vector.scalar_tensor_tensor(
                out=pooled[g][:, :n], in0=fin[:, 16:L], scalar=1.0 / w_, in1=U[:, 16:L],
                op0=ALU.mult, op1=ALU.subtract), reads=[fin, U], writes=[pooled[g]])
            S.op(pool, lambda U=U: nc.gpsimd.tensor_copy(out=U[:, 0:16], in_=U[:, n:n + 16]), reads=[U], partial=[U])
            ps = proj_ps.next()
            mm(ps[:, :n], pw_sb[:, g, :], pooled[g][:, :n], True, True, [pw_sb, pooled[g]], ps)
            S.op(act, lambda ps=ps, g=g: nc.scalar.activation(out=bout[:, g, :n], in_=ps[:, :n], func=AF.Copy,
                                                               scale=vec2[:, V2_PSCALE + g:V2_PSCALE + g + 1]),
                 reads=[ps, vec2], partial=[bout])
        S.store(BT, BT[:, :, off:off + n], bout, bout[:, :, :n])
    S.pop()

    S.push()
    iota_sb = S.sbuf("iota_sb", [128, MBW], F32)
    S.load(iota_sb, iota_sb[:], iota_in, iota_in[:, :])
    scores2 = [S.sbuf(f"scores{i}", [128, SEQ], F32) for i in range(2)]
    mbias = S.sbuf("mbias", [128, SEQ], BF16)
    junk = S.sbuf("junk", [128, SEQ // 2], BF16)
    mb2 = [S.sbuf(f"mb{i}", [128, MBW], BF16) for i in range(2)]
    tmpu = S.sbuf("tmpu", [128, MBW], F32)
    qt_ring = Ring([S.sbuf(f"qt{i}", [128, 4, 128], BF16) for i in range(2)])
    iqt_ring = Ring([S.sbuf(f"iqt{i}", [128, 8, 128], BF16) for i in range(2)])
    kb_ring = Ring([S.sbuf(f"kblk{i}", [128, 4, 512], BF16) for i in range(2)])
    vb_ring = Ring([S.sbuf(f"vblk{i}", [128, 4, 512], BF16) for i in range(2)])
    r_ring = Ring([S.sbuf(f"R{i}", [128, 512], BF16) for i in range(4)])
    p_ring = Ring([S.sbuf(f"P{i}", [128, 512], BF16) for i in range(3)])
    pt_ring = Ring([S.sbuf(f"PT{i}", [128, 512], BF16) for i in range(3)])
    diag2 = [S.sbuf(f"diag{i}", [128, 16, 128], BF16) for i in range(2)]
    sm = S.sbuf("sm", [128, 16], F32)
    hs = S.sbuf("hs", [128, 32], F32)
    pow2 = S.sbuf("pow2", [128, 32], F32)
    cntb = S.sbuf("cntb", [128, 2], F32)
    midr = Ring([S.sbuf(f"mid{i}", [128, 1], F32) for i in range(2)])
    eb = S.sbuf("eb", [128, 1], F32)
    rs = S.sbuf("rs", [128, 4, 16], F32)
    rsum = S.sbuf("rsum", [128, 4], F32)
    rrec = S.sbuf("rrec", [128, 4], F32)
    negone = S.sbuf("negone", [128, 4], F32)
    rjunk = S.sbuf("rjunk", [128, 16], F32)
    a_tok = S.sbuf("a_tok", [128, 512], BF16)
    aT_ring = Ring([S.sbuf(f"aT{i}", [128, 4, 128], BF16) for i in range(2)])
    for i in range(32):
        S.op(pool, lambda i=i: nc.gpsimd.memset(pow2[:, i:i + 1], 2.0 ** (-i)), partial=[pow2])
    S.op(pool, lambda: nc.gpsimd.memset(negone[:], -1.0), writes=[negone])
    s_ring = Ring([PS[0], PS[1]])
    sc_ring = Ring([PS[2]])
    l_ring = Ring([PS[4], PS[5]])
    Obank = PS[6]
    ps3b = Buf("ps3b", lambda: PS[3].ap_fn()[:, :].bitcast(BF16))
    PSBv = [S.view("psb0", PSB, (slice(None), slice(0, 512))), S.view("ps3b0", ps3b, (slice(None), slice(0, 512)))]
    ptp_ring = Ring(PSBv)
    SM_MX, SM_MN1, SM_MN2, SM_MN, SM_H, SM_TAU = range(6)
    MASKV = -30000.0

    def slot_geom(j):
        nkt = slot_nkt(j)
        nkb = (nkt + 3) // 4
        ub = slot_first_uncertain(j) // 4
        return nkb, nkb * 512, ub, (nkb - ub) * 512

    def stage_A(j):
        nkb, N, ub, W = slot_geom(j)
        assert W <= MBW
        scores = scores2[j % 2]; mb = mb2[j % 2]; diag = diag2[j % 2]
        iqt = iqt_ring.next()
        S.load(iqt, iqt[:], IQT, IQT[:, :, j * 128:(j + 1) * 128])
        for h in range(16):
            S.op(pool, lambda h=h: nc.gpsimd.tensor_scalar(out=diag[:, h, :], in0=ident[:], scalar1=iw_sb[:, j, h:h + 1],
                                                          scalar2=None, op0=ALU.mult),
                 reads=[ident, iw_sb], partial=[diag])
        S.op(pool, lambda: nc.gpsimd.tensor_scalar(out=mb[:, :W], in0=iota_sb[:, :W], scalar1=qadj_sb[:, j:j + 1],
                                                  scalar2=MASKV, op0=ALU.is_gt, op1=ALU.mult),
             reads=[iota_sb, qadj_sb], writes=[mb])
        yield
        items = [(kb, h) for kb in range(nkb) for h in range(16)]
        pend = None
        sc = None
        for (kb, h) in items + [(None, None)]:
            cur = None
            if kb is not None:
                sp_ = s_ring.next()
                ikp = ikT0 if h % 2 == 0 else ikT1
                mm(sp_[:, :], iqt[:, h // 2, :], ikp[:, kb * 512:(kb + 1) * 512], True, True,
                   [iqt, ikp], sp_)
                R = r_ring.next()
                S.op(act, lambda sp_=sp_, R=R: nc.scalar.activation(out=R[:], in_=sp_[:, :], func=AF.Relu),
                     reads=[sp_], writes=[R])
                cur = (kb, h, R)
            if pend is not None:
                pkb, ph, pR = pend
                if ph == 0:
                    sc = sc_ring.next()
                mm(sc[:, :], diag[:, ph, :], pR[:], ph == 0, (ph == 15 and pkb < ub), [diag, pR], sc)
                if ph == 15:
                    if pkb >= ub:
                        mm(sc[:, :], ident[:], mb[:, (pkb - ub) * 512:(pkb - ub + 1) * 512], False, True, [ident, mb], sc)
                    S.op(act, lambda sc=sc, pkb=pkb: nc.scalar.activation(out=scores[:, pkb * 512:(pkb + 1) * 512], in_=sc[:, :],
                                                                          func=AF.Copy), reads=[sc], partial=[scores])
            pend = cur
            if not (SKEW or SKEW_A) and pend is not None:
                pkb, ph, pR = pend
                if ph == 0:
                    sc = sc_ring.next()
                mm(sc[:, :], diag[:, ph, :], pR[:], ph == 0, (ph == 15 and pkb < ub), [diag, pR], sc)
                if ph == 15:
                    if pkb >= ub:
                        mm(sc[:, :], ident[:], mb[:, (pkb - ub) * 512:(pkb - ub + 1) * 512], False, True, [ident, mb], sc)
                    S.op(act, lambda sc=sc, pkb=pkb: nc.scalar.activation(out=scores[:, pkb * 512:(pkb + 1) * 512], in_=sc[:, :],
                                                                          func=AF.Copy), reads=[sc], partial=[scores])
                pend = None
            yield

    def stage_B(j):
        nkb, N, ub, W = slot_geom(j)
        scores = scores2[j % 2]; mb = mb2[j % 2]
        S.op(dve, lambda: nc.vector.tensor_reduce(out=sm[:, SM_MX:SM_MX + 1], in_=scores[:, :N], axis=AX.X, op=ALU.max),
             reads=[scores], partial=[sm])
        S.op(dve, lambda: nc.vector.scalar_tensor_tensor(out=tmpu[:, :W], in0=mb[:, :W], scalar=-2.0,
                                                         in1=scores[:, ub * 512:N], op0=ALU.mult, op1=ALU.add),
             reads=[mb, scores], writes=[tmpu])
        S.op(dve, lambda: nc.vector.tensor_reduce(out=sm[:, SM_MN2:SM_MN2 + 1], in_=tmpu[:, :W], axis=AX.X, op=ALU.min),
             reads=[tmpu], partial=[sm])
        if ub > 0:
            S.op(dve, lambda: nc.vector.tensor_reduce(out=sm[:, SM_MN1:SM_MN1 + 1], in_=scores[:, :ub * 512], axis=AX.X, op=ALU.min),
                 reads=[scores], partial=[sm])
            S.op(dve, lambda: nc.vector.tensor_tensor(out=sm[:, SM_MN:SM_MN + 1], in0=sm[:, SM_MN1:SM_MN1 + 1],
                                                      in1=sm[:, SM_MN2:SM_MN2 + 1], op=ALU.min), reads=[sm], partial=[sm])
        else:
            S.op(dve, lambda: nc.vector.tensor_copy(out=sm[:, SM_MN:SM_MN + 1], in_=sm[:, SM_MN2:SM_MN2 + 1]),
                 reads=[sm], partial=[sm])
        S.op(dve, lambda: nc.vector.tensor_scalar(out=sm[:, SM_H:SM_H + 1], in0=sm[:, SM_MX:SM_MX + 1],
                                                  scalar1=sm[:, SM_MN:SM_MN + 1], scalar2=0.50005, op0=ALU.subtract, op1=ALU.mult),
             reads=[sm], partial=[sm])
        mid = midr.next()
        S.op(dve, lambda mid=mid: nc.vector.tensor_scalar(out=mid[:], in0=sm[:, SM_MX:SM_MX + 1],
                                                          scalar1=sm[:, SM_MN:SM_MN + 1], scalar2=0.5, op0=ALU.add, op1=ALU.mult),
             reads=[sm], writes=[mid])
        S.op(dve, lambda: nc.vector.tensor_scalar(out=hs[:], in0=pow2[:], scalar1=sm[:, SM_H:SM_H + 1], scalar2=None, op0=ALU.mult),
             reads=[pow2, sm], writes=[hs])
        yield
        N1 = min(N, SEQ // 2)
        for it in range(NITER):
            S.op(dve, lambda mid=mid: nc.vector.tensor_scalar(out=junk[:, :N1], in0=scores[:, :N1], scalar1=mid[:, 0:1], scalar2=0.0,
                                                              op0=ALU.is_ge, op1=ALU.add, accum_out=cntb[:, 0:1]),
                 reads=[scores, mid], writes=[junk, cntb])
            if N > N1:
                S.op(dve, lambda mid=mid: nc.vector.tensor_scalar(out=junk[:, :N - N1], in0=scores[:, N1:N], scalar1=mid[:, 0:1],
                                                                  scalar2=cntb[:, 0:1], op0=ALU.is_ge, op1=ALU.add, accum_out=cntb[:, 1:2]),
                     reads=[scores, mid, cntb], writes=[junk], partial=[cntb])
                ccol = 1
            else:
                ccol = 0
            S.op(dve, lambda ccol=ccol: nc.vector.tensor_scalar(out=eb[:], in0=cntb[:, ccol:ccol + 1], scalar1=255.5, scalar2=0.5,
                                                                op0=ALU.is_ge, op1=ALU.subtract), reads=[cntb], writes=[eb])
            nmid = midr.next()
            S.op(dve, lambda mid=mid, nmid=nmid, it=it: nc.vector.scalar_tensor_tensor(
                out=nmid[:], in0=eb[:], scalar=hs[:, it:it + 1], in1=mid[:], op0=ALU.mult, op1=ALU.add),
                reads=[eb, hs, mid], writes=[nmid])
            mid = nmid
            yield
        S.op(dve, lambda mid=mid: nc.vector.scalar_tensor_tensor(
            out=sm[:, SM_TAU:SM_TAU + 1], in0=sm[:, SM_H:SM_H + 1], scalar=-(2.0 ** (-NITER)), in1=mid[:],
            op0=ALU.mult, op1=ALU.add), reads=[sm, mid], partial=[sm])
        S.op(dve, lambda: nc.vector.tensor_scalar(out=mbias[:, :N], in0=scores[:, :N], scalar1=sm[:, SM_TAU:SM_TAU + 1],
                                                  scalar2=MASKV, op0=ALU.is_lt, op1=ALU.mult),
             reads=[scores, sm], writes=[mbias])
        yield

    def stage_C(j):
        nkb, N, ub, W = slot_geom(j)
        qt = qt_ring.next()
        S.load(qt, qt[:], QT, QT[:, :, j * 128:(j + 1) * 128])
        S.op(pool, lambda: nc.gpsimd.memset(rs[:], 0.0), writes=[rs])
        yield
        items = [(kb, h) for kb in range(nkb) for h in range(4)]
        nI = len(items)
        st = {}
        blk = {}
        first_o = [True]

        def qk(i):
            kb, h = items[i]
            if h == 0:
                kblk = kb_ring.next(); vblk = vb_ring.next()
                S.load(kblk, kblk[:], KT, KT[:, :, kb * 512:(kb + 1) * 512])
                S.load(vblk, vblk[:], Vd, Vd.ap_fn()[kb * 512:(kb + 1) * 512, :].rearrange("(t p) d -> p t d", p=128))
                blk[kb] = (kblk, vblk)
            kblk, vblk = blk[kb]
            Lp = l_ring.next()
            mm(Lp[:, :], qt[:, h, :], kblk[:, h, :], True, False, [qt, kblk], Lp)
            mm(Lp[:, :], ident[:], mbias[:, kb * 512:(kb + 1) * 512], False, True, [ident, mbias], Lp)
            Pb = p_ring.next()
            S.op(act, lambda: nc.scalar.activation(out=Pb[:], in_=Lp[:, :], func=AF.Exp, scale=128.0 ** -0.5,
                                                   accum_out=rs[:, h, kb:kb + 1]),
                 reads=[Lp], writes=[Pb], partial=[rs])
            st[i] = [Pb, None]

        def tr(i):
            Pb = st[i][0]
            ptp = ptp_ring.next()
            for tt in range(4):
                S.op(pe, lambda tt=tt: nc.tensor.transpose(out=ptp[:, tt * 128:(tt + 1) * 128],
                                                           in_=Pb[:, tt * 128:(tt + 1) * 128], identity=ident[:]),
                     reads=[Pb, ident], writes=[ptp] if tt == 0 else (), partial=[ptp] if tt else ())
            PTb = pt_ring.next()
            S.op(act, lambda: nc.scalar.activation(out=PTb[:], in_=ptp[:, :], func=AF.Copy), reads=[ptp], writes=[PTb])
            st[i][1] = PTb

        def pv(i):
            kb, h = items[i]
            PTb = st[i][1]
            kblk, vblk = blk[kb]
            for tt in range(4):
                fo = first_o[0]
                first_o[0] = False
                S.op(pe, lambda tt=tt, fo=fo: nc.tensor.matmul(
                    Obank[:, h * 128:(h + 1) * 128], lhsT=PTb[:, tt * 128:(tt + 1) * 128],
                    rhs=vblk[:, tt, h * 128:(h + 1) * 128], start=fo, stop=(i == nI - 1 and tt == 3),
                    skip_group_check=True),
                    reads=[PTb, vblk], writes=[Obank] if fo else (), partial=() if fo else [Obank])
            del st[i]

        if SKEW:
            for g in range(nI + 2):
                if g < nI:
                    qk(g)
                if 1 <= g <= nI:
                    tr(g - 1)
                if g >= 2:
                    pv(g - 2)
                yield
        else:
            for g in range(nI):
                qk(g)
                tr(g)
                pv(g)
                yield
        for h in range(4):
            S.op(act, lambda h=h: nc.scalar.activation(out=rjunk[:, :], in_=rs[:, h, :], func=AF.Copy, accum_out=rsum[:, h:h + 1]),
                 reads=[rs], writes=[rjunk], partial=[rsum])
        S.op(pool, lambda: nc.gpsimd.tensor_tensor(out=rrec[:], in0=rsum[:], in1=negone[:], op=ALU.pow),
             reads=[rsum, negone], writes=[rrec])
        for h in range(4):
            S.op(act, lambda h=h: nc.scalar.activation(out=a_tok[:, h * 128:(h + 1) * 128], in_=Obank[:, h * 128:(h + 1) * 128],
                                                       func=AF.Copy, scale=rrec[:, h:h + 1]),
                 reads=[Obank, rrec], partial=[a_tok])
        ptp = ptp_ring.next()
        for h in range(4):
            S.op(pe, lambda h=h, ptp=ptp: nc.tensor.transpose(out=ptp[:, h * 128:(h + 1) * 128],
                                                              in_=a_tok[:, h * 128:(h + 1) * 128], identity=ident[:]),
                 reads=[a_tok, ident], writes=[ptp] if h == 0 else (), partial=[ptp] if h else ())
        aT = aT_ring.next()
        S.op(act, lambda ptp=ptp, aT=aT: nc.scalar.activation(out=aT[:].rearrange("p a b -> p (a b)"), in_=ptp[:, :], func=AF.Copy),
             reads=[ptp], writes=[aT])
        S.store(AT, AT[:, :, j * 128:(j + 1) * 128], aT, aT[:])
        yield

    def run_all(g):
        for _ in g:
            pass

    slot_list = list(slots) if slots is not None else list(range(NSLOT))
    ns = len(slot_list)
    run_all(stage_A(slot_list[0]))
    for si in range(ns + 1):
        gC = stage_C(slot_list[si - 1]) if si - 1 >= 0 else None
        gA = stage_A(slot_list[si + 1]) if si + 1 < ns else None
        gB = stage_B(slot_list[si]) if si < ns else None
        nC = slot_geom(slot_list[si - 1])[0] * 4 + 4 if gC is not None else 0
        nA = slot_geom(slot_list[si + 1])[0] * 16 + 2 if gA is not None else 0
        nB = NITER + 2 if gB is not None else 0
        live = {"A": gA, "B": gB, "C": gC}
        tot = {"A": nA, "B": nB, "C": nC}
        done = {"A": 0, "B": 0, "C": 0}
        wgt = {"A": 1.0, "B": 1.0, "C": 0.6}
        while any(g is not None for g in live.values()):
            best = None
            for k, g in live.items():
                if g is None:
                    continue
                frac = wgt[k] * done[k] / max(1, tot[k])
                if best is None or frac < best[0]:
                    best = (frac, k)
            k = best[1]
            if SEQ_PE and k == "A" and live["C"] is not None:
                k = "C"
            if k == "B" and done["B"] >= NITER + 1 and live["C"] is not None:
                k = "C"
            try:
                next(live[k])
                done[k] += 1
            except StopIteration:
                live[k] = None
    S.pop()
    S.pop()

    if stop_after == "3":
        dbg_a = S.dram("dbg_a", [128, 4, NOWN], BF16, kind="ExternalOutput")
        dbg_b = S.dram("dbg_b", [128, 4, NOWN], BF16, kind="ExternalOutput")
        S.push()
        big = S.sbuf("dbgbig", [128, 4, NOWN], BF16)
        S.load(big, big[:], AT, AT[:, :, :])
        S.store(dbg_a, dbg_a[:, :, :], big, big[:])
        S.load(big, big[:], BT, BT[:, :, :])
        S.store(dbg_b, dbg_b[:, :, :], big, big[:])
        S.finish([dbg_a, dbg_b])
        return nc

    GLU = S.dram("GLU", [128, 8, NOWN], BF16)
    hout_ring_holder = {}

    def residual_out(ps, n, c, res_buf, res_ap, hout):
        S.op(dve, lambda: nc.vector.tensor_tensor(out=hout[:, c, :n], in0=ps[:, :n], in1=res_ap, op=ALU.add),
             reads=[ps, res_buf], partial=[hout])

    S.push()
    wout_sb = S.sbuf("wout_sb", [128, 8, D], BF16)
    load_w_cast(wout_sb, 0, w_out_ab, w_out_ab.ap_fn(), 0, D)
    xt_ring = Ring([S.sbuf(f"xt4_{i}", [128, 8, 512], F32) for i in range(2)])
    ab_ring = Ring([S.sbuf(f"ab{i}", [128, 8, 512], BF16) for i in range(2)])
    hout_ring = Ring([S.sbuf(f"hout4_{i}", [128, 8, 512], F32) for i in range(2)])
    for (off, n, ci, is_halo, is_first) in own_blocks():
        xt = xt_ring.next(); ab = ab_ring.next(); hout = hout_ring.next()
        S.load(xt, xt[:, :, :n], xT_own, xT_own[:, :, off:off + n])
        S.load(ab, ab[:, 0:4, :n], AT, AT[:, :, off:off + n])
        S.load(ab, ab[:, 4:8, :n], BT, BT[:, :, off:off + n])
        for o in range(8):
            ps = proj_ps.next()
            proj(ps, wout_sb, o * 128, 128, [ab[:, c, :n] for c in range(8)], n, [ab])
            residual_out(ps, n, o, xt, xt[:, o, :n], hout)
        S.store(HT, HT[:, :, off:off + n], hout, hout[:, :, :n])
    S.pop()

    def cross_phase(l, blocks):
        S.push()
        gq, gk = (V2_CQ0, V2_CK0) if l == 0 else (V2_CQ1, V2_CK1)
        ncross = V_NCROSS0 if l == 0 else V_NCROSS1
        nmem = V_NMEM0 if l == 0 else V_NMEM1
        cm = alloc_common()
        kcT = S.sbuf("kcT", [128, 8, MEM], BF16)
        vc = S.sbuf("vc", [128, 2, D], BF16)
        sqc = S.sbuf("sqc", [128, 2, 512], BF16)
        rq = S.sbuf("rq", [128, 512], F32)
        tq = S.sbuf("tq", [128, 512], F32)
        S.push()
        wkv = S.sbuf("wkv", [128, 8, 2 * D], BF16)
        load_w_cast(wkv, 0, cross_wk, cross_wk.ap_fn()[l], 0, D)
        load_w_cast(wkv, D, cross_wv, cross_wv.ap_fn()[l], 0, D)
        norm_block(cm, memT, 0, MEM, nmem)
        xn_b = cm.xn_b
        n = MEM
        for hh in range(4):
            pss = [proj_ps.next(), proj_ps.next()]
            for dc in range(2):
                proj(pss[dc], wkv, (hh * 2 + dc) * 128, 128, [xn_b[:, c, :n] for c in range(8)], n, [xn_b])
                S.op(act, lambda dc=dc, pss=pss: nc.scalar.activation(out=sqc[:, dc, :n], in_=pss[dc][:, :n], func=AF.Square),
                     reads=[pss[dc]], partial=[sqc])
            ps2 = PS[5]
            for dc in range(2):
                mm(ps2[:, :n], ones_c[:], sqc[:, dc, :n], dc == 0, dc == 1, [ones_c, sqc], ps2)
            rstd_from_ms(ps2, n, rq, tq)
            for dc in range(2):
                S.op(dve, lambda dc=dc, pss=pss, hh=hh: nc.vector.scalar_tensor_tensor(
                    out=kcT[:, hh * 2 + dc, :n], in0=pss[dc][:, :n], scalar=vec2[:, gk + dc:gk + dc + 1], in1=rq[:, :n],
                    op0=ALU.mult, op1=ALU.mult), reads=[pss[dc], vec2, rq], partial=[kcT])
        for mt in range(2):
            for hf in range(2):
                ps = proj_ps.next()
                for c in range(8):
                    mm(ps[:, :], xn_b[:, c, mt * 128:(mt + 1) * 128], wkv[:, c, D + hf * 512:D + (hf + 1) * 512], c == 0, c == 7,
                       [wkv, xn_b], ps)
                S.op(act, lambda ps=ps, mt=mt, hf=hf: nc.scalar.activation(out=vc[:, mt, hf * 512:(hf + 1) * 512], in_=ps[:, :], func=AF.Copy),
                     reads=[ps], partial=[vc])
        S.pop()
        wqo = S.sbuf("wqo", [128, 8, 2 * D], BF16)
        load_w_cast(wqo, 0, cross_wq, cross_wq.ap_fn()[l], 0, D)
        load_w_cast(wqo, D, cross_wo, cross_wo.ap_fn()[l], 0, D)
        qn = S.sbuf("qn", [128, 8, 512], BF16)
        on = S.sbuf("on", [128, 8, 512], BF16)
        pc_ring = Ring([S.sbuf(f"pc{i}", [128, 2, 512], BF16) for i in range(2)])
        rden = S.sbuf("rden", [128, 512], F32)
        hout_ring = Ring([S.sbuf(f"houtc_{i}", [128, 8, 512], F32) for i in range(2)])
        for (off, n, ci, is_halo, is_first) in blocks:
            xt = norm_block(cm, HT, off, n, ncross)
            xn_b = cm.xn_b
            hout = hout_ring.next()
            for hh in range(4):
                pss = [proj_ps.next(), proj_ps.next()]
                for dc in range(2):
                    proj(pss[dc], wqo, (hh * 2 + dc) * 128, 128, [xn_b[:, c, :n] for c in range(8)], n, [xn_b])
                    S.op(act, lambda dc=dc, pss=pss: nc.scalar.activation(out=sqc[:, dc, :n], in_=pss[dc][:, :n], func=AF.Square),
                         reads=[pss[dc]], partial=[sqc])
                ps2 = PS[5]
                for dc in range(2):
                    mm(ps2[:, :n], ones_c[:], sqc[:, dc, :n], dc == 0, dc == 1, [ones_c, sqc], ps2)
                rstd_from_ms(ps2, n, rq, tq)
                for dc in range(2):
                    S.op(dve, lambda dc=dc, pss=pss, hh=hh: nc.vector.scalar_tensor_tensor(
                        out=qn[:, hh * 2 + dc, :n], in0=pss[dc][:, :n], scalar=vec2[:, gq + dc:gq + dc + 1], in1=rq[:, :n],
                        op0=ALU.mult, op1=ALU.mult), reads=[pss[dc], vec2, rq], partial=[qn])
                pc = pc_ring.next()
                for mt in range(2):
                    ps = proj_ps.next()
                    for dc in range(2):
                        mm(ps[:, :n], kcT[:, hh * 2 + dc, mt * 128:(mt + 1) * 128], qn[:, hh * 2 + dc, :n], dc == 0, dc == 1,
                           [kcT, qn], ps)
                    S.op(act, lambda ps=ps, mt=mt, pc=pc: nc.scalar.activation(out=pc[:, mt, :n], in_=ps[:, :n], func=AF.Exp,
                                                                                scale=1.0 / 16.0), reads=[ps], partial=[pc])
                psd = PS[6]
                for mt in range(2):
                    mm(psd[:, :n], ones_1[:], pc[:, mt, :n], mt == 0, mt == 1, [ones_1, pc], psd)
                S.op(dve, lambda psd=psd: nc.vector.reciprocal(out=rden[:, :n], in_=psd[:, :n]), reads=[psd], writes=[rden])
                for dc in range(2):
                    ps = proj_ps.next()
                    for mt in range(2):
                        mm(ps[:, :n], vc[:, mt, hh * 256 + dc * 128:hh * 256 + (dc + 1) * 128], pc[:, mt, :n], mt == 0, mt == 1,
                           [vc, pc], ps)
                    S.op(dve, lambda ps=ps, dc=dc, hh=hh: nc.vector.tensor_tensor(out=on[:, hh * 2 + dc, :n], in0=ps[:, :n],
                                                                                 in1=rden[:, :n], op=ALU.mult),
                         reads=[ps, rden], partial=[on])
            for o in range(8):
                ps = proj_ps.next()
                proj(ps, wqo, D + o * 128, 128, [on[:, c, :n] for c in range(8)], n, [on])
                residual_out(ps, n, o, xt, xt[:, o, :n], hout)
            S.store(HT, HT[:, :, off:off + n], hout, hout[:, :, :n])
        S.pop()

    def ffn_phase(l, blocks, final):
        nffn = V_NFFN0 if l == 0 else V_NFFN1
        S.push()
        cm = alloc_common()
        wgu = S.sbuf("wgu", [128, 8, 2 * DFF], BF16)
        load_w_cast(wgu, 0, ffn_wg, ffn_wg.ap_fn()[l], 0, DFF)
        load_w_cast(wgu, DFF, ffn_wu, ffn_wu.ap_fn()[l], 0, DFF)
        hid_ring = Ring([S.sbuf(f"hid{i}", [128, 22, 512], BF16) for i in range(2)])
        sg_ring = Ring([S.sbuf(f"sg{i}", [128, 512], F32) for i in range(2)])
        for (off, n, ci, is_halo, is_first) in blocks:
            norm_block(cm, HT, off, n, nffn)
            xn_b = cm.xn_b
            hid = hid_ring.next()
            xl = [xn_b[:, c, :n] for c in range(8)]
            for f in range(22):
                psg = proj_ps.next(); psu = proj_ps.next()
                proj(psg, wgu, f * 128, 128, xl, n, [xn_b])
                proj(psu, wgu, DFF + f * 128, 128, xl, n, [xn_b])
                sg = sg_ring.next()
                S.op(act, lambda psg=psg, sg=sg: nc.scalar.activation(out=sg[:, :n], in_=psg[:, :n], func=AF.Silu),
                     reads=[psg], writes=[sg])
                S.op(dve, lambda psu=psu, sg=sg, f=f: nc.vector.tensor_tensor(out=hid[:, f, :n], in0=psu[:, :n], in1=sg[:, :n], op=ALU.mult),
                     reads=[psu, sg], partial=[hid])
            S.store(HID, HID[:, :, off:off + n], hid, hid[:, :, :n])
        S.pop()
        S.push()
        wd = S.sbuf("wd", [128, 22, D], BF16)
        load_w_cast(wd, 0, ffn_wd, ffn_wd.ap_fn()[l], 0, D)
        hid_ring = Ring([S.sbuf(f"hidb{i}", [128, 22, 512], BF16) for i in range(2)])
        xt_ring = Ring([S.sbuf(f"xtd_{i}", [128, 8, 512], F32) for i in range(2)])
        hout_ring = Ring([S.sbuf(f"houtd_{i}", [128, 8, 512], F32) for i in range(2)])
        for (off, n, ci, is_halo, is_first) in blocks:
            hid = hid_ring.next(); xt = xt_ring.next(); hout = hout_ring.next()
            S.load(hid, hid[:, :, :n], HID, HID[:, :, off:off + n])
            S.load(xt, xt[:, :, :n], HT, HT[:, :, off:off + n])
            for o in range(8):
                ps = proj_ps.next()
                proj(ps, wd, o * 128, 128, [hid[:, f, :n] for f in range(22)], n, [hid])
                residual_out(ps, n, o, xt, xt[:, o, :n], hout)
            if final:
                slot0 = off // 128
                ci_, i_ = divmod(slot0, SLOTS_PER_CH)
                oo = (ci_ * CH_TILES + i_ - 1) * 128
                S.store(out_hT, out_hT[:, :, oo:oo + n], hout, hout[:, :, :n])
            else:
                S.store(HT, HT[:, :, off:off + n], hout, hout[:, :, :n])
        S.pop()

    cross_phase(0, own_blocks())
    ffn_phase(0, own_blocks(), False)

    S.push()
    cm = alloc_common()
    wci = S.sbuf("wci", [128, 8, 2 * D], BF16)
    load_w_cast(wci, 0, conv_w_in, conv_w_in.ap_fn(), 0, 2 * D)
    hflag = S.sbuf("hflag", [128, 2], F32)
    S.load(hflag, hflag[:], haloflag_in, haloflag_in[:, :])
    sig_ring = Ring([S.sbuf(f"sig{i}", [128, 512], F32) for i in range(2)])
    glu_ring = Ring([S.sbuf(f"glu{i}", [128, 8, 512], BF16) for i in range(2)])
    for (off, n, ci, is_halo, is_first) in own_blocks():
        norm_block(cm, HT, off, n, V_NMIX1)
        xn_b = cm.xn_b
        xl = [xn_b[:, c, :n] for c in range(8)]
        glu = glu_ring.next()
        for c8 in range(8):
            psa = proj_ps.next(); psg = proj_ps.next()
            proj(psa, wci, c8 * 128, 128, xl, n, [xn_b])
            proj(psg, wci, D + c8 * 128, 128, xl, n, [xn_b])
            sig = sig_ring.next()
            S.op(act, lambda psg=psg, sig=sig, c8=c8: nc.scalar.activation(out=sig[:, :n], in_=psg[:, :n], func=AF.Sigmoid,
                                                                          bias=vec2[:, V2_BIN + 8 + c8:V2_BIN + 9 + c8], scale=1.0),
                 reads=[psg, vec2], writes=[sig])
            S.op(dve, lambda psa=psa, sig=sig, c8=c8: nc.vector.scalar_tensor_tensor(
                out=glu[:, c8, :n], in0=psa[:, :n], scalar=vec2[:, V2_BIN + c8:V2_BIN + c8 + 1], in1=sig[:, :n],
                op0=ALU.add, op1=ALU.mult), reads=[psa, vec2, sig], partial=[glu])
        if is_halo:
            S.op(dve, lambda glu=glu: nc.vector.tensor_scalar(out=glu[:, :, :n], in0=glu[:, :, :n], scalar1=hflag[:, ci:ci + 1],
                                                             scalar2=None, op0=ALU.mult), reads=[hflag, glu], partial=[glu])
        S.store(GLU, GLU[:, :, off:off + n], glu, glu[:, :, :n])
    S.pop()

    S.push()
    wco = S.sbuf("wco", [128, 8, D], BF16)
    load_w_cast(wco, 0, conv_w_out, conv_w_out.ap_fn(), 0, D)
    dw_sb = S.sbuf("dw_sb", [128, 8, 31], F32)
    S.load(dw_sb, dw_sb[:], conv_dw, conv_dw[:, :, :])
    dg = S.sbuf("dg", [128, 8, 31, 128], BF16)
    for c8 in range(8):
        for k in range(31):
            S.op(act, lambda c8=c8, k=k: nc.scalar.activation(out=dg[:, c8, k, :], in_=ident[:], func=AF.Copy,
                                                              scale=dw_sb[:, c8, k:k + 1]), reads=[ident, dw_sb], partial=[dg])
    gin_ring = Ring([S.sbuf(f"gin{i}", [128, 8, 544], BF16) for i in range(2)])
    hc = S.sbuf("hc", [128, 8, 512], F32)
    hcb = S.sbuf("hcb", [128, 8, 512], BF16)
    hsq = S.sbuf("hsq", [128, 8, 512], BF16)
    mean_sb = S.sbuf("mean_sb", [128, 512], F32)
    var_sb = S.sbuf("var_sb", [128, 512], F32)
    rstd2 = S.sbuf("rstd2", [128, 512], F32)
    tv = S.sbuf("tv", [128, 512], F32)
    dtmp = Ring([S.sbuf(f"dtmp{i}", [128, 512], F32) for i in range(2)])
    sl = S.sbuf("sl", [128, 8, 512], BF16)
    xt_ring = Ring([S.sbuf(f"xtv_{i}", [128, 8, 512], F32) for i in range(1)])
    hout_ring = Ring([S.sbuf(f"houtv_{i}", [128, 8, 512], F32) for i in range(1)])
    for (off, n, ci, is_halo, is_first) in own_blocks(False):
        gin = gin_ring.next(); xt = xt_ring.next(); hout = hout_ring.next()
        S.load(gin, gin[:, :, :n + 30], GLU, GLU[:, :, off - 30:off + n])
        S.load(xt, xt[:, :, :n], HT, HT[:, :, off:off + n])
        for c8 in range(8):
            ps = proj_ps.next()
            for k in range(31):
                mm(ps[:, :n], dg[:, c8, k, :], gin[:, c8, k:k + n], k == 0, k == 30, [dg, gin], ps)
            S.op(act, lambda ps=ps, c8=c8: nc.scalar.activation(out=hc[:, c8, :n], in_=ps[:, :n], func=AF.Identity,
                                                                bias=vec2[:, V2_DWB + c8:V2_DWB + c8 + 1], scale=1.0),
                 reads=[ps, vec2], partial=[hc])
        S.op(dve, lambda: nc.vector.tensor_copy(out=hcb[:, :, :n], in_=hc[:, :, :n]), reads=[hc], writes=[hcb])
        S.op(act, lambda: nc.scalar.activation(out=hsq[:, :, :n], in_=hc[:, :, :n], func=AF.Square), reads=[hc], writes=[hsq])
        psm, psq = PS[5], PS[6]
        for c8 in range(8):
            mm(psm[:, :n], ones_m[:], hcb[:, c8, :n], c8 == 0, c8 == 7, [ones_m, hcb], psm)
        for c8 in range(8):
            mm(psq[:, :n], ones_m[:], hsq[:, c8, :n], c8 == 0, c8 == 7, [ones_m, hsq], psq)
        S.op(act, lambda: nc.scalar.activation(out=mean_sb[:, :n], in_=psm[:, :n], func=AF.Copy), reads=[psm], writes=[mean_sb])
        S.op(dve, lambda: nc.vector.tensor_tensor(out=var_sb[:, :n], in0=mean_sb[:, :n], in1=mean_sb[:, :n], op=ALU.mult),
             reads=[mean_sb], writes=[var_sb])
        S.op(dve, lambda: nc.vector.tensor_tensor(out=var_sb[:, :n], in0=psq[:, :n], in1=var_sb[:, :n], op=ALU.subtract),
             reads=[psq, var_sb], writes=[var_sb])
        S.op(act, lambda: nc.scalar.activation(out=tv[:, :n], in_=var_sb[:, :n], func=AF.Sqrt, bias=eps_t[:, 0:1], scale=1.0),
             reads=[var_sb, eps_t], writes=[tv])
        S.op(dve, lambda: nc.vector.reciprocal(out=rstd2[:, :n], in_=tv[:, :n]), reads=[tv], writes=[rstd2])
        for c8 in range(8):
            dt_ = dtmp.next()
            S.op(pool, lambda c8=c8, dt_=dt_: nc.gpsimd.tensor_tensor(out=dt_[:, :n], in0=hc[:, c8, :n], in1=mean_sb[:, :n], op=ALU.subtract),
                 reads=[hc, mean_sb], writes=[dt_])
            S.op(dve, lambda c8=c8, dt_=dt_: nc.vector.tensor_tensor(out=dt_[:, :n], in0=dt_[:, :n], in1=rstd2[:, :n], op=ALU.mult),
                 reads=[dt_, rstd2], writes=[dt_])
            S.op(act, lambda c8=c8, dt_=dt_: nc.scalar.activation(out=sl[:, c8, :n], in_=dt_[:, :n], func=AF.Silu,
                                                                  bias=vec2[:, V2_LNB + c8:V2_LNB + c8 + 1],
                                                                  scale=vec2[:, V2_LNG + c8:V2_LNG + c8 + 1]),
                 reads=[dt_, vec2], partial=[sl])
        for o in range(8):
            ps = proj_ps.next()
            proj(ps, wco, o * 128, 128, [sl[:, c, :n] for c in range(8)], n, [sl])
            residual_out(ps, n, o, xt, xt[:, o, :n], hout)
        S.store(HT, HT[:, :, off:off + n], hout, hout[:, :, :n])
    S.pop()

    cross_phase(1, own_blocks(False))
    ffn_phase(1, own_blocks(False), True)
    S.finish([out_hT])
    return nc


def _fm(a):
    t = a.shape[0]
    return np.ascontiguousarray(a.T.reshape(8, 128, t).transpose(1, 0, 2))


def _colvec(v):
    return np.ascontiguousarray(v.reshape(-1, 128).T)


def prepare_inputs(inputs, stop_after=None):
    f = lambda k: np.asarray(inputs[k], dtype=np.float32)
    x = f("x"); mem = f("mem")
    w_in = f("w_in_ab")[0]
    sp = np.cumsum((512, 512, 512, 512, 1024, 16, 64))[:-1]
    wq, wk, wv, wu, wiq, wiw, wik = np.split(w_in, sp, axis=1)
    w_keys = np.ascontiguousarray(np.concatenate([wk, wik, wik, wv], axis=1))
    w_own = np.ascontiguousarray(np.concatenate([wq, wiq, wu, wiw], axis=1))
    vecs = np.concatenate([_colvec(f(k)[l]) for k in ("norm_mix", "norm_cross", "norm_mem", "norm_ffn")
                           for l in range(2)], axis=1)
    vecs = np.ascontiguousarray(vecs.astype(np.float32))
    v2 = np.zeros((128, 64), np.float32)
    v2[:, 0] = f("a_q_norm")[0]; v2[:, 1] = f("a_k_norm")[0]
    v2[:, 2:6] = _colvec(f("pool_scale")[0])
    v2[:, 6:8] = _colvec(f("cross_q_norm")[0]); v2[:, 8:10] = _colvec(f("cross_q_norm")[1])
    v2[:, 10:12] = _colvec(f("cross_k_norm")[0]); v2[:, 12:14] = _colvec(f("cross_k_norm")[1])
    v2[:, 14:30] = _colvec(f("conv_b_in")[0])
    v2[:, 30:38] = _colvec(f("conv_dw_b")[0])
    v2[:, 38:46] = _colvec(f("conv_ln_g")[0])
    v2[:, 46:54] = _colvec(f("conv_ln_b")[0])
    conv_dw = np.ascontiguousarray(f("conv_dw_w")[0].T.reshape(8, 128, 31).transpose(1, 0, 2))
    seqpos = np.arange(SEQ)
    c128s, s128s = rope_tables(seqpos, 128)
    c64s, s64s = rope_tables(seqpos, 64)
    shared = dict(
        iota=np.ascontiguousarray(np.broadcast_to(np.arange(MBW, dtype=np.float32), (128, MBW))),
        c128s=c128s, s128s=s128s, c64s=c64s, s64s=s64s,
        p128=perm_matrix(128), p64=perm_matrix(64), ident=np.eye(128, dtype=np.float32),
        w_keys=w_keys, w_own=w_own, vecs=vecs, vecs2=v2,
        pool_w=f("pool_w")[0], w_out_ab=f("w_out_ab")[0], conv_w_in=f("conv_w_in")[0], conv_dw=conv_dw,
        conv_w_out=f("conv_w_out")[0], cross_wq=f("cross_wq"), cross_wk=f("cross_wk"),
        cross_wv=f("cross_wv"), cross_wo=f("cross_wo"), ffn_wg=f("ffn_w_gate"), ffn_wu=f("ffn_w_up"),
        ffn_wd=f("ffn_w_down"),
    )
    in_maps = []
    for core in range(8):
        b, half = divmod(core, 2)
        tiles = own_tiles(half)
        xo = np.zeros((NOWN, D), np.float32)
        pos = np.zeros(NOWN, np.int64)
        for s, t in enumerate(tiles):
            if t >= 0:
                xo[s * 128:(s + 1) * 128] = x[b, t * 128:(t + 1) * 128]
                pos[s * 128:(s + 1) * 128] = np.arange(t * 128, (t + 1) * 128)
        qa = np.zeros((128, NSLOT), np.float32)
        for s in range(NSLOT):
            base = (slot_first_uncertain(s) // 4) * 512
            qa[:, s] = pos[s * 128:(s + 1) * 128] - base
        c128o, s128o = rope_tables(pos, 128)
        c64o, s64o = rope_tables(pos, 64)
        pc = np.ones((128, 2, 4, 16), np.float32)
        hf = np.ones((128, 2), np.float32)
        if half == 0:
            hf[:, 0] = 0.0
            for g, w in enumerate((2, 4, 8, 16)):
                for t in range(16):
                    pc[:, 0, g, t] = w / min(t + 1, w)
        m = dict(shared)
        m.update(xT_seq=_fm(x[b]), xT_own=_fm(xo), memT=_fm(mem[b]), qadj=qa,
                 c128o=c128o, s128o=s128o, c64o=c64o, s64o=s64o, poolcorr=pc, haloflag=hf)
        in_maps.append(m)
    return in_maps


_NC_CACHE = {}


def kernel(**inputs):
    in_maps = prepare_inputs(inputs)
    if "nc" not in _NC_CACHE:
        _NC_CACHE["nc"] = build_program()
    nc = _NC_CACHE["nc"]
    res = run_bass_kernel_spmd(nc, in_maps, core_ids=list(range(8)))
    out = np.zeros((4, SEQ, D), np.float32)
    for core in range(8):
        b, half = divmod(core, 2)
        o = res.results[core]["out_hT"]
        o = o.transpose(2, 1, 0).reshape(32 * 128, D)
        for ci in range(2):
            start = (2 * ci + half) * CH_TILES * 128
            out[b, start:start + 2048] = o[ci * 2048:(ci + 1) * 2048]
    return out
```

```python
import numpy as np
import ml_dtypes
import concourse.bass as bass
import concourse.mybir as mybir
from concourse.bass_utils import run_bass_kernel_spmd

F32 = mybir.dt.float32
BF16 = mybir.dt.bfloat16
AF = mybir.ActivationFunctionType
ALU = mybir.AluOpType
AX = mybir.AxisListType

D = 1024
SEQ = 8192
NT_SEQ = SEQ // 128
CH_TILES = 16
SLOTS_PER_CH = CH_TILES + 1
NSLOT = 2 * SLOTS_PER_CH
NOWN = NSLOT * 128
MEM = 256
DFF = 2816
EPS = 1e-6
NEG = -1.0e30
NITER = 20
MBW = 3072

SAME_ENGINE_SYNC = True
SEQ_PE = True
SKEW = True
SKEW_A = True


class Buf:
    def __init__(self, name, ap_fn):
        self.name = name
        self.ap_fn = ap_fn
        self.w = {}
        self.r = {}
        self.dsem = None
        self.dval = 0

    def __getitem__(self, idx):
        return self.ap_fn()[idx]


class Eng:
    def __init__(self, name, inst, is_pe=False):
        self.name = name
        self.inst = inst
        self.sem = None
        self.cnt = 0
        self.known = {}
        self.is_pe = is_pe


class Sched:
    EPOCH = 30000

    def __init__(self, nc):
        self.nc = nc
        self.pe = Eng("pe", nc.tensor, True)
        self.act = Eng("act", nc.scalar)
        self.dve = Eng("dve", nc.vector)
        self.pool = Eng("pool", nc.gpsimd)
        self.sp = Eng("sp", nc.sync)
        self.engs = [self.pe, self.act, self.dve, self.pool, self.sp]
        self.nsem = 0
        self.all_dma_bufs = []
        self.nbuf = 0
        self.scopes = []
        self.live = []
        self.all_sems = []

    def new_sem(self, name):
        self.nsem += 1
        sm_ = self.nc.alloc_semaphore(f"{name}_{self.nsem}")
        self.all_sems.append(sm_)
        return sm_

    def sbuf(self, name, shape, dtype):
        self.nbuf += 1
        if self.scopes:
            t = self.scopes[-1].enter_context(self.nc.sbuf_tensor(f"{name}_{self.nbuf}", list(shape), dtype))
        else:
            t = self.nc.alloc_sbuf_tensor(f"{name}_{self.nbuf}", list(shape), dtype)
        b = Buf(name, lambda: t)
        self.live.append(b)
        return b

    def push(self):
        import contextlib
        self.scopes.append(contextlib.ExitStack())

    def pop(self):
        self.barrier()
        self.scopes.pop().close()

    def barrier(self):
        evs = {}
        for e in self.engs:
            if e.sem is not None and e.cnt > 0:
                evs[id(e.sem)] = (e.sem, e.cnt)
        for b in self.live:
            if b.dsem is not None and b.dval > 0:
                evs[id(b.dsem)] = (b.dsem, b.dval)
        for e in self.engs:
            for k, (sm, v) in evs.items():
                if e.sem is not None and sm is e.sem:
                    continue
                if e.known.get(k, 0) >= v:
                    continue
                e.inst.wait_ge(sm, v)
                e.known[k] = v

    def psum(self, name, shape, dtype):
        self.nbuf += 1
        t = self.nc.alloc_psum_tensor(f"{name}_{self.nbuf}", list(shape), dtype)
        return Buf(name, lambda: t)

    def dram(self, name, shape, dtype, kind="Internal"):
        t = self.nc.dram_tensor(name, list(shape), dtype, kind=kind)
        a = t.ap()
        return Buf(name, lambda: a)

    def view(self, name, buf, idx):
        return Buf(name, lambda: buf.ap_fn()[idx])

    def _collect(self, eng, reads, writes):
        need = {}

        def add(d):
            for s, v in d.items():
                k = id(s)
                if k not in need or need[k][1] < v:
                    need[k] = (s, v)
        for b in reads:
            add(b.w)
        for b in writes:
            add(b.w)
            add(b.r)
        for k, (s, v) in need.items():
            if eng.sem is not None and s is eng.sem:
                if eng.is_pe or not SAME_ENGINE_SYNC:
                    continue
            if eng.known.get(k, 0) >= v:
                continue
            eng.inst.wait_ge(s, v)
            eng.known[k] = v

    def op(self, eng, fn, reads=(), writes=(), partial=()):
        allw = list(writes) + list(partial)
        self._collect(eng, reads, allw)
        if eng.sem is None or eng.cnt >= self.EPOCH:
            eng.sem = self.new_sem(eng.name)
            eng.cnt = 0
        ins = fn()
        eng.cnt += 1
        ins.then_inc(eng.sem, 1)
        s, v = eng.sem, eng.cnt
        for b in reads:
            if b.r.get(s, 0) < v:
                b.r[s] = v
        for b in writes:
            b.w = {s: v}
            b.r = {}
        for b in partial:
            b.w[s] = v
        return ins

    def dma(self, out_buf, out_ap, in_buf, in_ap, eng=None, sbuf_side=None, **kw):
        eng = eng or self.sp
        owner = sbuf_side
        self._collect(eng, [in_buf], [out_buf])
        if owner.dsem is None or owner.dval >= self.EPOCH:
            owner.dsem = self.new_sem("d" + owner.name)
            owner.dval = 0
        ins = eng.inst.dma_start(out=out_ap, in_=in_ap, **kw)
        owner.dval += 16
        ins.then_inc(owner.dsem, 16)
        s, v = owner.dsem, owner.dval
        if in_buf.r.get(s, 0) < v:
            in_buf.r[s] = v
        out_buf.w[s] = v
        return ins

    def load(self, dst, dst_ap, src, src_ap, eng=None, **kw):
        return self.dma(dst, dst_ap, src, src_ap, eng=eng, sbuf_side=dst, **kw)

    def store(self, dst, dst_ap, src, src_ap, eng=None, **kw):
        return self.dma(dst, dst_ap, src, src_ap, eng=eng, sbuf_side=src, **kw)

    def finish(self, bufs):
        self._collect(self.sp, bufs, [])


class Ring:
    def __init__(self, bufs):
        self.bufs = bufs
        self.i = 0

    def next(self):
        b = self.bufs[self.i % len(self.bufs)]
        self.i += 1
        return b


def own_tiles(half):
    tiles = []
    for ci in range(2):
        start = (2 * ci + half) * CH_TILES
        tiles.append(start - 1)
        tiles.extend(range(start, start + CH_TILES))
    return tiles


def slot_nkt(slot):
    ci, i = divmod(slot, SLOTS_PER_CH)
    t1 = (2 * ci + 1) * CH_TILES + i - 1
    return t1 + 1


def slot_first_uncertain(slot):
    ci, i = divmod(slot, SLOTS_PER_CH)
    t0 = 2 * ci * CH_TILES + i - 1
    return max(t0, 0)


def rope_tables(pos, head_dim):
    rot = head_dim // 4
    half = rot // 2
    inv = 500000.0 ** (-np.arange(half, dtype=np.float32) * 2.0 / rot)
    ang = pos.astype(np.float32)[None, :] * inv[:, None].astype(np.float32)
    cos = np.cos(ang).astype(np.float32)
    sin = np.sin(ang).astype(np.float32)
    C = np.ones((128, len(pos)), np.float32)
    S = np.zeros((128, len(pos)), np.float32)
    for h0 in range(0, 128, head_dim):
        C[h0:h0 + half] = cos
        C[h0 + half:h0 + rot] = cos
        S[h0:h0 + half] = -sin
        S[h0 + half:h0 + rot] = sin
    return C, S


def perm_matrix(head_dim):
    rot = head_dim // 4
    half = rot // 2
    P = np.zeros((128, 128), np.float32)
    for h0 in range(0, 128, head_dim):
        for i in range(half):
            P[h0 + half + i, h0 + i] = 1.0
            P[h0 + i, h0 + half + i] = 1.0
    return P


def build_program(stop_after=None, slots=None):
    nc = bass.Bass("TRN2", target_bir_lowering=False)
    S = Sched(nc)
    pe, act, dve, pool, sp = S.pe, S.act, S.dve, S.pool, S.sp
    dbg = {}

    def din(name, shape, dtype=F32):
        return S.dram(name, shape, dtype, kind="ExternalInput")

    xT_seq = din("xT_seq", [128, 8, SEQ])
    xT_own = din("xT_own", [128, 8, NOWN])
    memT = din("memT", [128, 8, MEM])
    qadj = din("qadj", [128, NSLOT])
    iota_in = din("iota", [128, MBW])
    c128s = din("c128s", [128, SEQ]); s128s = din("s128s", [128, SEQ])
    c64s = din("c64s", [128, SEQ]); s64s = din("s64s", [128, SEQ])
    c128o = din("c128o", [128, NOWN]); s128o = din("s128o", [128, NOWN])
    c64o = din("c64o", [128, NOWN]); s64o = din("s64o", [128, NOWN])
    p128_in = din("p128", [128, 128]); p64_in = din("p64", [128, 128])
    ident_in = din("ident", [128, 128])
    poolcorr_in = din("poolcorr", [128, 2, 4, 16])
    haloflag_in = din("haloflag", [128, 2])
    w_keys = din("w_keys", [D, 1152])
    w_own = din("w_own", [D, 2064])
    vecs = din("vecs", [128, 64])
    pool_w = din("pool_w", [4, 128, 128])
    w_out_ab = din("w_out_ab", [D, D])
    conv_w_in = din("conv_w_in", [D, 2 * D])
    conv_dw = din("conv_dw", [128, 8, 31])
    conv_w_out = din("conv_w_out", [D, D])
    cross_wq = din("cross_wq", [2, D, D]); cross_wk = din("cross_wk", [2, D, D])
    cross_wv = din("cross_wv", [2, D, D]); cross_wo = din("cross_wo", [2, D, D])
    ffn_wg = din("ffn_wg", [2, D, DFF]); ffn_wu = din("ffn_wu", [2, D, DFF])
    ffn_wd = din("ffn_wd", [2, DFF, D])
    out_hT = S.dram("out_hT", [128, 8, 32 * 128], F32, kind="ExternalOutput")

    KT = S.dram("KT", [128, 4, SEQ], BF16)
    Vd = S.dram("Vd", [SEQ, 512], BF16)
    QT = S.dram("QT", [128, 4, NOWN], BF16)
    IQT = S.dram("IQT", [128, 8, NOWN], BF16)
    BT = S.dram("BT", [128, 4, NOWN], BF16)
    AT = S.dram("AT", [128, 4, NOWN], BF16)
    HT = S.dram("HT", [128, 8, NOWN], F32)
    HID = S.dram("HID", [128, 22, NOWN], BF16)

    ones_m = S.sbuf("ones_m", [128, 128], BF16)
    ones_h = S.sbuf("ones_h", [128, 128], BF16)
    ones_c = S.sbuf("ones_c", [128, 128], BF16)
    ones_1 = S.sbuf("ones_1", [128, 128], BF16)
    ident = S.sbuf("ident", [128, 128], BF16)
    p128 = S.sbuf("p128", [128, 128], BF16)
    p64 = S.sbuf("p64", [128, 128], BF16)
    cst_f = S.sbuf("cst_f", [128, 3, 128], F32)
    vec = S.sbuf("vec", [128, 64], F32)
    qadj_sb = S.sbuf("qadj_sb", [128, NSLOT], F32)
    eps_t = S.sbuf("eps_t", [128, 1], F32)

    S.op(pool, lambda: nc.gpsimd.memset(ones_m[:], 1.0 / 1024), writes=[ones_m])
    S.op(pool, lambda: nc.gpsimd.memset(ones_h[:], 1.0 / 128), writes=[ones_h])
    S.op(pool, lambda: nc.gpsimd.memset(ones_c[:], 1.0 / 256), writes=[ones_c])
    S.op(pool, lambda: nc.gpsimd.memset(ones_1[:], 1.0), writes=[ones_1])
    S.op(pool, lambda: nc.gpsimd.memset(eps_t[:], EPS), writes=[eps_t])
    S.load(cst_f, cst_f[:, 0, :], ident_in, ident_in[:, :])
    S.load(cst_f, cst_f[:, 1, :], p128_in, p128_in[:, :])
    S.load(cst_f, cst_f[:, 2, :], p64_in, p64_in[:, :])
    S.load(vec, vec[:], vecs, vecs[:, :])
    S.load(qadj_sb, qadj_sb[:], qadj, qadj[:, :])
    S.op(dve, lambda: nc.vector.tensor_copy(out=ident[:], in_=cst_f[:, 0, :]), reads=[cst_f], writes=[ident])
    S.op(dve, lambda: nc.vector.tensor_copy(out=p128[:], in_=cst_f[:, 1, :]), reads=[cst_f], writes=[p128])
    S.op(dve, lambda: nc.vector.tensor_copy(out=p64[:], in_=cst_f[:, 2, :]), reads=[cst_f], writes=[p64])

    V_NMIX0, V_NMIX1, V_NCROSS0, V_NCROSS1, V_NMEM0, V_NMEM1, V_NFFN0, V_NFFN1 = [8 * i for i in range(8)]
    vec2_in = din("vecs2", [128, 64])
    vec2 = S.sbuf("vec2", [128, 64], F32)
    S.load(vec2, vec2[:], vec2_in, vec2_in[:, :])
    V2_AQ, V2_AK = 0, 1
    V2_PSCALE = 2
    V2_CQ0, V2_CQ1, V2_CK0, V2_CK1 = 6, 8, 10, 12
    V2_BIN = 14
    V2_DWB = 30
    V2_LNG = 38
    V2_LNB = 46

    PS = [S.psum(f"ps{i}", [128, 512], F32) for i in range(7)]
    PSB = S.psum("psb", [128, 1024], BF16)

    def load_w_cast(dst, col_dst, w_buf, w_ap, col0, M):
        K = w_ap.shape[0]
        for kc in range(K // 128):
            m0 = 0
            while m0 < M:
                mm_ = min(2048, M - m0)
                S.load(dst, dst[:, kc, col_dst + m0:col_dst + m0 + mm_], w_buf,
                       w_ap[kc * 128:(kc + 1) * 128, col0 + m0:col0 + m0 + mm_], eng=pool)
                m0 += mm_

    def mm(ps_ap, lhsT, rhs, start, stop, reads, ps_buf):
        S.op(pe, lambda: nc.tensor.matmul(ps_ap, lhsT=lhsT, rhs=rhs, start=start, stop=stop),
             reads=reads, partial=[ps_buf] if not start else (), writes=[ps_buf] if start else ())

    def rstd_from_ms(ps_buf, n, out_buf, tmp_buf):
        S.op(act, lambda: nc.scalar.activation(out=tmp_buf[:, :n], in_=ps_buf[:, :n], func=AF.Sqrt,
                                               bias=eps_t[:, 0:1], scale=1.0),
             reads=[ps_buf, eps_t], writes=[tmp_buf])
        S.op(dve, lambda: nc.vector.reciprocal(out=out_buf[:, :n], in_=tmp_buf[:, :n]),
             reads=[tmp_buf], writes=[out_buf])

    def own_blocks(include_halo=True):
        res = []
        for ci in range(2):
            base = ci * SLOTS_PER_CH * 128
            if include_halo:
                res.append((base, 128, ci, True, False))
            for i in range(4):
                res.append((base + 128 + 512 * i, 512, ci, False, i == 0))
        return res

    class Common:
        pass

    def alloc_common():
        cm = Common()
        cm.xt_ring = Ring([S.sbuf(f"xt{i}", [128, 8, 512], F32) for i in range(2)])
        cm.sq_b = S.sbuf("sq", [128, 8, 512], BF16)
        cm.xn_b = S.sbuf("xn", [128, 8, 512], BF16)
        cm.rstd_b = S.sbuf("rstd", [128, 512], F32)
        cm.tmp_b = S.sbuf("tmpf", [128, 512], F32)
        return cm

    def norm_block(cm, src_buf, off, n, gcol):
        xt = cm.xt_ring.next()
        S.load(xt, xt[:, :, :n], src_buf, src_buf[:, :, off:off + n])
        S.op(act, lambda: nc.scalar.activation(out=cm.sq_b[:, :, :n], in_=xt[:, :, :n], func=AF.Square),
             reads=[xt], writes=[cm.sq_b])
        ps = PS[0]
        for c in range(8):
            mm(ps[:, :n], ones_m[:], cm.sq_b[:, c, :n], c == 0, c == 7, [ones_m, cm.sq_b], ps)
        rstd_from_ms(ps, n, cm.rstd_b, cm.tmp_b)
        for c in range(8):
            S.op(dve, lambda c=c: nc.vector.scalar_tensor_tensor(
                out=cm.xn_b[:, c, :n], in0=xt[:, c, :n], scalar=vec[:, gcol + c:gcol + c + 1],
                in1=cm.rstd_b[:, :n], op0=ALU.mult, op1=ALU.mult),
                reads=[xt, vec, cm.rstd_b], partial=[cm.xn_b])
        return xt

    def alloc_tmps():
        return (S.sbuf("sqh", [128, 512], BF16), S.sbuf("kn", [128, 512], BF16), S.sbuf("rk", [128, 512], F32),
                S.sbuf("tk", [128, 512], F32), S.sbuf("t1", [128, 512], F32), S.sbuf("t2", [128, 512], F32))

    def norm_head_rope(ps, n, gcol2, ones_t, pm, c_ap, s_ap, cs_bufs, out_ap, out_buf, do_norm, tmps):
        sqh, kn, rk, tk, t1, t2 = tmps
        ps2, ps3 = PS[5], PS[6]
        if do_norm:
            S.op(act, lambda: nc.scalar.activation(out=sqh[:, :n], in_=ps[:, :n], func=AF.Square),
                 reads=[ps], writes=[sqh])
            mm(ps2[:, :n], ones_t[:], sqh[:, :n], True, True, [ones_t, sqh], ps2)
            rstd_from_ms(ps2, n, rk, tk)
            S.op(dve, lambda: nc.vector.scalar_tensor_tensor(
                out=kn[:, :n], in0=ps[:, :n], scalar=vec2[:, gcol2:gcol2 + 1], in1=rk[:, :n],
                op0=ALU.mult, op1=ALU.mult), reads=[ps, vec2, rk], writes=[kn])
        else:
            S.op(act, lambda: nc.scalar.activation(out=kn[:, :n], in_=ps[:, :n], func=AF.Copy),
                 reads=[ps], writes=[kn])
        mm(ps3[:, :n], pm[:], kn[:, :n], True, True, [pm, kn], ps3)
        S.op(pool, lambda: nc.gpsimd.tensor_tensor(out=t1[:, :n], in0=kn[:, :n], in1=c_ap, op=ALU.mult),
             reads=[kn] + cs_bufs, writes=[t1])
        S.op(dve, lambda: nc.vector.tensor_tensor(out=t2[:, :n], in0=ps3[:, :n], in1=s_ap, op=ALU.mult),
             reads=[ps3] + cs_bufs, writes=[t2])
        if isinstance(out_ap, list):
            for (oap, obuf, p0, p1) in out_ap:
                S.op(dve, lambda oap=oap, p0=p0, p1=p1: nc.vector.tensor_tensor(out=oap, in0=t1[p0:p1, :n], in1=t2[p0:p1, :n], op=ALU.add),
                     reads=[t1, t2], partial=[obuf])
        else:
            S.op(dve, lambda: nc.vector.tensor_tensor(out=out_ap, in0=t1[:, :n], in1=t2[:, :n], op=ALU.add),
                 reads=[t1, t2], partial=[out_buf])

    proj_ps = Ring([PS[1], PS[2], PS[3], PS[4]])

    def proj(ps, w_sb, col0, ncols, rhs_list, n, rbufs):
        kc = len(rhs_list)
        for c in range(kc):
            mm(ps[:, :n], w_sb[:, c, col0:col0 + ncols], rhs_list[c], c == 0, c == kc - 1, [w_sb] + rbufs, ps)

    S.push()
    ikT0 = S.sbuf("ikT0", [128, SEQ], BF16)
    ikT1 = S.sbuf("ikT1", [128, SEQ], BF16)
    iw_sb = S.sbuf("iw_sb", [128, NSLOT, 16], F32)
    S.op(pool, lambda: nc.gpsimd.memset(ikT0[:], 0.0), writes=[ikT0])
    S.op(pool, lambda: nc.gpsimd.memset(ikT1[:], 0.0), writes=[ikT1])

    S.push()
    cm = alloc_common()
    tmps = alloc_tmps()
    cs_ring = Ring([S.sbuf(f"cs{i}", [128, 4, 512], F32) for i in range(2)])
    wk_sb = S.sbuf("wk_sb", [128, 8, 1152], BF16)
    load_w_cast(wk_sb, 0, w_keys, w_keys.ap_fn(), 0, 1152)
    kout_ring = Ring([S.sbuf(f"kout{i}", [128, 4, 512], BF16) for i in range(2)])
    vout_ring = Ring([S.sbuf(f"vout{i}", [128, 4, 512], BF16) for i in range(2)])
    for blk in range(SEQ // 512):
        off = blk * 512
        n = 512
        norm_block(cm, xT_seq, off, n, V_NMIX0)
        xn_b = cm.xn_b
        cs = cs_ring.next()
        S.load(cs, cs[:, 0, :], c128s, c128s[:, off:off + n])
        S.load(cs, cs[:, 1, :], s128s, s128s[:, off:off + n])
        S.load(cs, cs[:, 2, :], c64s, c64s[:, off:off + n])
        S.load(cs, cs[:, 3, :], s64s, s64s[:, off:off + n])
        kout = kout_ring.next()
        for kc in range(4):
            ps = proj_ps.next()
            proj(ps, wk_sb, kc * 128, 128, [xn_b[:, c, :n] for c in range(8)], n, [xn_b])
            norm_head_rope(ps, n, V2_AK, ones_h, p128, cs[:, 0, :n], cs[:, 1, :n], [cs],
                           kout[:, kc, :n], kout, True, tmps)
        S.store(KT, KT[:, :, off:off + n], kout, kout[:, :, :n])
        ps = proj_ps.next()
        proj(ps, wk_sb, 512, 128, [xn_b[:, c, :n] for c in range(8)], n, [xn_b])
        norm_head_rope(ps, n, 0, None, p64, cs[:, 2, :n], cs[:, 3, :n], [cs],
                       [(ikT0[0:64, off:off + n], ikT0, 0, 64), (ikT1[64:128, off:off + n], ikT1, 64, 128)],
                       None, False, tmps)
        vout = vout_ring.next()
        for tt in range(4):
            ps = proj_ps.next()
            for c in range(8):
                mm(ps[:, :], xn_b[:, c, tt * 128:(tt + 1) * 128], wk_sb[:, c, 640:1152], c == 0, c == 7,
                   [wk_sb, xn_b], ps)
            S.op(act, lambda tt=tt, ps=ps: nc.scalar.activation(out=vout[:, tt, :], in_=ps[:, :], func=AF.Copy),
                 reads=[ps], partial=[vout])
        S.store(Vd, Vd.ap_fn()[off:off + n, :].rearrange("(t p) d -> p t d", p=128), vout, vout[:, :, :])
    S.pop()

    S.push()
    cm = alloc_common()
    tmps = alloc_tmps()
    cs_ring = Ring([S.sbuf(f"cs{i}", [128, 4, 512], F32) for i in range(1)])
    wo_sb = S.sbuf("wo_sb", [128, 8, 2064], BF16)
    load_w_cast(wo_sb, 0, w_own, w_own.ap_fn(), 0, 2064)
    pw_sb = S.sbuf("pw_sb", [128, 4, 128], BF16)
    for g in range(4):
        S.load(pw_sb, pw_sb[:, g, :], pool_w, pool_w[g, :, :], eng=pool)
    pcorr = S.sbuf("pcorr", [128, 2, 4, 16], F32)
    S.load(pcorr, pcorr[:], poolcorr_in, poolcorr_in[:, :, :, :])
    qout_ring = Ring([S.sbuf(f"qout{i}", [128, 4, 512], BF16) for i in range(2)])
    iqout_ring = Ring([S.sbuf(f"iqout{i}", [128, 8, 512], BF16) for i in range(1)])
    bout_ring = Ring([S.sbuf(f"bout{i}", [128, 4, 512], BF16) for i in range(2)])
    Ug = [S.sbuf(f"U{g}", [128, 528], F32) for g in range(4)]
    sA = [S.sbuf(f"sA{g}", [128, 528], F32) for g in range(4)]
    sB = [S.sbuf(f"sB{g}", [128, 528], F32) for g in range(4)]
    pooled = [S.sbuf(f"pooled{g}", [128, 512], BF16) for g in range(4)]
    for (off, n, ci, is_halo, is_first) in own_blocks():
        norm_block(cm, xT_own, off, n, V_NMIX0)
        xn_b = cm.xn_b
        xl = [xn_b[:, c, :n] for c in range(8)]
        cs = cs_ring.next()
        S.load(cs, cs[:, 0, :n], c128o, c128o[:, off:off + n])
        S.load(cs, cs[:, 1, :n], s128o, s128o[:, off:off + n])
        S.load(cs, cs[:, 2, :n], c64o, c64o[:, off:off + n])
        S.load(cs, cs[:, 3, :n], s64o, s64o[:, off:off + n])
        qout = qout_ring.next()
        for kc in range(4):
            ps = proj_ps.next()
            proj(ps, wo_sb, kc * 128, 128, xl, n, [xn_b])
            norm_head_rope(ps, n, V2_AQ, ones_h, p128, cs[:, 0, :n], cs[:, 1, :n], [cs],
                           qout[:, kc, :n], qout, True, tmps)
        S.store(QT, QT[:, :, off:off + n], qout, qout[:, :, :n])
        iqout = iqout_ring.next()
        for kc in range(8):
            ps = proj_ps.next()
            proj(ps, wo_sb, 512 + kc * 128, 128, xl, n, [xn_b])
            norm_head_rope(ps, n, 0, None, p64, cs[:, 2, :n], cs[:, 3, :n], [cs],
                           iqout[:, kc, :n], iqout, False, tmps)
        S.store(IQT, IQT[:, :, off:off + n], iqout, iqout[:, :, :n])
        for tt in range(n // 128):
            slot = off // 128 + tt
            ps = proj_ps.next()
            for c in range(8):
                mm(ps[:, :16], xn_b[:, c, tt * 128:(tt + 1) * 128], wo_sb[:, c, 2048:2064], c == 0, c == 7,
                   [wo_sb, xn_b], ps)
            S.op(act, lambda ps=ps, slot=slot: nc.scalar.activation(out=iw_sb[:, slot, :], in_=ps[:, :16],
                                                                    func=AF.Copy, scale=1.0 / 32.0),
                 reads=[ps], partial=[iw_sb])
        L = 16 + n
        bout = bout_ring.next()
        for g in range(4):
            U = Ug[g]
            if is_halo:
                S.op(pool, lambda U=U: nc.gpsimd.memset(U[:, 0:16], 0.0), partial=[U])
            ps = proj_ps.next()
            proj(ps, wo_sb, 1536 + g * 128, 128, xl, n, [xn_b])
            S.op(act, lambda ps=ps, U=U: nc.scalar.activation(out=U[:, 16:L], in_=ps[:, :n], func=AF.Copy),
                 reads=[ps], partial=[U])
            a_, b_ = sA[g], sB[g]
            S.op(pool, lambda U=U, a_=a_: nc.gpsimd.tensor_tensor(out=a_[:, 1:L], in0=U[:, 1:L], in1=U[:, 0:L - 1], op=ALU.add),
                 reads=[U], writes=[a_])
            fin = a_
            if g >= 1:
                S.op(pool, lambda a_=a_, b_=b_: nc.gpsimd.tensor_tensor(out=b_[:, 3:L], in0=a_[:, 3:L], in1=a_[:, 1:L - 2], op=ALU.add),
                     reads=[a_], writes=[b_])
                fin = b_
            if g >= 2:
                S.op(pool, lambda a_=a_, b_=b_: nc.gpsimd.tensor_tensor(out=a_[:, 7:L], in0=b_[:, 7:L], in1=b_[:, 3:L - 4], op=ALU.add),
                     reads=[b_], writes=[a_])
                fin = a_
            if g >= 3:
                S.op(pool, lambda a_=a_, b_=b_: nc.gpsimd.tensor_tensor(out=b_[:, 15:L], in0=a_[:, 15:L], in1=a_[:, 7:L - 8], op=ALU.add),
                     reads=[a_], writes=[b_])
                fin = b_
            if is_first:
                S.op(dve, lambda fin=fin, g=g: nc.vector.tensor_tensor(out=fin[:, 16:32], in0=fin[:, 16:32],
                                                                       in1=pcorr[:, ci, g, :], op=ALU.mult),
                     reads=[pcorr, fin], partial=[fin])
            w_ = float(2 ** (g + 1))
            S.op(dve, lambda fin=fin, U=U, g=g: nc.vector.scalar_tensor_tensor(
                out=pooled[g][:, :n], in0=fin[:, 16:L], scalar=1.0 / w_, in1=U[:, 16:L],
                op0=ALU.mult, op1=ALU.subtract), reads=[fin, U], writes=[pooled[g]])
            S.op(pool, lambda U=U: nc.gpsimd.tensor_copy(out=U[:, 0:16], in_=U[:, n:n + 16]), reads=[U], partial=[U])
            ps = proj_ps.next()
            mm(ps[:, :n], pw_sb[:, g, :], pooled[g][:, :n], True, True, [pw_sb, pooled[g]], ps)
            S.op(act, lambda ps=ps, g=g: nc.scalar.activation(out=bout[:, g, :n], in_=ps[:, :n], func=AF.Copy,
                                                               scale=vec2[:, V2_PSCALE + g:V2_PSCALE + g + 1]),
                 reads=[ps, vec2], partial=[bout])
        S.store(BT, BT[:, :, off:off + n], bout, bout[:, :, :n])
    S.pop()

    S.push()
    iota_sb = S.sbuf("iota_sb", [128, MBW], F32)
    S.load(iota_sb, iota_sb[:], iota_in, iota_in[:, :])
    scores2 = [S.sbuf(f"scores{i}", [128, SEQ], F32) for i in range(2)]
    mbias = S.sbuf("mbias", [128, SEQ], BF16)
    junk = S.sbuf("junk", [128, SEQ // 2], BF16)
    mb2 = [S.sbuf(f"mb{i}", [128, MBW], BF16) for i in range(2)]
    tmpu = S.sbuf("tmpu", [128, MBW], F32)
    qt_ring = Ring([S.sbuf(f"qt{i}", [128, 4, 128], BF16) for i in range(2)])
    iqt_ring = Ring([S.sbuf(f"iqt{i}", [128, 8, 128], BF16) for i in range(2)])
    kb_ring = Ring([S.sbuf(f"kblk{i}", [128, 4, 512], BF16) for i in range(2)])
    vb_ring = Ring([S.sbuf(f"vblk{i}", [128, 4, 512], BF16) for i in range(2)])
    r_ring = Ring([S.sbuf(f"R{i}", [128, 512], BF16) for i in range(4)])
    p_ring = Ring([S.sbuf(f"P{i}", [128, 512], BF16) for i in range(3)])
    pt_ring = Ring([S.sbuf(f"PT{i}", [128, 512], BF16) for i in range(3)])
    diag2 = [S.sbuf(f"diag{i}", [128, 16, 128], BF16) for i in range(2)]
    sm = S.sbuf("sm", [128, 16], F32)
    hs = S.sbuf("hs", [128, 32], F32)
    pow2 = S.sbuf("pow2", [128, 32], F32)
    cntb = S.sbuf("cntb", [128, 2], F32)
    midr = Ring([S.sbuf(f"mid{i}", [128, 1], F32) for i in range(2)])
    eb = S.sbuf("eb", [128, 1], F32)
    rs = S.sbuf("rs", [128, 4, 16], F32)
    rsum = S.sbuf("rsum", [128, 4], F32)
    rrec = S.sbuf("rrec", [128, 4], F32)
    negone = S.sbuf("negone", [128, 4], F32)
    rjunk = S.sbuf("rjunk", [128, 16], F32)
    a_tok = S.sbuf("a_tok", [128, 512], BF16)
    aT_ring = Ring([S.sbuf(f"aT{i}", [128, 4, 128], BF16) for i in range(2)])
    for i in range(32):
        S.op(pool, lambda i=i: nc.gpsimd.memset(pow2[:, i:i + 1], 2.0 ** (-i)), partial=[pow2])
    S.op(pool, lambda: nc.gpsimd.memset(negone[:], -1.0), writes=[negone])
    s_ring = Ring([PS[0], PS[1]])
    sc_ring = Ring([PS[2]])
    l_ring = Ring([PS[4], PS[5]])
    Obank = PS[6]
    ps3b = Buf("ps3b", lambda: PS[3].ap_fn()[:, :].bitcast(BF16))
    PSBv = [S.view("psb0", PSB, (slice(None), slice(0, 512))), S.view("ps3b0", ps3b, (slice(None), slice(0, 512)))]
    ptp_ring = Ring(PSBv)
    SM_MX, SM_MN1, SM_MN2, SM_MN, SM_H, SM_TAU = range(6)
    MASKV = -30000.0

    def slot_geom(j):
        nkt = slot_nkt(j)
        nkb = (nkt + 3) // 4
        ub = slot_first_uncertain(j) // 4
        return nkb, nkb * 512, ub, (nkb - ub) * 512

    def stage_A(j):
        nkb, N, ub, W = slot_geom(j)
        assert W <= MBW
        scores = scores2[j % 2]; mb = mb2[j % 2]; diag = diag2[j % 2]
        iqt = iqt_ring.next()
        S.load(iqt, iqt[:], IQT, IQT[:, :, j * 128:(j + 1) * 128])
        for h in range(16):
            S.op(pool, lambda h=h: nc.gpsimd.tensor_scalar(out=diag[:, h, :], in0=ident[:], scalar1=iw_sb[:, j, h:h + 1],
                                                          scalar2=None, op0=ALU.mult),
                 reads=[ident, iw_sb], partial=[diag])
        S.op(pool, lambda: nc.gpsimd.tensor_scalar(out=mb[:, :W], in0=iota_sb[:, :W], scalar1=qadj_sb[:, j:j + 1],
                                                  scalar2=MASKV, op0=ALU.is_gt, op1=ALU.mult),
             reads=[iota_sb, qadj_sb], writes=[mb])
        yield
        items = [(kb, h) for kb in range(nkb) for h in range(16)]
        pend = None
        sc = None
        for (kb, h) in items + [(None, None)]:
            cur = None
            if kb is not None:
                sp_ = s_ring.next()
                ikp = ikT0 if h % 2 == 0 else ikT1
                mm(sp_[:, :], iqt[:, h // 2, :], ikp[:, kb * 512:(kb + 1) * 512], True, True,
                   [iqt, ikp], sp_)
                R = r_ring.next()
                S.op(act, lambda sp_=sp_, R=R: nc.scalar.activation(out=R[:], in_=sp_[:, :], func=AF.Relu),
                     reads=[sp_], writes=[R])
                cur = (kb, h, R)
            if pend is not None:
                pkb, ph, pR = pend
                if ph == 0:
                    sc = sc_ring.next()
                mm(sc[:, :], diag[:, ph, :], pR[:], ph == 0, (ph == 15 and pkb < ub), [diag, pR], sc)
                if ph == 15:
                    if pkb >= ub:
                        mm(sc[:, :], ident[:], mb[:, (pkb - ub) * 512:(pkb - ub + 1) * 512], False, True, [ident, mb], sc)
                    S.op(act, lambda sc=sc, pkb=pkb: nc.scalar.activation(out=scores[:, pkb * 512:(pkb + 1) * 512], in_=sc[:, :],
                                                                          func=AF.Copy), reads=[sc], partial=[scores])
            pend = cur
            if not (SKEW or SKEW_A) and pend is not None:
                pkb, ph, pR = pend
                if ph == 0:
                    sc = sc_ring.next()
                mm(sc[:, :], diag[:, ph, :], pR[:], ph == 0, (ph == 15 and pkb < ub), [diag, pR], sc)
                if ph == 15:
                    if pkb >= ub:
                        mm(sc[:, :], ident[:], mb[:, (pkb - ub) * 512:(pkb - ub + 1) * 512], False, True, [ident, mb], sc)
                    S.op(act, lambda sc=sc, pkb=pkb: nc.scalar.activation(out=scores[:, pkb * 512:(pkb + 1) * 512], in_=sc[:, :],
                                                                          func=AF.Copy), reads=[sc], partial=[scores])
                pend = None
            yield

    def stage_B(j):
        nkb, N, ub, W = slot_geom(j)
        scores = scores2[j % 2]; mb = mb2[j % 2]
        S.op(dve, lambda: nc.vector.tensor_reduce(out=sm[:, SM_MX:SM_MX + 1], in_=scores[:, :N], axis=AX.X, op=ALU.max),
             reads=[scores], partial=[sm])
        S.op(dve, lambda: nc.vector.scalar_tensor_tensor(out=tmpu[:, :W], in0=mb[:, :W], scalar=-2.0,
                                                         in1=scores[:, ub * 512:N], op0=ALU.mult, op1=ALU.add),
             reads=[mb, scores], writes=[tmpu])
        S.op(dve, lambda: nc.vector.tensor_reduce(out=sm[:, SM_MN2:SM_MN2 + 1], in_=tmpu[:, :W], axis=AX.X, op=ALU.min),
             reads=[tmpu], partial=[sm])
        if ub > 0:
            S.op(dve, lambda: nc.vector.tensor_reduce(out=sm[:, SM_MN1:SM_MN1 + 1], in_=scores[:, :ub * 512], axis=AX.X, op=ALU.min),
                 reads=[scores], partial=[sm])
            S.op(dve, lambda: nc.vector.tensor_tensor(out=sm[:, SM_MN:SM_MN + 1], in0=sm[:, SM_MN1:SM_MN1 + 1],
                                                      in1=sm[:, SM_MN2:SM_MN2 + 1], op=ALU.min), reads=[sm], partial=[sm])
        else:
            S.op(dve, lambda: nc.vector.tensor_copy(out=sm[:, SM_MN:SM_MN + 1], in_=sm[:, SM_MN2:SM_MN2 + 1]),
                 reads=[sm], partial=[sm])
        S.op(dve, lambda: nc.vector.tensor_scalar(out=sm[:, SM_H:SM_H + 1], in0=sm[:, SM_MX:SM_MX + 1],
                                                  scalar1=sm[:, SM_MN:SM_MN + 1], scalar2=0.50005, op0=ALU.subtract, op1=ALU.mult),
             reads=[sm], partial=[sm])
        mid = midr.next()
        S.op(dve, lambda mid=mid: nc.vector.tensor_scalar(out=mid[:], in0=sm[:, SM_MX:SM_MX + 1],
                                                          scalar1=sm[:, SM_MN:SM_MN + 1], scalar2=0.5, op0=ALU.add, op1=ALU.mult),
             reads=[sm], writes=[mid])
        S.op(dve, lambda: nc.vector.tensor_scalar(out=hs[:], in0=pow2[:], scalar1=sm[:, SM_H:SM_H + 1], scalar2=None, op0=ALU.mult),
             reads=[pow2, sm], writes=[hs])
        yield
        N1 = min(N, SEQ // 2)
        for it in range(NITER):
            S.op(dve, lambda mid=mid: nc.vector.tensor_scalar(out=junk[:, :N1], in0=scores[:, :N1], scalar1=mid[:, 0:1], scalar2=0.0,
                                                              op0=ALU.is_ge, op1=ALU.add, accum_out=cntb[:, 0:1]),
                 reads=[scores, mid], writes=[junk, cntb])
            if N > N1:
                S.op(dve, lambda mid=mid: nc.vector.tensor_scalar(out=junk[:, :N - N1], in0=scores[:, N1:N], scalar1=mid[:, 0:1],
                                                                  scalar2=cntb[:, 0:1], op0=ALU.is_ge, op1=ALU.add, accum_out=cntb[:, 1:2]),
                     reads=[scores, mid, cntb], writes=[junk], partial=[cntb])
                ccol = 1
            else:
                ccol = 0
            S.op(dve, lambda ccol=ccol: nc.vector.tensor_scalar(out=eb[:], in0=cntb[:, ccol:ccol + 1], scalar1=255.5, scalar2=0.5,
                                                                op0=ALU.is_ge, op1=ALU.subtract), reads=[cntb], writes=[eb])
            nmid = midr.next()
            S.op(dve, lambda mid=mid, nmid=nmid, it=it: nc.vector.scalar_tensor_tensor(
                out=nmid[:], in0=eb[:], scalar=hs[:, it:it + 1], in1=mid[:], op0=ALU.mult, op1=ALU.add),
                reads=[eb, hs, mid], writes=[nmid])
            mid = nmid
            yield
        S.op(dve, lambda mid=mid: nc.vector.scalar_tensor_tensor(
            out=sm[:, SM_TAU:SM_TAU + 1], in0=sm[:, SM_H:SM_H + 1], scalar=-(2.0 ** (-NITER)), in1=mid[:],
            op0=ALU.mult, op1=ALU.add), reads=[sm, mid], partial=[sm])
        S.op(dve, lambda: nc.vector.tensor_scalar(out=mbias[:, :N], in0=scores[:, :N], scalar1=sm[:, SM_TAU:SM_TAU + 1],
                                                  scalar2=MASKV, op0=ALU.is_lt, op1=ALU.mult),
             reads=[scores, sm], writes=[mbias])
        yield

    def stage_C(j):
        nkb, N, ub, W = slot_geom(j)
        qt = qt_ring.next()
        S.load(qt, qt[:], QT, QT[:, :, j * 128:(j + 1) * 128])
        S.op(pool, lambda: nc.gpsimd.memset(rs[:], 0.0), writes=[rs])
        yield
        items = [(kb, h) for kb in range(nkb) for h in range(4)]
        nI = len(items)
        st = {}
        blk = {}
        first_o = [True]

        def qk(i):
            kb, h = items[i]
            if h == 0:
                kblk = kb_ring.next(); vblk = vb_ring.next()
                S.load(kblk, kblk[:], KT, KT[:, :, kb * 512:(kb + 1) * 512])
                S.load(vblk, vblk[:], Vd, Vd.ap_fn()[kb * 512:(kb + 1) * 512, :].rearrange("(t p) d -> p t d", p=128))
                blk[kb] = (kblk, vblk)
            kblk, vblk = blk[kb]
            Lp = l_ring.next()
            mm(Lp[:, :], qt[:, h, :], kblk[:, h, :], True, False, [qt, kblk], Lp)
            mm(Lp[:, :], ident[:], mbias[:, kb * 512:(kb + 1) * 512], False, True, [ident, mbias], Lp)
            Pb = p_ring.next()
            S.op(act, lambda: nc.scalar.activation(out=Pb[:], in_=Lp[:, :], func=AF.Exp, scale=128.0 ** -0.5,
                                                   accum_out=rs[:, h, kb:kb + 1]),
                 reads=[Lp], writes=[Pb], partial=[rs])
            st[i] = [Pb, None]

        def tr(i):
            Pb = st[i][0]
            ptp = ptp_ring.next()
            for tt in range(4):
                S.op(pe, lambda tt=tt: nc.tensor.transpose(out=ptp[:, tt * 128:(tt + 1) * 128],
                                                           in_=Pb[:, tt * 128:(tt + 1) * 128], identity=ident[:]),
                     reads=[Pb, ident], writes=[ptp] if tt == 0 else (), partial=[ptp] if tt else ())
            PTb = pt_ring.next()
            S.op(act, lambda: nc.scalar.activation(out=PTb[:], in_=ptp[:, :], func=AF.Copy), reads=[ptp], writes=[PTb])
            st[i][1] = PTb

        def pv(i):
            kb, h = items[i]
            PTb = st[i][1]
            kblk, vblk = blk[kb]
            for tt in range(4):
                fo = first_o[0]
                first_o[0] = False
                S.op(pe, lambda tt=tt, fo=fo: nc.tensor.matmul(
                    Obank[:, h * 128:(h + 1) * 128], lhsT=PTb[:, tt * 128:(tt + 1) * 128],
                    rhs=vblk[:, tt, h * 128:(h + 1) * 128], start=fo, stop=(i == nI - 1 and tt == 3),
                    skip_group_check=True),
                    reads=[PTb, vblk], writes=[Obank] if fo else (), partial=() if fo else [Obank])
            del st[i]

        if SKEW:
            for g in range(nI + 2):
                if g < nI:
                    qk(g)
                if 1 <= g <= nI:
                    tr(g - 1)
                if g >= 2:
                    pv(g - 2)
                yield
        else:
            for g in range(nI):
                qk(g)
                tr(g)
                pv(g)
                yield
        for h in range(4):
            S.op(act, lambda h=h: nc.scalar.activation(out=rjunk[:, :], in_=rs[:, h, :], func=AF.Copy, accum_out=rsum[:, h:h + 1]),
                 reads=[rs], writes=[rjunk], partial=[rsum])
        S.op(pool, lambda: nc.gpsimd.tensor_tensor(out=rrec[:], in0=rsum[:], in1=negone[:], op=ALU.pow),
             reads=[rsum, negone], writes=[rrec])
        for h in range(4):
            S.op(act, lambda h=h: nc.scalar.activation(out=a_tok[:, h * 128:(h + 1) * 128], in_=Obank[:, h * 128:(h + 1) * 128],
                                                       func=AF.Copy, scale=rrec[:, h:h + 1]),
                 reads=[Obank, rrec], partial=[a_tok])
        ptp = ptp_ring.next()
        for h in range(4):
            S.op(pe, lambda h=h, ptp=ptp: nc.tensor.transpose(out=ptp[:, h * 128:(h + 1) * 128],
                                                              in_=a_tok[:, h * 128:(h + 1) * 128], identity=ident[:]),
                 reads=[a_tok, ident], writes=[ptp] if h == 0 else (), partial=[ptp] if h else ())
        aT = aT_ring.next()
        S.op(act, lambda ptp=ptp, aT=aT: nc.scalar.activation(out=aT[:].rearrange("p a b -> p (a b)"), in_=ptp[:, :], func=AF.Copy),
             reads=[ptp], writes=[aT])
        S.store(AT, AT[:, :, j * 128:(j + 1) * 128], aT, aT[:])
        yield

    def run_all(g):
        for _ in g:
            pass

    slot_list = list(slots) if slots is not None else list(range(NSLOT))
    ns = len(slot_list)
    run_all(stage_A(slot_list[0]))
    for si in range(ns + 1):
        gC = stage_C(slot_list[si - 1]) if si - 1 >= 0 else None
        gA = stage_A(slot_list[si + 1]) if si + 1 < ns else None
        gB = stage_B(slot_list[si]) if si < ns else None
        nC = slot_geom(slot_list[si - 1])[0] * 4 + 4 if gC is not None else 0
        nA = slot_geom(slot_list[si + 1])[0] * 16 + 2 if gA is not None else 0
        nB = NITER + 2 if gB is not None else 0
        live = {"A": gA, "B": gB, "C": gC}
        tot = {"A": nA, "B": nB, "C": nC}
        done = {"A": 0, "B": 0, "C": 0}
        wgt = {"A": 1.0, "B": 1.0, "C": 0.6}
        while any(g is not None for g in live.values()):
            best = None
            for k, g in live.items():
                if g is None:
                    continue
                frac = wgt[k] * done[k] / max(1, tot[k])
                if best is None or frac < best[0]:
                    best = (frac, k)
            k = best[1]
            if SEQ_PE and k == "A" and live["C"] is not None:
                k = "C"
            if k == "B" and done["B"] >= NITER + 1 and live["C"] is not None:
                k = "C"
            try:
                next(live[k])
                done[k] += 1
            except StopIteration:
                live[k] = None
    S.pop()
    S.pop()

    if stop_after == "3":
        dbg_a = S.dram("dbg_a", [128, 4, NOWN], BF16, kind="ExternalOutput")
        dbg_b = S.dram("dbg_b", [128, 4, NOWN], BF16, kind="ExternalOutput")
        S.push()
        big = S.sbuf("dbgbig", [128, 4, NOWN], BF16)
        S.load(big, big[:], AT, AT[:, :, :])
        S.store(dbg_a, dbg_a[:, :, :], big, big[:])
        S.load(big, big[:], BT, BT[:, :, :])
        S.store(dbg_b, dbg_b[:, :, :], big, big[:])
        S.finish([dbg_a, dbg_b])
        return nc

    GLU = S.dram("GLU", [128, 8, NOWN], BF16)
    hout_ring_holder = {}

    def residual_out(ps, n, c, res_buf, res_ap, hout):
        S.op(dve, lambda: nc.vector.tensor_tensor(out=hout[:, c, :n], in0=ps[:, :n], in1=res_ap, op=ALU.add),
             reads=[ps, res_buf], partial=[hout])

    S.push()
    wout_sb = S.sbuf("wout_sb", [128, 8, D], BF16)
    load_w_cast(wout_sb, 0, w_out_ab, w_out_ab.ap_fn(), 0, D)
    xt_ring = Ring([S.sbuf(f"xt4_{i}", [128, 8, 512], F32) for i in range(2)])
    ab_ring = Ring([S.sbuf(f"ab{i}", [128, 8, 512], BF16) for i in range(2)])
    hout_ring = Ring([S.sbuf(f"hout4_{i}", [128, 8, 512], F32) for i in range(2)])
    for (off, n, ci, is_halo, is_first) in own_blocks():
        xt = xt_ring.next(); ab = ab_ring.next(); hout = hout_ring.next()
        S.load(xt, xt[:, :, :n], xT_own, xT_own[:, :, off:off + n])
        S.load(ab, ab[:, 0:4, :n], AT, AT[:, :, off:off + n])
        S.load(ab, ab[:, 4:8, :n], BT, BT[:, :, off:off + n])
        for o in range(8):
            ps = proj_ps.next()
            proj(ps, wout_sb, o * 128, 128, [ab[:, c, :n] for c in range(8)], n, [ab])
            residual_out(ps, n, o, xt, xt[:, o, :n], hout)
        S.store(HT, HT[:, :, off:off + n], hout, hout[:, :, :n])
    S.pop()

    def cross_phase(l, blocks):
        S.push()
        gq, gk = (V2_CQ0, V2_CK0) if l == 0 else (V2_CQ1, V2_CK1)
        ncross = V_NCROSS0 if l == 0 else V_NCROSS1
        nmem = V_NMEM0 if l == 0 else V_NMEM1
        cm = alloc_common()
        kcT = S.sbuf("kcT", [128, 8, MEM], BF16)
        vc = S.sbuf("vc", [128, 2, D], BF16)
        sqc = S.sbuf("sqc", [128, 2, 512], BF16)
        rq = S.sbuf("rq", [128, 512], F32)
        tq = S.sbuf("tq", [128, 512], F32)
        S.push()
        wkv = S.sbuf("wkv", [128, 8, 2 * D], BF16)
        load_w_cast(wkv, 0, cross_wk, cross_wk.ap_fn()[l], 0, D)
        load_w_cast(wkv, D, cross_wv, cross_wv.ap_fn()[l], 0, D)
        norm_block(cm, memT, 0, MEM, nmem)
        xn_b = cm.xn_b
        n = MEM
        for hh in range(4):
            pss = [proj_ps.next(), proj_ps.next()]
            for dc in range(2):
                proj(pss[dc], wkv, (hh * 2 + dc) * 128, 128, [xn_b[:, c, :n] for c in range(8)], n, [xn_b])
                S.op(act, lambda dc=dc, pss=pss: nc.scalar.activation(out=sqc[:, dc, :n], in_=pss[dc][:, :n], func=AF.Square),
                     reads=[pss[dc]], partial=[sqc])
            ps2 = PS[5]
            for dc in range(2):
                mm(ps2[:, :n], ones_c[:], sqc[:, dc, :n], dc == 0, dc == 1, [ones_c, sqc], ps2)
            rstd_from_ms(ps2, n, rq, tq)
            for dc in range(2):
                S.op(dve, lambda dc=dc, pss=pss, hh=hh: nc.vector.scalar_tensor_tensor(
                    out=kcT[:, hh * 2 + dc, :n], in0=pss[dc][:, :n], scalar=vec2[:, gk + dc:gk + dc + 1], in1=rq[:, :n],
                    op0=ALU.mult, op1=ALU.mult), reads=[pss[dc], vec2, rq], partial=[kcT])
        for mt in range(2):
            for hf in range(2):
                ps = proj_ps.next()
                for c in range(8):
                    mm(ps[:, :], xn_b[:, c, mt * 128:(mt + 1) * 128], wkv[:, c, D + hf * 512:D + (hf + 1) * 512], c == 0, c == 7,
                       [wkv, xn_b], ps)
                S.op(act, lambda ps=ps, mt=mt, hf=hf: nc.scalar.activation(out=vc[:, mt, hf * 512:(hf + 1) * 512], in_=ps[:, :], func=AF.Copy),
                     reads=[ps], partial=[vc])
        S.pop()
        wqo = S.sbuf("wqo", [128, 8, 2 * D], BF16)
        load_w_cast(wqo, 0, cross_wq, cross_wq.ap_fn()[l], 0, D)
        load_w_cast(wqo, D, cross_wo, cross_wo.ap_fn()[l], 0, D)
        qn = S.sbuf("qn", [128, 8, 512], BF16)
        on = S.sbuf("on", [128, 8, 512], BF16)
        pc_ring = Ring([S.sbuf(f"pc{i}", [128, 2, 512], BF16) for i in range(2)])
        rden = S.sbuf("rden", [128, 512], F32)
        hout_ring = Ring([S.sbuf(f"houtc_{i}", [128, 8, 512], F32) for i in range(2)])
        for (off, n, ci, is_halo, is_first) in blocks:
            xt = norm_block(cm, HT, off, n, ncross)
            xn_b = cm.xn_b
            hout = hout_ring.next()
            for hh in range(4):
                pss = [proj_ps.next(), proj_ps.next()]
                for dc in range(2):
                    proj(pss[dc], wqo, (hh * 2 + dc) * 128, 128, [xn_b[:, c, :n] for c in range(8)], n, [xn_b])
                    S.op(act, lambda dc=dc, pss=pss: nc.scalar.activation(out=sqc[:, dc, :n], in_=pss[dc][:, :n], func=AF.Square),
                         reads=[pss[dc]], partial=[sqc])
                ps2 = PS[5]
                for dc in range(2):
                    mm(ps2[:, :n], ones_c[:], sqc[:, dc, :n], dc == 0, dc == 1, [ones_c, sqc], ps2)
                rstd_from_ms(ps2, n, rq, tq)
                for dc in range(2):
                    S.op(dve, lambda dc=dc, pss=pss, hh=hh: nc.vector.scalar_tensor_tensor(
                        out=qn[:, hh * 2 + dc, :n], in0=pss[dc][:, :n], scalar=vec2[:, gq + dc:gq + dc + 1], in1=rq[:, :n],
                        op0=ALU.mult, op1=ALU.mult), reads=[pss[dc], vec2, rq], partial=[qn])
                pc = pc_ring.next()
                for mt in range(2):
                    ps = proj_ps.next()
                    for dc in range(2):
                        mm(ps[:, :n], kcT[:, hh * 2 + dc, mt * 128:(mt + 1) * 128], qn[:, hh * 2 + dc, :n], dc == 0, dc == 1,
                           [kcT, qn], ps)
                    S.op(act, lambda ps=ps, mt=mt, pc=pc: nc.scalar.activation(out=pc[:, mt, :n], in_=ps[:, :n], func=AF.Exp,
                                                                                scale=1.0 / 16.0), reads=[ps], partial=[pc])
                psd = PS[6]
                for mt in range(2):
                    mm(psd[:, :n], ones_1[:], pc[:, mt, :n], mt == 0, mt == 1, [ones_1, pc], psd)
                S.op(dve, lambda psd=psd: nc.vector.reciprocal(out=rden[:, :n], in_=psd[:, :n]), reads=[psd], writes=[rden])
                for dc in range(2):
                    ps = proj_ps.next()
                    for mt in range(2):
                        mm(ps[:, :n], vc[:, mt, hh * 256 + dc * 128:hh * 256 + (dc + 1) * 128], pc[:, mt, :n], mt == 0, mt == 1,
                           [vc, pc], ps)
                    S.op(dve, lambda ps=ps, dc=dc, hh=hh: nc.vector.tensor_tensor(out=on[:, hh * 2 + dc, :n], in0=ps[:, :n],
                                                                                 in1=rden[:, :n], op=ALU.mult),
                         reads=[ps, rden], partial=[on])
            for o in range(8):
                ps = proj_ps.next()
                proj(ps, wqo, D + o * 128, 128, [on[:, c, :n] for c in range(8)], n, [on])
                residual_out(ps, n, o, xt, xt[:, o, :n], hout)
            S.store(HT, HT[:, :, off:off + n], hout, hout[:, :, :n])
        S.pop()

    def ffn_phase(l, blocks, final):
        nffn = V_NFFN0 if l == 0 else V_NFFN1
        S.push()
        cm = alloc_common()
        wgu = S.sbuf("wgu", [128, 8, 2 * DFF], BF16)
        load_w_cast(wgu, 0, ffn_wg, ffn_wg.ap_fn()[l], 0, DFF)
        load_w_cast(wgu, DFF, ffn_wu, ffn_wu.ap_fn()[l], 0, DFF)
        hid_ring = Ring([S.sbuf(f"hid{i}", [128, 22, 512], BF16) for i in range(2)])
        sg_ring = Ring([S.sbuf(f"sg{i}", [128, 512], F32) for i in range(2)])
        for (off, n, ci, is_halo, is_first) in blocks:
            norm_block(cm, HT, off, n, nffn)
            xn_b = cm.xn_b
            hid = hid_ring.next()
            xl = [xn_b[:, c, :n] for c in range(8)]
            for f in range(22):
                psg = proj_ps.next(); psu = proj_ps.next()
                proj(psg, wgu, f * 128, 128, xl, n, [xn_b])
                proj(psu, wgu, DFF + f * 128, 128, xl, n, [xn_b])
                sg = sg_ring.next()
                S.op(act, lambda psg=psg, sg=sg: nc.scalar.activation(out=sg[:, :n], in_=psg[:, :n], func=AF.Silu),
                     reads=[psg], writes=[sg])
                S.op(dve, lambda psu=psu, sg=sg, f=f: nc.vector.tensor_tensor(out=hid[:, f, :n], in0=psu[:, :n], in1=sg[:, :n], op=ALU.mult),
                     reads=[psu, sg], partial=[hid])
            S.store(HID, HID[:, :, off:off + n], hid, hid[:, :, :n])
        S.pop()
        S.push()
        wd = S.sbuf("wd", [128, 22, D], BF16)
        load_w_cast(wd, 0, ffn_wd, ffn_wd.ap_fn()[l], 0, D)
        hid_ring = Ring([S.sbuf(f"hidb{i}", [128, 22, 512], BF16) for i in range(2)])
        xt_ring = Ring([S.sbuf(f"xtd_{i}", [128, 8, 512], F32) for i in range(2)])
        hout_ring = Ring([S.sbuf(f"houtd_{i}", [128, 8, 512], F32) for i in range(2)])
        for (off, n, ci, is_halo, is_first) in blocks:
            hid = hid_ring.next(); xt = xt_ring.next(); hout = hout_ring.next()
            S.load(hid, hid[:, :, :n], HID, HID[:, :, off:off + n])
            S.load(xt, xt[:, :, :n], HT, HT[:, :, off:off + n])
            for o in range(8):
                ps = proj_ps.next()
                proj(ps, wd, o * 128, 128, [hid[:, f, :n] for f in range(22)], n, [hid])
                residual_out(ps, n, o, xt, xt[:, o, :n], hout)
            if final:
                slot0 = off // 128
                ci_, i_ = divmod(slot0, SLOTS_PER_CH)
                oo = (ci_ * CH_TILES + i_ - 1) * 128
                S.store(out_hT, out_hT[:, :, oo:oo + n], hout, hout[:, :, :n])
            else:
                S.store(HT, HT[:, :, off:off + n], hout, hout[:, :, :n])
        S.pop()

    cross_phase(0, own_blocks())
    ffn_phase(0, own_blocks(), False)

    S.push()
    cm = alloc_common()
    wci = S.sbuf("wci", [128, 8, 2 * D], BF16)
    load_w_cast(wci, 0, conv_w_in, conv_w_in.ap_fn(), 0, 2 * D)
    hflag = S.sbuf("hflag", [128, 2], F32)
    S.load(hflag, hflag[:], haloflag_in, haloflag_in[:, :])
    sig_ring = Ring([S.sbuf(f"sig{i}", [128, 512], F32) for i in range(2)])
    glu_ring = Ring([S.sbuf(f"glu{i}", [128, 8, 512], BF16) for i in range(2)])
    for (off, n, ci, is_halo, is_first) in own_blocks():
        norm_block(cm, HT, off, n, V_NMIX1)
        xn_b = cm.xn_b
        xl = [xn_b[:, c, :n] for c in range(8)]
        glu = glu_ring.next()
        for c8 in range(8):
            psa = proj_ps.next(); psg = proj_ps.next()
            proj(psa, wci, c8 * 128, 128, xl, n, [xn_b])
            proj(psg, wci, D + c8 * 128, 128, xl, n, [xn_b])
            sig = sig_ring.next()
            S.op(act, lambda psg=psg, sig=sig, c8=c8: nc.scalar.activation(out=sig[:, :n], in_=psg[:, :n], func=AF.Sigmoid,
                                                                          bias=vec2[:, V2_BIN + 8 + c8:V2_BIN + 9 + c8], scale=1.0),
                 reads=[psg, vec2], writes=[sig])
            S.op(dve, lambda psa=psa, sig=sig, c8=c8: nc.vector.scalar_tensor_tensor(
                out=glu[:, c8, :n], in0=psa[:, :n], scalar=vec2[:, V2_BIN + c8:V2_BIN + c8 + 1], in1=sig[:, :n],
                op0=ALU.add, op1=ALU.mult), reads=[psa, vec2, sig], partial=[glu])
        if is_halo:
            S.op(dve, lambda glu=glu: nc.vector.tensor_scalar(out=glu[:, :, :n], in0=glu[:, :, :n], scalar1=hflag[:, ci:ci + 1],
                                                             scalar2=None, op0=ALU.mult), reads=[hflag, glu], partial=[glu])
        S.store(GLU, GLU[:, :, off:off + n], glu, glu[:, :, :n])
    S.pop()

    S.push()
    wco = S.sbuf("wco", [128, 8, D], BF16)
    load_w_cast(wco, 0, conv_w_out, conv_w_out.ap_fn(), 0, D)
    dw_sb = S.sbuf("dw_sb", [128, 8, 31], F32)
    S.load(dw_sb, dw_sb[:], conv_dw, conv_dw[:, :, :])
    dg = S.sbuf("dg", [128, 8, 31, 128], BF16)
    for c8 in range(8):
        for k in range(31):
            S.op(act, lambda c8=c8, k=k: nc.scalar.activation(out=dg[:, c8, k, :], in_=ident[:], func=AF.Copy,
                                                              scale=dw_sb[:, c8, k:k + 1]), reads=[ident, dw_sb], partial=[dg])
    gin_ring = Ring([S.sbuf(f"gin{i}", [128, 8, 544], BF16) for i in range(2)])
    hc = S.sbuf("hc", [128, 8, 512], F32)
    hcb = S.sbuf("hcb", [128, 8, 512], BF16)
    hsq = S.sbuf("hsq", [128, 8, 512], BF16)
    mean_sb = S.sbuf("mean_sb", [128, 512], F32)
    var_sb = S.sbuf("var_sb", [128, 512], F32)
    rstd2 = S.sbuf("rstd2", [128, 512], F32)
    tv = S.sbuf("tv", [128, 512], F32)
    dtmp = Ring([S.sbuf(f"dtmp{i}", [128, 512], F32) for i in range(2)])
    sl = S.sbuf("sl", [128, 8, 512], BF16)
    xt_ring = Ring([S.sbuf(f"xtv_{i}", [128, 8, 512], F32) for i in range(1)])
    hout_ring = Ring([S.sbuf(f"houtv_{i}", [128, 8, 512], F32) for i in range(1)])
    for (off, n, ci, is_halo, is_first) in own_blocks(False):
        gin = gin_ring.next(); xt = xt_ring.next(); hout = hout_ring.next()
        S.load(gin, gin[:, :, :n + 30], GLU, GLU[:, :, off - 30:off + n])
        S.load(xt, xt[:, :, :n], HT, HT[:, :, off:off + n])
        for c8 in range(8):
            ps = proj_ps.next()
            for k in range(31):
                mm(ps[:, :n], dg[:, c8, k, :], gin[:, c8, k:k + n], k == 0, k == 30, [dg, gin], ps)
            S.op(act, lambda ps=ps, c8=c8: nc.scalar.activation(out=hc[:, c8, :n], in_=ps[:, :n], func=AF.Identity,
                                                                bias=vec2[:, V2_DWB + c8:V2_DWB + c8 + 1], scale=1.0),
                 reads=[ps, vec2], partial=[hc])
        S.op(dve, lambda: nc.vector.tensor_copy(out=hcb[:, :, :n], in_=hc[:, :, :n]), reads=[hc], writes=[hcb])
        S.op(act, lambda: nc.scalar.activation(out=hsq[:, :, :n], in_=hc[:, :, :n], func=AF.Square), reads=[hc], writes=[hsq])
        psm, psq = PS[5], PS[6]
        for c8 in range(8):
            mm(psm[:, :n], ones_m[:], hcb[:, c8, :n], c8 == 0, c8 == 7, [ones_m, hcb], psm)
        for c8 in range(8):
            mm(psq[:, :n], ones_m[:], hsq[:, c8, :n], c8 == 0, c8 == 7, [ones_m, hsq], psq)
        S.op(act, lambda: nc.scalar.activation(out=mean_sb[:, :n], in_=psm[:, :n], func=AF.Copy), reads=[psm], writes=[mean_sb])
        S.op(dve, lambda: nc.vector.tensor_tensor(out=var_sb[:, :n], in0=mean_sb[:, :n], in1=mean_sb[:, :n], op=ALU.mult),
             reads=[mean_sb], writes=[var_sb])
        S.op(dve, lambda: nc.vector.tensor_tensor(out=var_sb[:, :n], in0=psq[:, :n], in1=var_sb[:, :n], op=ALU.subtract),
             reads=[psq, var_sb], writes=[var_sb])
        S.op(act, lambda: nc.scalar.activation(out=tv[:, :n], in_=var_sb[:, :n], func=AF.Sqrt, bias=eps_t[:, 0:1], scale=1.0),
             reads=[var_sb, eps_t], writes=[tv])
        S.op(dve, lambda: nc.vector.reciprocal(out=rstd2[:, :n], in_=tv[:, :n]), reads=[tv], writes=[rstd2])
        for c8 in range(8):
            dt_ = dtmp.next()
            S.op(pool, lambda c8=c8, dt_=dt_: nc.gpsimd.tensor_tensor(out=dt_[:, :n], in0=hc[:, c8, :n], in1=mean_sb[:, :n], op=ALU.subtract),
                 reads=[hc, mean_sb], writes=[dt_])
            S.op(dve, lambda c8=c8, dt_=dt_: nc.vector.tensor_tensor(out=dt_[:, :n], in0=dt_[:, :n], in1=rstd2[:, :n], op=ALU.mult),
                 reads=[dt_, rstd2], writes=[dt_])
            S.op(act, lambda c8=c8, dt_=dt_: nc.scalar.activation(out=sl[:, c8, :n], in_=dt_[:, :n], func=AF.Silu,
                                                                  bias=vec2[:, V2_LNB + c8:V2_LNB + c8 + 1],
                                                                  scale=vec2[:, V2_LNG + c8:V2_LNG + c8 + 1]),
                 reads=[dt_, vec2], partial=[sl])
        for o in range(8):
            ps = proj_ps.next()
            proj(ps, wco, o * 128, 128, [sl[:, c, :n] for c in range(8)], n, [sl])
            residual_out(ps, n, o, xt, xt[:, o, :n], hout)
        S.store(HT, HT[:, :, off:off + n], hout, hout[:, :, :n])
    S.pop()

    cross_phase(1, own_blocks(False))
    ffn_phase(1, own_blocks(False), True)
    S.finish([out_hT])
    return nc


def _fm(a):
    t = a.shape[0]
    return np.ascontiguousarray(a.T.reshape(8, 128, t).transpose(1, 0, 2))


def _colvec(v):
    return np.ascontiguousarray(v.reshape(-1, 128).T)


def prepare_inputs(inputs, stop_after=None):
    f = lambda k: np.asarray(inputs[k], dtype=np.float32)
    x = f("x"); mem = f("mem")
    w_in = f("w_in_ab")[0]
    sp = np.cumsum((512, 512, 512, 512, 1024, 16, 64))[:-1]
    wq, wk, wv, wu, wiq, wiw, wik = np.split(w_in, sp, axis=1)
    w_keys = np.ascontiguousarray(np.concatenate([wk, wik, wik, wv], axis=1))
    w_own = np.ascontiguousarray(np.concatenate([wq, wiq, wu, wiw], axis=1))
    vecs = np.concatenate([_colvec(f(k)[l]) for k in ("norm_mix", "norm_cross", "norm_mem", "norm_ffn")
                           for l in range(2)], axis=1)
    vecs = np.ascontiguousarray(vecs.astype(np.float32))
    v2 = np.zeros((128, 64), np.float32)
    v2[:, 0] = f("a_q_norm")[0]; v2[:, 1] = f("a_k_norm")[0]
    v2[:, 2:6] = _colvec(f("pool_scale")[0])
    v2[:, 6:8] = _colvec(f("cross_q_norm")[0]); v2[:, 8:10] = _colvec(f("cross_q_norm")[1])
    v2[:, 10:12] = _colvec(f("cross_k_norm")[0]); v2[:, 12:14] = _colvec(f("cross_k_norm")[1])
    v2[:, 14:30] = _colvec(f("conv_b_in")[0])
    v2[:, 30:38] = _colvec(f("conv_dw_b")[0])
    v2[:, 38:46] = _colvec(f("conv_ln_g")[0])
    v2[:, 46:54] = _colvec(f("conv_ln_b")[0])
    conv_dw = np.ascontiguousarray(f("conv_dw_w")[0].T.reshape(8, 128, 31).transpose(1, 0, 2))
    seqpos = np.arange(SEQ)
    c128s, s128s = rope_tables(seqpos, 128)
    c64s, s64s = rope_tables(seqpos, 64)
    shared = dict(
        iota=np.ascontiguousarray(np.broadcast_to(np.arange(MBW, dtype=np.float32), (128, MBW))),
        c128s=c128s, s128s=s128s, c64s=c64s, s64s=s64s,
        p128=perm_matrix(128), p64=perm_matrix(64), ident=np.eye(128, dtype=np.float32),
        w_keys=w_keys, w_own=w_own, vecs=vecs, vecs2=v2,
        pool_w=f("pool_w")[0], w_out_ab=f("w_out_ab")[0], conv_w_in=f("conv_w_in")[0], conv_dw=conv_dw,
        conv_w_out=f("conv_w_out")[0], cross_wq=f("cross_wq"), cross_wk=f("cross_wk"),
        cross_wv=f("cross_wv"), cross_wo=f("cross_wo"), ffn_wg=f("ffn_w_gate"), ffn_wu=f("ffn_w_up"),
        ffn_wd=f("ffn_w_down"),
    )
    in_maps = []
    for core in range(8):
        b, half = divmod(core, 2)
        tiles = own_tiles(half)
        xo = np.zeros((NOWN, D), np.float32)
        pos = np.zeros(NOWN, np.int64)
        for s, t in enumerate(tiles):
            if t >= 0:
                xo[s * 128:(s + 1) * 128] = x[b, t * 128:(t + 1) * 128]
                pos[s * 128:(s + 1) * 128] = np.arange(t * 128, (t + 1) * 128)
        qa = np.zeros((128, NSLOT), np.float32)
        for s in range(NSLOT):
            base = (slot_first_uncertain(s) // 4) * 512
            qa[:, s] = pos[s * 128:(s + 1) * 128] - base
        c128o, s128o = rope_tables(pos, 128)
        c64o, s64o = rope_tables(pos, 64)
        pc = np.ones((128, 2, 4, 16), np.float32)
        hf = np.ones((128, 2), np.float32)
        if half == 0:
            hf[:, 0] = 0.0
            for g, w in enumerate((2, 4, 8, 16)):
                for t in range(16):
                    pc[:, 0, g, t] = w / min(t + 1, w)
        m = dict(shared)
        m.update(xT_seq=_fm(x[b]), xT_own=_fm(xo), memT=_fm(mem[b]), qadj=qa,
                 c128o=c128o, s128o=s128o, c64o=c64o, s64o=s64o, poolcorr=pc, haloflag=hf)
        in_maps.append(m)
    return in_maps


_NC_CACHE = {}


def kernel(**inputs):
    in_maps = prepare_inputs(inputs)
    if "nc" not in _NC_CACHE:
        _NC_CACHE["nc"] = build_program()
    nc = _NC_CACHE["nc"]
    res = run_bass_kernel_spmd(nc, in_maps, core_ids=list(range(8)))
    out = np.zeros((4, SEQ, D), np.float32)
    for core in range(8):
        b, half = divmod(core, 2)
        o = res.results[core]["out_hT"]
        o = o.transpose(2, 1, 0).reshape(32 * 128, D)
        for ci in range(2):
            start = (2 * ci + half) * CH_TILES * 128
            out[b, start:start + 2048] = o[ci * 2048:(ci + 1) * 2048]
    return out
```

```python
import numpy as np
import ml_dtypes
import concourse.bass as bass
import concourse.mybir as mybir
from concourse.bass_utils import run_bass_kernel_spmd

F32 = mybir.dt.float32
BF16 = mybir.dt.bfloat16
AF = mybir.ActivationFunctionType
ALU = mybir.AluOpType
AX = mybir.AxisListType

D = 1024
SEQ = 8192
NT_SEQ = SEQ // 128
CH_TILES = 16
SLOTS_PER_CH = CH_TILES + 1
NSLOT = 2 * SLOTS_PER_CH
NOWN = NSLOT * 128
MEM = 256
DFF = 2816
EPS = 1e-6
NEG = -1.0e30
NITER = 24
MBW = 3072

SAME_ENGINE_SYNC = True
SEQ_PE = True
SKEW = True
SKEW_A = True


class Buf:
    def __init__(self, name, ap_fn):
        self.name = name
        self.ap_fn = ap_fn
        self.w = {}
        self.r = {}
        self.dsem = None
        self.dval = 0

    def __getitem__(self, idx):
        return self.ap_fn()[idx]


class Eng:
    def __init__(self, name, inst, is_pe=False):
        self.name = name
        self.inst = inst
        self.sem = None
        self.cnt = 0
        self.known = {}
        self.is_pe = is_pe


class Sched:
    EPOCH = 30000

    def __init__(self, nc):
        self.nc = nc
        self.pe = Eng("pe", nc.tensor, True)
        self.act = Eng("act", nc.scalar)
        self.dve = Eng("dve", nc.vector)
        self.pool = Eng("pool", nc.gpsimd)
        self.sp = Eng("sp", nc.sync)
        self.engs = [self.pe, self.act, self.dve, self.pool, self.sp]
        self.nsem = 0
        self.all_dma_bufs = []
        self.nbuf = 0
        self.scopes = []
        self.live = []
        self.all_sems = []

    def new_sem(self, name):
        self.nsem += 1
        sm_ = self.nc.alloc_semaphore(f"{name}_{self.nsem}")
        self.all_sems.append(sm_)
        return sm_

    def sbuf(self, name, shape, dtype):
        self.nbuf += 1
        if self.scopes:
            t = self.scopes[-1].enter_context(self.nc.sbuf_tensor(f"{name}_{self.nbuf}", list(shape), dtype))
        else:
            t = self.nc.alloc_sbuf_tensor(f"{name}_{self.nbuf}", list(shape), dtype)
        b = Buf(name, lambda: t)
        self.live.append(b)
        return b

    def push(self):
        import contextlib
        self.scopes.append(contextlib.ExitStack())

    def pop(self):
        self.barrier()
        self.scopes.pop().close()

    def barrier(self):
        evs = {}
        for e in self.engs:
            if e.sem is not None and e.cnt > 0:
                evs[id(e.sem)] = (e.sem, e.cnt)
        for b in self.live:
            if b.dsem is not None and b.dval > 0:
                evs[id(b.dsem)] = (b.dsem, b.dval)
        for e in self.engs:
            for k, (sm, v) in evs.items():
                if e.sem is not None and sm is e.sem:
                    continue
                if e.known.get(k, 0) >= v:
                    continue
                e.inst.wait_ge(sm, v)
                e.known[k] = v

    def psum(self, name, shape, dtype):
        self.nbuf += 1
        t = self.nc.alloc_psum_tensor(f"{name}_{self.nbuf}", list(shape), dtype)
        return Buf(name, lambda: t)

    def dram(self, name, shape, dtype, kind="Internal"):
        t = self.nc.dram_tensor(name, list(shape), dtype, kind=kind)
        a = t.ap()
        return Buf(name, lambda: a)

    def view(self, name, buf, idx):
        return Buf(name, lambda: buf.ap_fn()[idx])

    def _collect(self, eng, reads, writes):
        need = {}

        def add(d):
            for s, v in d.items():
                k = id(s)
                if k not in need or need[k][1] < v:
                    need[k] = (s, v)
        for b in reads:
            add(b.w)
        for b in writes:
            add(b.w)
            add(b.r)
        for k, (s, v) in need.items():
            if eng.sem is not None and s is eng.sem:
                if eng.is_pe or not SAME_ENGINE_SYNC:
                    continue
            if eng.known.get(k, 0) >= v:
                continue
            eng.inst.wait_ge(s, v)
            eng.known[k] = v

    def op(self, eng, fn, reads=(), writes=(), partial=()):
        allw = list(writes) + list(partial)
        self._collect(eng, reads, allw)
        if eng.sem is None or eng.cnt >= self.EPOCH:
            eng.sem = self.new_sem(eng.name)
            eng.cnt = 0
        ins = fn()
        eng.cnt += 1
        ins.then_inc(eng.sem, 1)
        s, v = eng.sem, eng.cnt
        for b in reads:
            if b.r.get(s, 0) < v:
                b.r[s] = v
        for b in writes:
            b.w = {s: v}
            b.r = {}
        for b in partial:
            b.w[s] = v
        return ins

    def dma(self, out_buf, out_ap, in_buf, in_ap, eng=None, sbuf_side=None, **kw):
        eng = eng or self.sp
        owner = sbuf_side
        self._collect(eng, [in_buf], [out_buf])
        if owner.dsem is None or owner.dval >= self.EPOCH:
            owner.dsem = self.new_sem("d" + owner.name)
            owner.dval = 0
        ins = eng.inst.dma_start(out=out_ap, in_=in_ap, **kw)
        owner.dval += 16
        ins.then_inc(owner.dsem, 16)
        s, v = owner.dsem, owner.dval
        if in_buf.r.get(s, 0) < v:
            in_buf.r[s] = v
        out_buf.w[s] = v
        return ins

    def load(self, dst, dst_ap, src, src_ap, eng=None, **kw):
        return self.dma(dst, dst_ap, src, src_ap, eng=eng, sbuf_side=dst, **kw)

    def store(self, dst, dst_ap, src, src_ap, eng=None, **kw):
        return self.dma(dst, dst_ap, src, src_ap, eng=eng, sbuf_side=src, **kw)

    def finish(self, bufs):
        self._collect(self.sp, bufs, [])


class Ring:
    def __init__(self, bufs):
        self.bufs = bufs
        self.i = 0

    def next(self):
        b = self.bufs[self.i % len(self.bufs)]
        self.i += 1
        return b


def own_tiles(half):
    tiles = []
    for ci in range(2):
        start = (2 * ci + half) * CH_TILES
        tiles.append(start - 1)
        tiles.extend(range(start, start + CH_TILES))
    return tiles


def slot_nkt(slot):
    ci, i = divmod(slot, SLOTS_PER_CH)
    t1 = (2 * ci + 1) * CH_TILES + i - 1
    return t1 + 1


def slot_first_uncertain(slot):
    ci, i = divmod(slot, SLOTS_PER_CH)
    t0 = 2 * ci * CH_TILES + i - 1
    return max(t0, 0)


def rope_tables(pos, head_dim):
    rot = head_dim // 4
    half = rot // 2
    inv = 500000.0 ** (-np.arange(half, dtype=np.float32) * 2.0 / rot)
    ang = pos.astype(np.float32)[None, :] * inv[:, None].astype(np.float32)
    cos = np.cos(ang).astype(np.float32)
    sin = np.sin(ang).astype(np.float32)
    C = np.ones((128, len(pos)), np.float32)
    S = np.zeros((128, len(pos)), np.float32)
    for h0 in range(0, 128, head_dim):
        C[h0:h0 + half] = cos
        C[h0 + half:h0 + rot] = cos
        S[h0:h0 + half] = -sin
        S[h0 + half:h0 + rot] = sin
    return C, S


def perm_matrix(head_dim):
    rot = head_dim // 4
    half = rot // 2
    P = np.zeros((128, 128), np.float32)
    for h0 in range(0, 128, head_dim):
        for i in range(half):
            P[h0 + half + i, h0 + i] = 1.0
            P[h0 + i, h0 + half + i] = 1.0
    return P


def build_program(stop_after=None, slots=None):
    nc = bass.Bass("TRN2", target_bir_lowering=False)
    S = Sched(nc)
    pe, act, dve, pool, sp = S.pe, S.act, S.dve, S.pool, S.sp
    dbg = {}

    def din(name, shape, dtype=F32):
        return S.dram(name, shape, dtype, kind="ExternalInput")

    xT_seq = din("xT_seq", [128, 8, SEQ])
    xT_own = din("xT_own", [128, 8, NOWN])
    memT = din("memT", [128, 8, MEM])
    qadj = din("qadj", [128, NSLOT])
    iota_in = din("iota", [128, MBW])
    c128s = din("c128s", [128, SEQ]); s128s = din("s128s", [128, SEQ])
    c64s = din("c64s", [128, SEQ]); s64s = din("s64s", [128, SEQ])
    c128o = din("c128o", [128, NOWN]); s128o = din("s128o", [128, NOWN])
    c64o = din("c64o", [128, NOWN]); s64o = din("s64o", [128, NOWN])
    p128_in = din("p128", [128, 128]); p64_in = din("p64", [128, 128])
    ident_in = din("ident", [128, 128])
    poolcorr_in = din("poolcorr", [128, 2, 4, 16])
    haloflag_in = din("haloflag", [128, 2])
    w_keys = din("w_keys", [D, 1152])
    w_own = din("w_own", [D, 2064])
    vecs = din("vecs", [128, 64])
    pool_w = din("pool_w", [4, 128, 128])
    w_out_ab = din("w_out_ab", [D, D])
    conv_w_in = din("conv_w_in", [D, 2 * D])
    conv_dw = din("conv_dw", [128, 8, 31])
    conv_w_out = din("conv_w_out", [D, D])
    cross_wq = din("cross_wq", [2, D, D]); cross_wk = din("cross_wk", [2, D, D])
    cross_wv = din("cross_wv", [2, D, D]); cross_wo = din("cross_wo", [2, D, D])
    ffn_wg = din("ffn_wg", [2, D, DFF]); ffn_wu = din("ffn_wu", [2, D, DFF])
    ffn_wd = din("ffn_wd", [2, DFF, D])
    out_hT = S.dram("out_hT", [128, 8, 32 * 128], F32, kind="ExternalOutput")

    KT = S.dram("KT", [128, 4, SEQ], BF16)
    Vd = S.dram("Vd", [SEQ, 512], BF16)
    QT = S.dram("QT", [128, 4, NOWN], BF16)
    IQT = S.dram("IQT", [128, 8, NOWN], BF16)
    BT = S.dram("BT", [128, 4, NOWN], BF16)
    AT = S.dram("AT", [128, 4, NOWN], BF16)
    HT = S.dram("HT", [128, 8, NOWN], F32)
    HID = S.dram("HID", [128, 22, NOWN], BF16)

    ones_m = S.sbuf("ones_m", [128, 128], BF16)
    ones_h = S.sbuf("ones_h", [128, 128], BF16)
    ones_c = S.sbuf("ones_c", [128, 128], BF16)
    ones_1 = S.sbuf("ones_1", [128, 128], BF16)
    ident = S.sbuf("ident", [128, 128], BF16)
    p128 = S.sbuf("p128", [128, 128], BF16)
    p64 = S.sbuf("p64", [128, 128], BF16)
    cst_f = S.sbuf("cst_f", [128, 3, 128], F32)
    vec = S.sbuf("vec", [128, 64], F32)
    qadj_sb = S.sbuf("qadj_sb", [128, NSLOT], F32)
    eps_t = S.sbuf("eps_t", [128, 1], F32)

    S.op(pool, lambda: nc.gpsimd.memset(ones_m[:], 1.0 / 1024), writes=[ones_m])
    S.op(pool, lambda: nc.gpsimd.memset(ones_h[:], 1.0 / 128), writes=[ones_h])
    S.op(pool, lambda: nc.gpsimd.memset(ones_c[:], 1.0 / 256), writes=[ones_c])
    S.op(pool, lambda: nc.gpsimd.memset(ones_1[:], 1.0), writes=[ones_1])
    S.op(pool, lambda: nc.gpsimd.memset(eps_t[:], EPS), writes=[eps_t])
    S.load(cst_f, cst_f[:, 0, :], ident_in, ident_in[:, :])
    S.load(cst_f, cst_f[:, 1, :], p128_in, p128_in[:, :])
    S.load(cst_f, cst_f[:, 2, :], p64_in, p64_in[:, :])
    S.load(vec, vec[:], vecs, vecs[:, :])
    S.load(qadj_sb, qadj_sb[:], qadj, qadj[:, :])
    S.op(dve, lambda: nc.vector.tensor_copy(out=ident[:], in_=cst_f[:, 0, :]), reads=[cst_f], writes=[ident])
    S.op(dve, lambda: nc.vector.tensor_copy(out=p128[:], in_=cst_f[:, 1, :]), reads=[cst_f], writes=[p128])
    S.op(dve, lambda: nc.vector.tensor_copy(out=p64[:], in_=cst_f[:, 2, :]), reads=[cst_f], writes=[p64])

    V_NMIX0, V_NMIX1, V_NCROSS0, V_NCROSS1, V_NMEM0, V_NMEM1, V_NFFN0, V_NFFN1 = [8 * i for i in range(8)]
    vec2_in = din("vecs2", [128, 64])
    vec2 = S.sbuf("vec2", [128, 64], F32)
    S.load(vec2, vec2[:], vec2_in, vec2_in[:, :])
    V2_AQ, V2_AK = 0, 1
    V2_PSCALE = 2
    V2_CQ0, V2_CQ1, V2_CK0, V2_CK1 = 6, 8, 10, 12
    V2_BIN = 14
    V2_DWB = 30
    V2_LNG = 38
    V2_LNB = 46

    PS = [S.psum(f"ps{i}", [128, 512], F32) for i in range(7)]
    PSB = S.psum("psb", [128, 1024], BF16)

    def load_w_cast(dst, col_dst, w_buf, w_ap, col0, M):
        K = w_ap.shape[0]
        for kc in range(K // 128):
            m0 = 0
            while m0 < M:
                mm_ = min(2048, M - m0)
                S.load(dst, dst[:, kc, col_dst + m0:col_dst + m0 + mm_], w_buf,
                       w_ap[kc * 128:(kc + 1) * 128, col0 + m0:col0 + m0 + mm_], eng=pool)
                m0 += mm_

    def mm(ps_ap, lhsT, rhs, start, stop, reads, ps_buf):
        S.op(pe, lambda: nc.tensor.matmul(ps_ap, lhsT=lhsT, rhs=rhs, start=start, stop=stop),
             reads=reads, partial=[ps_buf] if not start else (), writes=[ps_buf] if start else ())

    def rstd_from_ms(ps_buf, n, out_buf, tmp_buf):
        S.op(act, lambda: nc.scalar.activation(out=tmp_buf[:, :n], in_=ps_buf[:, :n], func=AF.Sqrt,
                                               bias=eps_t[:, 0:1], scale=1.0),
             reads=[ps_buf, eps_t], writes=[tmp_buf])
        S.op(dve, lambda: nc.vector.reciprocal(out=out_buf[:, :n], in_=tmp_buf[:, :n]),
             reads=[tmp_buf], writes=[out_buf])

    def own_blocks(include_halo=True):
        res = []
        for ci in range(2):
            base = ci * SLOTS_PER_CH * 128
            if include_halo:
                res.append((base, 128, ci, True, False))
            for i in range(4):
                res.append((base + 128 + 512 * i, 512, ci, False, i == 0))
        return res

    class Common:
        pass

    def alloc_common():
        cm = Common()
        cm.xt_ring = Ring([S.sbuf(f"xt{i}", [128, 8, 512], F32) for i in range(2)])
        cm.sq_b = S.sbuf("sq", [128, 8, 512], BF16)
        cm.xn_b = S.sbuf("xn", [128, 8, 512], BF16)
        cm.rstd_b = S.sbuf("rstd", [128, 512], F32)
        cm.tmp_b = S.sbuf("tmpf", [128, 512], F32)
        return cm

    def norm_block(cm, src_buf, off, n, gcol):
        xt = cm.xt_ring.next()
        S.load(xt, xt[:, :, :n], src_buf, src_buf[:, :, off:off + n])
        S.op(act, lambda: nc.scalar.activation(out=cm.sq_b[:, :, :n], in_=xt[:, :, :n], func=AF.Square),
             reads=[xt], writes=[cm.sq_b])
        ps = PS[0]
        for c in range(8):
            mm(ps[:, :n], ones_m[:], cm.sq_b[:, c, :n], c == 0, c == 7, [ones_m, cm.sq_b], ps)
        rstd_from_ms(ps, n, cm.rstd_b, cm.tmp_b)
        for c in range(8):
            S.op(dve, lambda c=c: nc.vector.scalar_tensor_tensor(
                out=cm.xn_b[:, c, :n], in0=xt[:, c, :n], scalar=vec[:, gcol + c:gcol + c + 1],
                in1=cm.rstd_b[:, :n], op0=ALU.mult, op1=ALU.mult),
                reads=[xt, vec, cm.rstd_b], partial=[cm.xn_b])
        return xt

    def alloc_tmps():
        return (S.sbuf("sqh", [128, 512], BF16), S.sbuf("kn", [128, 512], BF16), S.sbuf("rk", [128, 512], F32),
                S.sbuf("tk", [128, 512], F32), S.sbuf("t1", [128, 512], F32), S.sbuf("t2", [128, 512], F32))

    def norm_head_rope(ps, n, gcol2, ones_t, pm, c_ap, s_ap, cs_bufs, out_ap, out_buf, do_norm, tmps):
        sqh, kn, rk, tk, t1, t2 = tmps
        ps2, ps3 = PS[5], PS[6]
        if do_norm:
            S.op(act, lambda: nc.scalar.activation(out=sqh[:, :n], in_=ps[:, :n], func=AF.Square),
                 reads=[ps], writes=[sqh])
            mm(ps2[:, :n], ones_t[:], sqh[:, :n], True, True, [ones_t, sqh], ps2)
            rstd_from_ms(ps2, n, rk, tk)
            S.op(dve, lambda: nc.vector.scalar_tensor_tensor(
                out=kn[:, :n], in0=ps[:, :n], scalar=vec2[:, gcol2:gcol2 + 1], in1=rk[:, :n],
                op0=ALU.mult, op1=ALU.mult), reads=[ps, vec2, rk], writes=[kn])
        else:
            S.op(act, lambda: nc.scalar.activation(out=kn[:, :n], in_=ps[:, :n], func=AF.Copy),
                 reads=[ps], writes=[kn])
        mm(ps3[:, :n], pm[:], kn[:, :n], True, True, [pm, kn], ps3)
        S.op(pool, lambda: nc.gpsimd.tensor_tensor(out=t1[:, :n], in0=kn[:, :n], in1=c_ap, op=ALU.mult),
             reads=[kn] + cs_bufs, writes=[t1])
        S.op(dve, lambda: nc.vector.tensor_tensor(out=t2[:, :n], in0=ps3[:, :n], in1=s_ap, op=ALU.mult),
             reads=[ps3] + cs_bufs, writes=[t2])
        if isinstance(out_ap, list):
            for (oap, obuf, p0, p1) in out_ap:
                S.op(dve, lambda oap=oap, p0=p0, p1=p1: nc.vector.tensor_tensor(out=oap, in0=t1[p0:p1, :n], in1=t2[p0:p1, :n], op=ALU.add),
                     reads=[t1, t2], partial=[obuf])
        else:
            S.op(dve, lambda: nc.vector.tensor_tensor(out=out_ap, in0=t1[:, :n], in1=t2[:, :n], op=ALU.add),
                 reads=[t1, t2], partial=[out_buf])

    proj_ps = Ring([PS[1], PS[2], PS[3], PS[4]])

    def proj(ps, w_sb, col0, ncols, rhs_list, n, rbufs):
        kc = len(rhs_list)
        for c in range(kc):
            mm(ps[:, :n], w_sb[:, c, col0:col0 + ncols], rhs_list[c], c == 0, c == kc - 1, [w_sb] + rbufs, ps)

    S.push()
    ikT0 = S.sbuf("ikT0", [128, SEQ], BF16)
    ikT1 = S.sbuf("ikT1", [128, SEQ], BF16)
    iw_sb = S.sbuf("iw_sb", [128, NSLOT, 16], F32)
    S.op(pool, lambda: nc.gpsimd.memset(ikT0[:], 0.0), writes=[ikT0])
    S.op(pool, lambda: nc.gpsimd.memset(ikT1[:], 0.0), writes=[ikT1])

    S.push()
    cm = alloc_common()
    tmps = alloc_tmps()
    cs_ring = Ring([S.sbuf(f"cs{i}", [128, 4, 512], F32) for i in range(2)])
    wk_sb = S.sbuf("wk_sb", [128, 8, 1152], BF16)
    load_w_cast(wk_sb, 0, w_keys, w_keys.ap_fn(), 0, 1152)
    kout_ring = Ring([S.sbuf(f"kout{i}", [128, 4, 512], BF16) for i in range(2)])
    vout_ring = Ring([S.sbuf(f"vout{i}", [128, 4, 512], BF16) for i in range(2)])
    for blk in range(SEQ // 512):
        off = blk * 512
        n = 512
        norm_block(cm, xT_seq, off, n, V_NMIX0)
        xn_b = cm.xn_b
        cs = cs_ring.next()
        S.load(cs, cs[:, 0, :], c128s, c128s[:, off:off + n])
        S.load(cs, cs[:, 1, :], s128s, s128s[:, off:off + n])
        S.load(cs, cs[:, 2, :], c64s, c64s[:, off:off + n])
        S.load(cs, cs[:, 3, :], s64s, s64s[:, off:off + n])
        kout = kout_ring.next()
        for kc in range(4):
            ps = proj_ps.next()
            proj(ps, wk_sb, kc * 128, 128, [xn_b[:, c, :n] for c in range(8)], n, [xn_b])
            norm_head_rope(ps, n, V2_AK, ones_h, p128, cs[:, 0, :n], cs[:, 1, :n], [cs],
                           kout[:, kc, :n], kout, True, tmps)
        S.store(KT, KT[:, :, off:off + n], kout, kout[:, :, :n])
        ps = proj_ps.next()
        proj(ps, wk_sb, 512, 128, [xn_b[:, c, :n] for c in range(8)], n, [xn_b])
        norm_head_rope(ps, n, 0, None, p64, cs[:, 2, :n], cs[:, 3, :n], [cs],
                       [(ikT0[0:64, off:off + n], ikT0, 0, 64), (ikT1[64:128, off:off + n], ikT1, 64, 128)],
                       None, False, tmps)
        vout = vout_ring.next()
        for tt in range(4):
            ps = proj_ps.next()
            for c in range(8):
                mm(ps[:, :], xn_b[:, c, tt * 128:(tt + 1) * 128], wk_sb[:, c, 640:1152], c == 0, c == 7,
                   [wk_sb, xn_b], ps)
            S.op(act, lambda tt=tt, ps=ps: nc.scalar.activation(out=vout[:, tt, :], in_=ps[:, :], func=AF.Copy),
                 reads=[ps], partial=[vout])
        S.store(Vd, Vd.ap_fn()[off:off + n, :].rearrange("(t p) d -> p t d", p=128), vout, vout[:, :, :])
    S.pop()

    S.push()
    cm = alloc_common()
    tmps = alloc_tmps()
    cs_ring = Ring([S.sbuf(f"cs{i}", [128, 4, 512], F32) for i in range(1)])
    wo_sb = S.sbuf("wo_sb", [128, 8, 2064], BF16)
    load_w_cast(wo_sb, 0, w_own, w_own.ap_fn(), 0, 2064)
    pw_sb = S.sbuf("pw_sb", [128, 4, 128], BF16)
    for g in range(4):
        S.load(pw_sb, pw_sb[:, g, :], pool_w, pool_w[g, :, :], eng=pool)
    pcorr = S.sbuf("pcorr", [128, 2, 4, 16], F32)
    S.load(pcorr, pcorr[:], poolcorr_in, poolcorr_in[:, :, :, :])
    qout_ring = Ring([S.sbuf(f"qout{i}", [128, 4, 512], BF16) for i in range(2)])
    iqout_ring = Ring([S.sbuf(f"iqout{i}", [128, 8, 512], BF16) for i in range(1)])
    bout_ring = Ring([S.sbuf(f"bout{i}", [128, 4, 512], BF16) for i in range(2)])
    Ug = [S.sbuf(f"U{g}", [128, 528], F32) for g in range(4)]
    sA = [S.sbuf(f"sA{g}", [128, 528], F32) for g in range(4)]
    sB = [S.sbuf(f"sB{g}", [128, 528], F32) for g in range(4)]
    pooled = [S.sbuf(f"pooled{g}", [128, 512], BF16) for g in range(4)]
    for (off, n, ci, is_halo, is_first) in own_blocks():
        norm_block(cm, xT_own, off, n, V_NMIX0)
        xn_b = cm.xn_b
        xl = [xn_b[:, c, :n] for c in range(8)]
        cs = cs_ring.next()
        S.load(cs, cs[:, 0, :n], c128o, c128o[:, off:off + n])
        S.load(cs, cs[:, 1, :n], s128o, s128o[:, off:off + n])
        S.load(cs, cs[:, 2, :n], c64o, c64o[:, off:off + n])
        S.load(cs, cs[:, 3, :n], s64o, s64o[:, off:off + n])
        qout = qout_ring.next()
        for kc in range(4):
            ps = proj_ps.next()
            proj(ps, wo_sb, kc * 128, 128, xl, n, [xn_b])
            norm_head_rope(ps, n, V2_AQ, ones_h, p128, cs[:, 0, :n], cs[:, 1, :n], [cs],
                           qout[:, kc, :n], qout, True, tmps)
        S.store(QT, QT[:, :, off:off + n], qout, qout[:, :, :n])
        iqout = iqout_ring.next()
        for kc in range(8):
            ps = proj_ps.next()
            proj(ps, wo_sb, 512 + kc * 128, 128, xl, n, [xn_b])
            norm_head_rope(ps, n, 0, None, p64, cs[:, 2, :n], cs[:, 3, :n], [cs],
                           iqout[:, kc, :n], iqout, False, tmps)
        S.store(IQT, IQT[:, :, off:off + n], iqout, iqout[:, :, :n])
        for tt in range(n // 128):
            slot = off // 128 + tt
            ps = proj_ps.next()
            for c in range(8):
                mm(ps[:, :16], xn_b[:, c, tt * 128:(tt + 1) * 128], wo_sb[:, c, 2048:2064], c == 0, c == 7,
                   [wo_sb, xn_b], ps)
            S.op(act, lambda ps=ps, slot=slot: nc.scalar.activation(out=iw_sb[:, slot, :], in_=ps[:, :16],
                                                                    func=AF.Copy, scale=1.0 / 32.0),
                 reads=[ps], partial=[iw_sb])
        L = 16 + n
        bout = bout_ring.next()
        for g in range(4):
            U = Ug[g]
            if is_halo:
                S.op(pool, lambda U=U: nc.gpsimd.memset(U[:, 0:16], 0.0), partial=[U])
            ps = proj_ps.next()
            proj(ps, wo_sb, 1536 + g * 128, 128, xl, n, [xn_b])
            S.op(act, lambda ps=ps, U=U: nc.scalar.activation(out=U[:, 16:L], in_=ps[:, :n], func=AF.Copy),
                 reads=[ps], partial=[U])
            a_, b_ = sA[g], sB[g]
            S.op(pool, lambda U=U, a_=a_: nc.gpsimd.tensor_tensor(out=a_[:, 1:L], in0=U[:, 1:L], in1=U[:, 0:L - 1], op=ALU.add),
                 reads=[U], writes=[a_])
            fin = a_
            if g >= 1:
                S.op(pool, lambda a_=a_, b_=b_: nc.gpsimd.tensor_tensor(out=b_[:, 3:L], in0=a_[:, 3:L], in1=a_[:, 1:L - 2], op=ALU.add),
                     reads=[a_], writes=[b_])
                fin = b_
            if g >= 2:
                S.op(pool, lambda a_=a_, b_=b_: nc.gpsimd.tensor_tensor(out=a_[:, 7:L], in0=b_[:, 7:L], in1=b_[:, 3:L - 4], op=ALU.add),
                     reads=[b_], writes=[a_])
                fin = a_
            if g >= 3:
                S.op(pool, lambda a_=a_, b_=b_: nc.gpsimd.tensor_tensor(out=b_[:, 15:L], in0=a_[:, 15:L], in1=a_[:, 7:L - 8], op=ALU.add),
                     reads=[a_], writes=[b_])
                fin = b_
            if is_first:
                S.op(dve, lambda fin=fin, g=g: nc.vector.tensor_tensor(out=fin[:, 16:32], in0=fin[:, 16:32],
                                                                       in1=pcorr[:, ci, g, :], op=ALU.mult),
                     reads=[pcorr, fin], partial=[fin])
            w_ = float(2 ** (g + 1))
            S.op(dve, lambda fin=fin, U=U, g=g: nc.vector.scalar_tensor_tensor(
                out=pooled[g][:, :n], in0=fin[:, 16:L], scalar=1.0 / w_, in1=U[:, 16:L],
                op0=ALU.mult, op1=ALU.subtract), reads=[fin, U], writes=[pooled[g]])
            S.op(pool, lambda U=U: nc.gpsimd.tensor_copy(out=U[:, 0:16], in_=U[:, n:n + 16]), reads=[U], partial=[U])
            ps = proj_ps.next()
            mm(ps[:, :n], pw_sb[:, g, :], pooled[g][:, :n], True, True, [pw_sb, pooled[g]], ps)
            S.op(act, lambda ps=ps, g=g: nc.scalar.activation(out=bout[:, g, :n], in_=ps[:, :n], func=AF.Copy,
                                                               scale=vec2[:, V2_PSCALE + g:V2_PSCALE + g + 1]),
                 reads=[ps, vec2], partial=[bout])
        S.store(BT, BT[:, :, off:off + n], bout, bout[:, :, :n])
    S.pop()

    S.push()
    iota_sb = S.sbuf("iota_sb", [128, MBW], F32)
    S.load(iota_sb, iota_sb[:], iota_in, iota_in[:, :])
    scores2 = [S.sbuf(f"scores{i}", [128, SEQ], F32) for i in range(2)]
    mbias = S.sbuf("mbias", [128, SEQ], BF16)
    junk = S.sbuf("junk", [128, SEQ // 2], BF16)
    mb2 = [S.sbuf(f"mb{i}", [128, MBW], BF16) for i in range(2)]
    tmpu = S.sbuf("tmpu", [128, MBW], F32)
    qt_ring = Ring([S.sbuf(f"qt{i}", [128, 4, 128], BF16) for i in range(2)])
    iqt_ring = Ring([S.sbuf(f"iqt{i}", [128, 8, 128], BF16) for i in range(2)])
    kb_ring = Ring([S.sbuf(f"kblk{i}", [128, 4, 512], BF16) for i in range(2)])
    vb_ring = Ring([S.sbuf(f"vblk{i}", [128, 4, 512], BF16) for i in range(2)])
    r_ring = Ring([S.sbuf(f"R{i}", [128, 512], BF16) for i in range(4)])
    p_ring = Ring([S.sbuf(f"P{i}", [128, 512], BF16) for i in range(3)])
    pt_ring = Ring([S.sbuf(f"PT{i}", [128, 512], BF16) for i in range(3)])
    diag2 = [S.sbuf(f"diag{i}", [128, 16, 128], BF16) for i in range(2)]
    sm = S.sbuf("sm", [128, 16], F32)
    hs = S.sbuf("hs", [128, 32], F32)
    pow2 = S.sbuf("pow2", [128, 32], F32)
    cntb = S.sbuf("cntb", [128, 2], F32)
    midr = Ring([S.sbuf(f"mid{i}", [128, 1], F32) for i in range(2)])
    eb = S.sbuf("eb", [128, 1], F32)
    rs = S.sbuf("rs", [128, 4, 16], F32)
    rsum = S.sbuf("rsum", [128, 4], F32)
    rrec = S.sbuf("rrec", [128, 4], F32)
    negone = S.sbuf("negone", [128, 4], F32)
    rjunk = S.sbuf("rjunk", [128, 16], F32)
    a_tok = S.sbuf("a_tok", [128, 512], BF16)
    aT_ring = Ring([S.sbuf(f"aT{i}", [128, 4, 128], BF16) for i in range(2)])
    for i in range(32):
        S.op(pool, lambda i=i: nc.gpsimd.memset(pow2[:, i:i + 1], 2.0 ** (-i)), partial=[pow2])
    S.op(pool, lambda: nc.gpsimd.memset(negone[:], -1.0), writes=[negone])
    s_ring = Ring([PS[0], PS[1]])
    sc_ring = Ring([PS[2]])
    l_ring = Ring([PS[4], PS[5]])
    Obank = PS[6]
    ps3b = Buf("ps3b", lambda: PS[3].ap_fn()[:, :].bitcast(BF16))
    PSBv = [S.view("psb0", PSB, (slice(None), slice(0, 512))), S.view("ps3b0", ps3b, (slice(None), slice(0, 512)))]
    ptp_ring = Ring(PSBv)
    SM_MX, SM_MN1, SM_MN2, SM_MN, SM_H, SM_TAU = range(6)
    MASKV = -30000.0

    def slot_geom(j):
        nkt = slot_nkt(j)
        nkb = (nkt + 3) // 4
        ub = slot_first_uncertain(j) // 4
        return nkb, nkb * 512, ub, (nkb - ub) * 512

    def stage_A(j):
        nkb, N, ub, W = slot_geom(j)
        assert W <= MBW
        scores = scores2[j % 2]; mb = mb2[j % 2]; diag = diag2[j % 2]
        iqt = iqt_ring.next()
        S.load(iqt, iqt[:], IQT, IQT[:, :, j * 128:(j + 1) * 128])
        for h in range(16):
            S.op(pool, lambda h=h: nc.gpsimd.tensor_scalar(out=diag[:, h, :], in0=ident[:], scalar1=iw_sb[:, j, h:h + 1],
                                                          scalar2=None, op0=ALU.mult),
                 reads=[ident, iw_sb], partial=[diag])
        S.op(pool, lambda: nc.gpsimd.tensor_scalar(out=mb[:, :W], in0=iota_sb[:, :W], scalar1=qadj_sb[:, j:j + 1],
                                                  scalar2=MASKV, op0=ALU.is_gt, op1=ALU.mult),
             reads=[iota_sb, qadj_sb], writes=[mb])
        yield
        items = [(kb, h) for kb in range(nkb) for h in range(16)]
        pend = None
        sc = None
        for (kb, h) in items + [(None, None)]:
            cur = None
            if kb is not None:
                sp_ = s_ring.next()
                ikp = ikT0 if h % 2 == 0 else ikT1
                mm(sp_[:, :], iqt[:, h // 2, :], ikp[:, kb * 512:(kb + 1) * 512], True, True,
                   [iqt, ikp], sp_)
                R = r_ring.next()
                S.op(act, lambda sp_=sp_, R=R: nc.scalar.activation(out=R[:], in_=sp_[:, :], func=AF.Relu),
                     reads=[sp_], writes=[R])
                cur = (kb, h, R)
            if pend is not None:
                pkb, ph, pR = pend
                if ph == 0:
                    sc = sc_ring.next()
                mm(sc[:, :], diag[:, ph, :], pR[:], ph == 0, (ph == 15 and pkb < ub), [diag, pR], sc)
                if ph == 15:
                    if pkb >= ub:
                        mm(sc[:, :], ident[:], mb[:, (pkb - ub) * 512:(pkb - ub + 1) * 512], False, True, [ident, mb], sc)
                    S.op(act, lambda sc=sc, pkb=pkb: nc.scalar.activation(out=scores[:, pkb * 512:(pkb + 1) * 512], in_=sc[:, :],
                                                                          func=AF.Copy), reads=[sc], partial=[scores])
            pend = cur
            if not (SKEW or SKEW_A) and pend is not None:
                pkb, ph, pR = pend
                if ph == 0:
                    sc = sc_ring.next()
                mm(sc[:, :], diag[:, ph, :], pR[:], ph == 0, (ph == 15 and pkb < ub), [diag, pR], sc)
                if ph == 15:
                    if pkb >= ub:
                        mm(sc[:, :], ident[:], mb[:, (pkb - ub) * 512:(pkb - ub + 1) * 512], False, True, [ident, mb], sc)
                    S.op(act, lambda sc=sc, pkb=pkb: nc.scalar.activation(out=scores[:, pkb * 512:(pkb + 1) * 512], in_=sc[:, :],
                                                                          func=AF.Copy), reads=[sc], partial=[scores])
                pend = None
            yield

    def stage_B(j):
        nkb, N, ub, W = slot_geom(j)
        scores = scores2[j % 2]; mb = mb2[j % 2]
        S.op(dve, lambda: nc.vector.tensor_reduce(out=sm[:, SM_MX:SM_MX + 1], in_=scores[:, :N], axis=AX.X, op=ALU.max),
             reads=[scores], partial=[sm])
        S.op(dve, lambda: nc.vector.scalar_tensor_tensor(out=tmpu[:, :W], in0=mb[:, :W], scalar=-2.0,
                                                         in1=scores[:, ub * 512:N], op0=ALU.mult, op1=ALU.add),
             reads=[mb, scores], writes=[tmpu])
        S.op(dve, lambda: nc.vector.tensor_reduce(out=sm[:, SM_MN2:SM_MN2 + 1], in_=tmpu[:, :W], axis=AX.X, op=ALU.min),
             reads=[tmpu], partial=[sm])
        if ub > 0:
            S.op(dve, lambda: nc.vector.tensor_reduce(out=sm[:, SM_MN1:SM_MN1 + 1], in_=scores[:, :ub * 512], axis=AX.X, op=ALU.min),
                 reads=[scores], partial=[sm])
            S.op(dve, lambda: nc.vector.tensor_tensor(out=sm[:, SM_MN:SM_MN + 1], in0=sm[:, SM_MN1:SM_MN1 + 1],
                                                      in1=sm[:, SM_MN2:SM_MN2 + 1], op=ALU.min), reads=[sm], partial=[sm])
        else:
            S.op(dve, lambda: nc.vector.tensor_copy(out=sm[:, SM_MN:SM_MN + 1], in_=sm[:, SM_MN2:SM_MN2 + 1]),
                 reads=[sm], partial=[sm])
        S.op(dve, lambda: nc.vector.tensor_scalar(out=sm[:, SM_H:SM_H + 1], in0=sm[:, SM_MX:SM_MX + 1],
                                                  scalar1=sm[:, SM_MN:SM_MN + 1], scalar2=0.50005, op0=ALU.subtract, op1=ALU.mult),
             reads=[sm], partial=[sm])
        mid = midr.next()
        S.op(dve, lambda mid=mid: nc.vector.tensor_scalar(out=mid[:], in0=sm[:, SM_MX:SM_MX + 1],
                                                          scalar1=sm[:, SM_MN:SM_MN + 1], scalar2=0.5, op0=ALU.add, op1=ALU.mult),
             reads=[sm], writes=[mid])
        S.op(dve, lambda: nc.vector.tensor_scalar(out=hs[:], in0=pow2[:], scalar1=sm[:, SM_H:SM_H + 1], scalar2=None, op0=ALU.mult),
             reads=[pow2, sm], writes=[hs])
        yield
        N1 = min(N, SEQ // 2)
        for it in range(NITER):
            S.op(dve, lambda mid=mid: nc.vector.tensor_scalar(out=junk[:, :N1], in0=scores[:, :N1], scalar1=mid[:, 0:1], scalar2=0.0,
                                                              op0=ALU.is_ge, op1=ALU.add, accum_out=cntb[:, 0:1]),
                 reads=[scores, mid], writes=[junk, cntb])
            if N > N1:
                S.op(dve, lambda mid=mid: nc.vector.tensor_scalar(out=junk[:, :N - N1], in0=scores[:, N1:N], scalar1=mid[:, 0:1],
                                                                  scalar2=cntb[:, 0:1], op0=ALU.is_ge, op1=ALU.add, accum_out=cntb[:, 1:2]),
                     reads=[scores, mid, cntb], writes=[junk], partial=[cntb])
                ccol = 1
            else:
                ccol = 0
            S.op(dve, lambda ccol=ccol: nc.vector.tensor_scalar(out=eb[:], in0=cntb[:, ccol:ccol + 1], scalar1=255.5, scalar2=0.5,
                                                                op0=ALU.is_ge, op1=ALU.subtract), reads=[cntb], writes=[eb])
            nmid = midr.next()
            S.op(dve, lambda mid=mid, nmid=nmid, it=it: nc.vector.scalar_tensor_tensor(
                out=nmid[:], in0=eb[:], scalar=hs[:, it:it + 1], in1=mid[:], op0=ALU.mult, op1=ALU.add),
                reads=[eb, hs, mid], writes=[nmid])
            mid = nmid
            yield
        S.op(dve, lambda mid=mid: nc.vector.scalar_tensor_tensor(
            out=sm[:, SM_TAU:SM_TAU + 1], in0=sm[:, SM_H:SM_H + 1], scalar=-(2.0 ** (-NITER)), in1=mid[:],
            op0=ALU.mult, op1=ALU.add), reads=[sm, mid], partial=[sm])
        S.op(dve, lambda: nc.vector.tensor_scalar(out=mbias[:, :N], in0=scores[:, :N], scalar1=sm[:, SM_TAU:SM_TAU + 1],
                                                  scalar2=MASKV, op0=ALU.is_lt, op1=ALU.mult),
             reads=[scores, sm], writes=[mbias])
        yield

    def stage_C(j):
        nkb, N, ub, W = slot_geom(j)
        qt = qt_ring.next()
        S.load(qt, qt[:], QT, QT[:, :, j * 128:(j + 1) * 128])
        S.op(pool, lambda: nc.gpsimd.memset(rs[:], 0.0), writes=[rs])
        yield
        items = [(kb, h) for kb in range(nkb) for h in range(4)]
        nI = len(items)
        st = {}
        blk = {}
        first_o = [True]

        def qk(i):
            kb, h = items[i]
            if h == 0:
                kblk = kb_ring.next(); vblk = vb_ring.next()
                S.load(kblk, kblk[:], KT, KT[:, :, kb * 512:(kb + 1) * 512])
                S.load(vblk, vblk[:], Vd, Vd.ap_fn()[kb * 512:(kb + 1) * 512, :].rearrange("(t p) d -> p t d", p=128))
                blk[kb] = (kblk, vblk)
            kblk, vblk = blk[kb]
            Lp = l_ring.next()
            mm(Lp[:, :], qt[:, h, :], kblk[:, h, :], True, False, [qt, kblk], Lp)
            mm(Lp[:, :], ident[:], mbias[:, kb * 512:(kb + 1) * 512], False, True, [ident, mbias], Lp)
            Pb = p_ring.next()
            S.op(act, lambda: nc.scalar.activation(out=Pb[:], in_=Lp[:, :], func=AF.Exp, scale=128.0 ** -0.5,
                                                   accum_out=rs[:, h, kb:kb + 1]),
                 reads=[Lp], writes=[Pb], partial=[rs])
            st[i] = [Pb, None]

        def tr(i):
            Pb = st[i][0]
            ptp = ptp_ring.next()
            for tt in range(4):
                S.op(pe, lambda tt=tt: nc.tensor.transpose(out=ptp[:, tt * 128:(tt + 1) * 128],
                                                           in_=Pb[:, tt * 128:(tt + 1) * 128], identity=ident[:]),
                     reads=[Pb, ident], writes=[ptp] if tt == 0 else (), partial=[ptp] if tt else ())
            PTb = pt_ring.next()
            S.op(act, lambda: nc.scalar.activation(out=PTb[:], in_=ptp[:, :], func=AF.Copy), reads=[ptp], writes=[PTb])
            st[i][1] = PTb

        def pv(i):
            kb, h = items[i]
            PTb = st[i][1]
            kblk, vblk = blk[kb]
            for tt in range(4):
                fo = first_o[0]
                first_o[0] = False
                S.op(pe, lambda tt=tt, fo=fo: nc.tensor.matmul(
                    Obank[:, h * 128:(h + 1) * 128], lhsT=PTb[:, tt * 128:(tt + 1) * 128],
                    rhs=vblk[:, tt, h * 128:(h + 1) * 128], start=fo, stop=(i == nI - 1 and tt == 3),
                    skip_group_check=True),
                    reads=[PTb, vblk], writes=[Obank] if fo else (), partial=() if fo else [Obank])
            del st[i]

        if SKEW:
            for g in range(nI + 2):
                if g < nI:
                    qk(g)
                if 1 <= g <= nI:
                    tr(g - 1)
                if g >= 2:
                    pv(g - 2)
                yield
        else:
            for g in range(nI):
                qk(g)
                tr(g)
                pv(g)
                yield
        for h in range(4):
            S.op(act, lambda h=h: nc.scalar.activation(out=rjunk[:, :], in_=rs[:, h, :], func=AF.Copy, accum_out=rsum[:, h:h + 1]),
                 reads=[rs], writes=[rjunk], partial=[rsum])
        S.op(pool, lambda: nc.gpsimd.tensor_tensor(out=rrec[:], in0=rsum[:], in1=negone[:], op=ALU.pow),
             reads=[rsum, negone], writes=[rrec])
        for h in range(4):
            S.op(act, lambda h=h: nc.scalar.activation(out=a_tok[:, h * 128:(h + 1) * 128], in_=Obank[:, h * 128:(h + 1) * 128],
                                                       func=AF.Copy, scale=rrec[:, h:h + 1]),
                 reads=[Obank, rrec], partial=[a_tok])
        ptp = ptp_ring.next()
        for h in range(4):
            S.op(pe, lambda h=h, ptp=ptp: nc.tensor.transpose(out=ptp[:, h * 128:(h + 1) * 128],
                                                              in_=a_tok[:, h * 128:(h + 1) * 128], identity=ident[:]),
                 reads=[a_tok, ident], writes=[ptp] if h == 0 else (), partial=[ptp] if h else ())
        aT = aT_ring.next()
        S.op(act, lambda ptp=ptp, aT=aT: nc.scalar.activation(out=aT[:].rearrange("p a b -> p (a b)"), in_=ptp[:, :], func=AF.Copy),
             reads=[ptp], writes=[aT])
        S.store(AT, AT[:, :, j * 128:(j + 1) * 128], aT, aT[:])
        yield

    def run_all(g):
        for _ in g:
            pass

    slot_list = list(slots) if slots is not None else list(range(NSLOT))
    ns = len(slot_list)
    run_all(stage_A(slot_list[0]))
    for si in range(ns + 1):
        gC = stage_C(slot_list[si - 1]) if si - 1 >= 0 else None
        gA = stage_A(slot_list[si + 1]) if si + 1 < ns else None
        gB = stage_B(slot_list[si]) if si < ns else None
        nC = slot_geom(slot_list[si - 1])[0] * 4 + 4 if gC is not None else 0
        nA = slot_geom(slot_list[si + 1])[0] * 16 + 2 if gA is not None else 0
        nB = NITER + 2 if gB is not None else 0
        live = {"A": gA, "B": gB, "C": gC}
        tot = {"A": nA, "B": nB, "C": nC}
        done = {"A": 0, "B": 0, "C": 0}
        wgt = {"A": 1.0, "B": 1.0, "C": 0.6}
        while any(g is not None for g in live.values()):
            best = None
            for k, g in live.items():
                if g is None:
                    continue
                frac = wgt[k] * done[k] / max(1, tot[k])
                if best is None or frac < best[0]:
                    best = (frac, k)
            k = best[1]
            if SEQ_PE and k == "A" and live["C"] is not None:
                k = "C"
            if k == "B" and done["B"] >= NITER + 1 and live["C"] is not None:
                k = "C"
            try:
                next(live[k])
                done[k] += 1
            except StopIteration:
                live[k] = None
    S.pop()
    S.pop()

    if stop_after == "3":
        dbg_a = S.dram("dbg_a", [128, 4, NOWN], BF16, kind="ExternalOutput")
        dbg_b = S.dram("dbg_b", [128, 4, NOWN], BF16, kind="ExternalOutput")
        S.push()
        big = S.sbuf("dbgbig", [128, 4, NOWN], BF16)
        S.load(big, big[:], AT, AT[:, :, :])
        S.store(dbg_a, dbg_a[:, :, :], big, big[:])
        S.load(big, big[:], BT, BT[:, :, :])
        S.store(dbg_b, dbg_b[:, :, :], big, big[:])
        S.finish([dbg_a, dbg_b])
        return nc

    GLU = S.dram("GLU", [128, 8, NOWN], BF16)
    hout_ring_holder = {}

    def residual_out(ps, n, c, res_buf, res_ap, hout):
        S.op(dve, lambda: nc.vector.tensor_tensor(out=hout[:, c, :n], in0=ps[:, :n], in1=res_ap, op=ALU.add),
             reads=[ps, res_buf], partial=[hout])

    S.push()
    wout_sb = S.sbuf("wout_sb", [128, 8, D], BF16)
    load_w_cast(wout_sb, 0, w_out_ab, w_out_ab.ap_fn(), 0, D)
    xt_ring = Ring([S.sbuf(f"xt4_{i}", [128, 8, 512], F32) for i in range(2)])
    ab_ring = Ring([S.sbuf(f"ab{i}", [128, 8, 512], BF16) for i in range(2)])
    hout_ring = Ring([S.sbuf(f"hout4_{i}", [128, 8, 512], F32) for i in range(2)])
    for (off, n, ci, is_halo, is_first) in own_blocks():
        xt = xt_ring.next(); ab = ab_ring.next(); hout = hout_ring.next()
        S.load(xt, xt[:, :, :n], xT_own, xT_own[:, :, off:off + n])
        S.load(ab, ab[:, 0:4, :n], AT, AT[:, :, off:off + n])
        S.load(ab, ab[:, 4:8, :n], BT, BT[:, :, off:off + n])
        for o in range(8):
            ps = proj_ps.next()
            proj(ps, wout_sb, o * 128, 128, [ab[:, c, :n] for c in range(8)], n, [ab])
            residual_out(ps, n, o, xt, xt[:, o, :n], hout)
        S.store(HT, HT[:, :, off:off + n], hout, hout[:, :, :n])
    S.pop()

    def cross_phase(l, blocks):
        S.push()
        gq, gk = (V2_CQ0, V2_CK0) if l == 0 else (V2_CQ1, V2_CK1)
        ncross = V_NCROSS0 if l == 0 else V_NCROSS1
        nmem = V_NMEM0 if l == 0 else V_NMEM1
        cm = alloc_common()
        kcT = S.sbuf("kcT", [128, 8, MEM], BF16)
        vc = S.sbuf("vc", [128, 2, D], BF16)
        sqc = S.sbuf("sqc", [128, 2, 512], BF16)
        rq = S.sbuf("rq", [128, 512], F32)
        tq = S.sbuf("tq", [128, 512], F32)
        S.push()
        wkv = S.sbuf("wkv", [128, 8, 2 * D], BF16)
        load_w_cast(wkv, 0, cross_wk, cross_wk.ap_fn()[l], 0, D)
        load_w_cast(wkv, D, cross_wv, cross_wv.ap_fn()[l], 0, D)
        norm_block(cm, memT, 0, MEM, nmem)
        xn_b = cm.xn_b
        n = MEM
        for hh in range(4):
            pss = [proj_ps.next(), proj_ps.next()]
            for dc in range(2):
                proj(pss[dc], wkv, (hh * 2 + dc) * 128, 128, [xn_b[:, c, :n] for c in range(8)], n, [xn_b])
                S.op(act, lambda dc=dc, pss=pss: nc.scalar.activation(out=sqc[:, dc, :n], in_=pss[dc][:, :n], func=AF.Square),
                     reads=[pss[dc]], partial=[sqc])
            ps2 = PS[5]
            for dc in range(2):
                mm(ps2[:, :n], ones_c[:], sqc[:, dc, :n], dc == 0, dc == 1, [ones_c, sqc], ps2)
            rstd_from_ms(ps2, n, rq, tq)
            for dc in range(2):
                S.op(dve, lambda dc=dc, pss=pss, hh=hh: nc.vector.scalar_tensor_tensor(
                    out=kcT[:, hh * 2 + dc, :n], in0=pss[dc][:, :n], scalar=vec2[:, gk + dc:gk + dc + 1], in1=rq[:, :n],
                    op0=ALU.mult, op1=ALU.mult), reads=[pss[dc], vec2, rq], partial=[kcT])
        for mt in range(2):
            for hf in range(2):
                ps = proj_ps.next()
                for c in range(8):
                    mm(ps[:, :], xn_b[:, c, mt * 128:(mt + 1) * 128], wkv[:, c, D + hf * 512:D + (hf + 1) * 512], c == 0, c == 7,
                       [wkv, xn_b], ps)
                S.op(act, lambda ps=ps, mt=mt, hf=hf: nc.scalar.activation(out=vc[:, mt, hf * 512:(hf + 1) * 512], in_=ps[:, :], func=AF.Copy),
                     reads=[ps], partial=[vc])
        S.pop()
        wqo = S.sbuf("wqo", [128, 8, 2 * D], BF16)
        load_w_cast(wqo, 0, cross_wq, cross_wq.ap_fn()[l], 0, D)
        load_w_cast(wqo, D, cross_wo, cross_wo.ap_fn()[l], 0, D)
        qn = S.sbuf("qn", [128, 8, 512], BF16)
        on = S.sbuf("on", [128, 8, 512], BF16)
        pc_ring = Ring([S.sbuf(f"pc{i}", [128, 2, 512], BF16) for i in range(2)])
        rden = S.sbuf("rden", [128, 512], F32)
        hout_ring = Ring([S.sbuf(f"houtc_{i}", [128, 8, 512], F32) for i in range(2)])
        qn_h = [S.sbuf(f"qnh{h}", [128, 2, 512], BF16) for h in range(4)]
        on_h = [S.sbuf(f"onh{h}", [128, 2, 512], BF16) for h in range(4)]
        sqc_r = Ring([sqc, S.sbuf("sqc2", [128, 2, 512], BF16)])
        rq_r = Ring([rq, S.sbuf("rq2", [128, 512], F32)])
        tq_r = Ring([tq, S.sbuf("tq2", [128, 512], F32)])
        rden_r = Ring([rden, S.sbuf("rden2", [128, 512], F32)])
        for (off, n, ci, is_halo, is_first) in blocks:
            xt = norm_block(cm, HT, off, n, ncross)
            xn_b = cm.xn_b
            hout = hout_ring.next()

            def front(hh):
                pss = [proj_ps.next(), proj_ps.next()]
                sq_ = sqc_r.next(); rq_ = rq_r.next(); tq_ = tq_r.next()
                for dc in range(2):
                    proj(pss[dc], wqo, (hh * 2 + dc) * 128, 128, [xn_b[:, c, :n] for c in range(8)], n, [xn_b])
                    S.op(act, lambda dc=dc: nc.scalar.activation(out=sq_[:, dc, :n], in_=pss[dc][:, :n], func=AF.Square),
                         reads=[pss[dc]], partial=[sq_])
                ps2 = PS[5]
                for dc in range(2):
                    mm(ps2[:, :n], ones_c[:], sq_[:, dc, :n], dc == 0, dc == 1, [ones_c, sq_], ps2)
                rstd_from_ms(ps2, n, rq_, tq_)
                for dc in range(2):
                    S.op(dve, lambda dc=dc: nc.vector.scalar_tensor_tensor(
                        out=qn_h[hh][:, dc, :n], in0=pss[dc][:, :n], scalar=vec2[:, gq + dc:gq + dc + 1], in1=rq_[:, :n],
                        op0=ALU.mult, op1=ALU.mult), reads=[pss[dc], vec2, rq_], partial=[qn_h[hh]])

            def back(hh):
                pc = pc_ring.next()
                rd_ = rden_r.next()
                for mt in range(2):
                    ps = proj_ps.next()
                    for dc in range(2):
                        mm(ps[:, :n], kcT[:, hh * 2 + dc, mt * 128:(mt + 1) * 128], qn_h[hh][:, dc, :n], dc == 0, dc == 1,
                           [kcT, qn_h[hh]], ps)
                    S.op(act, lambda ps=ps, mt=mt: nc.scalar.activation(out=pc[:, mt, :n], in_=ps[:, :n], func=AF.Exp,
                                                                        scale=1.0 / 16.0), reads=[ps], partial=[pc])
                psd = PS[6]
                for mt in range(2):
                    mm(psd[:, :n], ones_1[:], pc[:, mt, :n], mt == 0, mt == 1, [ones_1, pc], psd)
                S.op(dve, lambda: nc.vector.reciprocal(out=rd_[:, :n], in_=psd[:, :n]), reads=[psd], writes=[rd_])
                for dc in range(2):
                    ps = proj_ps.next()
                    for mt in range(2):
                        mm(ps[:, :n], vc[:, mt, hh * 256 + dc * 128:hh * 256 + (dc + 1) * 128], pc[:, mt, :n], mt == 0, mt == 1,
                           [vc, pc], ps)
                    S.op(dve, lambda ps=ps, dc=dc: nc.vector.tensor_tensor(out=on_h[hh][:, dc, :n], in0=ps[:, :n],
                                                                          in1=rd_[:, :n], op=ALU.mult),
                         reads=[ps, rd_], partial=[on_h[hh]])

            front(0)
            for hh in range(4):
                if hh + 1 < 4:
                    front(hh + 1)
                back(hh)
            for o in range(8):
                ps = proj_ps.next()
                proj(ps, wqo, D + o * 128, 128, [on_h[c // 2][:, c % 2, :n] for c in range(8)], n, on_h)
                residual_out(ps, n, o, xt, xt[:, o, :n], hout)
            S.store(HT, HT[:, :, off:off + n], hout, hout[:, :, :n])
        S.pop()

    def ffn_phase(l, blocks, final):
        nffn = V_NFFN0 if l == 0 else V_NFFN1
        S.push()
        cm = alloc_common()
        wgu = S.sbuf("wgu", [128, 8, 2 * DFF], BF16)
        load_w_cast(wgu, 0, ffn_wg, ffn_wg.ap_fn()[l], 0, DFF)
        load_w_cast(wgu, DFF, ffn_wu, ffn_wu.ap_fn()[l], 0, DFF)
        hid_ring = Ring([S.sbuf(f"hid{i}", [128, 22, 512], BF16) for i in range(2)])
        sg_ring = Ring([S.sbuf(f"sg{i}", [128, 512], F32) for i in range(2)])
        for (off, n, ci, is_halo, is_first) in blocks:
            norm_block(cm, HT, off, n, nffn)
            xn_b = cm.xn_b
            hid = hid_ring.next()
            xl = [xn_b[:, c, :n] for c in range(8)]
            for f in range(22):
                psg = proj_ps.next(); psu = proj_ps.next()
                proj(psg, wgu, f * 128, 128, xl, n, [xn_b])
                proj(psu, wgu, DFF + f * 128, 128, xl, n, [xn_b])
                sg = sg_ring.next()
                S.op(act, lambda psg=psg, sg=sg: nc.scalar.activation(out=sg[:, :n], in_=psg[:, :n], func=AF.Silu),
                     reads=[psg], writes=[sg])
                S.op(dve, lambda psu=psu, sg=sg, f=f: nc.vector.tensor_tensor(out=hid[:, f, :n], in0=psu[:, :n], in1=sg[:, :n], op=ALU.mult),
                     reads=[psu, sg], partial=[hid])
            S.store(HID, HID[:, :, off:off + n], hid, hid[:, :, :n])
        S.pop()
        S.push()
        wd = S.sbuf("wd", [128, 22, D], BF16)
        load_w_cast(wd, 0, ffn_wd, ffn_wd.ap_fn()[l], 0, D)
        hid_ring = Ring([S.sbuf(f"hidb{i}", [128, 22, 512], BF16) for i in range(2)])
        xt_ring = Ring([S.sbuf(f"xtd_{i}", [128, 8, 512], F32) for i in range(2)])
        hout_ring = Ring([S.sbuf(f"houtd_{i}", [128, 8, 512], F32) for i in range(2)])
        for (off, n, ci, is_halo, is_first) in blocks:
            hid = hid_ring.next(); xt = xt_ring.next(); hout = hout_ring.next()
            S.load(hid, hid[:, :, :n], HID, HID[:, :, off:off + n])
            S.load(xt, xt[:, :, :n], HT, HT[:, :, off:off + n])
            for o in range(8):
                ps = proj_ps.next()
                proj(ps, wd, o * 128, 128, [hid[:, f, :n] for f in range(22)], n, [hid])
                residual_out(ps, n, o, xt, xt[:, o, :n], hout)
            if final:
                slot0 = off // 128
                ci_, i_ = divmod(slot0, SLOTS_PER_CH)
                oo = (ci_ * CH_TILES + i_ - 1) * 128
                S.store(out_hT, out_hT[:, :, oo:oo + n], hout, hout[:, :, :n])
            else:
                S.store(HT, HT[:, :, off:off + n], hout, hout[:, :, :n])
        S.pop()

    cross_phase(0, own_blocks())
    ffn_phase(0, own_blocks(), False)

    S.push()
    cm = alloc_common()
    wci = S.sbuf("wci", [128, 8, 2 * D], BF16)
    load_w_cast(wci, 0, conv_w_in, conv_w_in.ap_fn(), 0, 2 * D)
    hflag = S.sbuf("hflag", [128, 2], F32)
    S.load(hflag, hflag[:], haloflag_in, haloflag_in[:, :])
    sig_ring = Ring([S.sbuf(f"sig{i}", [128, 512], F32) for i in range(2)])
    glu_ring = Ring([S.sbuf(f"glu{i}", [128, 8, 512], BF16) for i in range(2)])
    for (off, n, ci, is_halo, is_first) in own_blocks():
        norm_block(cm, HT, off, n, V_NMIX1)
        xn_b = cm.xn_b
        xl = [xn_b[:, c, :n] for c in range(8)]
        glu = glu_ring.next()
        for c8 in range(8):
            psa = proj_ps.next(); psg = proj_ps.next()
            proj(psa, wci, c8 * 128, 128, xl, n, [xn_b])
            proj(psg, wci, D + c8 * 128, 128, xl, n, [xn_b])
            sig = sig_ring.next()
            S.op(act, lambda psg=psg, sig=sig, c8=c8: nc.scalar.activation(out=sig[:, :n], in_=psg[:, :n], func=AF.Sigmoid,
                                                                          bias=vec2[:, V2_BIN + 8 + c8:V2_BIN + 9 + c8], scale=1.0),
                 reads=[psg, vec2], writes=[sig])
            S.op(dve, lambda psa=psa, sig=sig, c8=c8: nc.vector.scalar_tensor_tensor(
                out=glu[:, c8, :n], in0=psa[:, :n], scalar=vec2[:, V2_BIN + c8:V2_BIN + c8 + 1], in1=sig[:, :n],
                op0=ALU.add, op1=ALU.mult), reads=[psa, vec2, sig], partial=[glu])
        if is_halo:
            S.op(dve, lambda glu=glu: nc.vector.tensor_scalar(out=glu[:, :, :n], in0=glu[:, :, :n], scalar1=hflag[:, ci:ci + 1],
                                                             scalar2=None, op0=ALU.mult), reads=[hflag, glu], partial=[glu])
        S.store(GLU, GLU[:, :, off:off + n], glu, glu[:, :, :n])
    S.pop()

    S.push()
    wco = S.sbuf("wco", [128, 8, D], BF16)
    load_w_cast(wco, 0, conv_w_out, conv_w_out.ap_fn(), 0, D)
    dw_sb = S.sbuf("dw_sb", [128, 8, 31], F32)
    S.load(dw_sb, dw_sb[:], conv_dw, conv_dw[:, :, :])
    dg = S.sbuf("dg", [128, 8, 31, 128], BF16)
    for c8 in range(8):
        for k in range(31):
            S.op(act, lambda c8=c8, k=k: nc.scalar.activation(out=dg[:, c8, k, :], in_=ident[:], func=AF.Copy,
                                                              scale=dw_sb[:, c8, k:k + 1]), reads=[ident, dw_sb], partial=[dg])
    gin_ring = Ring([S.sbuf(f"gin{i}", [128, 8, 544], BF16) for i in range(2)])
    hc = S.sbuf("hc", [128, 8, 512], F32)
    hcb = S.sbuf("hcb", [128, 8, 512], BF16)
    hsq = S.sbuf("hsq", [128, 8, 512], BF16)
    mean_sb = S.sbuf("mean_sb", [128, 512], F32)
    var_sb = S.sbuf("var_sb", [128, 512], F32)
    rstd2 = S.sbuf("rstd2", [128, 512], F32)
    tv = S.sbuf("tv", [128, 512], F32)
    dtmp = Ring([S.sbuf(f"dtmp{i}", [128, 512], F32) for i in range(2)])
    sl = S.sbuf("sl", [128, 8, 512], BF16)
    xt_ring = Ring([S.sbuf(f"xtv_{i}", [128, 8, 512], F32) for i in range(1)])
    hout_ring = Ring([S.sbuf(f"houtv_{i}", [128, 8, 512], F32) for i in range(1)])
    for (off, n, ci, is_halo, is_first) in own_blocks(False):
        gin = gin_ring.next(); xt = xt_ring.next(); hout = hout_ring.next()
        S.load(gin, gin[:, :, :n + 30], GLU, GLU[:, :, off - 30:off + n])
        S.load(xt, xt[:, :, :n], HT, HT[:, :, off:off + n])
        for c8 in range(8):
            ps = proj_ps.next()
            for k in range(31):
                mm(ps[:, :n], dg[:, c8, k, :], gin[:, c8, k:k + n], k == 0, k == 30, [dg, gin], ps)
            S.op(act, lambda ps=ps, c8=c8: nc.scalar.activation(out=hc[:, c8, :n], in_=ps[:, :n], func=AF.Identity,
                                                                bias=vec2[:, V2_DWB + c8:V2_DWB + c8 + 1], scale=1.0),
                 reads=[ps, vec2], partial=[hc])
        S.op(dve, lambda: nc.vector.tensor_copy(out=hcb[:, :, :n], in_=hc[:, :, :n]), reads=[hc], writes=[hcb])
        S.op(act, lambda: nc.scalar.activation(out=hsq[:, :, :n], in_=hc[:, :, :n], func=AF.Square), reads=[hc], writes=[hsq])
        psm, psq = PS[5], PS[6]
        for c8 in range(8):
            mm(psm[:, :n], ones_m[:], hcb[:, c8, :n], c8 == 0, c8 == 7, [ones_m, hcb], psm)
        for c8 in range(8):
            mm(psq[:, :n], ones_m[:], hsq[:, c8, :n], c8 == 0, c8 == 7, [ones_m, hsq], psq)
        S.op(act, lambda: nc.scalar.activation(out=mean_sb[:, :n], in_=psm[:, :n], func=AF.Copy), reads=[psm], writes=[mean_sb])
        S.op(dve, lambda: nc.vector.tensor_tensor(out=var_sb[:, :n], in0=mean_sb[:, :n], in1=mean_sb[:, :n], op=ALU.mult),
             reads=[mean_sb], writes=[var_sb])
        S.op(dve, lambda: nc.vector.tensor_tensor(out=var_sb[:, :n], in0=psq[:, :n], in1=var_sb[:, :n], op=ALU.subtract),
             reads=[psq, var_sb], writes=[var_sb])
        S.op(act, lambda: nc.scalar.activation(out=tv[:, :n], in_=var_sb[:, :n], func=AF.Sqrt, bias=eps_t[:, 0:1], scale=1.0),
             reads=[var_sb, eps_t], writes=[tv])
        S.op(dve, lambda: nc.vector.reciprocal(out=rstd2[:, :n], in_=tv[:, :n]), reads=[tv], writes=[rstd2])
        for c8 in range(8):
            dt_ = dtmp.next()
            S.op(pool, lambda c8=c8, dt_=dt_: nc.gpsimd.tensor_tensor(out=dt_[:, :n], in0=hc[:, c8, :n], in1=mean_sb[:, :n], op=ALU.subtract),
                 reads=[hc, mean_sb], writes=[dt_])
            S.op(dve, lambda c8=c8, dt_=dt_: nc.vector.tensor_tensor(out=dt_[:, :n], in0=dt_[:, :n], in1=rstd2[:, :n], op=ALU.mult),
                 reads=[dt_, rstd2], writes=[dt_])
            S.op(act, lambda c8=c8, dt_=dt_: nc.scalar.activation(out=sl[:, c8, :n], in_=dt_[:, :n], func=AF.Silu,
                                                                  bias=vec2[:, V2_LNB + c8:V2_LNB + c8 + 1],
                                                                  scale=vec2[:, V2_LNG + c8:V2_LNG + c8 + 1]),
                 reads=[dt_, vec2], partial=[sl])
        for o in range(8):
            ps = proj_ps.next()
            proj(ps, wco, o * 128, 128, [sl[:, c, :n] for c in range(8)], n, [sl])
            residual_out(ps, n, o, xt, xt[:, o, :n], hout)
        S.store(HT, HT[:, :, off:off + n], hout, hout[:, :, :n])
    S.pop()

    cross_phase(1, own_blocks(False))
    ffn_phase(1, own_blocks(False), True)
    S.finish([out_hT])
    return nc


def _fm(a):
    t = a.shape[0]
    return np.ascontiguousarray(a.T.reshape(8, 128, t).transpose(1, 0, 2))


def _colvec(v):
    return np.ascontiguousarray(v.reshape(-1, 128).T)


def prepare_inputs(inputs, stop_after=None):
    f = lambda k: np.asarray(inputs[k], dtype=np.float32)
    x = f("x"); mem = f("mem")
    w_in = f("w_in_ab")[0]
    sp = np.cumsum((512, 512, 512, 512, 1024, 16, 64))[:-1]
    wq, wk, wv, wu, wiq, wiw, wik = np.split(w_in, sp, axis=1)
    w_keys = np.ascontiguousarray(np.concatenate([wk, wik, wik, wv], axis=1))
    w_own = np.ascontiguousarray(np.concatenate([wq, wiq, wu, wiw], axis=1))
    vecs = np.concatenate([_colvec(f(k)[l]) for k in ("norm_mix", "norm_cross", "norm_mem", "norm_ffn")
                           for l in range(2)], axis=1)
    vecs = np.ascontiguousarray(vecs.astype(np.float32))
    v2 = np.zeros((128, 64), np.float32)
    v2[:, 0] = f("a_q_norm")[0]; v2[:, 1] = f("a_k_norm")[0]
    v2[:, 2:6] = _colvec(f("pool_scale")[0])
    v2[:, 6:8] = _colvec(f("cross_q_norm")[0]); v2[:, 8:10] = _colvec(f("cross_q_norm")[1])
    v2[:, 10:12] = _colvec(f("cross_k_norm")[0]); v2[:, 12:14] = _colvec(f("cross_k_norm")[1])
    v2[:, 14:30] = _colvec(f("conv_b_in")[0])
    v2[:, 30:38] = _colvec(f("conv_dw_b")[0])
    v2[:, 38:46] = _colvec(f("conv_ln_g")[0])
    v2[:, 46:54] = _colvec(f("conv_ln_b")[0])
    conv_dw = np.ascontiguousarray(f("conv_dw_w")[0].T.reshape(8, 128, 31).transpose(1, 0, 2))
    seqpos = np.arange(SEQ)
    c128s, s128s = rope_tables(seqpos, 128)
    c64s, s64s = rope_tables(seqpos, 64)
    shared = dict(
        iota=np.ascontiguousarray(np.broadcast_to(np.arange(MBW, dtype=np.float32), (128, MBW))),
        c128s=c128s, s128s=s128s, c64s=c64s, s64s=s64s,
        p128=perm_matrix(128), p64=perm_matrix(64), ident=np.eye(128, dtype=np.float32),
        w_keys=w_keys, w_own=w_own, vecs=vecs, vecs2=v2,
        pool_w=f("pool_w")[0], w_out_ab=f("w_out_ab")[0], conv_w_in=f("conv_w_in")[0], conv_dw=conv_dw,
        conv_w_out=f("conv_w_out")[0], cross_wq=f("cross_wq"), cross_wk=f("cross_wk"),
        cross_wv=f("cross_wv"), cross_wo=f("cross_wo"), ffn_wg=f("ffn_w_gate"), ffn_wu=f("ffn_w_up"),
        ffn_wd=f("ffn_w_down"),
    )
    in_maps = []
    for core in range(8):
        b, half = divmod(core, 2)
        tiles = own_tiles(half)
        xo = np.zeros((NOWN, D), np.float32)
        pos = np.zeros(NOWN, np.int64)
        for s, t in enumerate(tiles):
            if t >= 0:
                xo[s * 128:(s + 1) * 128] = x[b, t * 128:(t + 1) * 128]
                pos[s * 128:(s + 1) * 128] = np.arange(t * 128, (t + 1) * 128)
        qa = np.zeros((128, NSLOT), np.float32)
        for s in range(NSLOT):
            base = (slot_first_uncertain(s) // 4) * 512
            qa[:, s] = pos[s * 128:(s + 1) * 128] - base
        c128o, s128o = rope_tables(pos, 128)
        c64o, s64o = rope_tables(pos, 64)
        pc = np.ones((128, 2, 4, 16), np.float32)
        hf = np.ones((128, 2), np.float32)
        if half == 0:
            hf[:, 0] = 0.0
            for g, w in enumerate((2, 4, 8, 16)):
                for t in range(16):
                    pc[:, 0, g, t] = w / min(t + 1, w)
        m = dict(shared)
        m.update(xT_seq=_fm(x[b]), xT_own=_fm(xo), memT=_fm(mem[b]), qadj=qa,
                 c128o=c128o, s128o=s128o, c64o=c64o, s64o=s64o, poolcorr=pc, haloflag=hf)
        in_maps.append(m)
    return in_maps


_NC_CACHE = {}


def kernel(**inputs):
    in_maps = prepare_inputs(inputs)
    if "nc" not in _NC_CACHE:
        _NC_CACHE["nc"] = build_program()
    nc = _NC_CACHE["nc"]
    res = run_bass_kernel_spmd(nc, in_maps, core_ids=list(range(8)))
    out = np.zeros((4, SEQ, D), np.float32)
    for core in range(8):
        b, half = divmod(core, 2)
        o = res.results[core]["out_hT"]
        o = o.transpose(2, 1, 0).reshape(32 * 128, D)
        for ci in range(2):
            start = (2 * ci + half) * CH_TILES * 128
            out[b, start:start + 2048] = o[ci * 2048:(ci + 1) * 2048]
    return out
```

```python
import numpy as np
import ml_dtypes
import concourse.bass as bass
import concourse.mybir as mybir
from concourse.bass_utils import run_bass_kernel_spmd

F32 = mybir.dt.float32
BF16 = mybir.dt.bfloat16
AF = mybir.ActivationFunctionType
ALU = mybir.AluOpType
AX = mybir.AxisListType

D = 1024
SEQ = 8192
NT_SEQ = SEQ // 128
CH_TILES = 16
SLOTS_PER_CH = CH_TILES + 1
NSLOT = 2 * SLOTS_PER_CH
NOWN = NSLOT * 128
MEM = 256
DFF = 2816
EPS = 1e-6
NEG = -1.0e30
NITER = 24
MBW = 3072

SAME_ENGINE_SYNC = True
SEQ_PE = True
SKEW = True
SKEW_A = True


class Buf:
    def __init__(self, name, ap_fn):
        self.name = name
        self.ap_fn = ap_fn
        self.w = {}
        self.r = {}
        self.dsem = None
        self.dval = 0

    def __getitem__(self, idx):
        return self.ap_fn()[idx]


class Eng:
    def __init__(self, name, inst, is_pe=False):
        self.name = name
        self.inst = inst
        self.sem = None
        self.cnt = 0
        self.known = {}
        self.is_pe = is_pe


class Sched:
    EPOCH = 30000

    def __init__(self, nc):
        self.nc = nc
        self.pe = Eng("pe", nc.tensor, True)
        self.act = Eng("act", nc.scalar)
        self.dve = Eng("dve", nc.vector)
        self.pool = Eng("pool", nc.gpsimd)
        self.sp = Eng("sp", nc.sync)
        self.engs = [self.pe, self.act, self.dve, self.pool, self.sp]
        self.nsem = 0
        self.all_dma_bufs = []
        self.nbuf = 0
        self.scopes = []
        self.live = []
        self.all_sems = []

    def new_sem(self, name):
        self.nsem += 1
        sm_ = self.nc.alloc_semaphore(f"{name}_{self.nsem}")
        self.all_sems.append(sm_)
        return sm_

    def sbuf(self, name, shape, dtype):
        self.nbuf += 1
        if self.scopes:
            t = self.scopes[-1].enter_context(self.nc.sbuf_tensor(f"{name}_{self.nbuf}", list(shape), dtype))
        else:
            t = self.nc.alloc_sbuf_tensor(f"{name}_{self.nbuf}", list(shape), dtype)
        b = Buf(name, lambda: t)
        self.live.append(b)
        return b

    def push(self):
        import contextlib
        self.scopes.append(contextlib.ExitStack())

    def pop(self):
        self.barrier()
        self.scopes.pop().close()

    def barrier(self):
        evs = {}
        for e in self.engs:
            if e.sem is not None and e.cnt > 0:
                evs[id(e.sem)] = (e.sem, e.cnt)
        for b in self.live:
            if b.dsem is not None and b.dval > 0:
                evs[id(b.dsem)] = (b.dsem, b.dval)
        for e in self.engs:
            for k, (sm, v) in evs.items():
                if e.sem is not None and sm is e.sem:
                    continue
                if e.known.get(k, 0) >= v:
                    continue
                e.inst.wait_ge(sm, v)
                e.known[k] = v

    def psum(self, name, shape, dtype):
        self.nbuf += 1
        t = self.nc.alloc_psum_tensor(f"{name}_{self.nbuf}", list(shape), dtype)
        return Buf(name, lambda: t)

    def dram(self, name, shape, dtype, kind="Internal"):
        t = self.nc.dram_tensor(name, list(shape), dtype, kind=kind)
        a = t.ap()
        return Buf(name, lambda: a)

    def view(self, name, buf, idx):
        return Buf(name, lambda: buf.ap_fn()[idx])

    def _collect(self, eng, reads, writes):
        need = {}

        def add(d):
            for s, v in d.items():
                k = id(s)
                if k not in need or need[k][1] < v:
                    need[k] = (s, v)
        for b in reads:
            add(b.w)
        for b in writes:
            add(b.w)
            add(b.r)
        for k, (s, v) in need.items():
            if eng.sem is not None and s is eng.sem:
                if eng.is_pe or not SAME_ENGINE_SYNC:
                    continue
            if eng.known.get(k, 0) >= v:
                continue
            eng.inst.wait_ge(s, v)
            eng.known[k] = v

    def op(self, eng, fn, reads=(), writes=(), partial=()):
        allw = list(writes) + list(partial)
        self._collect(eng, reads, allw)
        if eng.sem is None or eng.cnt >= self.EPOCH:
            eng.sem = self.new_sem(eng.name)
            eng.cnt = 0
        ins = fn()
        eng.cnt += 1
        ins.then_inc(eng.sem, 1)
        s, v = eng.sem, eng.cnt
        for b in reads:
            if b.r.get(s, 0) < v:
                b.r[s] = v
        for b in writes:
            b.w = {s: v}
            b.r = {}
        for b in partial:
            b.w[s] = v
        return ins

    def dma(self, out_buf, out_ap, in_buf, in_ap, eng=None, sbuf_side=None, **kw):
        eng = eng or self.sp
        owner = sbuf_side
        self._collect(eng, [in_buf], [out_buf])
        if owner.dsem is None or owner.dval >= self.EPOCH:
            owner.dsem = self.new_sem("d" + owner.name)
            owner.dval = 0
        ins = eng.inst.dma_start(out=out_ap, in_=in_ap, **kw)
        owner.dval += 16
        ins.then_inc(owner.dsem, 16)
        s, v = owner.dsem, owner.dval
        if in_buf.r.get(s, 0) < v:
            in_buf.r[s] = v
        out_buf.w[s] = v
        return ins

    def load(self, dst, dst_ap, src, src_ap, eng=None, **kw):
        return self.dma(dst, dst_ap, src, src_ap, eng=eng, sbuf_side=dst, **kw)

    def store(self, dst, dst_ap, src, src_ap, eng=None, **kw):
        return self.dma(dst, dst_ap, src, src_ap, eng=eng, sbuf_side=src, **kw)

    def finish(self, bufs):
        self._collect(self.sp, bufs, [])


class Ring:
    def __init__(self, bufs):
        self.bufs = bufs
        self.i = 0

    def next(self):
        b = self.bufs[self.i % len(self.bufs)]
        self.i += 1
        return b


def own_tiles(half):
    tiles = []
    for ci in range(2):
        start = (2 * ci + half) * CH_TILES
        tiles.append(start - 1)
        tiles.extend(range(start, start + CH_TILES))
    return tiles


def slot_nkt(slot):
    ci, i = divmod(slot, SLOTS_PER_CH)
    t1 = (2 * ci + 1) * CH_TILES + i - 1
    return t1 + 1


def slot_first_uncertain(slot):
    ci, i = divmod(slot, SLOTS_PER_CH)
    t0 = 2 * ci * CH_TILES + i - 1
    return max(t0, 0)


def rope_tables(pos, head_dim):
    rot = head_dim // 4
    half = rot // 2
    inv = 500000.0 ** (-np.arange(half, dtype=np.float32) * 2.0 / rot)
    ang = pos.astype(np.float32)[None, :] * inv[:, None].astype(np.float32)
    cos = np.cos(ang).astype(np.float32)
    sin = np.sin(ang).astype(np.float32)
    C = np.ones((128, len(pos)), np.float32)
    S = np.zeros((128, len(pos)), np.float32)
    for h0 in range(0, 128, head_dim):
        C[h0:h0 + half] = cos
        C[h0 + half:h0 + rot] = cos
        S[h0:h0 + half] = -sin
        S[h0 + half:h0 + rot] = sin
    return C, S


def perm_matrix(head_dim):
    rot = head_dim // 4
    half = rot // 2
    P = np.zeros((128, 128), np.float32)
    for h0 in range(0, 128, head_dim):
        for i in range(half):
            P[h0 + half + i, h0 + i] = 1.0
            P[h0 + i, h0 + half + i] = 1.0
    return P


def build_program(stop_after=None, slots=None):
    nc = bass.Bass("TRN2", target_bir_lowering=False)
    S = Sched(nc)
    pe, act, dve, pool, sp = S.pe, S.act, S.dve, S.pool, S.sp
    dbg = {}

    def din(name, shape, dtype=F32):
        return S.dram(name, shape, dtype, kind="ExternalInput")

    xT_seq = din("xT_seq", [128, 8, SEQ])
    xT_own = din("xT_own", [128, 8, NOWN])
    memT = din("memT", [128, 8, MEM])
    qadj = din("qadj", [128, NSLOT])
    iota_in = din("iota", [128, MBW])
    c128s = din("c128s", [128, SEQ]); s128s = din("s128s", [128, SEQ])
    c64s = din("c64s", [128, SEQ]); s64s = din("s64s", [128, SEQ])
    c128o = din("c128o", [128, NOWN]); s128o = din("s128o", [128, NOWN])
    c64o = din("c64o", [128, NOWN]); s64o = din("s64o", [128, NOWN])
    p128_in = din("p128", [128, 128]); p64_in = din("p64", [128, 128])
    ident_in = din("ident", [128, 128])
    poolcorr_in = din("poolcorr", [128, 2, 4, 16])
    haloflag_in = din("haloflag", [128, 2])
    w_keys = din("w_keys", [D, 1152])
    w_own = din("w_own", [D, 2064])
    vecs = din("vecs", [128, 64])
    pool_w = din("pool_w", [4, 128, 128])
    w_out_ab = din("w_out_ab", [D, D])
    conv_w_in = din("conv_w_in", [D, 2 * D])
    conv_dw = din("conv_dw", [128, 8, 31])
    conv_w_out = din("conv_w_out", [D, D])
    cross_wq = din("cross_wq", [2, D, D]); cross_wk = din("cross_wk", [2, D, D])
    cross_wv = din("cross_wv", [2, D, D]); cross_wo = din("cross_wo", [2, D, D])
    ffn_wg = din("ffn_wg", [2, D, DFF]); ffn_wu = din("ffn_wu", [2, D, DFF])
    ffn_wd = din("ffn_wd", [2, DFF, D])
    out_hT = S.dram("out_hT", [128, 8, 32 * 128], F32, kind="ExternalOutput")

    KT = S.dram("KT", [128, 4, SEQ], BF16)
    Vd = S.dram("Vd", [SEQ, 512], BF16)
    QT = S.dram("QT", [128, 4, NOWN], BF16)
    IQT = S.dram("IQT", [128, 8, NOWN], BF16)
    BT = S.dram("BT", [128, 4, NOWN], BF16)
    AT = S.dram("AT", [128, 4, NOWN], BF16)
    HT = S.dram("HT", [128, 8, NOWN], F32)
    HID = S.dram("HID", [128, 22, NOWN], BF16)

    ones_m = S.sbuf("ones_m", [128, 128], BF16)
    ones_h = S.sbuf("ones_h", [128, 128], BF16)
    ones_c = S.sbuf("ones_c", [128, 128], BF16)
    ones_1 = S.sbuf("ones_1", [128, 128], BF16)
    ident = S.sbuf("ident", [128, 128], BF16)
    p128 = S.sbuf("p128", [128, 128], BF16)
    p64 = S.sbuf("p64", [128, 128], BF16)
    cst_f = S.sbuf("cst_f", [128, 3, 128], F32)
    vec = S.sbuf("vec", [128, 64], F32)
    qadj_sb = S.sbuf("qadj_sb", [128, NSLOT], F32)
    eps_t = S.sbuf("eps_t", [128, 1], F32)

    S.op(pool, lambda: nc.gpsimd.memset(ones_m[:], 1.0 / 1024), writes=[ones_m])
    S.op(pool, lambda: nc.gpsimd.memset(ones_h[:], 1.0 / 128), writes=[ones_h])
    S.op(pool, lambda: nc.gpsimd.memset(ones_c[:], 1.0 / 256), writes=[ones_c])
    S.op(pool, lambda: nc.gpsimd.memset(ones_1[:], 1.0), writes=[ones_1])
    S.op(pool, lambda: nc.gpsimd.memset(eps_t[:], EPS), writes=[eps_t])
    S.load(cst_f, cst_f[:, 0, :], ident_in, ident_in[:, :])
    S.load(cst_f, cst_f[:, 1, :], p128_in, p128_in[:, :])
    S.load(cst_f, cst_f[:, 2, :], p64_in, p64_in[:, :])
    S.load(vec, vec[:], vecs, vecs[:, :])
    S.load(qadj_sb, qadj_sb[:], qadj, qadj[:, :])
    S.op(dve, lambda: nc.vector.tensor_copy(out=ident[:], in_=cst_f[:, 0, :]), reads=[cst_f], writes=[ident])
    S.op(dve, lambda: nc.vector.tensor_copy(out=p128[:], in_=cst_f[:, 1, :]), reads=[cst_f], writes=[p128])
    S.op(dve, lambda: nc.vector.tensor_copy(out=p64[:], in_=cst_f[:, 2, :]), reads=[cst_f], writes=[p64])

    V_NMIX0, V_NMIX1, V_NCROSS0, V_NCROSS1, V_NMEM0, V_NMEM1, V_NFFN0, V_NFFN1 = [8 * i for i in range(8)]
    vec2_in = din("vecs2", [128, 64])
    vec2 = S.sbuf("vec2", [128, 64], F32)
    S.load(vec2, vec2[:], vec2_in, vec2_in[:, :])
    V2_AQ, V2_AK = 0, 1
    V2_PSCALE = 2
    V2_CQ0, V2_CQ1, V2_CK0, V2_CK1 = 6, 8, 10, 12
    V2_BIN = 14
    V2_DWB = 30
    V2_LNG = 38
    V2_LNB = 46

    PS = [S.psum(f"ps{i}", [128, 512], F32) for i in range(7)]
    PSB = S.psum("psb", [128, 1024], BF16)

    def load_w_cast(dst, col_dst, w_buf, w_ap, col0, M):
        K = w_ap.shape[0]
        for kc in range(K // 128):
            m0 = 0
            while m0 < M:
                mm_ = min(2048, M - m0)
                S.load(dst, dst[:, kc, col_dst + m0:col_dst + m0 + mm_], w_buf,
                       w_ap[kc * 128:(kc + 1) * 128, col0 + m0:col0 + m0 + mm_], eng=pool)
                m0 += mm_

    def mm(ps_ap, lhsT, rhs, start, stop, reads, ps_buf):
        S.op(pe, lambda: nc.tensor.matmul(ps_ap, lhsT=lhsT, rhs=rhs, start=start, stop=stop),
             reads=reads, partial=[ps_buf] if not start else (), writes=[ps_buf] if start else ())

    def rstd_from_ms(ps_buf, n, out_buf, tmp_buf):
        S.op(act, lambda: nc.scalar.activation(out=tmp_buf[:, :n], in_=ps_buf[:, :n], func=AF.Sqrt,
                                               bias=eps_t[:, 0:1], scale=1.0),
             reads=[ps_buf, eps_t], writes=[tmp_buf])
        S.op(dve, lambda: nc.vector.reciprocal(out=out_buf[:, :n], in_=tmp_buf[:, :n]),
             reads=[tmp_buf], writes=[out_buf])

    def own_blocks(include_halo=True):
        res = []
        for ci in range(2):
            base = ci * SLOTS_PER_CH * 128
            if include_halo:
                res.append((base, 128, ci, True, False))
            for i in range(4):
                res.append((base + 128 + 512 * i, 512, ci, False, i == 0))
        return res

    class Common:
        pass

    def alloc_common():
        cm = Common()
        cm.xt_ring = Ring([S.sbuf(f"xt{i}", [128, 8, 512], F32) for i in range(2)])
        cm.sq_b = S.sbuf("sq", [128, 8, 512], BF16)
        cm.xn_b = S.sbuf("xn", [128, 8, 512], BF16)
        cm.rstd_b = S.sbuf("rstd", [128, 512], F32)
        cm.tmp_b = S.sbuf("tmpf", [128, 512], F32)
        return cm

    def norm_block(cm, src_buf, off, n, gcol):
        xt = cm.xt_ring.next()
        S.load(xt, xt[:, :, :n], src_buf, src_buf[:, :, off:off + n])
        S.op(act, lambda: nc.scalar.activation(out=cm.sq_b[:, :, :n], in_=xt[:, :, :n], func=AF.Square),
             reads=[xt], writes=[cm.sq_b])
        ps = PS[0]
        for c in range(8):
            mm(ps[:, :n], ones_m[:], cm.sq_b[:, c, :n], c == 0, c == 7, [ones_m, cm.sq_b], ps)
        rstd_from_ms(ps, n, cm.rstd_b, cm.tmp_b)
        for c in range(8):
            S.op(dve, lambda c=c: nc.vector.scalar_tensor_tensor(
                out=cm.xn_b[:, c, :n], in0=xt[:, c, :n], scalar=vec[:, gcol + c:gcol + c + 1],
                in1=cm.rstd_b[:, :n], op0=ALU.mult, op1=ALU.mult),
                reads=[xt, vec, cm.rstd_b], partial=[cm.xn_b])
        return xt

    def alloc_tmps():
        return (S.sbuf("sqh", [128, 512], BF16), S.sbuf("kn", [128, 512], BF16), S.sbuf("rk", [128, 512], F32),
                S.sbuf("tk", [128, 512], F32), S.sbuf("t1", [128, 512], F32), S.sbuf("t2", [128, 512], F32))

    def norm_head_rope(ps, n, gcol2, ones_t, pm, c_ap, s_ap, cs_bufs, out_ap, out_buf, do_norm, tmps, part="both"):
        sqh, kn, rk, tk, t1, t2 = tmps
        ps2, ps3 = PS[5], PS[6]
        if part == "back":
            pass
        elif do_norm:
            S.op(act, lambda: nc.scalar.activation(out=sqh[:, :n], in_=ps[:, :n], func=AF.Square),
                 reads=[ps], writes=[sqh])
            mm(ps2[:, :n], ones_t[:], sqh[:, :n], True, True, [ones_t, sqh], ps2)
            rstd_from_ms(ps2, n, rk, tk)
            S.op(dve, lambda: nc.vector.scalar_tensor_tensor(
                out=kn[:, :n], in0=ps[:, :n], scalar=vec2[:, gcol2:gcol2 + 1], in1=rk[:, :n],
                op0=ALU.mult, op1=ALU.mult), reads=[ps, vec2, rk], writes=[kn])
        else:
            S.op(act, lambda: nc.scalar.activation(out=kn[:, :n], in_=ps[:, :n], func=AF.Copy),
                 reads=[ps], writes=[kn])
        if part == "front":
            return
        mm(ps3[:, :n], pm[:], kn[:, :n], True, True, [pm, kn], ps3)
        S.op(pool, lambda: nc.gpsimd.tensor_tensor(out=t1[:, :n], in0=kn[:, :n], in1=c_ap, op=ALU.mult),
             reads=[kn] + cs_bufs, writes=[t1])
        S.op(dve, lambda: nc.vector.tensor_tensor(out=t2[:, :n], in0=ps3[:, :n], in1=s_ap, op=ALU.mult),
             reads=[ps3] + cs_bufs, writes=[t2])
        if isinstance(out_ap, list):
            for (oap, obuf, p0, p1) in out_ap:
                S.op(dve, lambda oap=oap, p0=p0, p1=p1: nc.vector.tensor_tensor(out=oap, in0=t1[p0:p1, :n], in1=t2[p0:p1, :n], op=ALU.add),
                     reads=[t1, t2], partial=[obuf])
        else:
            S.op(dve, lambda: nc.vector.tensor_tensor(out=out_ap, in0=t1[:, :n], in1=t2[:, :n], op=ALU.add),
                 reads=[t1, t2], partial=[out_buf])

    proj_ps = Ring([PS[1], PS[2], PS[3], PS[4]])

    def rope_pipeline(items, tmps2):
        pss = {}

        def front(i):
            proj_fn, args = items[i]
            ps = proj_ps.next()
            proj_fn(ps)
            pss[i] = ps
            norm_head_rope(ps, *args, tmps2[i % 2], part="front")

        def back(i):
            proj_fn, args = items[i]
            norm_head_rope(pss.pop(i), *args, tmps2[i % 2], part="back")
        front(0)
        for i in range(len(items)):
            if i + 1 < len(items):
                front(i + 1)
            back(i)

    def proj(ps, w_sb, col0, ncols, rhs_list, n, rbufs):
        kc = len(rhs_list)
        for c in range(kc):
            mm(ps[:, :n], w_sb[:, c, col0:col0 + ncols], rhs_list[c], c == 0, c == kc - 1, [w_sb] + rbufs, ps)

    S.push()
    ikT0 = S.sbuf("ikT0", [128, SEQ], BF16)
    ikT1 = S.sbuf("ikT1", [128, SEQ], BF16)
    iw_sb = S.sbuf("iw_sb", [128, NSLOT, 16], F32)
    S.op(pool, lambda: nc.gpsimd.memset(ikT0[:], 0.0), writes=[ikT0])
    S.op(pool, lambda: nc.gpsimd.memset(ikT1[:], 0.0), writes=[ikT1])

    S.push()
    cm = alloc_common()
    tmps = alloc_tmps()
    tmps2 = [tmps, alloc_tmps()]
    cs_ring = Ring([S.sbuf(f"cs{i}", [128, 4, 512], F32) for i in range(2)])
    wk_sb = S.sbuf("wk_sb", [128, 8, 1152], BF16)
    load_w_cast(wk_sb, 0, w_keys, w_keys.ap_fn(), 0, 1152)
    kout_ring = Ring([S.sbuf(f"kout{i}", [128, 4, 512], BF16) for i in range(2)])
    vout_ring = Ring([S.sbuf(f"vout{i}", [128, 4, 512], BF16) for i in range(2)])
    for blk in range(SEQ // 512):
        off = blk * 512
        n = 512
        norm_block(cm, xT_seq, off, n, V_NMIX0)
        xn_b = cm.xn_b
        cs = cs_ring.next()
        S.load(cs, cs[:, 0, :], c128s, c128s[:, off:off + n])
        S.load(cs, cs[:, 1, :], s128s, s128s[:, off:off + n])
        S.load(cs, cs[:, 2, :], c64s, c64s[:, off:off + n])
        S.load(cs, cs[:, 3, :], s64s, s64s[:, off:off + n])
        kout = kout_ring.next()
        xl_ = [xn_b[:, c, :n] for c in range(8)]
        items = []
        for kc in range(4):
            items.append((lambda ps, kc=kc: proj(ps, wk_sb, kc * 128, 128, xl_, n, [xn_b]),
                          (n, V2_AK, ones_h, p128, cs[:, 0, :n], cs[:, 1, :n], [cs], kout[:, kc, :n], kout, True)))
        items.append((lambda ps: proj(ps, wk_sb, 512, 128, xl_, n, [xn_b]),
                      (n, 0, None, p64, cs[:, 2, :n], cs[:, 3, :n], [cs],
                       [(ikT0[0:64, off:off + n], ikT0, 0, 64), (ikT1[64:128, off:off + n], ikT1, 64, 128)], None, False)))
        rope_pipeline(items, tmps2)
        S.store(KT, KT[:, :, off:off + n], kout, kout[:, :, :n])
        vout = vout_ring.next()
        for tt in range(4):
            ps = proj_ps.next()
            for c in range(8):
                mm(ps[:, :], xn_b[:, c, tt * 128:(tt + 1) * 128], wk_sb[:, c, 640:1152], c == 0, c == 7,
                   [wk_sb, xn_b], ps)
            S.op(act, lambda tt=tt, ps=ps: nc.scalar.activation(out=vout[:, tt, :], in_=ps[:, :], func=AF.Copy),
                 reads=[ps], partial=[vout])
        S.store(Vd, Vd.ap_fn()[off:off + n, :].rearrange("(t p) d -> p t d", p=128), vout, vout[:, :, :])
    S.pop()

    S.push()
    cm = alloc_common()
    tmps = alloc_tmps()
    tmps2 = [tmps, alloc_tmps()]
    cs_ring = Ring([S.sbuf(f"cs{i}", [128, 4, 512], F32) for i in range(1)])
    wo_sb = S.sbuf("wo_sb", [128, 8, 2064], BF16)
    load_w_cast(wo_sb, 0, w_own, w_own.ap_fn(), 0, 2064)
    pw_sb = S.sbuf("pw_sb", [128, 4, 128], BF16)
    for g in range(4):
        S.load(pw_sb, pw_sb[:, g, :], pool_w, pool_w[g, :, :], eng=pool)
    pcorr = S.sbuf("pcorr", [128, 2, 4, 16], F32)
    S.load(pcorr, pcorr[:], poolcorr_in, poolcorr_in[:, :, :, :])
    qout_ring = Ring([S.sbuf(f"qout{i}", [128, 4, 512], BF16) for i in range(2)])
    iqout_ring = Ring([S.sbuf(f"iqout{i}", [128, 8, 512], BF16) for i in range(1)])
    bout_ring = Ring([S.sbuf(f"bout{i}", [128, 4, 512], BF16) for i in range(2)])
    Ug = [S.sbuf(f"U{g}", [128, 528], F32) for g in range(4)]
    sA = [S.sbuf(f"sA{g}", [128, 528], F32) for g in range(4)]
    sB = [S.sbuf(f"sB{g}", [128, 528], F32) for g in range(4)]
    pooled = [S.sbuf(f"pooled{g}", [128, 512], BF16) for g in range(4)]
    for (off, n, ci, is_halo, is_first) in own_blocks():
        norm_block(cm, xT_own, off, n, V_NMIX0)
        xn_b = cm.xn_b
        xl = [xn_b[:, c, :n] for c in range(8)]
        cs = cs_ring.next()
        S.load(cs, cs[:, 0, :n], c128o, c128o[:, off:off + n])
        S.load(cs, cs[:, 1, :n], s128o, s128o[:, off:off + n])
        S.load(cs, cs[:, 2, :n], c64o, c64o[:, off:off + n])
        S.load(cs, cs[:, 3, :n], s64o, s64o[:, off:off + n])
        qout = qout_ring.next()
        iqout = iqout_ring.next()
        items = []
        for kc in range(4):
            items.append((lambda ps, kc=kc: proj(ps, wo_sb, kc * 128, 128, xl, n, [xn_b]),
                          (n, V2_AQ, ones_h, p128, cs[:, 0, :n], cs[:, 1, :n], [cs], qout[:, kc, :n], qout, True)))
        for kc in range(8):
            items.append((lambda ps, kc=kc: proj(ps, wo_sb, 512 + kc * 128, 128, xl, n, [xn_b]),
                          (n, 0, None, p64, cs[:, 2, :n], cs[:, 3, :n], [cs], iqout[:, kc, :n], iqout, False)))
        rope_pipeline(items, tmps2)
        S.store(QT, QT[:, :, off:off + n], qout, qout[:, :, :n])
        S.store(IQT, IQT[:, :, off:off + n], iqout, iqout[:, :, :n])
        for tt in range(n // 128):
            slot = off // 128 + tt
            ps = proj_ps.next()
            for c in range(8):
                mm(ps[:, :16], xn_b[:, c, tt * 128:(tt + 1) * 128], wo_sb[:, c, 2048:2064], c == 0, c == 7,
                   [wo_sb, xn_b], ps)
            S.op(act, lambda ps=ps, slot=slot: nc.scalar.activation(out=iw_sb[:, slot, :], in_=ps[:, :16],
                                                                    func=AF.Copy, scale=1.0 / 32.0),
                 reads=[ps], partial=[iw_sb])
        L = 16 + n
        bout = bout_ring.next()
        for g in range(4):
            U = Ug[g]
            if is_halo:
                S.op(pool, lambda U=U: nc.gpsimd.memset(U[:, 0:16], 0.0), partial=[U])
            ps = proj_ps.next()
            proj(ps, wo_sb, 1536 + g * 128, 128, xl, n, [xn_b])
            S.op(act, lambda ps=ps, U=U: nc.scalar.activation(out=U[:, 16:L], in_=ps[:, :n], func=AF.Copy),
                 reads=[ps], partial=[U])
            a_, b_ = sA[g], sB[g]
            S.op(pool, lambda U=U, a_=a_: nc.gpsimd.tensor_tensor(out=a_[:, 1:L], in0=U[:, 1:L], in1=U[:, 0:L - 1], op=ALU.add),
                 reads=[U], writes=[a_])
            fin = a_
            if g >= 1:
                S.op(pool, lambda a_=a_, b_=b_: nc.gpsimd.tensor_tensor(out=b_[:, 3:L], in0=a_[:, 3:L], in1=a_[:, 1:L - 2], op=ALU.add),
                     reads=[a_], writes=[b_])
                fin = b_
            if g >= 2:
                S.op(pool, lambda a_=a_, b_=b_: nc.gpsimd.tensor_tensor(out=a_[:, 7:L], in0=b_[:, 7:L], in1=b_[:, 3:L - 4], op=ALU.add),
                     reads=[b_], writes=[a_])
                fin = a_
            if g >= 3:
                S.op(pool, lambda a_=a_, b_=b_: nc.gpsimd.tensor_tensor(out=b_[:, 15:L], in0=a_[:, 15:L], in1=a_[:, 7:L - 8], op=ALU.add),
                     reads=[a_], writes=[b_])
                fin = b_
            if is_first:
                S.op(dve, lambda fin=fin, g=g: nc.vector.tensor_tensor(out=fin[:, 16:32], in0=fin[:, 16:32],
                                                                       in1=pcorr[:, ci, g, :], op=ALU.mult),
                     reads=[pcorr, fin], partial=[fin])
            w_ = float(2 ** (g + 1))
            S.op(dve, lambda fin=fin, U=U, g=g: nc.vector.scalar_tensor_tensor(
                out=pooled[g][:, :n], in0=fin[:, 16:L], scalar=1.0 / w_, in1=U[:, 16:L],
                op0=ALU.mult, op1=ALU.subtract), reads=[fin, U], writes=[pooled[g]])
            S.op(pool, lambda U=U: nc.gpsimd.tensor_copy(out=U[:, 0:16], in_=U[:, n:n + 16]), reads=[U], partial=[U])
            ps = proj_ps.next()
            mm(ps[:, :n], pw_sb[:, g, :], pooled[g][:, :n], True, True, [pw_sb, pooled[g]], ps)
            S.op(act, lambda ps=ps, g=g: nc.scalar.activation(out=bout[:, g, :n], in_=ps[:, :n], func=AF.Copy,
                                                               scale=vec2[:, V2_PSCALE + g:V2_PSCALE + g + 1]),
                 reads=[ps, vec2], partial=[bout])
        S.store(BT, BT[:, :, off:off + n], bout, bout[:, :, :n])
    S.pop()

    S.push()
    iota_sb = S.sbuf("iota_sb", [128, MBW], F32)
    S.load(iota_sb, iota_sb[:], iota_in, iota_in[:, :])
    scores2 = [S.sbuf(f"scores{i}", [128, SEQ], F32) for i in range(2)]
    mbias = S.sbuf("mbias", [128, SEQ], BF16)
    junk = S.sbuf("junk", [128, SEQ // 2], BF16)
    mb2 = [S.sbuf(f"mb{i}", [128, MBW], BF16) for i in range(2)]
    tmpu = S.sbuf("tmpu", [128, MBW], F32)
    qt_ring = Ring([S.sbuf(f"qt{i}", [128, 4, 128], BF16) for i in range(2)])
    iqt_ring = Ring([S.sbuf(f"iqt{i}", [128, 8, 128], BF16) for i in range(2)])
    kb_ring = Ring([S.sbuf(f"kblk{i}", [128, 4, 512], BF16) for i in range(2)])
    vb_ring = Ring([S.sbuf(f"vblk{i}", [128, 4, 512], BF16) for i in range(2)])
    r_ring = Ring([S.sbuf(f"R{i}", [128, 512], BF16) for i in range(4)])
    p_ring = Ring([S.sbuf(f"P{i}", [128, 512], BF16) for i in range(3)])
    pt_ring = Ring([S.sbuf(f"PT{i}", [128, 512], BF16) for i in range(3)])
    diag2 = [S.sbuf(f"diag{i}", [128, 16, 128], BF16) for i in range(2)]
    sm = S.sbuf("sm", [128, 16], F32)
    hs = S.sbuf("hs", [128, 32], F32)
    pow2 = S.sbuf("pow2", [128, 32], F32)
    cntb = S.sbuf("cntb", [128, 2], F32)
    midr = Ring([S.sbuf(f"mid{i}", [128, 1], F32) for i in range(2)])
    eb = S.sbuf("eb", [128, 1], F32)
    rs = S.sbuf("rs", [128, 4, 16], F32)
    rsum = S.sbuf("rsum", [128, 4], F32)
    rrec = S.sbuf("rrec", [128, 4], F32)
    negone = S.sbuf("negone", [128, 4], F32)
    rjunk = S.sbuf("rjunk", [128, 16], F32)
    a_tok = S.sbuf("a_tok", [128, 512], BF16)
    aT_ring = Ring([S.sbuf(f"aT{i}", [128, 4, 128], BF16) for i in range(2)])
    for i in range(32):
        S.op(pool, lambda i=i: nc.gpsimd.memset(pow2[:, i:i + 1], 2.0 ** (-i)), partial=[pow2])
    S.op(pool, lambda: nc.gpsimd.memset(negone[:], -1.0), writes=[negone])
    s_ring = Ring([PS[0], PS[1]])
    sc_ring = Ring([PS[2]])
    l_ring = Ring([PS[4], PS[5]])
    Obank = PS[6]
    ps3b = Buf("ps3b", lambda: PS[3].ap_fn()[:, :].bitcast(BF16))
    PSBv = [S.view("psb0", PSB, (slice(None), slice(0, 512))), S.view("ps3b0", ps3b, (slice(None), slice(0, 512)))]
    ptp_ring = Ring(PSBv)
    SM_MX, SM_MN1, SM_MN2, SM_MN, SM_H, SM_TAU = range(6)
    MASKV = -30000.0

    def slot_geom(j):
        nkt = slot_nkt(j)
        nkb = (nkt + 3) // 4
        ub = slot_first_uncertain(j) // 4
        return nkb, nkb * 512, ub, (nkb - ub) * 512

    def stage_A(j):
        nkb, N, ub, W = slot_geom(j)
        assert W <= MBW
        scores = scores2[j % 2]; mb = mb2[j % 2]; diag = diag2[j % 2]
        iqt = iqt_ring.next()
        S.load(iqt, iqt[:], IQT, IQT[:, :, j * 128:(j + 1) * 128])
        for h in range(16):
            S.op(pool, lambda h=h: nc.gpsimd.tensor_scalar(out=diag[:, h, :], in0=ident[:], scalar1=iw_sb[:, j, h:h + 1],
                                                          scalar2=None, op0=ALU.mult),
                 reads=[ident, iw_sb], partial=[diag])
        S.op(pool, lambda: nc.gpsimd.tensor_scalar(out=mb[:, :W], in0=iota_sb[:, :W], scalar1=qadj_sb[:, j:j + 1],
                                                  scalar2=MASKV, op0=ALU.is_gt, op1=ALU.mult),
             reads=[iota_sb, qadj_sb], writes=[mb])
        yield
        items = [(kb, h) for kb in range(nkb) for h in range(16)]
        pend = None
        sc = None
        for (kb, h) in items + [(None, None)]:
            cur = None
            if kb is not None:
                sp_ = s_ring.next()
                ikp = ikT0 if h % 2 == 0 else ikT1
                mm(sp_[:, :], iqt[:, h // 2, :], ikp[:, kb * 512:(kb + 1) * 512], True, True,
                   [iqt, ikp], sp_)
                R = r_ring.next()
                S.op(act, lambda sp_=sp_, R=R: nc.scalar.activation(out=R[:], in_=sp_[:, :], func=AF.Relu),
                     reads=[sp_], writes=[R])
                cur = (kb, h, R)
            if pend is not None:
                pkb, ph, pR = pend
                if ph == 0:
                    sc = sc_ring.next()
                mm(sc[:, :], diag[:, ph, :], pR[:], ph == 0, (ph == 15 and pkb < ub), [diag, pR], sc)
                if ph == 15:
                    if pkb >= ub:
                        mm(sc[:, :], ident[:], mb[:, (pkb - ub) * 512:(pkb - ub + 1) * 512], False, True, [ident, mb], sc)
                    S.op(act, lambda sc=sc, pkb=pkb: nc.scalar.activation(out=scores[:, pkb * 512:(pkb + 1) * 512], in_=sc[:, :],
                                                                          func=AF.Copy), reads=[sc], partial=[scores])
            pend = cur
            if not (SKEW or SKEW_A) and pend is not None:
                pkb, ph, pR = pend
                if ph == 0:
                    sc = sc_ring.next()
                mm(sc[:, :], diag[:, ph, :], pR[:], ph == 0, (ph == 15 and pkb < ub), [diag, pR], sc)
                if ph == 15:
                    if pkb >= ub:
                        mm(sc[:, :], ident[:], mb[:, (pkb - ub) * 512:(pkb - ub + 1) * 512], False, True, [ident, mb], sc)
                    S.op(act, lambda sc=sc, pkb=pkb: nc.scalar.activation(out=scores[:, pkb * 512:(pkb + 1) * 512], in_=sc[:, :],
                                                                          func=AF.Copy), reads=[sc], partial=[scores])
                pend = None
            yield

    def stage_B(j):
        nkb, N, ub, W = slot_geom(j)
        scores = scores2[j % 2]; mb = mb2[j % 2]
        S.op(dve, lambda: nc.vector.tensor_reduce(out=sm[:, SM_MX:SM_MX + 1], in_=scores[:, :N], axis=AX.X, op=ALU.max),
             reads=[scores], partial=[sm])
        S.op(dve, lambda: nc.vector.scalar_tensor_tensor(out=tmpu[:, :W], in0=mb[:, :W], scalar=-2.0,
                                                         in1=scores[:, ub * 512:N], op0=ALU.mult, op1=ALU.add),
             reads=[mb, scores], writes=[tmpu])
        S.op(dve, lambda: nc.vector.tensor_reduce(out=sm[:, SM_MN2:SM_MN2 + 1], in_=tmpu[:, :W], axis=AX.X, op=ALU.min),
             reads=[tmpu], partial=[sm])
        if ub > 0:
            S.op(dve, lambda: nc.vector.tensor_reduce(out=sm[:, SM_MN1:SM_MN1 + 1], in_=scores[:, :ub * 512], axis=AX.X, op=ALU.min),
                 reads=[scores], partial=[sm])
            S.op(dve, lambda: nc.vector.tensor_tensor(out=sm[:, SM_MN:SM_MN + 1], in0=sm[:, SM_MN1:SM_MN1 + 1],
                                                      in1=sm[:, SM_MN2:SM_MN2 + 1], op=ALU.min), reads=[sm], partial=[sm])
        else:
            S.op(dve, lambda: nc.vector.tensor_copy(out=sm[:, SM_MN:SM_MN + 1], in_=sm[:, SM_MN2:SM_MN2 + 1]),
                 reads=[sm], partial=[sm])
        S.op(dve, lambda: nc.vector.tensor_scalar(out=sm[:, SM_H:SM_H + 1], in0=sm[:, SM_MX:SM_MX + 1],
                                                  scalar1=sm[:, SM_MN:SM_MN + 1], scalar2=0.50005, op0=ALU.subtract, op1=ALU.mult),
             reads=[sm], partial=[sm])
        mid = midr.next()
        S.op(dve, lambda mid=mid: nc.vector.tensor_scalar(out=mid[:], in0=sm[:, SM_MX:SM_MX + 1],
                                                          scalar1=sm[:, SM_MN:SM_MN + 1], scalar2=0.5, op0=ALU.add, op1=ALU.mult),
             reads=[sm], writes=[mid])
        S.op(dve, lambda: nc.vector.tensor_scalar(out=hs[:], in0=pow2[:], scalar1=sm[:, SM_H:SM_H + 1], scalar2=None, op0=ALU.mult),
             reads=[pow2, sm], writes=[hs])
        yield
        N1 = min(N, SEQ // 2)
        for it in range(NITER):
            S.op(dve, lambda mid=mid: nc.vector.tensor_scalar(out=junk[:, :N1], in0=scores[:, :N1], scalar1=mid[:, 0:1], scalar2=0.0,
                                                              op0=ALU.is_ge, op1=ALU.add, accum_out=cntb[:, 0:1]),
                 reads=[scores, mid], writes=[junk, cntb])
            if N > N1:
                S.op(dve, lambda mid=mid: nc.vector.tensor_scalar(out=junk[:, :N - N1], in0=scores[:, N1:N], scalar1=mid[:, 0:1],
                                                                  scalar2=cntb[:, 0:1], op0=ALU.is_ge, op1=ALU.add, accum_out=cntb[:, 1:2]),
                     reads=[scores, mid, cntb], writes=[junk], partial=[cntb])
                ccol = 1
            else:
                ccol = 0
            S.op(dve, lambda ccol=ccol: nc.vector.tensor_scalar(out=eb[:], in0=cntb[:, ccol:ccol + 1], scalar1=255.5, scalar2=0.5,
                                                                op0=ALU.is_ge, op1=ALU.subtract), reads=[cntb], writes=[eb])
            nmid = midr.next()
            S.op(dve, lambda mid=mid, nmid=nmid, it=it: nc.vector.scalar_tensor_tensor(
                out=nmid[:], in0=eb[:], scalar=hs[:, it:it + 1], in1=mid[:], op0=ALU.mult, op1=ALU.add),
                reads=[eb, hs, mid], writes=[nmid])
            mid = nmid
            yield
        S.op(dve, lambda mid=mid: nc.vector.scalar_tensor_tensor(
            out=sm[:, SM_TAU:SM_TAU + 1], in0=sm[:, SM_H:SM_H + 1], scalar=-(2.0 ** (-NITER)), in1=mid[:],
            op0=ALU.mult, op1=ALU.add), reads=[sm, mid], partial=[sm])
        S.op(dve, lambda: nc.vector.tensor_scalar(out=mbias[:, :N], in0=scores[:, :N], scalar1=sm[:, SM_TAU:SM_TAU + 1],
                                                  scalar2=MASKV, op0=ALU.is_lt, op1=ALU.mult),
             reads=[scores, sm], writes=[mbias])
        yield

    def stage_C(j):
        nkb, N, ub, W = slot_geom(j)
        qt = qt_ring.next()
        S.load(qt, qt[:], QT, QT[:, :, j * 128:(j + 1) * 128])
        S.op(pool, lambda: nc.gpsimd.memset(rs[:], 0.0), writes=[rs])
        yield
        items = [(kb, h) for kb in range(nkb) for h in range(4)]
        nI = len(items)
        st = {}
        blk = {}
        first_o = [True]

        def qk(i):
            kb, h = items[i]
            if h == 0:
                kblk = kb_ring.next(); vblk = vb_ring.next()
                S.load(kblk, kblk[:], KT, KT[:, :, kb * 512:(kb + 1) * 512])
                S.load(vblk, vblk[:], Vd, Vd.ap_fn()[kb * 512:(kb + 1) * 512, :].rearrange("(t p) d -> p t d", p=128))
                blk[kb] = (kblk, vblk)
            kblk, vblk = blk[kb]
            Lp = l_ring.next()
            mm(Lp[:, :], qt[:, h, :], kblk[:, h, :], True, False, [qt, kblk], Lp)
            mm(Lp[:, :], ident[:], mbias[:, kb * 512:(kb + 1) * 512], False, True, [ident, mbias], Lp)
            Pb = p_ring.next()
            S.op(act, lambda: nc.scalar.activation(out=Pb[:], in_=Lp[:, :], func=AF.Exp, scale=128.0 ** -0.5,
                                                   accum_out=rs[:, h, kb:kb + 1]),
                 reads=[Lp], writes=[Pb], partial=[rs])
            st[i] = [Pb, None]

        def tr(i):
            Pb = st[i][0]
            ptp = ptp_ring.next()
            for tt in range(4):
                S.op(pe, lambda tt=tt: nc.tensor.transpose(out=ptp[:, tt * 128:(tt + 1) * 128],
                                                           in_=Pb[:, tt * 128:(tt + 1) * 128], identity=ident[:]),
                     reads=[Pb, ident], writes=[ptp] if tt == 0 else (), partial=[ptp] if tt else ())
            PTb = pt_ring.next()
            S.op(act, lambda: nc.scalar.activation(out=PTb[:], in_=ptp[:, :], func=AF.Copy), reads=[ptp], writes=[PTb])
            st[i][1] = PTb

        def pv(i):
            kb, h = items[i]
            PTb = st[i][1]
            kblk, vblk = blk[kb]
            for tt in range(4):
                fo = first_o[0]
                first_o[0] = False
                S.op(pe, lambda tt=tt, fo=fo: nc.tensor.matmul(
                    Obank[:, h * 128:(h + 1) * 128], lhsT=PTb[:, tt * 128:(tt + 1) * 128],
                    rhs=vblk[:, tt, h * 128:(h + 1) * 128], start=fo, stop=(i == nI - 1 and tt == 3),
                    skip_group_check=True),
                    reads=[PTb, vblk], writes=[Obank] if fo else (), partial=() if fo else [Obank])
            del st[i]

        if SKEW:
            for g in range(nI + 2):
                if g < nI:
                    qk(g)
                if 1 <= g <= nI:
                    tr(g - 1)
                if g >= 2:
                    pv(g - 2)
                yield
        else:
            for g in range(nI):
                qk(g)
                tr(g)
                pv(g)
                yield
        for h in range(4):
            S.op(act, lambda h=h: nc.scalar.activation(out=rjunk[:, :], in_=rs[:, h, :], func=AF.Copy, accum_out=rsum[:, h:h + 1]),
                 reads=[rs], writes=[rjunk], partial=[rsum])
        S.op(pool, lambda: nc.gpsimd.tensor_tensor(out=rrec[:], in0=rsum[:], in1=negone[:], op=ALU.pow),
             reads=[rsum, negone], writes=[rrec])
        for h in range(4):
            S.op(act, lambda h=h: nc.scalar.activation(out=a_tok[:, h * 128:(h + 1) * 128], in_=Obank[:, h * 128:(h + 1) * 128],
                                                       func=AF.Copy, scale=rrec[:, h:h + 1]),
                 reads=[Obank, rrec], partial=[a_tok])
        ptp = ptp_ring.next()
        for h in range(4):
            S.op(pe, lambda h=h, ptp=ptp: nc.tensor.transpose(out=ptp[:, h * 128:(h + 1) * 128],
                                                              in_=a_tok[:, h * 128:(h + 1) * 128], identity=ident[:]),
                 reads=[a_tok, ident], writes=[ptp] if h == 0 else (), partial=[ptp] if h else ())
        aT = aT_ring.next()
        S.op(act, lambda ptp=ptp, aT=aT: nc.scalar.activation(out=aT[:].rearrange("p a b -> p (a b)"), in_=ptp[:, :], func=AF.Copy),
             reads=[ptp], writes=[aT])
        S.store(AT, AT[:, :, j * 128:(j + 1) * 128], aT, aT[:])
        yield

    def run_all(g):
        for _ in g:
            pass

    slot_list = list(slots) if slots is not None else list(range(NSLOT))
    ns = len(slot_list)
    run_all(stage_A(slot_list[0]))
    for si in range(ns + 1):
        gC = stage_C(slot_list[si - 1]) if si - 1 >= 0 else None
        gA = stage_A(slot_list[si + 1]) if si + 1 < ns else None
        gB = stage_B(slot_list[si]) if si < ns else None
        nC = slot_geom(slot_list[si - 1])[0] * 4 + 4 if gC is not None else 0
        nA = slot_geom(slot_list[si + 1])[0] * 16 + 2 if gA is not None else 0
        nB = NITER + 2 if gB is not None else 0
        live = {"A": gA, "B": gB, "C": gC}
        tot = {"A": nA, "B": nB, "C": nC}
        done = {"A": 0, "B": 0, "C": 0}
        wgt = {"A": 1.0, "B": 1.0, "C": 0.6}
        while any(g is not None for g in live.values()):
            best = None
            for k, g in live.items():
                if g is None:
                    continue
                frac = wgt[k] * done[k] / max(1, tot[k])
                if best is None or frac < best[0]:
                    best = (frac, k)
            k = best[1]
            if SEQ_PE and k == "A" and live["C"] is not None:
                k = "C"
            if k == "B" and done["B"] >= NITER + 1 and live["C"] is not None:
                k = "C"
            try:
                next(live[k])
                done[k] += 1
            except StopIteration:
                live[k] = None
    S.pop()
    S.pop()

    if stop_after == "3":
        dbg_a = S.dram("dbg_a", [128, 4, NOWN], BF16, kind="ExternalOutput")
        dbg_b = S.dram("dbg_b", [128, 4, NOWN], BF16, kind="ExternalOutput")
        S.push()
        big = S.sbuf("dbgbig", [128, 4, NOWN], BF16)
        S.load(big, big[:], AT, AT[:, :, :])
        S.store(dbg_a, dbg_a[:, :, :], big, big[:])
        S.load(big, big[:], BT, BT[:, :, :])
        S.store(dbg_b, dbg_b[:, :, :], big, big[:])
        S.finish([dbg_a, dbg_b])
        return nc

    GLU = S.dram("GLU", [128, 8, NOWN], BF16)
    hout_ring_holder = {}

    def residual_out(ps, n, c, res_buf, res_ap, hout):
        S.op(dve, lambda: nc.vector.tensor_tensor(out=hout[:, c, :n], in0=ps[:, :n], in1=res_ap, op=ALU.add),
             reads=[ps, res_buf], partial=[hout])

    S.push()
    wout_sb = S.sbuf("wout_sb", [128, 8, D], BF16)
    load_w_cast(wout_sb, 0, w_out_ab, w_out_ab.ap_fn(), 0, D)
    xt_ring = Ring([S.sbuf(f"xt4_{i}", [128, 8, 512], F32) for i in range(2)])
    ab_ring = Ring([S.sbuf(f"ab{i}", [128, 8, 512], BF16) for i in range(2)])
    hout_ring = Ring([S.sbuf(f"hout4_{i}", [128, 8, 512], F32) for i in range(2)])
    for (off, n, ci, is_halo, is_first) in own_blocks():
        xt = xt_ring.next(); ab = ab_ring.next(); hout = hout_ring.next()
        S.load(xt, xt[:, :, :n], xT_own, xT_own[:, :, off:off + n])
        S.load(ab, ab[:, 0:4, :n], AT, AT[:, :, off:off + n])
        S.load(ab, ab[:, 4:8, :n], BT, BT[:, :, off:off + n])
        for o in range(8):
            ps = proj_ps.next()
            proj(ps, wout_sb, o * 128, 128, [ab[:, c, :n] for c in range(8)], n, [ab])
            residual_out(ps, n, o, xt, xt[:, o, :n], hout)
        S.store(HT, HT[:, :, off:off + n], hout, hout[:, :, :n])
    S.pop()

    def cross_phase(l, blocks):
        S.push()
        gq, gk = (V2_CQ0, V2_CK0) if l == 0 else (V2_CQ1, V2_CK1)
        ncross = V_NCROSS0 if l == 0 else V_NCROSS1
        nmem = V_NMEM0 if l == 0 else V_NMEM1
        cm = alloc_common()
        kcT = S.sbuf("kcT", [128, 8, MEM], BF16)
        vc = S.sbuf("vc", [128, 2, D], BF16)
        sqc = S.sbuf("sqc", [128, 2, 512], BF16)
        rq = S.sbuf("rq", [128, 512], F32)
        tq = S.sbuf("tq", [128, 512], F32)
        S.push()
        wkv = S.sbuf("wkv", [128, 8, 2 * D], BF16)
        load_w_cast(wkv, 0, cross_wk, cross_wk.ap_fn()[l], 0, D)
        load_w_cast(wkv, D, cross_wv, cross_wv.ap_fn()[l], 0, D)
        norm_block(cm, memT, 0, MEM, nmem)
        xn_b = cm.xn_b
        n = MEM
        for hh in range(4):
            pss = [proj_ps.next(), proj_ps.next()]
            for dc in range(2):
                proj(pss[dc], wkv, (hh * 2 + dc) * 128, 128, [xn_b[:, c, :n] for c in range(8)], n, [xn_b])
                S.op(act, lambda dc=dc, pss=pss: nc.scalar.activation(out=sqc[:, dc, :n], in_=pss[dc][:, :n], func=AF.Square),
                     reads=[pss[dc]], partial=[sqc])
            ps2 = PS[5]
            for dc in range(2):
                mm(ps2[:, :n], ones_c[:], sqc[:, dc, :n], dc == 0, dc == 1, [ones_c, sqc], ps2)
            rstd_from_ms(ps2, n, rq, tq)
            for dc in range(2):
                S.op(dve, lambda dc=dc, pss=pss, hh=hh: nc.vector.scalar_tensor_tensor(
                    out=kcT[:, hh * 2 + dc, :n], in0=pss[dc][:, :n], scalar=vec2[:, gk + dc:gk + dc + 1], in1=rq[:, :n],
                    op0=ALU.mult, op1=ALU.mult), reads=[pss[dc], vec2, rq], partial=[kcT])
        for mt in range(2):
            for hf in range(2):
                ps = proj_ps.next()
                for c in range(8):
                    mm(ps[:, :], xn_b[:, c, mt * 128:(mt + 1) * 128], wkv[:, c, D + hf * 512:D + (hf + 1) * 512], c == 0, c == 7,
                       [wkv, xn_b], ps)
                S.op(act, lambda ps=ps, mt=mt, hf=hf: nc.scalar.activation(out=vc[:, mt, hf * 512:(hf + 1) * 512], in_=ps[:, :], func=AF.Copy),
                     reads=[ps], partial=[vc])
        S.pop()
        wqo = S.sbuf("wqo", [128, 8, 2 * D], BF16)
        load_w_cast(wqo, 0, cross_wq, cross_wq.ap_fn()[l], 0, D)
        load_w_cast(wqo, D, cross_wo, cross_wo.ap_fn()[l], 0, D)
        qn = S.sbuf("qn", [128, 8, 512], BF16)
        on = S.sbuf("on", [128, 8, 512], BF16)
        pc_ring = Ring([S.sbuf(f"pc{i}", [128, 2, 512], BF16) for i in range(2)])
        rden = S.sbuf("rden", [128, 512], F32)
        hout_ring = Ring([S.sbuf(f"houtc_{i}", [128, 8, 512], F32) for i in range(2)])
        qn_h = [S.sbuf(f"qnh{h}", [128, 2, 512], BF16) for h in range(4)]
        on_h = [S.sbuf(f"onh{h}", [128, 2, 512], BF16) for h in range(4)]
        sqc_r = Ring([sqc, S.sbuf("sqc2", [128, 2, 512], BF16)])
        rq_r = Ring([rq, S.sbuf("rq2", [128, 512], F32)])
        tq_r = Ring([tq, S.sbuf("tq2", [128, 512], F32)])
        rden_r = Ring([rden, S.sbuf("rden2", [128, 512], F32)])
        for (off, n, ci, is_halo, is_first) in blocks:
            xt = norm_block(cm, HT, off, n, ncross)
            xn_b = cm.xn_b
            hout = hout_ring.next()

            def front(hh):
                pss = [proj_ps.next(), proj_ps.next()]
                sq_ = sqc_r.next(); rq_ = rq_r.next(); tq_ = tq_r.next()
                for dc in range(2):
                    proj(pss[dc], wqo, (hh * 2 + dc) * 128, 128, [xn_b[:, c, :n] for c in range(8)], n, [xn_b])
                    S.op(act, lambda dc=dc: nc.scalar.activation(out=sq_[:, dc, :n], in_=pss[dc][:, :n], func=AF.Square),
                         reads=[pss[dc]], partial=[sq_])
                ps2 = PS[5]
                for dc in range(2):
                    mm(ps2[:, :n], ones_c[:], sq_[:, dc, :n], dc == 0, dc == 1, [ones_c, sq_], ps2)
                rstd_from_ms(ps2, n, rq_, tq_)
                for dc in range(2):
                    S.op(dve, lambda dc=dc: nc.vector.scalar_tensor_tensor(
                        out=qn_h[hh][:, dc, :n], in0=pss[dc][:, :n], scalar=vec2[:, gq + dc:gq + dc + 1], in1=rq_[:, :n],
                        op0=ALU.mult, op1=ALU.mult), reads=[pss[dc], vec2, rq_], partial=[qn_h[hh]])

            def back(hh):
                pc = pc_ring.next()
                rd_ = rden_r.next()
                for mt in range(2):
                    ps = proj_ps.next()
                    for dc in range(2):
                        mm(ps[:, :n], kcT[:, hh * 2 + dc, mt * 128:(mt + 1) * 128], qn_h[hh][:, dc, :n], dc == 0, dc == 1,
                           [kcT, qn_h[hh]], ps)
                    S.op(act, lambda ps=ps, mt=mt: nc.scalar.activation(out=pc[:, mt, :n], in_=ps[:, :n], func=AF.Exp,
                                                                        scale=1.0 / 16.0), reads=[ps], partial=[pc])
                psd = PS[6]
                for mt in range(2):
                    mm(psd[:, :n], ones_1[:], pc[:, mt, :n], mt == 0, mt == 1, [ones_1, pc], psd)
                S.op(dve, lambda: nc.vector.reciprocal(out=rd_[:, :n], in_=psd[:, :n]), reads=[psd], writes=[rd_])
                for dc in range(2):
                    ps = proj_ps.next()
                    for mt in range(2):
                        mm(ps[:, :n], vc[:, mt, hh * 256 + dc * 128:hh * 256 + (dc + 1) * 128], pc[:, mt, :n], mt == 0, mt == 1,
                           [vc, pc], ps)
                    S.op(dve, lambda ps=ps, dc=dc: nc.vector.tensor_tensor(out=on_h[hh][:, dc, :n], in0=ps[:, :n],
                                                                          in1=rd_[:, :n], op=ALU.mult),
                         reads=[ps, rd_], partial=[on_h[hh]])

            front(0)
            for hh in range(4):
                if hh + 1 < 4:
                    front(hh + 1)
                back(hh)
            for o in range(8):
                ps = proj_ps.next()
                proj(ps, wqo, D + o * 128, 128, [on_h[c // 2][:, c % 2, :n] for c in range(8)], n, on_h)
                residual_out(ps, n, o, xt, xt[:, o, :n], hout)
            S.store(HT, HT[:, :, off:off + n], hout, hout[:, :, :n])
        S.pop()

    def ffn_phase(l, blocks, final):
        nffn = V_NFFN0 if l == 0 else V_NFFN1
        S.push()
        cm = alloc_common()
        wgu = S.sbuf("wgu", [128, 8, 2 * DFF], BF16)
        load_w_cast(wgu, 0, ffn_wg, ffn_wg.ap_fn()[l], 0, DFF)
        load_w_cast(wgu, DFF, ffn_wu, ffn_wu.ap_fn()[l], 0, DFF)
        hid_ring = Ring([S.sbuf(f"hid{i}", [128, 22, 512], BF16) for i in range(2)])
        sg_ring = Ring([S.sbuf(f"sg{i}", [128, 512], F32) for i in range(2)])
        for (off, n, ci, is_halo, is_first) in blocks:
            norm_block(cm, HT, off, n, nffn)
            xn_b = cm.xn_b
            hid = hid_ring.next()
            xl = [xn_b[:, c, :n] for c in range(8)]
            for f in range(22):
                psg = proj_ps.next(); psu = proj_ps.next()
                proj(psg, wgu, f * 128, 128, xl, n, [xn_b])
                proj(psu, wgu, DFF + f * 128, 128, xl, n, [xn_b])
                sg = sg_ring.next()
                S.op(act, lambda psg=psg, sg=sg: nc.scalar.activation(out=sg[:, :n], in_=psg[:, :n], func=AF.Silu),
                     reads=[psg], writes=[sg])
                S.op(dve, lambda psu=psu, sg=sg, f=f: nc.vector.tensor_tensor(out=hid[:, f, :n], in0=psu[:, :n], in1=sg[:, :n], op=ALU.mult),
                     reads=[psu, sg], partial=[hid])
            S.store(HID, HID[:, :, off:off + n], hid, hid[:, :, :n])
        S.pop()
        S.push()
        wd = S.sbuf("wd", [128, 22, D], BF16)
        load_w_cast(wd, 0, ffn_wd, ffn_wd.ap_fn()[l], 0, D)
        hid_ring = Ring([S.sbuf(f"hidb{i}", [128, 22, 512], BF16) for i in range(2)])
        xt_ring = Ring([S.sbuf(f"xtd_{i}", [128, 8, 512], F32) for i in range(2)])
        hout_ring = Ring([S.sbuf(f"houtd_{i}", [128, 8, 512], F32) for i in range(2)])
        for (off, n, ci, is_halo, is_first) in blocks:
            hid = hid_ring.next(); xt = xt_ring.next(); hout = hout_ring.next()
            S.load(hid, hid[:, :, :n], HID, HID[:, :, off:off + n])
            S.load(xt, xt[:, :, :n], HT, HT[:, :, off:off + n])
            for o in range(8):
                ps = proj_ps.next()
                proj(ps, wd, o * 128, 128, [hid[:, f, :n] for f in range(22)], n, [hid])
                residual_out(ps, n, o, xt, xt[:, o, :n], hout)
            if final:
                slot0 = off // 128
                ci_, i_ = divmod(slot0, SLOTS_PER_CH)
                oo = (ci_ * CH_TILES + i_ - 1) * 128
                S.store(out_hT, out_hT[:, :, oo:oo + n], hout, hout[:, :, :n])
            else:
                S.store(HT, HT[:, :, off:off + n], hout, hout[:, :, :n])
        S.pop()

    cross_phase(0, own_blocks())
    ffn_phase(0, own_blocks(), False)

    S.push()
    cm = alloc_common()
    wci = S.sbuf("wci", [128, 8, 2 * D], BF16)
    load_w_cast(wci, 0, conv_w_in, conv_w_in.ap_fn(), 0, 2 * D)
    hflag = S.sbuf("hflag", [128, 2], F32)
    S.load(hflag, hflag[:], haloflag_in, haloflag_in[:, :])
    sig_ring = Ring([S.sbuf(f"sig{i}", [128, 512], F32) for i in range(2)])
    glu_ring = Ring([S.sbuf(f"glu{i}", [128, 8, 512], BF16) for i in range(2)])
    for (off, n, ci, is_halo, is_first) in own_blocks():
        norm_block(cm, HT, off, n, V_NMIX1)
        xn_b = cm.xn_b
        xl = [xn_b[:, c, :n] for c in range(8)]
        glu = glu_ring.next()
        for c8 in range(8):
            psa = proj_ps.next(); psg = proj_ps.next()
            proj(psa, wci, c8 * 128, 128, xl, n, [xn_b])
            proj(psg, wci, D + c8 * 128, 128, xl, n, [xn_b])
            sig = sig_ring.next()
            S.op(act, lambda psg=psg, sig=sig, c8=c8: nc.scalar.activation(out=sig[:, :n], in_=psg[:, :n], func=AF.Sigmoid,
                                                                          bias=vec2[:, V2_BIN + 8 + c8:V2_BIN + 9 + c8], scale=1.0),
                 reads=[psg, vec2], writes=[sig])
            S.op(dve, lambda psa=psa, sig=sig, c8=c8: nc.vector.scalar_tensor_tensor(
                out=glu[:, c8, :n], in0=psa[:, :n], scalar=vec2[:, V2_BIN + c8:V2_BIN + c8 + 1], in1=sig[:, :n],
                op0=ALU.add, op1=ALU.mult), reads=[psa, vec2, sig], partial=[glu])
        if is_halo:
            S.op(dve, lambda glu=glu: nc.vector.tensor_scalar(out=glu[:, :, :n], in0=glu[:, :, :n], scalar1=hflag[:, ci:ci + 1],
                                                             scalar2=None, op0=ALU.mult), reads=[hflag, glu], partial=[glu])
        S.store(GLU, GLU[:, :, off:off + n], glu, glu[:, :, :n])
    S.pop()

    S.push()
    wco = S.sbuf("wco", [128, 8, D], BF16)
    load_w_cast(wco, 0, conv_w_out, conv_w_out.ap_fn(), 0, D)
    dw_sb = S.sbuf("dw_sb", [128, 8, 31], F32)
    S.load(dw_sb, dw_sb[:], conv_dw, conv_dw[:, :, :])
    dg = S.sbuf("dg", [128, 8, 31, 128], BF16)
    for c8 in range(8):
        for k in range(31):
            S.op(act, lambda c8=c8, k=k: nc.scalar.activation(out=dg[:, c8, k, :], in_=ident[:], func=AF.Copy,
                                                              scale=dw_sb[:, c8, k:k + 1]), reads=[ident, dw_sb], partial=[dg])
    gin_ring = Ring([S.sbuf(f"gin{i}", [128, 8, 544], BF16) for i in range(2)])
    hc = S.sbuf("hc", [128, 8, 512], F32)
    hcb = S.sbuf("hcb", [128, 8, 512], BF16)
    hsq = S.sbuf("hsq", [128, 8, 512], BF16)
    mean_sb = S.sbuf("mean_sb", [128, 512], F32)
    var_sb = S.sbuf("var_sb", [128, 512], F32)
    rstd2 = S.sbuf("rstd2", [128, 512], F32)
    tv = S.sbuf("tv", [128, 512], F32)
    dtmp = Ring([S.sbuf(f"dtmp{i}", [128, 512], F32) for i in range(2)])
    sl = S.sbuf("sl", [128, 8, 512], BF16)
    xt_ring = Ring([S.sbuf(f"xtv_{i}", [128, 8, 512], F32) for i in range(1)])
    hout_ring = Ring([S.sbuf(f"houtv_{i}", [128, 8, 512], F32) for i in range(1)])
    for (off, n, ci, is_halo, is_first) in own_blocks(False):
        gin = gin_ring.next(); xt = xt_ring.next(); hout = hout_ring.next()
        S.load(gin, gin[:, :, :n + 30], GLU, GLU[:, :, off - 30:off + n])
        S.load(xt, xt[:, :, :n], HT, HT[:, :, off:off + n])
        for c8 in range(8):
            ps = proj_ps.next()
            for k in range(31):
                mm(ps[:, :n], dg[:, c8, k, :], gin[:, c8, k:k + n], k == 0, k == 30, [dg, gin], ps)
            S.op(act, lambda ps=ps, c8=c8: nc.scalar.activation(out=hc[:, c8, :n], in_=ps[:, :n], func=AF.Identity,
                                                                bias=vec2[:, V2_DWB + c8:V2_DWB + c8 + 1], scale=1.0),
                 reads=[ps, vec2], partial=[hc])
        S.op(dve, lambda: nc.vector.tensor_copy(out=hcb[:, :, :n], in_=hc[:, :, :n]), reads=[hc], writes=[hcb])
        S.op(act, lambda: nc.scalar.activation(out=hsq[:, :, :n], in_=hc[:, :, :n], func=AF.Square), reads=[hc], writes=[hsq])
        psm, psq = PS[5], PS[6]
        for c8 in range(8):
            mm(psm[:, :n], ones_m[:], hcb[:, c8, :n], c8 == 0, c8 == 7, [ones_m, hcb], psm)
        for c8 in range(8):
            mm(psq[:, :n], ones_m[:], hsq[:, c8, :n], c8 == 0, c8 == 7, [ones_m, hsq], psq)
        S.op(act, lambda: nc.scalar.activation(out=mean_sb[:, :n], in_=psm[:, :n], func=AF.Copy), reads=[psm], writes=[mean_sb])
        S.op(dve, lambda: nc.vector.tensor_tensor(out=var_sb[:, :n], in0=mean_sb[:, :n], in1=mean_sb[:, :n], op=ALU.mult),
             reads=[mean_sb], writes=[var_sb])
        S.op(dve, lambda: nc.vector.tensor_tensor(out=var_sb[:, :n], in0=psq[:, :n], in1=var_sb[:, :n], op=ALU.subtract),
             reads=[psq, var_sb], writes=[var_sb])
        S.op(act, lambda: nc.scalar.activation(out=tv[:, :n], in_=var_sb[:, :n], func=AF.Sqrt, bias=eps_t[:, 0:1], scale=1.0),
             reads=[var_sb, eps_t], writes=[tv])
        S.op(dve, lambda: nc.vector.reciprocal(out=rstd2[:, :n], in_=tv[:, :n]), reads=[tv], writes=[rstd2])
        for c8 in range(8):
            dt_ = dtmp.next()
            S.op(pool, lambda c8=c8, dt_=dt_: nc.gpsimd.tensor_tensor(out=dt_[:, :n], in0=hc[:, c8, :n], in1=mean_sb[:, :n], op=ALU.subtract),
                 reads=[hc, mean_sb], writes=[dt_])
            S.op(dve, lambda c8=c8, dt_=dt_: nc.vector.tensor_tensor(out=dt_[:, :n], in0=dt_[:, :n], in1=rstd2[:, :n], op=ALU.mult),
                 reads=[dt_, rstd2], writes=[dt_])
            S.op(act, lambda c8=c8, dt_=dt_: nc.scalar.activation(out=sl[:, c8, :n], in_=dt_[:, :n], func=AF.Silu,
                                                                  bias=vec2[:, V2_LNB + c8:V2_LNB + c8 + 1],
                                                                  scale=vec2[:, V2_LNG + c8:V2_LNG + c8 + 1]),
                 reads=[dt_, vec2], partial=[sl])
        for o in range(8):
            ps = proj_ps.next()
            proj(ps, wco, o * 128, 128, [sl[:, c, :n] for c in range(8)], n, [sl])
            residual_out(ps, n, o, xt, xt[:, o, :n], hout)
        S.store(HT, HT[:, :, off:off + n], hout, hout[:, :, :n])
    S.pop()

    cross_phase(1, own_blocks(False))
    ffn_phase(1, own_blocks(False), True)
    S.finish([out_hT])
    return nc


def _fm(a):
    t = a.shape[0]
    return np.ascontiguousarray(a.T.reshape(8, 128, t).transpose(1, 0, 2))


def _colvec(v):
    return np.ascontiguousarray(v.reshape(-1, 128).T)


def prepare_inputs(inputs, stop_after=None):
    f = lambda k: np.asarray(inputs[k], dtype=np.float32)
    x = f("x"); mem = f("mem")
    w_in = f("w_in_ab")[0]
    sp = np.cumsum((512, 512, 512, 512, 1024, 16, 64))[:-1]
    wq, wk, wv, wu, wiq, wiw, wik = np.split(w_in, sp, axis=1)
    w_keys = np.ascontiguousarray(np.concatenate([wk, wik, wik, wv], axis=1))
    w_own = np.ascontiguousarray(np.concatenate([wq, wiq, wu, wiw], axis=1))
    vecs = np.concatenate([_colvec(f(k)[l]) for k in ("norm_mix", "norm_cross", "norm_mem", "norm_ffn")
                           for l in range(2)], axis=1)
    vecs = np.ascontiguousarray(vecs.astype(np.float32))
    v2 = np.zeros((128, 64), np.float32)
    v2[:, 0] = f("a_q_norm")[0]; v2[:, 1] = f("a_k_norm")[0]
    v2[:, 2:6] = _colvec(f("pool_scale")[0])
    v2[:, 6:8] = _colvec(f("cross_q_norm")[0]); v2[:, 8:10] = _colvec(f("cross_q_norm")[1])
    v2[:, 10:12] = _colvec(f("cross_k_norm")[0]); v2[:, 12:14] = _colvec(f("cross_k_norm")[1])
    v2[:, 14:30] = _colvec(f("conv_b_in")[0])
    v2[:, 30:38] = _colvec(f("conv_dw_b")[0])
    v2[:, 38:46] = _colvec(f("conv_ln_g")[0])
    v2[:, 46:54] = _colvec(f("conv_ln_b")[0])
    conv_dw = np.ascontiguousarray(f("conv_dw_w")[0].T.reshape(8, 128, 31).transpose(1, 0, 2))
    seqpos = np.arange(SEQ)
    c128s, s128s = rope_tables(seqpos, 128)
    c64s, s64s = rope_tables(seqpos, 64)
    shared = dict(
        iota=np.ascontiguousarray(np.broadcast_to(np.arange(MBW, dtype=np.float32), (128, MBW))),
        c128s=c128s, s128s=s128s, c64s=c64s, s64s=s64s,
        p128=perm_matrix(128), p64=perm_matrix(64), ident=np.eye(128, dtype=np.float32),
        w_keys=w_keys, w_own=w_own, vecs=vecs, vecs2=v2,
        pool_w=f("pool_w")[0], w_out_ab=f("w_out_ab")[0], conv_w_in=f("conv_w_in")[0], conv_dw=conv_dw,
        conv_w_out=f("conv_w_out")[0], cross_wq=f("cross_wq"), cross_wk=f("cross_wk"),
        cross_wv=f("cross_wv"), cross_wo=f("cross_wo"), ffn_wg=f("ffn_w_gate"), ffn_wu=f("ffn_w_up"),
        ffn_wd=f("ffn_w_down"),
    )
    in_maps = []
    for core in range(8):
        b, half = divmod(core, 2)
        tiles = own_tiles(half)
        xo = np.zeros((NOWN, D), np.float32)
        pos = np.zeros(NOWN, np.int64)
        for s, t in enumerate(tiles):
            if t >= 0:
                xo[s * 128:(s + 1) * 128] = x[b, t * 128:(t + 1) * 128]
                pos[s * 128:(s + 1) * 128] = np.arange(t * 128, (t + 1) * 128)
        qa = np.zeros((128, NSLOT), np.float32)
        for s in range(NSLOT):
            base = (slot_first_uncertain(s) // 4) * 512
            qa[:, s] = pos[s * 128:(s + 1) * 128] - base
        c128o, s128o = rope_tables(pos, 128)
        c64o, s64o = rope_tables(pos, 64)
        pc = np.ones((128, 2, 4, 16), np.float32)
        hf = np.ones((128, 2), np.float32)
        if half == 0:
            hf[:, 0] = 0.0
            for g, w in enumerate((2, 4, 8, 16)):
                for t in range(16):
                    pc[:, 0, g, t] = w / min(t + 1, w)
        m = dict(shared)
        m.update(xT_seq=_fm(x[b]), xT_own=_fm(xo), memT=_fm(mem[b]), qadj=qa,
                 c128o=c128o, s128o=s128o, c64o=c64o, s64o=s64o, poolcorr=pc, haloflag=hf)
        in_maps.append(m)
    return in_maps


_NC_CACHE = {}


def kernel(**inputs):
    in_maps = prepare_inputs(inputs)
    if "nc" not in _NC_CACHE:
        _NC_CACHE["nc"] = build_program()
    nc = _NC_CACHE["nc"]
    res = run_bass_kernel_spmd(nc, in_maps, core_ids=list(range(8)))
    out = np.zeros((4, SEQ, D), np.float32)
    for core in range(8):
        b, half = divmod(core, 2)
        o = res.results[core]["out_hT"]
        o = o.transpose(2, 1, 0).reshape(32 * 128, D)
        for ci in range(2):
            start = (2 * ci + half) * CH_TILES * 128
            out[b, start:start + 2048] = o[ci * 2048:(ci + 1) * 2048]
    return out
```

```python
import numpy as np
import ml_dtypes
import concourse.bass as bass
import concourse.mybir as mybir
from concourse.bass_utils import run_bass_kernel_spmd

F32 = mybir.dt.float32
BF16 = mybir.dt.bfloat16
AF = mybir.ActivationFunctionType
ALU = mybir.AluOpType
AX = mybir.AxisListType

D = 1024
SEQ = 8192
NT_SEQ = SEQ // 128
CH_TILES = 16
SLOTS_PER_CH = CH_TILES + 1
NSLOT = 2 * SLOTS_PER_CH
NOWN = NSLOT * 128
MEM = 256
DFF = 2816
EPS = 1e-6
NEG = -1.0e30
NITER = 24
MBW = 3072

SAME_ENGINE_SYNC = True
SEQ_PE = True
SKEW = True
SKEW_A = True


class Buf:
    def __init__(self, name, ap_fn):
        self.name = name
        self.ap_fn = ap_fn
        self.w = {}
        self.r = {}
        self.dsem = None
        self.dval = 0

    def __getitem__(self, idx):
        return self.ap_fn()[idx]


class Eng:
    def __init__(self, name, inst, is_pe=False):
        self.name = name
        self.inst = inst
        self.sem = None
        self.cnt = 0
        self.known = {}
        self.is_pe = is_pe


class Sched:
    EPOCH = 30000

    def __init__(self, nc):
        self.nc = nc
        self.pe = Eng("pe", nc.tensor, True)
        self.act = Eng("act", nc.scalar)
        self.dve = Eng("dve", nc.vector)
        self.pool = Eng("pool", nc.gpsimd)
        self.sp = Eng("sp", nc.sync)
        self.engs = [self.pe, self.act, self.dve, self.pool, self.sp]
        self.nsem = 0
        self.all_dma_bufs = []
        self.nbuf = 0
        self.scopes = []
        self.live = []
        self.all_sems = []

    def new_sem(self, name):
        self.nsem += 1
        sm_ = self.nc.alloc_semaphore(f"{name}_{self.nsem}")
        self.all_sems.append(sm_)
        return sm_

    def sbuf(self, name, shape, dtype):
        self.nbuf += 1
        if self.scopes:
            t = self.scopes[-1].enter_context(self.nc.sbuf_tensor(f"{name}_{self.nbuf}", list(shape), dtype))
        else:
            t = self.nc.alloc_sbuf_tensor(f"{name}_{self.nbuf}", list(shape), dtype)
        b = Buf(name, lambda: t)
        self.live.append(b)
        return b

    def push(self):
        import contextlib
        self.scopes.append(contextlib.ExitStack())

    def pop(self):
        self.barrier()
        self.scopes.pop().close()

    def barrier(self):
        evs = {}
        for e in self.engs:
            if e.sem is not None and e.cnt > 0:
                evs[id(e.sem)] = (e.sem, e.cnt)
        for b in self.live:
            if b.dsem is not None and b.dval > 0:
                evs[id(b.dsem)] = (b.dsem, b.dval)
        for e in self.engs:
            for k, (sm, v) in evs.items():
                if e.sem is not None and sm is e.sem:
                    continue
                if e.known.get(k, 0) >= v:
                    continue
                e.inst.wait_ge(sm, v)
                e.known[k] = v

    def psum(self, name, shape, dtype):
        self.nbuf += 1
        t = self.nc.alloc_psum_tensor(f"{name}_{self.nbuf}", list(shape), dtype)
        return Buf(name, lambda: t)

    def dram(self, name, shape, dtype, kind="Internal"):
        t = self.nc.dram_tensor(name, list(shape), dtype, kind=kind)
        a = t.ap()
        return Buf(name, lambda: a)

    def view(self, name, buf, idx):
        return Buf(name, lambda: buf.ap_fn()[idx])

    def _collect(self, eng, reads, writes):
        need = {}

        def add(d):
            for s, v in d.items():
                k = id(s)
                if k not in need or need[k][1] < v:
                    need[k] = (s, v)
        for b in reads:
            add(b.w)
        for b in writes:
            add(b.w)
            add(b.r)
        for k, (s, v) in need.items():
            if eng.sem is not None and s is eng.sem:
                if eng.is_pe or not SAME_ENGINE_SYNC:
                    continue
            if eng.known.get(k, 0) >= v:
                continue
            eng.inst.wait_ge(s, v)
            eng.known[k] = v

    def op(self, eng, fn, reads=(), writes=(), partial=()):
        allw = list(writes) + list(partial)
        self._collect(eng, reads, allw)
        if eng.sem is None or eng.cnt >= self.EPOCH:
            eng.sem = self.new_sem(eng.name)
            eng.cnt = 0
        ins = fn()
        eng.cnt += 1
        ins.then_inc(eng.sem, 1)
        s, v = eng.sem, eng.cnt
        for b in reads:
            if b.r.get(s, 0) < v:
                b.r[s] = v
        for b in writes:
            b.w = {s: v}
            b.r = {}
        for b in partial:
            b.w[s] = v
        return ins

    def dma(self, out_buf, out_ap, in_buf, in_ap, eng=None, sbuf_side=None, **kw):
        eng = eng or self.sp
        owner = sbuf_side
        self._collect(eng, [in_buf], [out_buf])
        if owner.dsem is None or owner.dval >= self.EPOCH:
            owner.dsem = self.new_sem("d" + owner.name)
            owner.dval = 0
        ins = eng.inst.dma_start(out=out_ap, in_=in_ap, **kw)
        owner.dval += 16
        ins.then_inc(owner.dsem, 16)
        s, v = owner.dsem, owner.dval
        if in_buf.r.get(s, 0) < v:
            in_buf.r[s] = v
        out_buf.w[s] = v
        return ins

    def load(self, dst, dst_ap, src, src_ap, eng=None, **kw):
        return self.dma(dst, dst_ap, src, src_ap, eng=eng, sbuf_side=dst, **kw)

    def store(self, dst, dst_ap, src, src_ap, eng=None, **kw):
        return self.dma(dst, dst_ap, src, src_ap, eng=eng, sbuf_side=src, **kw)

    def finish(self, bufs):
        self._collect(self.sp, bufs, [])


class Ring:
    def __init__(self, bufs):
        self.bufs = bufs
        self.i = 0

    def next(self):
        b = self.bufs[self.i % len(self.bufs)]
        self.i += 1
        return b


def own_tiles(half):
    tiles = []
    for ci in range(2):
        start = (2 * ci + half) * CH_TILES
        tiles.append(start - 1)
        tiles.extend(range(start, start + CH_TILES))
    return tiles


def slot_nkt(slot):
    ci, i = divmod(slot, SLOTS_PER_CH)
    t1 = (2 * ci + 1) * CH_TILES + i - 1
    return t1 + 1


def slot_first_uncertain(slot):
    ci, i = divmod(slot, SLOTS_PER_CH)
    t0 = 2 * ci * CH_TILES + i - 1
    return max(t0, 0)


def rope_tables(pos, head_dim):
    rot = head_dim // 4
    half = rot // 2
    inv = 500000.0 ** (-np.arange(half, dtype=np.float32) * 2.0 / rot)
    ang = pos.astype(np.float32)[None, :] * inv[:, None].astype(np.float32)
    cos = np.cos(ang).astype(np.float32)
    sin = np.sin(ang).astype(np.float32)
    C = np.ones((128, len(pos)), np.float32)
    S = np.zeros((128, len(pos)), np.float32)
    for h0 in range(0, 128, head_dim):
        C[h0:h0 + half] = cos
        C[h0 + half:h0 + rot] = cos
        S[h0:h0 + half] = -sin
        S[h0 + half:h0 + rot] = sin
    return C, S


def perm_matrix(head_dim):
    rot = head_dim // 4
    half = rot // 2
    P = np.zeros((128, 128), np.float32)
    for h0 in range(0, 128, head_dim):
        for i in range(half):
            P[h0 + half + i, h0 + i] = 1.0
            P[h0 + i, h0 + half + i] = 1.0
    return P


def build_program(stop_after=None, slots=None):
    nc = bass.Bass("TRN2", target_bir_lowering=False)
    S = Sched(nc)
    pe, act, dve, pool, sp = S.pe, S.act, S.dve, S.pool, S.sp
    dbg = {}

    def din(name, shape, dtype=F32):
        return S.dram(name, shape, dtype, kind="ExternalInput")

    xT_seq = din("xT_seq", [128, 8, SEQ])
    xT_own = din("xT_own", [128, 8, NOWN])
    memT = din("memT", [128, 8, MEM])
    qadj = din("qadj", [128, NSLOT])
    iota_in = din("iota", [128, MBW])
    c128s = din("c128s", [128, SEQ]); s128s = din("s128s", [128, SEQ])
    c64s = din("c64s", [128, SEQ]); s64s = din("s64s", [128, SEQ])
    c128o = din("c128o", [128, NOWN]); s128o = din("s128o", [128, NOWN])
    c64o = din("c64o", [128, NOWN]); s64o = din("s64o", [128, NOWN])
    p128_in = din("p128", [128, 128]); p64_in = din("p64", [128, 128])
    ident_in = din("ident", [128, 128])
    poolcorr_in = din("poolcorr", [128, 2, 4, 16])
    haloflag_in = din("haloflag", [128, 2])
    w_keys = din("w_keys", [D, 1152])
    w_own = din("w_own", [D, 2064])
    vecs = din("vecs", [128, 64])
    pool_w = din("pool_w", [4, 128, 128])
    w_out_ab = din("w_out_ab", [D, D])
    conv_w_in = din("conv_w_in", [D, 2 * D])
    conv_dw = din("conv_dw", [128, 8, 31])
    conv_w_out = din("conv_w_out", [D, D])
    cross_wq = din("cross_wq", [2, D, D]); cross_wk = din("cross_wk", [2, D, D])
    cross_wv = din("cross_wv", [2, D, D]); cross_wo = din("cross_wo", [2, D, D])
    ffn_wg = din("ffn_wg", [2, D, DFF]); ffn_wu = din("ffn_wu", [2, D, DFF])
    ffn_wd = din("ffn_wd", [2, DFF, D])
    out_hT = S.dram("out_hT", [128, 8, 32 * 128], F32, kind="ExternalOutput")

    KT = S.dram("KT", [128, 4, SEQ], BF16)
    Vd = S.dram("Vd", [SEQ, 512], BF16)
    QT = S.dram("QT", [128, 4, NOWN], BF16)
    IQT = S.dram("IQT", [128, 8, NOWN], BF16)
    BT = S.dram("BT", [128, 4, NOWN], BF16)
    AT = S.dram("AT", [128, 4, NOWN], BF16)
    HT = S.dram("HT", [128, 8, NOWN], F32)
    HID = S.dram("HID", [128, 22, NOWN], BF16)

    ones_m = S.sbuf("ones_m", [128, 128], BF16)
    ones_h = S.sbuf("ones_h", [128, 128], BF16)
    ones_c = S.sbuf("ones_c", [128, 128], BF16)
    ones_1 = S.sbuf("ones_1", [128, 128], BF16)
    ident = S.sbuf("ident", [128, 128], BF16)
    p128 = S.sbuf("p128", [128, 128], BF16)
    p64 = S.sbuf("p64", [128, 128], BF16)
    cst_f = S.sbuf("cst_f", [128, 3, 128], F32)
    vec = S.sbuf("vec", [128, 64], F32)
    qadj_sb = S.sbuf("qadj_sb", [128, NSLOT], F32)
    eps_t = S.sbuf("eps_t", [128, 1], F32)

    S.op(pool, lambda: nc.gpsimd.memset(ones_m[:], 1.0 / 1024), writes=[ones_m])
    S.op(pool, lambda: nc.gpsimd.memset(ones_h[:], 1.0 / 128), writes=[ones_h])
    S.op(pool, lambda: nc.gpsimd.memset(ones_c[:], 1.0 / 256), writes=[ones_c])
    S.op(pool, lambda: nc.gpsimd.memset(ones_1[:], 1.0), writes=[ones_1])
    S.op(pool, lambda: nc.gpsimd.memset(eps_t[:], EPS), writes=[eps_t])
    S.load(cst_f, cst_f[:, 0, :], ident_in, ident_in[:, :])
    S.load(cst_f, cst_f[:, 1, :], p128_in, p128_in[:, :])
    S.load(cst_f, cst_f[:, 2, :], p64_in, p64_in[:, :])
    S.load(vec, vec[:], vecs, vecs[:, :])
    S.load(qadj_sb, qadj_sb[:], qadj, qadj[:, :])
    S.op(dve, lambda: nc.vector.tensor_copy(out=ident[:], in_=cst_f[:, 0, :]), reads=[cst_f], writes=[ident])
    S.op(dve, lambda: nc.vector.tensor_copy(out=p128[:], in_=cst_f[:, 1, :]), reads=[cst_f], writes=[p128])
    S.op(dve, lambda: nc.vector.tensor_copy(out=p64[:], in_=cst_f[:, 2, :]), reads=[cst_f], writes=[p64])

    V_NMIX0, V_NMIX1, V_NCROSS0, V_NCROSS1, V_NMEM0, V_NMEM1, V_NFFN0, V_NFFN1 = [8 * i for i in range(8)]
    vec2_in = din("vecs2", [128, 64])
    vec2 = S.sbuf("vec2", [128, 64], F32)
    S.load(vec2, vec2[:], vec2_in, vec2_in[:, :])
    V2_AQ, V2_AK = 0, 1
    V2_PSCALE = 2
    V2_CQ0, V2_CQ1, V2_CK0, V2_CK1 = 6, 8, 10, 12
    V2_BIN = 14
    V2_DWB = 30
    V2_LNG = 38
    V2_LNB = 46

    PS = [S.psum(f"ps{i}", [128, 512], F32) for i in range(7)]
    PSB = S.psum("psb", [128, 1024], BF16)

    def load_w_cast(dst, col_dst, w_buf, w_ap, col0, M):
        K = w_ap.shape[0]
        for kc in range(K // 128):
            m0 = 0
            while m0 < M:
                mm_ = min(2048, M - m0)
                S.load(dst, dst[:, kc, col_dst + m0:col_dst + m0 + mm_], w_buf,
                       w_ap[kc * 128:(kc + 1) * 128, col0 + m0:col0 + m0 + mm_], eng=pool)
                m0 += mm_

    def mm(ps_ap, lhsT, rhs, start, stop, reads, ps_buf):
        S.op(pe, lambda: nc.tensor.matmul(ps_ap, lhsT=lhsT, rhs=rhs, start=start, stop=stop),
             reads=reads, partial=[ps_buf] if not start else (), writes=[ps_buf] if start else ())

    def rstd_from_ms(ps_buf, n, out_buf, tmp_buf):
        S.op(act, lambda: nc.scalar.activation(out=tmp_buf[:, :n], in_=ps_buf[:, :n], func=AF.Sqrt,
                                               bias=eps_t[:, 0:1], scale=1.0),
             reads=[ps_buf, eps_t], writes=[tmp_buf])
        S.op(dve, lambda: nc.vector.reciprocal(out=out_buf[:, :n], in_=tmp_buf[:, :n]),
             reads=[tmp_buf], writes=[out_buf])

    def own_blocks(include_halo=True):
        res = []
        for ci in range(2):
            base = ci * SLOTS_PER_CH * 128
            if include_halo:
                res.append((base, 128, ci, True, False))
            for i in range(4):
                res.append((base + 128 + 512 * i, 512, ci, False, i == 0))
        return res

    class Common:
        pass

    def alloc_common():
        cm = Common()
        cm.xt_ring = Ring([S.sbuf(f"xt{i}", [128, 8, 512], F32) for i in range(2)])
        cm.sq_b = S.sbuf("sq", [128, 8, 512], BF16)
        cm.xn_b = S.sbuf("xn", [128, 8, 512], BF16)
        cm.sqv = [S.view("sqv0", cm.sq_b, (slice(None), slice(0, 4))), S.view("sqv1", cm.sq_b, (slice(None), slice(4, 8)))]
        cm.rstd_b = S.sbuf("rstd", [128, 512], F32)
        cm.tmp_b = S.sbuf("tmpf", [128, 512], F32)
        return cm

    def norm_block(cm, src_buf, off, n, gcol):
        xt = cm.xt_ring.next()
        S.load(xt, xt[:, :, :n], src_buf, src_buf[:, :, off:off + n])
        sqv = cm.sqv
        for hf in range(2):
            S.op(act, lambda hf=hf: nc.scalar.activation(out=sqv[hf][:, :, :n], in_=xt[:, 4 * hf:4 * hf + 4, :n], func=AF.Square),
                 reads=[xt], writes=[sqv[hf]])
        ps = PS[0]
        for c in range(8):
            mm(ps[:, :n], ones_m[:], sqv[c // 4][:, c % 4, :n], c == 0, c == 7, [ones_m, sqv[c // 4]], ps)
        rstd_from_ms(ps, n, cm.rstd_b, cm.tmp_b)
        for c in range(8):
            S.op(dve, lambda c=c: nc.vector.scalar_tensor_tensor(
                out=cm.xn_b[:, c, :n], in0=xt[:, c, :n], scalar=vec[:, gcol + c:gcol + c + 1],
                in1=cm.rstd_b[:, :n], op0=ALU.mult, op1=ALU.mult),
                reads=[xt, vec, cm.rstd_b], partial=[cm.xn_b])
        return xt

    def alloc_tmps():
        return (S.sbuf("sqh", [128, 512], BF16), S.sbuf("kn", [128, 512], BF16), S.sbuf("rk", [128, 512], F32),
                S.sbuf("tk", [128, 512], F32), S.sbuf("t1", [128, 512], F32), S.sbuf("t2", [128, 512], F32))

    def norm_head_rope(ps, n, gcol2, ones_t, pm, c_ap, s_ap, cs_bufs, out_ap, out_buf, do_norm, tmps, part="both"):
        sqh, kn, rk, tk, t1, t2 = tmps
        ps2, ps3 = PS[5], PS[6]
        if part == "back":
            pass
        elif do_norm:
            S.op(act, lambda: nc.scalar.activation(out=sqh[:, :n], in_=ps[:, :n], func=AF.Square),
                 reads=[ps], writes=[sqh])
            mm(ps2[:, :n], ones_t[:], sqh[:, :n], True, True, [ones_t, sqh], ps2)
            rstd_from_ms(ps2, n, rk, tk)
            S.op(dve, lambda: nc.vector.scalar_tensor_tensor(
                out=kn[:, :n], in0=ps[:, :n], scalar=vec2[:, gcol2:gcol2 + 1], in1=rk[:, :n],
                op0=ALU.mult, op1=ALU.mult), reads=[ps, vec2, rk], writes=[kn])
        else:
            S.op(act, lambda: nc.scalar.activation(out=kn[:, :n], in_=ps[:, :n], func=AF.Copy),
                 reads=[ps], writes=[kn])
        if part == "front":
            return
        mm(ps3[:, :n], pm[:], kn[:, :n], True, True, [pm, kn], ps3)
        S.op(pool, lambda: nc.gpsimd.tensor_tensor(out=t1[:, :n], in0=kn[:, :n], in1=c_ap, op=ALU.mult),
             reads=[kn] + cs_bufs, writes=[t1])
        S.op(dve, lambda: nc.vector.tensor_tensor(out=t2[:, :n], in0=ps3[:, :n], in1=s_ap, op=ALU.mult),
             reads=[ps3] + cs_bufs, writes=[t2])
        if isinstance(out_ap, list):
            for (oap, obuf, p0, p1) in out_ap:
                S.op(dve, lambda oap=oap, p0=p0, p1=p1: nc.vector.tensor_tensor(out=oap, in0=t1[p0:p1, :n], in1=t2[p0:p1, :n], op=ALU.add),
                     reads=[t1, t2], partial=[obuf])
        else:
            S.op(dve, lambda: nc.vector.tensor_tensor(out=out_ap, in0=t1[:, :n], in1=t2[:, :n], op=ALU.add),
                 reads=[t1, t2], partial=[out_buf])

    proj_ps = Ring([PS[1], PS[2], PS[3], PS[4]])

    def rope_pipeline(items, tmps2):
        pss = {}

        def front(i):
            proj_fn, args = items[i]
            ps = proj_ps.next()
            proj_fn(ps)
            pss[i] = ps
            norm_head_rope(ps, *args, tmps2[i % 2], part="front")

        def back(i):
            proj_fn, args = items[i]
            norm_head_rope(pss.pop(i), *args, tmps2[i % 2], part="back")
        front(0)
        for i in range(len(items)):
            if i + 1 < len(items):
                front(i + 1)
            back(i)

    def proj(ps, w_sb, col0, ncols, rhs_list, n, rbufs):
        kc = len(rhs_list)
        for c in range(kc):
            mm(ps[:, :n], w_sb[:, c, col0:col0 + ncols], rhs_list[c], c == 0, c == kc - 1, [w_sb] + rbufs, ps)

    S.push()
    ikT0 = S.sbuf("ikT0", [128, SEQ], BF16)
    ikT1 = S.sbuf("ikT1", [128, SEQ], BF16)
    iw_sb = S.sbuf("iw_sb", [128, NSLOT, 16], F32)
    S.op(pool, lambda: nc.gpsimd.memset(ikT0[:], 0.0), writes=[ikT0])
    S.op(pool, lambda: nc.gpsimd.memset(ikT1[:], 0.0), writes=[ikT1])

    S.push()
    cm = alloc_common()
    tmps = alloc_tmps()
    tmps2 = [tmps, alloc_tmps()]
    cs_ring = Ring([S.sbuf(f"cs{i}", [128, 4, 512], F32) for i in range(2)])
    wk_sb = S.sbuf("wk_sb", [128, 8, 1152], BF16)
    load_w_cast(wk_sb, 0, w_keys, w_keys.ap_fn(), 0, 1152)
    kout_ring = Ring([S.sbuf(f"kout{i}", [128, 4, 512], BF16) for i in range(2)])
    vout_ring = Ring([S.sbuf(f"vout{i}", [128, 4, 512], BF16) for i in range(2)])
    for blk in range(SEQ // 512):
        off = blk * 512
        n = 512
        norm_block(cm, xT_seq, off, n, V_NMIX0)
        xn_b = cm.xn_b
        cs = cs_ring.next()
        S.load(cs, cs[:, 0, :], c128s, c128s[:, off:off + n])
        S.load(cs, cs[:, 1, :], s128s, s128s[:, off:off + n])
        S.load(cs, cs[:, 2, :], c64s, c64s[:, off:off + n])
        S.load(cs, cs[:, 3, :], s64s, s64s[:, off:off + n])
        kout = kout_ring.next()
        xl_ = [xn_b[:, c, :n] for c in range(8)]
        items = []
        for kc in range(4):
            items.append((lambda ps, kc=kc: proj(ps, wk_sb, kc * 128, 128, xl_, n, [xn_b]),
                          (n, V2_AK, ones_h, p128, cs[:, 0, :n], cs[:, 1, :n], [cs], kout[:, kc, :n], kout, True)))
        items.append((lambda ps: proj(ps, wk_sb, 512, 128, xl_, n, [xn_b]),
                      (n, 0, None, p64, cs[:, 2, :n], cs[:, 3, :n], [cs],
                       [(ikT0[0:64, off:off + n], ikT0, 0, 64), (ikT1[64:128, off:off + n], ikT1, 64, 128)], None, False)))
        rope_pipeline(items, tmps2)
        S.store(KT, KT[:, :, off:off + n], kout, kout[:, :, :n])
        vout = vout_ring.next()
        for tt in range(4):
            ps = proj_ps.next()
            for c in range(8):
                mm(ps[:, :], xn_b[:, c, tt * 128:(tt + 1) * 128], wk_sb[:, c, 640:1152], c == 0, c == 7,
                   [wk_sb, xn_b], ps)
            S.op(act, lambda tt=tt, ps=ps: nc.scalar.activation(out=vout[:, tt, :], in_=ps[:, :], func=AF.Copy),
                 reads=[ps], partial=[vout])
        S.store(Vd, Vd.ap_fn()[off:off + n, :].rearrange("(t p) d -> p t d", p=128), vout, vout[:, :, :])
    S.pop()

    S.push()
    cm = alloc_common()
    tmps = alloc_tmps()
    tmps2 = [tmps, alloc_tmps()]
    cs_ring = Ring([S.sbuf(f"cs{i}", [128, 4, 512], F32) for i in range(1)])
    wo_sb = S.sbuf("wo_sb", [128, 8, 2064], BF16)
    load_w_cast(wo_sb, 0, w_own, w_own.ap_fn(), 0, 2064)
    pw_sb = S.sbuf("pw_sb", [128, 4, 128], BF16)
    for g in range(4):
        S.load(pw_sb, pw_sb[:, g, :], pool_w, pool_w[g, :, :], eng=pool)
    pcorr = S.sbuf("pcorr", [128, 2, 4, 16], F32)
    S.load(pcorr, pcorr[:], poolcorr_in, poolcorr_in[:, :, :, :])
    qout_ring = Ring([S.sbuf(f"qout{i}", [128, 4, 512], BF16) for i in range(2)])
    iqout_ring = Ring([S.sbuf(f"iqout{i}", [128, 8, 512], BF16) for i in range(1)])
    bout_ring = Ring([S.sbuf(f"bout{i}", [128, 4, 512], BF16) for i in range(2)])
    Ug = [S.sbuf(f"U{g}", [128, 528], F32) for g in range(4)]
    sA = [S.sbuf(f"sA{g}", [128, 528], F32) for g in range(4)]
    sB = [S.sbuf(f"sB{g}", [128, 528], F32) for g in range(4)]
    pooled = [S.sbuf(f"pooled{g}", [128, 512], BF16) for g in range(4)]
    for (off, n, ci, is_halo, is_first) in own_blocks():
        norm_block(cm, xT_own, off, n, V_NMIX0)
        xn_b = cm.xn_b
        xl = [xn_b[:, c, :n] for c in range(8)]
        cs = cs_ring.next()
        S.load(cs, cs[:, 0, :n], c128o, c128o[:, off:off + n])
        S.load(cs, cs[:, 1, :n], s128o, s128o[:, off:off + n])
        S.load(cs, cs[:, 2, :n], c64o, c64o[:, off:off + n])
        S.load(cs, cs[:, 3, :n], s64o, s64o[:, off:off + n])
        qout = qout_ring.next()
        iqout = iqout_ring.next()
        items = []
        for kc in range(4):
            items.append((lambda ps, kc=kc: proj(ps, wo_sb, kc * 128, 128, xl, n, [xn_b]),
                          (n, V2_AQ, ones_h, p128, cs[:, 0, :n], cs[:, 1, :n], [cs], qout[:, kc, :n], qout, True)))
        for kc in range(8):
            items.append((lambda ps, kc=kc: proj(ps, wo_sb, 512 + kc * 128, 128, xl, n, [xn_b]),
                          (n, 0, None, p64, cs[:, 2, :n], cs[:, 3, :n], [cs], iqout[:, kc, :n], iqout, False)))
        rope_pipeline(items, tmps2)
        S.store(QT, QT[:, :, off:off + n], qout, qout[:, :, :n])
        S.store(IQT, IQT[:, :, off:off + n], iqout, iqout[:, :, :n])
        for tt in range(n // 128):
            slot = off // 128 + tt
            ps = proj_ps.next()
            for c in range(8):
                mm(ps[:, :16], xn_b[:, c, tt * 128:(tt + 1) * 128], wo_sb[:, c, 2048:2064], c == 0, c == 7,
                   [wo_sb, xn_b], ps)
            S.op(act, lambda ps=ps, slot=slot: nc.scalar.activation(out=iw_sb[:, slot, :], in_=ps[:, :16],
                                                                    func=AF.Copy, scale=1.0 / 32.0),
                 reads=[ps], partial=[iw_sb])
        L = 16 + n
        bout = bout_ring.next()
        for g in range(4):
            U = Ug[g]
            if is_halo:
                S.op(pool, lambda U=U: nc.gpsimd.memset(U[:, 0:16], 0.0), partial=[U])
            ps = proj_ps.next()
            proj(ps, wo_sb, 1536 + g * 128, 128, xl, n, [xn_b])
            S.op(act, lambda ps=ps, U=U: nc.scalar.activation(out=U[:, 16:L], in_=ps[:, :n], func=AF.Copy),
                 reads=[ps], partial=[U])
            a_, b_ = sA[g], sB[g]
            S.op(pool, lambda U=U, a_=a_: nc.gpsimd.tensor_tensor(out=a_[:, 1:L], in0=U[:, 1:L], in1=U[:, 0:L - 1], op=ALU.add),
                 reads=[U], writes=[a_])
            fin = a_
            if g >= 1:
                S.op(pool, lambda a_=a_, b_=b_: nc.gpsimd.tensor_tensor(out=b_[:, 3:L], in0=a_[:, 3:L], in1=a_[:, 1:L - 2], op=ALU.add),
                     reads=[a_], writes=[b_])
                fin = b_
            if g >= 2:
                S.op(pool, lambda a_=a_, b_=b_: nc.gpsimd.tensor_tensor(out=a_[:, 7:L], in0=b_[:, 7:L], in1=b_[:, 3:L - 4], op=ALU.add),
                     reads=[b_], writes=[a_])
                fin = a_
            if g >= 3:
                S.op(pool, lambda a_=a_, b_=b_: nc.gpsimd.tensor_tensor(out=b_[:, 15:L], in0=a_[:, 15:L], in1=a_[:, 7:L - 8], op=ALU.add),
                     reads=[a_], writes=[b_])
                fin = b_
            if is_first:
                S.op(dve, lambda fin=fin, g=g: nc.vector.tensor_tensor(out=fin[:, 16:32], in0=fin[:, 16:32],
                                                                       in1=pcorr[:, ci, g, :], op=ALU.mult),
                     reads=[pcorr, fin], partial=[fin])
            w_ = float(2 ** (g + 1))
            S.op(dve, lambda fin=fin, U=U, g=g: nc.vector.scalar_tensor_tensor(
                out=pooled[g][:, :n], in0=fin[:, 16:L], scalar=1.0 / w_, in1=U[:, 16:L],
                op0=ALU.mult, op1=ALU.subtract), reads=[fin, U], writes=[pooled[g]])
            S.op(pool, lambda U=U: nc.gpsimd.tensor_copy(out=U[:, 0:16], in_=U[:, n:n + 16]), reads=[U], partial=[U])
            ps = proj_ps.next()
            mm(ps[:, :n], pw_sb[:, g, :], pooled[g][:, :n], True, True, [pw_sb, pooled[g]], ps)
            S.op(act, lambda ps=ps, g=g: nc.scalar.activation(out=bout[:, g, :n], in_=ps[:, :n], func=AF.Copy,
                                                               scale=vec2[:, V2_PSCALE + g:V2_PSCALE + g + 1]),
                 reads=[ps, vec2], partial=[bout])
        S.store(BT, BT[:, :, off:off + n], bout, bout[:, :, :n])
    S.pop()

    S.push()
    iota_sb = S.sbuf("iota_sb", [128, MBW], F32)
    S.load(iota_sb, iota_sb[:], iota_in, iota_in[:, :])
    scores2 = [S.sbuf(f"scores{i}", [128, SEQ], F32) for i in range(2)]
    mbias = S.sbuf("mbias", [128, SEQ], BF16)
    junk = S.sbuf("junk", [128, SEQ // 2], BF16)
    mb2 = [S.sbuf(f"mb{i}", [128, MBW], BF16) for i in range(2)]
    tmpu = S.sbuf("tmpu", [128, MBW], F32)
    qt_ring = Ring([S.sbuf(f"qt{i}", [128, 4, 128], BF16) for i in range(2)])
    iqt_ring = Ring([S.sbuf(f"iqt{i}", [128, 8, 128], BF16) for i in range(2)])
    kb_ring = Ring([S.sbuf(f"kblk{i}", [128, 4, 512], BF16) for i in range(2)])
    vb_ring = Ring([S.sbuf(f"vblk{i}", [128, 4, 512], BF16) for i in range(2)])
    r_ring = Ring([S.sbuf(f"R{i}", [128, 512], BF16) for i in range(4)])
    p_ring = Ring([S.sbuf(f"P{i}", [128, 512], BF16) for i in range(3)])
    pt_ring = Ring([S.sbuf(f"PT{i}", [128, 512], BF16) for i in range(3)])
    diag2 = [S.sbuf(f"diag{i}", [128, 16, 128], BF16) for i in range(2)]
    sm = S.sbuf("sm", [128, 16], F32)
    hs = S.sbuf("hs", [128, 32], F32)
    pow2 = S.sbuf("pow2", [128, 32], F32)
    cntb = S.sbuf("cntb", [128, 2], F32)
    midr = Ring([S.sbuf(f"mid{i}", [128, 1], F32) for i in range(2)])
    eb = S.sbuf("eb", [128, 1], F32)
    rs = S.sbuf("rs", [128, 4, 16], F32)
    rsum = S.sbuf("rsum", [128, 4], F32)
    rrec = S.sbuf("rrec", [128, 4], F32)
    negone = S.sbuf("negone", [128, 4], F32)
    rjunk = S.sbuf("rjunk", [128, 16], F32)
    a_tok = S.sbuf("a_tok", [128, 512], BF16)
    aT_ring = Ring([S.sbuf(f"aT{i}", [128, 4, 128], BF16) for i in range(2)])
    for i in range(32):
        S.op(pool, lambda i=i: nc.gpsimd.memset(pow2[:, i:i + 1], 2.0 ** (-i)), partial=[pow2])
    S.op(pool, lambda: nc.gpsimd.memset(negone[:], -1.0), writes=[negone])
    s_ring = Ring([PS[0], PS[1]])
    sc_ring = Ring([PS[2]])
    l_ring = Ring([PS[4], PS[5]])
    Obank = PS[6]
    ps3b = Buf("ps3b", lambda: PS[3].ap_fn()[:, :].bitcast(BF16))
    PSBv = [S.view("psb0", PSB, (slice(None), slice(0, 512))), S.view("ps3b0", ps3b, (slice(None), slice(0, 512)))]
    ptp_ring = Ring(PSBv)
    SM_MX, SM_MN1, SM_MN2, SM_MN, SM_H, SM_TAU = range(6)
    MASKV = -30000.0

    def slot_geom(j):
        nkt = slot_nkt(j)
        nkb = (nkt + 3) // 4
        ub = slot_first_uncertain(j) // 4
        return nkb, nkb * 512, ub, (nkb - ub) * 512

    def stage_A(j):
        nkb, N, ub, W = slot_geom(j)
        assert W <= MBW
        scores = scores2[j % 2]; mb = mb2[j % 2]; diag = diag2[j % 2]
        iqt = iqt_ring.next()
        S.load(iqt, iqt[:], IQT, IQT[:, :, j * 128:(j + 1) * 128])
        for h in range(16):
            S.op(pool, lambda h=h: nc.gpsimd.tensor_scalar(out=diag[:, h, :], in0=ident[:], scalar1=iw_sb[:, j, h:h + 1],
                                                          scalar2=None, op0=ALU.mult),
                 reads=[ident, iw_sb], partial=[diag])
        S.op(pool, lambda: nc.gpsimd.tensor_scalar(out=mb[:, :W], in0=iota_sb[:, :W], scalar1=qadj_sb[:, j:j + 1],
                                                  scalar2=MASKV, op0=ALU.is_gt, op1=ALU.mult),
             reads=[iota_sb, qadj_sb], writes=[mb])
        yield
        items = [(kb, h) for kb in range(nkb) for h in range(16)]
        pend = None
        sc = None
        for (kb, h) in items + [(None, None)]:
            cur = None
            if kb is not None:
                sp_ = s_ring.next()
                ikp = ikT0 if h % 2 == 0 else ikT1
                mm(sp_[:, :], iqt[:, h // 2, :], ikp[:, kb * 512:(kb + 1) * 512], True, True,
                   [iqt, ikp], sp_)
                R = r_ring.next()
                S.op(act, lambda sp_=sp_, R=R: nc.scalar.activation(out=R[:], in_=sp_[:, :], func=AF.Relu),
                     reads=[sp_], writes=[R])
                cur = (kb, h, R)
            if pend is not None:
                pkb, ph, pR = pend
                if ph == 0:
                    sc = sc_ring.next()
                mm(sc[:, :], diag[:, ph, :], pR[:], ph == 0, (ph == 15 and pkb < ub), [diag, pR], sc)
                if ph == 15:
                    if pkb >= ub:
                        mm(sc[:, :], ident[:], mb[:, (pkb - ub) * 512:(pkb - ub + 1) * 512], False, True, [ident, mb], sc)
                    S.op(act, lambda sc=sc, pkb=pkb: nc.scalar.activation(out=scores[:, pkb * 512:(pkb + 1) * 512], in_=sc[:, :],
                                                                          func=AF.Copy), reads=[sc], partial=[scores])
            pend = cur
            if not (SKEW or SKEW_A) and pend is not None:
                pkb, ph, pR = pend
                if ph == 0:
                    sc = sc_ring.next()
                mm(sc[:, :], diag[:, ph, :], pR[:], ph == 0, (ph == 15 and pkb < ub), [diag, pR], sc)
                if ph == 15:
                    if pkb >= ub:
                        mm(sc[:, :], ident[:], mb[:, (pkb - ub) * 512:(pkb - ub + 1) * 512], False, True, [ident, mb], sc)
                    S.op(act, lambda sc=sc, pkb=pkb: nc.scalar.activation(out=scores[:, pkb * 512:(pkb + 1) * 512], in_=sc[:, :],
                                                                          func=AF.Copy), reads=[sc], partial=[scores])
                pend = None
            yield

    def stage_B(j):
        nkb, N, ub, W = slot_geom(j)
        scores = scores2[j % 2]; mb = mb2[j % 2]
        S.op(dve, lambda: nc.vector.tensor_reduce(out=sm[:, SM_MX:SM_MX + 1], in_=scores[:, :N], axis=AX.X, op=ALU.max),
             reads=[scores], partial=[sm])
        S.op(dve, lambda: nc.vector.scalar_tensor_tensor(out=tmpu[:, :W], in0=mb[:, :W], scalar=-2.0,
                                                         in1=scores[:, ub * 512:N], op0=ALU.mult, op1=ALU.add),
             reads=[mb, scores], writes=[tmpu])
        S.op(dve, lambda: nc.vector.tensor_reduce(out=sm[:, SM_MN2:SM_MN2 + 1], in_=tmpu[:, :W], axis=AX.X, op=ALU.min),
             reads=[tmpu], partial=[sm])
        if ub > 0:
            S.op(dve, lambda: nc.vector.tensor_reduce(out=sm[:, SM_MN1:SM_MN1 + 1], in_=scores[:, :ub * 512], axis=AX.X, op=ALU.min),
                 reads=[scores], partial=[sm])
            S.op(dve, lambda: nc.vector.tensor_tensor(out=sm[:, SM_MN:SM_MN + 1], in0=sm[:, SM_MN1:SM_MN1 + 1],
                                                      in1=sm[:, SM_MN2:SM_MN2 + 1], op=ALU.min), reads=[sm], partial=[sm])
        else:
            S.op(dve, lambda: nc.vector.tensor_copy(out=sm[:, SM_MN:SM_MN + 1], in_=sm[:, SM_MN2:SM_MN2 + 1]),
                 reads=[sm], partial=[sm])
        S.op(dve, lambda: nc.vector.tensor_scalar(out=sm[:, SM_H:SM_H + 1], in0=sm[:, SM_MX:SM_MX + 1],
                                                  scalar1=sm[:, SM_MN:SM_MN + 1], scalar2=0.50005, op0=ALU.subtract, op1=ALU.mult),
             reads=[sm], partial=[sm])
        mid = midr.next()
        S.op(dve, lambda mid=mid: nc.vector.tensor_scalar(out=mid[:], in0=sm[:, SM_MX:SM_MX + 1],
                                                          scalar1=sm[:, SM_MN:SM_MN + 1], scalar2=0.5, op0=ALU.add, op1=ALU.mult),
             reads=[sm], writes=[mid])
        S.op(dve, lambda: nc.vector.tensor_scalar(out=hs[:], in0=pow2[:], scalar1=sm[:, SM_H:SM_H + 1], scalar2=None, op0=ALU.mult),
             reads=[pow2, sm], writes=[hs])
        yield
        N1 = min(N, SEQ // 2)
        for it in range(NITER):
            S.op(dve, lambda mid=mid: nc.vector.tensor_scalar(out=junk[:, :N1], in0=scores[:, :N1], scalar1=mid[:, 0:1], scalar2=0.0,
                                                              op0=ALU.is_ge, op1=ALU.add, accum_out=cntb[:, 0:1]),
                 reads=[scores, mid], writes=[junk, cntb])
            if N > N1:
                S.op(dve, lambda mid=mid: nc.vector.tensor_scalar(out=junk[:, :N - N1], in0=scores[:, N1:N], scalar1=mid[:, 0:1],
                                                                  scalar2=cntb[:, 0:1], op0=ALU.is_ge, op1=ALU.add, accum_out=cntb[:, 1:2]),
                     reads=[scores, mid, cntb], writes=[junk], partial=[cntb])
                ccol = 1
            else:
                ccol = 0
            S.op(dve, lambda ccol=ccol: nc.vector.tensor_scalar(out=eb[:], in0=cntb[:, ccol:ccol + 1], scalar1=255.5, scalar2=0.5,
                                                                op0=ALU.is_ge, op1=ALU.subtract), reads=[cntb], writes=[eb])
            nmid = midr.next()
            S.op(dve, lambda mid=mid, nmid=nmid, it=it: nc.vector.scalar_tensor_tensor(
                out=nmid[:], in0=eb[:], scalar=hs[:, it:it + 1], in1=mid[:], op0=ALU.mult, op1=ALU.add),
                reads=[eb, hs, mid], writes=[nmid])
            mid = nmid
            yield
        S.op(dve, lambda mid=mid: nc.vector.scalar_tensor_tensor(
            out=sm[:, SM_TAU:SM_TAU + 1], in0=sm[:, SM_H:SM_H + 1], scalar=-(2.0 ** (-NITER)), in1=mid[:],
            op0=ALU.mult, op1=ALU.add), reads=[sm, mid], partial=[sm])
        S.op(dve, lambda: nc.vector.tensor_scalar(out=mbias[:, :N], in0=scores[:, :N], scalar1=sm[:, SM_TAU:SM_TAU + 1],
                                                  scalar2=MASKV, op0=ALU.is_lt, op1=ALU.mult),
             reads=[scores, sm], writes=[mbias])
        yield

    def stage_C(j):
        nkb, N, ub, W = slot_geom(j)
        qt = qt_ring.next()
        S.load(qt, qt[:], QT, QT[:, :, j * 128:(j + 1) * 128])
        S.op(pool, lambda: nc.gpsimd.memset(rs[:], 0.0), writes=[rs])
        yield
        items = [(kb, h) for kb in range(nkb) for h in range(4)]
        nI = len(items)
        st = {}
        blk = {}
        first_o = [True]

        def qk(i):
            kb, h = items[i]
            if h == 0:
                kblk = kb_ring.next(); vblk = vb_ring.next()
                S.load(kblk, kblk[:], KT, KT[:, :, kb * 512:(kb + 1) * 512])
                S.load(vblk, vblk[:], Vd, Vd.ap_fn()[kb * 512:(kb + 1) * 512, :].rearrange("(t p) d -> p t d", p=128))
                blk[kb] = (kblk, vblk)
            kblk, vblk = blk[kb]
            Lp = l_ring.next()
            mm(Lp[:, :], qt[:, h, :], kblk[:, h, :], True, False, [qt, kblk], Lp)
            mm(Lp[:, :], ident[:], mbias[:, kb * 512:(kb + 1) * 512], False, True, [ident, mbias], Lp)
            Pb = p_ring.next()
            S.op(act, lambda: nc.scalar.activation(out=Pb[:], in_=Lp[:, :], func=AF.Exp, scale=128.0 ** -0.5,
                                                   accum_out=rs[:, h, kb:kb + 1]),
                 reads=[Lp], writes=[Pb], partial=[rs])
            st[i] = [Pb, None]

        def tr(i):
            Pb = st[i][0]
            ptp = ptp_ring.next()
            for tt in range(4):
                S.op(pe, lambda tt=tt: nc.tensor.transpose(out=ptp[:, tt * 128:(tt + 1) * 128],
                                                           in_=Pb[:, tt * 128:(tt + 1) * 128], identity=ident[:]),
                     reads=[Pb, ident], writes=[ptp] if tt == 0 else (), partial=[ptp] if tt else ())
            PTb = pt_ring.next()
            S.op(act, lambda: nc.scalar.activation(out=PTb[:], in_=ptp[:, :], func=AF.Copy), reads=[ptp], writes=[PTb])
            st[i][1] = PTb

        def pv(i):
            kb, h = items[i]
            PTb = st[i][1]
            kblk, vblk = blk[kb]
            for tt in range(4):
                fo = first_o[0]
                first_o[0] = False
                S.op(pe, lambda tt=tt, fo=fo: nc.tensor.matmul(
                    Obank[:, h * 128:(h + 1) * 128], lhsT=PTb[:, tt * 128:(tt + 1) * 128],
                    rhs=vblk[:, tt, h * 128:(h + 1) * 128], start=fo, stop=(i == nI - 1 and tt == 3),
                    skip_group_check=True),
                    reads=[PTb, vblk], writes=[Obank] if fo else (), partial=() if fo else [Obank])
            del st[i]

        if SKEW:
            for g in range(nI + 2):
                if g < nI:
                    qk(g)
                if 1 <= g <= nI:
                    tr(g - 1)
                if g >= 2:
                    pv(g - 2)
                yield
        else:
            for g in range(nI):
                qk(g)
                tr(g)
                pv(g)
                yield
        for h in range(4):
            S.op(act, lambda h=h: nc.scalar.activation(out=rjunk[:, :], in_=rs[:, h, :], func=AF.Copy, accum_out=rsum[:, h:h + 1]),
                 reads=[rs], writes=[rjunk], partial=[rsum])
        S.op(pool, lambda: nc.gpsimd.tensor_tensor(out=rrec[:], in0=rsum[:], in1=negone[:], op=ALU.pow),
             reads=[rsum, negone], writes=[rrec])
        for h in range(4):
            S.op(act, lambda h=h: nc.scalar.activation(out=a_tok[:, h * 128:(h + 1) * 128], in_=Obank[:, h * 128:(h + 1) * 128],
                                                       func=AF.Copy, scale=rrec[:, h:h + 1]),
                 reads=[Obank, rrec], partial=[a_tok])
        ptp = ptp_ring.next()
        for h in range(4):
            S.op(pe, lambda h=h, ptp=ptp: nc.tensor.transpose(out=ptp[:, h * 128:(h + 1) * 128],
                                                              in_=a_tok[:, h * 128:(h + 1) * 128], identity=ident[:]),
                 reads=[a_tok, ident], writes=[ptp] if h == 0 else (), partial=[ptp] if h else ())
        aT = aT_ring.next()
        S.op(act, lambda ptp=ptp, aT=aT: nc.scalar.activation(out=aT[:].rearrange("p a b -> p (a b)"), in_=ptp[:, :], func=AF.Copy),
             reads=[ptp], writes=[aT])
        S.store(AT, AT[:, :, j * 128:(j + 1) * 128], aT, aT[:])
        yield

    def run_all(g):
        for _ in g:
            pass

    slot_list = list(slots) if slots is not None else list(range(NSLOT))
    ns = len(slot_list)
    run_all(stage_A(slot_list[0]))
    for si in range(ns + 1):
        gC = stage_C(slot_list[si - 1]) if si - 1 >= 0 else None
        gA = stage_A(slot_list[si + 1]) if si + 1 < ns else None
        gB = stage_B(slot_list[si]) if si < ns else None
        nC = slot_geom(slot_list[si - 1])[0] * 4 + 4 if gC is not None else 0
        nA = slot_geom(slot_list[si + 1])[0] * 16 + 2 if gA is not None else 0
        nB = NITER + 2 if gB is not None else 0
        live = {"A": gA, "B": gB, "C": gC}
        tot = {"A": nA, "B": nB, "C": nC}
        done = {"A": 0, "B": 0, "C": 0}
        wgt = {"A": 1.0, "B": 1.0, "C": 0.6}
        while any(g is not None for g in live.values()):
            best = None
            for k, g in live.items():
                if g is None:
                    continue
                frac = wgt[k] * done[k] / max(1, tot[k])
                if best is None or frac < best[0]:
                    best = (frac, k)
            k = best[1]
            if SEQ_PE and k == "A" and live["C"] is not None:
                k = "C"
            if k == "B" and done["B"] >= NITER + 1 and live["C"] is not None:
                k = "C"
            try:
                next(live[k])
                done[k] += 1
            except StopIteration:
                live[k] = None
    S.pop()
    S.pop()

    if stop_after == "3":
        dbg_a = S.dram("dbg_a", [128, 4, NOWN], BF16, kind="ExternalOutput")
        dbg_b = S.dram("dbg_b", [128, 4, NOWN], BF16, kind="ExternalOutput")
        S.push()
        big = S.sbuf("dbgbig", [128, 4, NOWN], BF16)
        S.load(big, big[:], AT, AT[:, :, :])
        S.store(dbg_a, dbg_a[:, :, :], big, big[:])
        S.load(big, big[:], BT, BT[:, :, :])
        S.store(dbg_b, dbg_b[:, :, :], big, big[:])
        S.finish([dbg_a, dbg_b])
        return nc

    GLU = S.dram("GLU", [128, 8, NOWN], BF16)
    hout_ring_holder = {}

    def residual_out(ps, n, c, res_buf, res_ap, hout):
        S.op(dve, lambda: nc.vector.tensor_tensor(out=hout[:, c, :n], in0=ps[:, :n], in1=res_ap, op=ALU.add),
             reads=[ps, res_buf], partial=[hout])

    S.push()
    wout_sb = S.sbuf("wout_sb", [128, 8, D], BF16)
    load_w_cast(wout_sb, 0, w_out_ab, w_out_ab.ap_fn(), 0, D)
    xt_ring = Ring([S.sbuf(f"xt4_{i}", [128, 8, 512], F32) for i in range(2)])
    ab_ring = Ring([S.sbuf(f"ab{i}", [128, 8, 512], BF16) for i in range(2)])
    hout_ring = Ring([S.sbuf(f"hout4_{i}", [128, 8, 512], F32) for i in range(2)])
    for (off, n, ci, is_halo, is_first) in own_blocks():
        xt = xt_ring.next(); ab = ab_ring.next(); hout = hout_ring.next()
        S.load(xt, xt[:, :, :n], xT_own, xT_own[:, :, off:off + n])
        S.load(ab, ab[:, 0:4, :n], AT, AT[:, :, off:off + n])
        S.load(ab, ab[:, 4:8, :n], BT, BT[:, :, off:off + n])
        for o in range(8):
            ps = proj_ps.next()
            proj(ps, wout_sb, o * 128, 128, [ab[:, c, :n] for c in range(8)], n, [ab])
            residual_out(ps, n, o, xt, xt[:, o, :n], hout)
        S.store(HT, HT[:, :, off:off + n], hout, hout[:, :, :n])
    S.pop()

    def cross_phase(l, blocks):
        S.push()
        gq, gk = (V2_CQ0, V2_CK0) if l == 0 else (V2_CQ1, V2_CK1)
        ncross = V_NCROSS0 if l == 0 else V_NCROSS1
        nmem = V_NMEM0 if l == 0 else V_NMEM1
        cm = alloc_common()
        kcT = S.sbuf("kcT", [128, 8, MEM], BF16)
        vc = S.sbuf("vc", [128, 2, D], BF16)
        sqc = S.sbuf("sqc", [128, 2, 512], BF16)
        rq = S.sbuf("rq", [128, 512], F32)
        tq = S.sbuf("tq", [128, 512], F32)
        wqo = S.sbuf("wqo", [128, 8, 2 * D], BF16)
        S.push()
        wkv = S.sbuf("wkv", [128, 8, 2 * D], BF16)
        load_w_cast(wkv, 0, cross_wk, cross_wk.ap_fn()[l], 0, D)
        load_w_cast(wkv, D, cross_wv, cross_wv.ap_fn()[l], 0, D)
        load_w_cast(wqo, 0, cross_wq, cross_wq.ap_fn()[l], 0, D)
        load_w_cast(wqo, D, cross_wo, cross_wo.ap_fn()[l], 0, D)
        norm_block(cm, memT, 0, MEM, nmem)
        xn_b = cm.xn_b
        n = MEM
        for hh in range(4):
            pss = [proj_ps.next(), proj_ps.next()]
            for dc in range(2):
                proj(pss[dc], wkv, (hh * 2 + dc) * 128, 128, [xn_b[:, c, :n] for c in range(8)], n, [xn_b])
                S.op(act, lambda dc=dc, pss=pss: nc.scalar.activation(out=sqc[:, dc, :n], in_=pss[dc][:, :n], func=AF.Square),
                     reads=[pss[dc]], partial=[sqc])
            ps2 = PS[5]
            for dc in range(2):
                mm(ps2[:, :n], ones_c[:], sqc[:, dc, :n], dc == 0, dc == 1, [ones_c, sqc], ps2)
            rstd_from_ms(ps2, n, rq, tq)
            for dc in range(2):
                S.op(dve, lambda dc=dc, pss=pss, hh=hh: nc.vector.scalar_tensor_tensor(
                    out=kcT[:, hh * 2 + dc, :n], in0=pss[dc][:, :n], scalar=vec2[:, gk + dc:gk + dc + 1], in1=rq[:, :n],
                    op0=ALU.mult, op1=ALU.mult), reads=[pss[dc], vec2, rq], partial=[kcT])
        for mt in range(2):
            for hf in range(2):
                ps = proj_ps.next()
                for c in range(8):
                    mm(ps[:, :], xn_b[:, c, mt * 128:(mt + 1) * 128], wkv[:, c, D + hf * 512:D + (hf + 1) * 512], c == 0, c == 7,
                       [wkv, xn_b], ps)
                S.op(act, lambda ps=ps, mt=mt, hf=hf: nc.scalar.activation(out=vc[:, mt, hf * 512:(hf + 1) * 512], in_=ps[:, :], func=AF.Copy),
                     reads=[ps], partial=[vc])
        S.pop()
        qn = S.sbuf("qn", [128, 8, 512], BF16)
        on = S.sbuf("on", [128, 8, 512], BF16)
        pc_ring = Ring([S.sbuf(f"pc{i}", [128, 2, 512], BF16) for i in range(2)])
        rden = S.sbuf("rden", [128, 512], F32)
        hout_ring = Ring([S.sbuf(f"houtc_{i}", [128, 8, 512], F32) for i in range(2)])
        qn_h = [S.sbuf(f"qnh{h}", [128, 2, 512], BF16) for h in range(4)]
        on_h = [S.sbuf(f"onh{h}", [128, 2, 512], BF16) for h in range(4)]
        sqc_r = Ring([sqc, S.sbuf("sqc2", [128, 2, 512], BF16)])
        rq_r = Ring([rq, S.sbuf("rq2", [128, 512], F32)])
        tq_r = Ring([tq, S.sbuf("tq2", [128, 512], F32)])
        rden_r = Ring([rden, S.sbuf("rden2", [128, 512], F32)])
        for (off, n, ci, is_halo, is_first) in blocks:
            xt = norm_block(cm, HT, off, n, ncross)
            xn_b = cm.xn_b
            hout = hout_ring.next()

            def front(hh):
                pss = [proj_ps.next(), proj_ps.next()]
                sq_ = sqc_r.next(); rq_ = rq_r.next(); tq_ = tq_r.next()
                for dc in range(2):
                    proj(pss[dc], wqo, (hh * 2 + dc) * 128, 128, [xn_b[:, c, :n] for c in range(8)], n, [xn_b])
                    S.op(act, lambda dc=dc: nc.scalar.activation(out=sq_[:, dc, :n], in_=pss[dc][:, :n], func=AF.Square),
                         reads=[pss[dc]], partial=[sq_])
                ps2 = PS[5]
                for dc in range(2):
                    mm(ps2[:, :n], ones_c[:], sq_[:, dc, :n], dc == 0, dc == 1, [ones_c, sq_], ps2)
                rstd_from_ms(ps2, n, rq_, tq_)
                for dc in range(2):
                    S.op(dve, lambda dc=dc: nc.vector.scalar_tensor_tensor(
                        out=qn_h[hh][:, dc, :n], in0=pss[dc][:, :n], scalar=vec2[:, gq + dc:gq + dc + 1], in1=rq_[:, :n],
                        op0=ALU.mult, op1=ALU.mult), reads=[pss[dc], vec2, rq_], partial=[qn_h[hh]])

            def back(hh):
                pc = pc_ring.next()
                rd_ = rden_r.next()
                for mt in range(2):
                    ps = proj_ps.next()
                    for dc in range(2):
                        mm(ps[:, :n], kcT[:, hh * 2 + dc, mt * 128:(mt + 1) * 128], qn_h[hh][:, dc, :n], dc == 0, dc == 1,
                           [kcT, qn_h[hh]], ps)
                    S.op(act, lambda ps=ps, mt=mt: nc.scalar.activation(out=pc[:, mt, :n], in_=ps[:, :n], func=AF.Exp,
                                                                        scale=1.0 / 16.0), reads=[ps], partial=[pc])
                psd = PS[6]
                for mt in range(2):
                    mm(psd[:, :n], ones_1[:], pc[:, mt, :n], mt == 0, mt == 1, [ones_1, pc], psd)
                S.op(dve, lambda: nc.vector.reciprocal(out=rd_[:, :n], in_=psd[:, :n]), reads=[psd], writes=[rd_])
                for dc in range(2):
                    ps = proj_ps.next()
                    for mt in range(2):
                        mm(ps[:, :n], vc[:, mt, hh * 256 + dc * 128:hh * 256 + (dc + 1) * 128], pc[:, mt, :n], mt == 0, mt == 1,
                           [vc, pc], ps)
                    S.op(dve, lambda ps=ps, dc=dc: nc.vector.tensor_tensor(out=on_h[hh][:, dc, :n], in0=ps[:, :n],
                                                                          in1=rd_[:, :n], op=ALU.mult),
                         reads=[ps, rd_], partial=[on_h[hh]])

            front(0)
            for hh in range(4):
                if hh + 1 < 4:
                    front(hh + 1)
                back(hh)
            for o in range(8):
                ps = proj_ps.next()
                proj(ps, wqo, D + o * 128, 128, [on_h[c // 2][:, c % 2, :n] for c in range(8)], n, on_h)
                residual_out(ps, n, o, xt, xt[:, o, :n], hout)
            S.store(HT, HT[:, :, off:off + n], hout, hout[:, :, :n])
        S.pop()

    def ffn_phase(l, blocks, final):
        nffn = V_NFFN0 if l == 0 else V_NFFN1
        S.push()
        cm = alloc_common()
        wgu = S.sbuf("wgu", [128, 8, 2 * DFF], BF16)
        load_w_cast(wgu, 0, ffn_wg, ffn_wg.ap_fn()[l], 0, DFF)
        load_w_cast(wgu, DFF, ffn_wu, ffn_wu.ap_fn()[l], 0, DFF)
        hid_ring = Ring([S.sbuf(f"hid{i}", [128, 22, 512], BF16) for i in range(2)])
        sg_ring = Ring([S.sbuf(f"sg{i}", [128, 512], F32) for i in range(2)])
        for (off, n, ci, is_halo, is_first) in blocks:
            norm_block(cm, HT, off, n, nffn)
            xn_b = cm.xn_b
            hid = hid_ring.next()
            xl = [xn_b[:, c, :n] for c in range(8)]
            for f in range(22):
                psg = proj_ps.next(); psu = proj_ps.next()
                proj(psg, wgu, f * 128, 128, xl, n, [xn_b])
                proj(psu, wgu, DFF + f * 128, 128, xl, n, [xn_b])
                sg = sg_ring.next()
                S.op(act, lambda psg=psg, sg=sg: nc.scalar.activation(out=sg[:, :n], in_=psg[:, :n], func=AF.Silu),
                     reads=[psg], writes=[sg])
                S.op(dve, lambda psu=psu, sg=sg, f=f: nc.vector.tensor_tensor(out=hid[:, f, :n], in0=psu[:, :n], in1=sg[:, :n], op=ALU.mult),
                     reads=[psu, sg], partial=[hid])
            S.store(HID, HID[:, :, off:off + n], hid, hid[:, :, :n])
        S.pop()
        S.push()
        wd = S.sbuf("wd", [128, 22, D], BF16)
        load_w_cast(wd, 0, ffn_wd, ffn_wd.ap_fn()[l], 0, D)
        hid_ring = Ring([S.sbuf(f"hidb{i}", [128, 22, 512], BF16) for i in range(2)])
        xt_ring = Ring([S.sbuf(f"xtd_{i}", [128, 8, 512], F32) for i in range(2)])
        hout_ring = Ring([S.sbuf(f"houtd_{i}", [128, 8, 512], F32) for i in range(2)])
        for (off, n, ci, is_halo, is_first) in blocks:
            hid = hid_ring.next(); xt = xt_ring.next(); hout = hout_ring.next()
            S.load(hid, hid[:, :, :n], HID, HID[:, :, off:off + n])
            S.load(xt, xt[:, :, :n], HT, HT[:, :, off:off + n])
            for o in range(8):
                ps = proj_ps.next()
                proj(ps, wd, o * 128, 128, [hid[:, f, :n] for f in range(22)], n, [hid])
                residual_out(ps, n, o, xt, xt[:, o, :n], hout)
            if final:
                slot0 = off // 128
                ci_, i_ = divmod(slot0, SLOTS_PER_CH)
                oo = (ci_ * CH_TILES + i_ - 1) * 128
                S.store(out_hT, out_hT[:, :, oo:oo + n], hout, hout[:, :, :n])
            else:
                S.store(HT, HT[:, :, off:off + n], hout, hout[:, :, :n])
        S.pop()

    cross_phase(0, own_blocks())
    ffn_phase(0, own_blocks(), False)

    S.push()
    cm = alloc_common()
    wci = S.sbuf("wci", [128, 8, 2 * D], BF16)
    load_w_cast(wci, 0, conv_w_in, conv_w_in.ap_fn(), 0, 2 * D)
    hflag = S.sbuf("hflag", [128, 2], F32)
    S.load(hflag, hflag[:], haloflag_in, haloflag_in[:, :])
    sig_ring = Ring([S.sbuf(f"sig{i}", [128, 512], F32) for i in range(2)])
    glu_ring = Ring([S.sbuf(f"glu{i}", [128, 8, 512], BF16) for i in range(2)])
    for (off, n, ci, is_halo, is_first) in own_blocks():
        norm_block(cm, HT, off, n, V_NMIX1)
        xn_b = cm.xn_b
        xl = [xn_b[:, c, :n] for c in range(8)]
        glu = glu_ring.next()
        for c8 in range(8):
            psa = proj_ps.next(); psg = proj_ps.next()
            proj(psa, wci, c8 * 128, 128, xl, n, [xn_b])
            proj(psg, wci, D + c8 * 128, 128, xl, n, [xn_b])
            sig = sig_ring.next()
            S.op(act, lambda psg=psg, sig=sig, c8=c8: nc.scalar.activation(out=sig[:, :n], in_=psg[:, :n], func=AF.Sigmoid,
                                                                          bias=vec2[:, V2_BIN + 8 + c8:V2_BIN + 9 + c8], scale=1.0),
                 reads=[psg, vec2], writes=[sig])
            S.op(dve, lambda psa=psa, sig=sig, c8=c8: nc.vector.scalar_tensor_tensor(
                out=glu[:, c8, :n], in0=psa[:, :n], scalar=vec2[:, V2_BIN + c8:V2_BIN + c8 + 1], in1=sig[:, :n],
                op0=ALU.add, op1=ALU.mult), reads=[psa, vec2, sig], partial=[glu])
        if is_halo:
            S.op(dve, lambda glu=glu: nc.vector.tensor_scalar(out=glu[:, :, :n], in0=glu[:, :, :n], scalar1=hflag[:, ci:ci + 1],
                                                             scalar2=None, op0=ALU.mult), reads=[hflag, glu], partial=[glu])
        S.store(GLU, GLU[:, :, off:off + n], glu, glu[:, :, :n])
    S.pop()

    S.push()
    wco = S.sbuf("wco", [128, 8, D], BF16)
    load_w_cast(wco, 0, conv_w_out, conv_w_out.ap_fn(), 0, D)
    dw_sb = S.sbuf("dw_sb", [128, 8, 31], F32)
    S.load(dw_sb, dw_sb[:], conv_dw, conv_dw[:, :, :])
    dg = S.sbuf("dg", [128, 8, 31, 128], BF16)
    for c8 in range(8):
        for k in range(31):
            S.op(act, lambda c8=c8, k=k: nc.scalar.activation(out=dg[:, c8, k, :], in_=ident[:], func=AF.Copy,
                                                              scale=dw_sb[:, c8, k:k + 1]), reads=[ident, dw_sb], partial=[dg])
    gin_ring = Ring([S.sbuf(f"gin{i}", [128, 8, 544], BF16) for i in range(2)])
    hc = S.sbuf("hc", [128, 8, 512], F32)
    hcb = S.sbuf("hcb", [128, 8, 512], BF16)
    hsq = S.sbuf("hsq", [128, 8, 512], BF16)
    mean_sb = S.sbuf("mean_sb", [128, 512], F32)
    var_sb = S.sbuf("var_sb", [128, 512], F32)
    rstd2 = S.sbuf("rstd2", [128, 512], F32)
    tv = S.sbuf("tv", [128, 512], F32)
    dtmp = Ring([S.sbuf(f"dtmp{i}", [128, 512], F32) for i in range(2)])
    sl = S.sbuf("sl", [128, 8, 512], BF16)
    xt_ring = Ring([S.sbuf(f"xtv_{i}", [128, 8, 512], F32) for i in range(1)])
    hout_ring = Ring([S.sbuf(f"houtv_{i}", [128, 8, 512], F32) for i in range(1)])
    for (off, n, ci, is_halo, is_first) in own_blocks(False):
        gin = gin_ring.next(); xt = xt_ring.next(); hout = hout_ring.next()
        S.load(gin, gin[:, :, :n + 30], GLU, GLU[:, :, off - 30:off + n])
        S.load(xt, xt[:, :, :n], HT, HT[:, :, off:off + n])
        for c8 in range(8):
            ps = proj_ps.next()
            for k in range(31):
                mm(ps[:, :n], dg[:, c8, k, :], gin[:, c8, k:k + n], k == 0, k == 30, [dg, gin], ps)
            S.op(act, lambda ps=ps, c8=c8: nc.scalar.activation(out=hc[:, c8, :n], in_=ps[:, :n], func=AF.Identity,
                                                                bias=vec2[:, V2_DWB + c8:V2_DWB + c8 + 1], scale=1.0),
                 reads=[ps, vec2], partial=[hc])
        S.op(dve, lambda: nc.vector.tensor_copy(out=hcb[:, :, :n], in_=hc[:, :, :n]), reads=[hc], writes=[hcb])
        S.op(act, lambda: nc.scalar.activation(out=hsq[:, :, :n], in_=hc[:, :, :n], func=AF.Square), reads=[hc], writes=[hsq])
        psm, psq = PS[5], PS[6]
        for c8 in range(8):
            mm(psm[:, :n], ones_m[:], hcb[:, c8, :n], c8 == 0, c8 == 7, [ones_m, hcb], psm)
        for c8 in range(8):
            mm(psq[:, :n], ones_m[:], hsq[:, c8, :n], c8 == 0, c8 == 7, [ones_m, hsq], psq)
        S.op(act, lambda: nc.scalar.activation(out=mean_sb[:, :n], in_=psm[:, :n], func=AF.Copy), reads=[psm], writes=[mean_sb])
        S.op(dve, lambda: nc.vector.tensor_tensor(out=var_sb[:, :n], in0=mean_sb[:, :n], in1=mean_sb[:, :n], op=ALU.mult),
             reads=[mean_sb], writes=[var_sb])
        S.op(dve, lambda: nc.vector.tensor_tensor(out=var_sb[:, :n], in0=psq[:, :n], in1=var_sb[:, :n], op=ALU.subtract),
             reads=[psq, var_sb], writes=[var_sb])
        S.op(act, lambda: nc.scalar.activation(out=tv[:, :n], in_=var_sb[:, :n], func=AF.Sqrt, bias=eps_t[:, 0:1], scale=1.0),
             reads=[var_sb, eps_t], writes=[tv])
        S.op(dve, lambda: nc.vector.reciprocal(out=rstd2[:, :n], in_=tv[:, :n]), reads=[tv], writes=[rstd2])
        for c8 in range(8):
            dt_ = dtmp.next()
            S.op(pool, lambda c8=c8, dt_=dt_: nc.gpsimd.tensor_tensor(out=dt_[:, :n], in0=hc[:, c8, :n], in1=mean_sb[:, :n], op=ALU.subtract),
                 reads=[hc, mean_sb], writes=[dt_])
            S.op(dve, lambda c8=c8, dt_=dt_: nc.vector.tensor_tensor(out=dt_[:, :n], in0=dt_[:, :n], in1=rstd2[:, :n], op=ALU.mult),
                 reads=[dt_, rstd2], writes=[dt_])
            S.op(act, lambda c8=c8, dt_=dt_: nc.scalar.activation(out=sl[:, c8, :n], in_=dt_[:, :n], func=AF.Silu,
                                                                  bias=vec2[:, V2_LNB + c8:V2_LNB + c8 + 1],
                                                                  scale=vec2[:, V2_LNG + c8:V2_LNG + c8 + 1]),
                 reads=[dt_, vec2], partial=[sl])
        for o in range(8):
            ps = proj_ps.next()
            proj(ps, wco, o * 128, 128, [sl[:, c, :n] for c in range(8)], n, [sl])
            residual_out(ps, n, o, xt, xt[:, o, :n], hout)
        S.store(HT, HT[:, :, off:off + n], hout, hout[:, :, :n])
    S.pop()

    cross_phase(1, own_blocks(False))
    ffn_phase(1, own_blocks(False), True)
    S.finish([out_hT])
    return nc


def _fm(a):
    t = a.shape[0]
    return np.ascontiguousarray(a.T.reshape(8, 128, t).transpose(1, 0, 2))


def _colvec(v):
    return np.ascontiguousarray(v.reshape(-1, 128).T)


def prepare_inputs(inputs, stop_after=None):
    f = lambda k: np.asarray(inputs[k], dtype=np.float32)
    x = f("x"); mem = f("mem")
    w_in = f("w_in_ab")[0]
    sp = np.cumsum((512, 512, 512, 512, 1024, 16, 64))[:-1]
    wq, wk, wv, wu, wiq, wiw, wik = np.split(w_in, sp, axis=1)
    w_keys = np.ascontiguousarray(np.concatenate([wk, wik, wik, wv], axis=1))
    w_own = np.ascontiguousarray(np.concatenate([wq, wiq, wu, wiw], axis=1))
    vecs = np.concatenate([_colvec(f(k)[l]) for k in ("norm_mix", "norm_cross", "norm_mem", "norm_ffn")
                           for l in range(2)], axis=1)
    vecs = np.ascontiguousarray(vecs.astype(np.float32))
    v2 = np.zeros((128, 64), np.float32)
    v2[:, 0] = f("a_q_norm")[0]; v2[:, 1] = f("a_k_norm")[0]
    v2[:, 2:6] = _colvec(f("pool_scale")[0])
    v2[:, 6:8] = _colvec(f("cross_q_norm")[0]); v2[:, 8:10] = _colvec(f("cross_q_norm")[1])
    v2[:, 10:12] = _colvec(f("cross_k_norm")[0]); v2[:, 12:14] = _colvec(f("cross_k_norm")[1])
    v2[:, 14:30] = _colvec(f("conv_b_in")[0])
    v2[:, 30:38] = _colvec(f("conv_dw_b")[0])
    v2[:, 38:46] = _colvec(f("conv_ln_g")[0])
    v2[:, 46:54] = _colvec(f("conv_ln_b")[0])
    conv_dw = np.ascontiguousarray(f("conv_dw_w")[0].T.reshape(8, 128, 31).transpose(1, 0, 2))
    seqpos = np.arange(SEQ)
    c128s, s128s = rope_tables(seqpos, 128)
    c64s, s64s = rope_tables(seqpos, 64)
    shared = dict(
        iota=np.ascontiguousarray(np.broadcast_to(np.arange(MBW, dtype=np.float32), (128, MBW))),
        c128s=c128s, s128s=s128s, c64s=c64s, s64s=s64s,
        p128=perm_matrix(128), p64=perm_matrix(64), ident=np.eye(128, dtype=np.float32),
        w_keys=w_keys, w_own=w_own, vecs=vecs, vecs2=v2,
        pool_w=f("pool_w")[0], w_out_ab=f("w_out_ab")[0], conv_w_in=f("conv_w_in")[0], conv_dw=conv_dw,
        conv_w_out=f("conv_w_out")[0], cross_wq=f("cross_wq"), cross_wk=f("cross_wk"),
        cross_wv=f("cross_wv"), cross_wo=f("cross_wo"), ffn_wg=f("ffn_w_gate"), ffn_wu=f("ffn_w_up"),
        ffn_wd=f("ffn_w_down"),
    )
    in_maps = []
    for core in range(8):
        b, half = divmod(core, 2)
        tiles = own_tiles(half)
        xo = np.zeros((NOWN, D), np.float32)
        pos = np.zeros(NOWN, np.int64)
        for s, t in enumerate(tiles):
            if t >= 0:
                xo[s * 128:(s + 1) * 128] = x[b, t * 128:(t + 1) * 128]
                pos[s * 128:(s + 1) * 128] = np.arange(t * 128, (t + 1) * 128)
        qa = np.zeros((128, NSLOT), np.float32)
        for s in range(NSLOT):
            base = (slot_first_uncertain(s) // 4) * 512
            qa[:, s] = pos[s * 128:(s + 1) * 128] - base
        c128o, s128o = rope_tables(pos, 128)
        c64o, s64o = rope_tables(pos, 64)
        pc = np.ones((128, 2, 4, 16), np.float32)
        hf = np.ones((128, 2), np.float32)
        if half == 0:
            hf[:, 0] = 0.0
            for g, w in enumerate((2, 4, 8, 16)):
                for t in range(16):
                    pc[:, 0, g, t] = w / min(t + 1, w)
        m = dict(shared)
        m.update(xT_seq=_fm(x[b]), xT_own=_fm(xo), memT=_fm(mem[b]), qadj=qa,
                 c128o=c128o, s128o=s128o, c64o=c64o, s64o=s64o, poolcorr=pc, haloflag=hf)
        in_maps.append(m)
    return in_maps


_NC_CACHE = {}


def kernel(**inputs):
    in_maps = prepare_inputs(inputs)
    if "nc" not in _NC_CACHE:
        _NC_CACHE["nc"] = build_program()
    nc = _NC_CACHE["nc"]
    res = run_bass_kernel_spmd(nc, in_maps, core_ids=list(range(8)))
    out = np.zeros((4, SEQ, D), np.float32)
    for core in range(8):
        b, half = divmod(core, 2)
        o = res.results[core]["out_hT"]
        o = o.transpose(2, 1, 0).reshape(32 * 128, D)
        for ci in range(2):
            start = (2 * ci + half) * CH_TILES * 128
            out[b, start:start + 2048] = o[ci * 2048:(ci + 1) * 2048]
    return out
```
